# Optimizing a Trainium2 kernel written in Bass

```python
import jax, jax.numpy as jnp
from jax import lax
import numpy as np

D_MODEL = 1024
BATCH = 4
SEQ = 4096
DEPTH = 4

N_META = 16
SB_HEADS = 8
SB_HEAD_DIM = 64
SB_WIDTH = SB_HEADS * SB_HEAD_DIM
SB_BLOCK = 128
GDN_HEADS = 4
GDN_HEAD_DIM = 128
GDN_WIDTH = GDN_HEADS * GDN_HEAD_DIM
GDN_CHUNK = 64
CONV_WIDTH = 4
MIX_WIDTH = SB_WIDTH + GDN_WIDTH
IN_WIDTH = 3 * SB_WIDTH + 4 * GDN_WIDTH + 2 * GDN_HEADS
N_GROUPS = 4
EXPERTS_PER_GROUP = 8
N_EXPERTS = N_GROUPS * EXPERTS_PER_GROUP
TOP_K = 2
D_EXPERT = 256
LN_EPS = 1e-5
RMS_EPS = 1e-6
DEEPNORM_ALPHA = float((2 * DEPTH) ** 0.25)
DEEPNORM_BETA = float((8 * DEPTH) ** -0.25)

kernel_name = "hymba_stickbreak_gdn_hmoe_deepnorm"


def layer_norm(x, g, b):
    xf = x.astype(jnp.float32)
    mu = jnp.mean(xf, axis=-1, keepdims=True)
    var = jnp.mean(jnp.square(xf - mu), axis=-1, keepdims=True)
    y = (xf - mu) * lax.rsqrt(var + LN_EPS) * g.astype(jnp.float32) + b.astype(jnp.float32)
    return y.astype(x.dtype)


def rms_norm(x, g):
    xf = x.astype(jnp.float32)
    return xf * lax.rsqrt(jnp.mean(jnp.square(xf), axis=-1, keepdims=True) + RMS_EPS) * g.astype(jnp.float32)


def l2_normalize(x):
    return x * lax.rsqrt(jnp.sum(jnp.square(x), axis=-1, keepdims=True) + RMS_EPS)


def causal_depthwise_conv(x, w):
    ch = x.shape[-1]
    return lax.conv_general_dilated(
        x, w[:, None, :].astype(x.dtype), window_strides=(1,),
        padding=[(CONV_WIDTH - 1, 0)], dimension_numbers=("NWC", "WIO", "NWC"),
        feature_group_count=ch)


def stick_breaking_attention(q, k, v):
    lp, dh = q.shape[2], q.shape[3]
    scale = dh ** -0.5
    outs = []
    for start in range(0, lp, SB_BLOCK):
        stop = start + SB_BLOCK
        qb = q[:, :, start:stop].astype(jnp.float32)
        kp = k[:, :, :stop].astype(jnp.float32)
        vp = v[:, :, :stop].astype(jnp.float32)
        z = jnp.einsum("bhqd,bhkd->bhqk", qb, kp) * scale
        visible = jnp.arange(stop)[None, :] < jnp.arange(start, stop)[:, None]
        log_not_beta = jnp.where(visible, -jax.nn.softplus(z), 0.0)
        between = lax.cumsum(log_not_beta, axis=3, reverse=True) - log_not_beta
        weights = jnp.where(visible, jnp.exp(jax.nn.log_sigmoid(z) + between), 0.0)
        outs.append(jnp.einsum("bhqk,bhkd->bhqd", weights, vp))
    return jnp.concatenate(outs, axis=2)


def gated_delta_rule_chunked(q, k, v, beta, g):
    c, dk, dv = q.shape[3], q.shape[4], v.shape[4]
    g_cum = jnp.cumsum(g, axis=-1)
    lower_incl = jnp.tril(jnp.ones((c, c), dtype=bool))
    lower_strict = jnp.tril(jnp.ones((c, c), dtype=bool), -1)
    diff = g_cum[..., :, None] - g_cum[..., None, :]
    decay_mask = jnp.where(lower_incl, jnp.exp(jnp.where(lower_incl, diff, 0.0)), 0.0)
    k_beta = k * beta[..., None]
    v_beta = v * beta[..., None]
    a_strict = jnp.where(lower_strict, jnp.einsum("bhnid,bhnjd->bhnij", k_beta, k) * decay_mask, 0.0)
    rhs = jnp.concatenate([k_beta * jnp.exp(g_cum)[..., None], v_beta], axis=-1)
    sol = lax.linalg.triangular_solve(jnp.eye(c, dtype=jnp.float32) + a_strict, rhs,
                                      left_side=True, lower=True)
    w_cd, u = sol[..., :dk], sol[..., dk:]
    qk_intra = jnp.einsum("bhnid,bhnjd->bhnij", q, k) * decay_mask
    q_dec = q * jnp.exp(g_cum)[..., None]
    k_to_end = k * jnp.exp(g_cum[..., -1:] - g_cum)[..., None]
    chunk_decay = jnp.exp(g_cum[..., -1])

    def step(state, inp):
        w_n, u_n, qd_n, qk_n, ke_n, cd_n = inp
        v_new = u_n - jnp.einsum("bhcd,bhde->bhce", w_n, state)
        o = jnp.einsum("bhcd,bhde->bhce", qd_n, state) + jnp.einsum("bhij,bhje->bhie", qk_n, v_new)
        state = state * cd_n[..., None, None] + jnp.einsum("bhcd,bhce->bhde", ke_n, v_new)
        return state, o

    xs = tuple(jnp.moveaxis(a, 2, 0) for a in (w_cd, u, q_dec, qk_intra, k_to_end, chunk_decay))
    init = jnp.zeros(q.shape[:2] + (dk, dv), jnp.float32)
    _, o = lax.scan(step, init, xs)
    return jnp.moveaxis(o, 0, 2)


def hybrid_mixer(x, w_in, conv_w, a_log, dt_bias, sb_norm_g, gdn_norm_g, w_out):
    b, l, _ = x.shape
    proj = x @ w_in
    s1 = 3 * SB_WIDTH
    s2 = s1 + 3 * GDN_WIDTH
    s3 = s2 + GDN_WIDTH
    s4 = s3 + GDN_HEADS
    sb_qkv, gdn_qkv, gdn_z, gdn_b, gdn_a = jnp.split(proj, [s1, s2, s3, s4], axis=-1)

    pad_sb = (-l) % SB_BLOCK
    sb_qkv = jnp.pad(sb_qkv, ((0, 0), (0, pad_sb), (0, 0)))
    sb_qkv = sb_qkv.reshape(b, l + pad_sb, 3, SB_HEADS, SB_HEAD_DIM).transpose(2, 0, 3, 1, 4)
    o_sb = stick_breaking_attention(sb_qkv[0], sb_qkv[1], sb_qkv[2])[:, :, :l]
    o_sb = rms_norm(o_sb, sb_norm_g).transpose(0, 2, 1, 3).reshape(b, l, SB_WIDTH)

    gdn_qkv = jax.nn.silu(causal_depthwise_conv(gdn_qkv, conv_w)).astype(jnp.float32)
    gdn_qkv = gdn_qkv.reshape(b, l, 3, GDN_HEADS, GDN_HEAD_DIM)
    q = l2_normalize(gdn_qkv[:, :, 0]) * (GDN_HEAD_DIM ** -0.5)
    k = l2_normalize(gdn_qkv[:, :, 1])
    v = gdn_qkv[:, :, 2]
    beta = jax.nn.sigmoid(gdn_b.astype(jnp.float32))
    g = -jnp.exp(a_log.astype(jnp.float32)) * jax.nn.softplus(
        gdn_a.astype(jnp.float32) + dt_bias.astype(jnp.float32))
    front = (-N_META) % GDN_CHUNK
    back = (-(l + front)) % GDN_CHUNK
    lc = l + front + back
    n_chunks = lc // GDN_CHUNK

    def to_chunks(a):
        a = jnp.pad(a, ((0, 0), (front, back)) + ((0, 0),) * (a.ndim - 2))
        a = a.reshape((b, n_chunks, GDN_CHUNK) + a.shape[2:])
        return jnp.moveaxis(a, 3, 1)

    o_gdn = gated_delta_rule_chunked(to_chunks(q), to_chunks(k), to_chunks(v),
                                     to_chunks(beta), to_chunks(g))
    o_gdn = o_gdn.transpose(0, 2, 3, 1, 4).reshape(b, lc, GDN_HEADS, GDN_HEAD_DIM)[:, front:front + l]
    z = gdn_z.reshape(b, l, GDN_HEADS, GDN_HEAD_DIM).astype(jnp.float32)
    o_gdn = (rms_norm(o_gdn, gdn_norm_g) * jax.nn.silu(z)).reshape(b, l, GDN_WIDTH)

    mixed = jnp.concatenate([o_sb, o_gdn], axis=-1)
    return (mixed @ w_out.astype(jnp.float32)).astype(x.dtype)


def hierarchical_moe(x, w_group, b_group, w_expert, b_expert, w1, w3, w2):
    t = x.shape[0]
    group_prob = jax.nn.softmax((x @ w_group + b_group).astype(jnp.float32), axis=-1)
    g_val, g_idx = lax.top_k(group_prob, 1)
    group_onehot = jax.nn.one_hot(g_idx[:, 0], N_GROUPS, dtype=jnp.float32)
    expert_logits = (x @ w_expert + b_expert).astype(jnp.float32).reshape(t, N_GROUPS, EXPERTS_PER_GROUP)
    in_group = jnp.einsum("tg,tge->te", group_onehot, expert_logits)
    e_val, e_idx = lax.top_k(in_group, TOP_K)
    e_w = jax.nn.softmax(e_val, axis=-1)
    within = jnp.einsum("tk,tke->te", e_w, jax.nn.one_hot(e_idx, EXPERTS_PER_GROUP, dtype=jnp.float32))
    combine = group_onehot[:, :, None] * (g_val * within)[:, None, :]
    y = jnp.zeros((t, x.shape[1]), jnp.float32)
    for gi in range(N_GROUPS):
        sl = slice(gi * EXPERTS_PER_GROUP, (gi + 1) * EXPERTS_PER_GROUP)
        h = jax.nn.silu(jnp.einsum("td,edf->tef", x, w1[sl]).astype(jnp.float32)) * \
            jnp.einsum("td,edf->tef", x, w3[sl]).astype(jnp.float32)
        y = y + jnp.einsum("tef,efd->td", h * combine[:, gi, :, None], w2[sl].astype(jnp.float32))
    return y.astype(x.dtype)


def setup_inputs(seed: int = 0) -> dict:
    key = jax.random.key(seed)
    ks = jax.random.split(key, 24)
    f32 = jnp.float32
    nrm = lambda k, shape, s: jax.random.normal(k, shape, f32) * s
    dt = jnp.exp(jax.random.uniform(ks[6], (DEPTH, GDN_HEADS), f32, np.log(1e-3), np.log(1e-1)))
    return {
        "x": jax.random.normal(ks[0], (BATCH, SEQ, D_MODEL), f32),
        "meta_tokens": nrm(ks[1], (N_META, D_MODEL), 1.0),
        "ln_in_g": 1.0 + nrm(ks[2], (D_MODEL,), 0.02),
        "ln_in_b": nrm(ks[3], (D_MODEL,), 0.02),
        "w_in": nrm(ks[4], (DEPTH, D_MODEL, IN_WIDTH), D_MODEL ** -0.5),
        "conv_w": nrm(ks[5], (DEPTH, CONV_WIDTH, 3 * GDN_WIDTH), CONV_WIDTH ** -0.5),
        "a_log": jnp.log(jax.random.uniform(ks[7], (DEPTH, GDN_HEADS), f32, 1.0, 16.0)),
        "dt_bias": dt + jnp.log(-jnp.expm1(-dt)),
        "sb_norm_g": 1.0 + nrm(ks[8], (DEPTH, SB_HEAD_DIM), 0.02),
        "gdn_norm_g": 1.0 + nrm(ks[9], (DEPTH, GDN_HEAD_DIM), 0.02),
        "w_out": nrm(ks[10], (DEPTH, MIX_WIDTH, D_MODEL), MIX_WIDTH ** -0.5 * DEEPNORM_BETA),
        "ln1_g": 1.0 + nrm(ks[11], (DEPTH, D_MODEL), 0.02),
        "ln1_b": nrm(ks[12], (DEPTH, D_MODEL), 0.02),
        "w_group": nrm(ks[13], (DEPTH, D_MODEL, N_GROUPS), D_MODEL ** -0.5),
        "b_group": nrm(ks[14], (DEPTH, N_GROUPS), 0.01),
        "w_expert": nrm(ks[15], (DEPTH, D_MODEL, N_EXPERTS), D_MODEL ** -0.5),
        "b_expert": nrm(ks[16], (DEPTH, N_EXPERTS), 0.01),
        "w1": nrm(ks[17], (DEPTH, N_EXPERTS, D_MODEL, D_EXPERT), D_MODEL ** -0.5),
        "w3": nrm(ks[18], (DEPTH, N_EXPERTS, D_MODEL, D_EXPERT), D_MODEL ** -0.5),
        "w2": nrm(ks[19], (DEPTH, N_EXPERTS, D_EXPERT, D_MODEL), D_EXPERT ** -0.5 * DEEPNORM_BETA),
        "ln2_g": 1.0 + nrm(ks[20], (DEPTH, D_MODEL), 0.02),
        "ln2_b": nrm(ks[21], (DEPTH, D_MODEL), 0.02),
    }


def reference(x, meta_tokens, ln_in_g, ln_in_b, w_in, conv_w, a_log, dt_bias, sb_norm_g,
              gdn_norm_g, w_out, ln1_g, ln1_b, w_group, b_group, w_expert, b_expert,
              w1, w3, w2, ln2_g, ln2_b):
    b = x.shape[0]
    meta = jnp.broadcast_to(meta_tokens.astype(x.dtype)[None], (b, N_META, D_MODEL))
    h = layer_norm(jnp.concatenate([meta, x], axis=1), ln_in_g, ln_in_b)
    l = h.shape[1]
    for i in range(DEPTH):
        mix = hybrid_mixer(h, w_in[i], conv_w[i], a_log[i], dt_bias[i], sb_norm_g[i],
                           gdn_norm_g[i], w_out[i])
        h = layer_norm(DEEPNORM_ALPHA * h + mix, ln1_g[i], ln1_b[i])
        ffn = hierarchical_moe(h.reshape(b * l, D_MODEL), w_group[i], b_group[i], w_expert[i],
                               b_expert[i], w1[i], w3[i], w2[i]).reshape(b, l, D_MODEL)
        h = layer_norm(DEEPNORM_ALPHA * h + ffn, ln2_g[i], ln2_b[i])
    return h[:, N_META:]
```

```python
import contextlib
import numpy as np
import ml_dtypes
import concourse.bass as bass
import concourse.mybir as mybir
from concourse.bass_utils import run_bass_kernel_spmd

F32 = mybir.dt.float32
BF16 = mybir.dt.bfloat16
AF = mybir.ActivationFunctionType
ALU = mybir.AluOpType
AX = mybir.AxisListType

D = 1024
KC = 8
DEPTH = 4
IN_W = 3592
ALPHA = float((2 * DEPTH) ** 0.25)
LN_EPS = 1e-5
RMS_EPS = 1e-6
NEG = -30000.0
INTERLEAVE_GDN = True
DMA_PAD = 3
GDN_YK = 1


class Ev:
    __slots__ = ("key", "val", "snap", "src")

    def __init__(self, key, val, snap, src):
        self.key, self.val, self.snap, self.src = key, val, snap, src


class Buf:
    __slots__ = ("w", "r", "name", "excl")

    def __init__(self, name=""):
        self.w = None
        self.r = {}
        self.name = name
        self.excl = False


class Tl:
    def __init__(self, h, name=""):
        self.h = h
        self.b = Buf(name)

    def __getitem__(self, idx):
        return self.h[idx]


ENG = ("pe", "act", "dve", "pool", "sp")


class Sched:
    def __init__(self, nc, es, ndma=8, same_eng=True):
        self.nc = nc
        self.e = {"pe": nc.tensor, "act": nc.scalar, "dve": nc.vector, "pool": nc.gpsimd, "sp": nc.sync}
        self.sem = {k: es.enter_context(nc.semaphore("sem_" + k)) for k in ENG}
        self.cnt = {k: 0 for k in ENG}
        self.seen = {k: {} for k in ENG}
        self.dq = {}
        self.semobj = {"c:" + k: self.sem[k] for k in ENG}
        for q in ("sp", "pool"):
            sems = [es.enter_context(nc.semaphore("dma_%s_%d" % (q, i))) for i in range(ndma)]
            self.dq[q] = {"sems": sems, "n": 0, "pending": [None] * ndma}
            for i, sm in enumerate(sems):
                self.semobj["d:%s:%d" % (q, i)] = sm
        self.same_eng = same_eng
        self.nwait = 0
        self.max_ops = None
        self.log = None
        self.nins = 0
        self.rr = 0

    def _wait(self, eng, ev):
        if ev is None:
            return
        seen = self.seen[eng]
        if seen.get(ev.key, 0) >= ev.val:
            return
        if ev.src == eng and (eng == "pe" or not self.same_eng):
            return
        self.e[eng].wait_ge(self.semobj[ev.key], ev.val)
        if self.log is not None:
            self.log.append((self.nins, eng, "WAIT %s >= %d" % (ev.key, ev.val)))
        self.nwait += 1
        new = dict(seen)
        for k, v in ev.snap.items():
            if new.get(k, 0) < v:
                new[k] = v
        if new.get(ev.key, 0) < ev.val:
            new[ev.key] = ev.val
        self.seen[eng] = new

    def _deps(self, eng, R, W):
        for b in R:
            self._wait(eng, b.w)
            if b.excl:
                for k, ev in list(b.r.items()):
                    if k != eng:
                        self._wait(eng, ev)
        for b in W:
            self._wait(eng, b.w)
            for ev in list(b.r.values()):
                self._wait(eng, ev)

    def op(self, eng, fn, R=(), W=()):
        if self.max_ops is not None and self.nins >= self.max_ops:
            return None
        R = [getattr(x, "b", x) for x in R]
        W = [getattr(x, "b", x) for x in W]
        self._deps(eng, R, W)
        ins = fn()
        if self.log is not None:
            self.log.append((self.nins, eng, str(ins)[:150]))
        self.cnt[eng] += 1
        self.nins += 1
        ins.then_inc(self.sem[eng], 1)
        ev = Ev("c:" + eng, self.cnt[eng], self.seen[eng], eng)
        for b in R:
            b.r[eng] = ev
        for b in W:
            b.w = ev
            b.r = {}
        return ev

    def dma(self, q, out, in_, R=(), W=(), **kw):
        if self.max_ops is not None and self.nins >= self.max_ops:
            return None
        R = [getattr(x, "b", x) for x in R]
        W = [getattr(x, "b", x) for x in W]
        dq = self.dq[q]
        ns = len(dq["sems"])
        i = dq["n"] % ns
        self._wait(q, dq["pending"][i])
        self._deps(q, R, W)
        ins = self.e[q].dma_start(out=out, in_=in_, **kw)
        val = 16 * (dq["n"] // ns + 1)
        ins.then_inc(dq["sems"][i], 16)
        ev = Ev("d:%s:%d" % (q, i), val, self.seen[q], "dma")
        dq["pending"][i] = ev
        dq["n"] += 1
        self.nins += 1
        for b in R:
            b.r[("dma", q, dq["n"])] = ev
        for b in W:
            b.w = ev
            b.r = {}
        return ev

    def barrier(self):
        evs = [Ev("c:" + k, self.cnt[k], {}, "x") for k in ENG if self.cnt[k] > 0]
        for dq in self.dq.values():
            evs += [p for p in dq["pending"] if p is not None]
        for eng in ENG:
            for ev in evs:
                self._wait(eng, ev)

    def finish(self):
        for dq in self.dq.values():
            for p in dq["pending"]:
                self._wait("sp", p)

    def alt(self):
        self.rr += 1
        return "act" if (self.rr & 1) else "dve"


class Ctx:
    pass


def _sb(K, es, name, shape, dt):
    K.uid = getattr(K, "uid", 0) + 1
    name = "%s_u%d" % (name, K.uid)
    return Tl(es.enter_context(K.nc.sbuf_tensor(name, list(shape), dt)), name)


def _evac(K, eng, out, in_, R, W, scale=None):
    nc, S = K.nc, K.S
    if eng == "act":
        if scale is None:
            S.op("act", lambda: nc.scalar.copy(out=out, in_=in_), R=R, W=W)
        else:
            S.op("act", lambda: nc.scalar.mul(out=out, in_=in_, mul=scale), R=R, W=W)
    else:
        if scale is None:
            S.op("dve", lambda: nc.vector.tensor_copy(out=out, in_=in_), R=R, W=W)
        else:
            S.op("dve", lambda: nc.vector.tensor_scalar_mul(out=out, in0=in_, scalar1=scale), R=R, W=W)


def _ps(K):
    K.psi = (K.psi + 1) % len(K.psum)
    return K.psum[K.psi]


def _ln_tile(K, x, out, g, b, sm):
    nc, S = K.nc, K.S
    st, mv, rstd, tmp = sm["st"], sm["mv"], sm["rstd"], sm["tmp"]
    S.op("dve", lambda: nc.vector.bn_stats(out=st[:, 0, :], in_=x[:, 0:512]), R=[x], W=[st])
    S.op("dve", lambda: nc.vector.bn_stats(out=st[:, 1, :], in_=x[:, 512:1024]), R=[x, st], W=[st])
    S.op("dve", lambda: nc.vector.bn_aggr(out=mv[:], in_=st[:].rearrange("p a b -> p (a b)")), R=[st], W=[mv])
    S.op("act", lambda: nc.scalar.activation(out=rstd[:], in_=mv[:, 1:2], func=AF.Sqrt, bias=K.eps_ln[:, 0:1], scale=1.0),
         R=[mv, K.eps_ln], W=[rstd])
    S.op("dve", lambda: nc.vector.reciprocal(out=rstd[:], in_=rstd[:]), R=[rstd], W=[rstd])
    S.op("dve", lambda: nc.vector.tensor_scalar(out=tmp[:], in0=x[:], scalar1=mv[:, 0:1], scalar2=rstd[:, 0:1],
                                                op0=ALU.subtract, op1=ALU.mult), R=[x, mv, rstd], W=[tmp])
    S.op("pool", lambda: nc.gpsimd.tensor_tensor(out=tmp[:], in0=tmp[:], in1=g[:], op=ALU.mult), R=[tmp, g], W=[tmp])
    S.op("pool", lambda: nc.gpsimd.tensor_tensor(out=out[:], in0=tmp[:], in1=b[:], op=ALU.add), R=[tmp, b], W=[out])


def _load_cast(K, dst_tl, dst_ap, src_ap, shape, Wb):
    nc, S = K.nc, K.S
    K.stgi = (K.stgi + 1) % len(K.stg)
    st = K.stg[K.stgi]
    n = int(np.prod(shape[1:]))
    if len(shape) == 3:
        v = st.h[:, 0:n].rearrange("p (a b) -> p a b", a=shape[1])
    else:
        v = st.h[:, 0:n]
    S.dma("sp", v, src_ap, W=[st])
    S.op("pool", lambda: nc.gpsimd.tensor_copy(out=dst_ap, in_=v), R=[st], W=[Wb])


def phase_input(K):
    nc, S, NT = K.nc, K.S, K.NT
    with contextlib.ExitStack() as es:
        g = _sb(K, es, "pi_g", [128, D], F32)
        b = _sb(K, es, "pi_b", [128, D], F32)
        S.dma("sp", g[:], K.d["ln_in_g"].partition_broadcast(128), W=[g])
        S.dma("sp", b[:], K.d["ln_in_b"].partition_broadcast(128), W=[b])
        xs = [_sb(K, es, "pi_x%d" % i, [128, D], F32) for i in range(2)]
        os_ = [_sb(K, es, "pi_o%d" % i, [128, D], F32) for i in range(2)]
        sm = {"st": _sb(K, es, "pi_st", [128, 2, 6], F32), "mv": _sb(K, es, "pi_mv", [128, 2], F32),
              "rstd": _sb(K, es, "pi_rs", [128, 1], F32), "tmp": _sb(K, es, "pi_tmp", [128, D], F32)}
        PT = K.SEQ + 64
        if NT * 128 > PT:
            zb = _sb(K, es, "pi_zb", [128, 512], BF16)
            S.op("pool", lambda: nc.gpsimd.memset(zb[:], 0.0), W=[zb])
            S.dma("sp", K.d["mixed"][PT:NT * 128, 512:1024], zb[0:NT * 128 - PT, :], R=[zb], W=[K.sbuf_mx_gd])
        for t in range(NT):
            x, o = xs[t % 2], os_[t % 2]
            lo = 128 * t - 64
            r0, r1 = max(lo, 0), min(lo + 128, K.SEQ)
            if t == 0 or r1 - lo < 128:
                S.op("pool", lambda: nc.gpsimd.memset(x[:], 0.0), W=[x])
            if t == 0:
                S.dma("sp", x[48:64, :], K.d["meta"][:, :], W=[x])
            if r1 > r0:
                S.dma("sp", x[r0 - lo:r1 - lo, :], K.d["x"][r0:r1, :], W=[x])
            _ln_tile(K, x, o, g, b, sm)
            S.dma("sp", K.d["h"][128 * t:128 * (t + 1), :], o[:], R=[o], W=[K.hbuf[t]])


def phase_A1(K, l):
    nc, S, NT = K.nc, K.S, K.NT
    with contextlib.ExitStack() as es:
        K.stg = [_sb(K, es, "stg%d" % i, [128, IN_W], F32) for i in range(2)]
        Wb = _sb(K, es, "a1_W", [128, KC, IN_W], BF16)
        wbufs = [Buf() for _ in range(KC)]
        wsrc = K.d["w_in"][l].rearrange("(k p) n -> p k n", p=128)
        for kc in range(KC):
            _load_cast(K, Wb, Wb[:, kc, :], wsrc[:, kc, :], [128, IN_W], wbufs[kc])
        dtb = _sb(K, es, "a1_dtb", [128, 4], F32)
        nea = _sb(K, es, "a1_nea", [128, 4], F32)
        S.dma("sp", dtb[:], K.d["dt_bias"][l].partition_broadcast(128), W=[dtb])
        S.dma("sp", nea[:], K.d["a_log"][l].partition_broadcast(128), W=[nea])
        S.op("act", lambda: nc.scalar.activation(out=nea[:], in_=nea[:], func=AF.Exp), R=[nea], W=[nea])
        S.op("dve", lambda: nc.vector.tensor_scalar_mul(out=nea[:], in0=nea[:], scalar1=-1.0), R=[nea], W=[nea])
        hts = [_sb(K, es, "a1_h%d" % i, [128, 4, D], F32) for i in range(1)]
        hTs = [_sb(K, es, "a1_hT%d" % i, [128, KC, 512], BF16) for i in range(2)]
        stq = [_sb(K, es, "a1_sq%d" % i, [128, 512], BF16) for i in range(3)]
        stg = [_sb(K, es, "a1_sg%d" % i, [128, 512], F32) for i in range(3)]
        stv = [_sb(K, es, "a1_sv%d" % i, [128, 4, 512], BF16) for i in range(2)]
        stz = [_sb(K, es, "a1_sz%d" % i, [128, 4, 512], F32) for i in range(2)]
        stb = [_sb(K, es, "a1_sb%d" % i, [128, 4, 8], F32) for i in range(2)]
        tb = _sb(K, es, "a1_tb", [128, 4, 4], F32)
        NG = (NT + 3) // 4
        for g in range(NG):
            nt = min(4, NT - 4 * g)
            n = nt * 128
            c0 = g * 512
            ht, hT = hts[0], hTs[g % 2]
            sv, sz, sbb = stv[g % 2], stz[g % 2], stb[g % 2]
            S.dma("sp", ht[:, 0:nt, :], K.d["h"][c0:c0 + n, :].rearrange("(t p) d -> p t d", p=128),
                  R=K.hbuf[4 * g:4 * g + nt], W=[ht])
            for kc in range(KC):
                ps = _ps(K)
                for t in range(nt):
                    S.op("pe", lambda: nc.tensor.transpose(out=ps[:, t * 128:(t + 1) * 128],
                                                           in_=ht[:, t, kc * 128:(kc + 1) * 128], identity=K.identf[:]),
                         R=[ht, K.identf], W=[ps])
                _evac(K, S.alt(), hT[:, kc, 0:n], ps[:, 0:n], [ps], [hT])
            for ci in range(20):
                if ci < 8:
                    col = ci * 128
                else:
                    col = 1536 + (ci - 8) * 128
                ps = _ps(K)
                for kc in range(KC):
                    S.op("pe", lambda: nc.tensor.matmul(ps[:, 0:n], lhsT=Wb[:, kc, col:col + 128], rhs=hT[:, kc, 0:n],
                                                        start=(kc == 0), stop=(kc == KC - 1)),
                         R=[wbufs[kc], hT], W=[ps])
                if ci < 8:
                    sq = stq[ci % 3]
                    _evac(K, S.alt(), sq[:, 0:n], ps[:, 0:n], [ps], [sq], scale=(0.125 if ci < 4 else None))
                    if ci < 4:
                        S.dma("sp", K.d["qT"][ci, :, c0:c0 + n], sq[:, 0:n], R=[sq], W=[K.sbuf_q])
                    else:
                        S.dma("sp", K.d["kT"][ci - 4, :, c0:c0 + n], sq[:, 0:n], R=[sq], W=[K.sbuf_k])
                else:
                    sg = stg[ci % 3]
                    _evac(K, S.alt(), sg[:, 0:n], ps[:, 0:n], [ps], [sg])
                    S.dma("sp", K.d["gT"][ci - 8, :, c0:c0 + n], sg[:, 0:n], R=[sg], W=[K.sbuf_g])
            for t in range(nt):
                ps = _ps(K)
                for kc in range(KC):
                    S.op("pe", lambda: nc.tensor.matmul(ps[:, :], lhsT=hT[:, kc, t * 128:(t + 1) * 128], rhs=Wb[:, kc, 1024:1536],
                                                        start=(kc == 0), stop=(kc == KC - 1)), R=[wbufs[kc], hT], W=[ps])
                _evac(K, S.alt(), sv[:, t, :], ps[:, :], [ps], [sv])
                ps = _ps(K)
                for kc in range(KC):
                    S.op("pe", lambda: nc.tensor.matmul(ps[:, :], lhsT=hT[:, kc, t * 128:(t + 1) * 128], rhs=Wb[:, kc, 3072:3584],
                                                        start=(kc == 0), stop=(kc == KC - 1)), R=[wbufs[kc], hT], W=[ps])
                S.op("act", lambda: nc.scalar.activation(out=sz[:, t, :], in_=ps[:, :], func=AF.Silu), R=[ps], W=[sz])
                ps = _ps(K)
                for kc in range(KC):
                    S.op("pe", lambda: nc.tensor.matmul(ps[:, 0:8], lhsT=hT[:, kc, t * 128:(t + 1) * 128], rhs=Wb[:, kc, 3584:3592],
                                                        start=(kc == 0), stop=(kc == KC - 1)), R=[wbufs[kc], hT], W=[ps])
                S.op("act", lambda: nc.scalar.activation(out=sbb[:, t, 0:4], in_=ps[:, 0:4], func=AF.Sigmoid), R=[ps], W=[sbb])
                S.op("dve", lambda: nc.vector.tensor_tensor(out=tb[:, t, :], in0=ps[:, 4:8], in1=dtb[:], op=ALU.add),
                     R=[ps, dtb], W=[tb])
            S.op("act", lambda: nc.scalar.activation(out=tb[:, 0:nt, :], in_=tb[:, 0:nt, :], func=AF.Exp), R=[tb], W=[tb])
            S.op("act", lambda: nc.scalar.activation(out=tb[:, 0:nt, :], in_=tb[:, 0:nt, :], func=AF.Ln, bias=K.one_c[:, 0:1], scale=1.0),
                 R=[tb, K.one_c], W=[tb])
            S.op("dve", lambda: nc.vector.tensor_tensor(out=sbb[:, 0:nt, 4:8], in0=tb[:, 0:nt, :],
                                                        in1=nea[:].unsqueeze(1).to_broadcast([128, nt, 4]), op=ALU.mult),
                 R=[tb, nea], W=[sbb])
            S.dma("sp", K.d["V"][c0:c0 + n, :].rearrange("(t p) c -> p t c", p=128), sv[:, 0:nt, :], R=[sv], W=[K.sbuf_v])
            S.dma("sp", K.d["zs"][c0:c0 + n, :].rearrange("(t p) c -> p t c", p=128), sz[:, 0:nt, :], R=[sz], W=[K.sbuf_z])
            S.dma("sp", K.d["bg"][c0:c0 + n, :].rearrange("(t p) c -> p t c", p=128), sbb[:, 0:nt, :], R=[sbb], W=[K.sbuf_bg])


class RR:
    def __init__(self):
        self.items = []

    def add(self, gen, w=1):
        self.items.append([gen, w])

    def run(self):
        while self.items:
            for item in list(self.items):
                for _ in range(item[1]):
                    try:
                        next(item[0])
                    except StopIteration:
                        self.items.remove(item)
                        break


def _psg(K):
    K.psgi = (K.psgi + 1) % len(K.psum_gd)
    return K.psum_gd[K.psgi]


def sb_stream(K, l, es):
    nc, S, NT = K.nc, K.S, K.NT
    P = NT * 128
    qT = _sb(K, es, "sb_q", [128, P], BF16)
    kT = _sb(K, es, "sb_k", [128, P], BF16)
    Vt = _sb(K, es, "sb_v", [128, NT, 128], BF16)
    sbg = _sb(K, es, "sb_g", [128, 64], F32)
    S.dma("sp", sbg[:], K.d["sb_norm_g"][l].partition_broadcast(128), W=[sbg])
    es_ = [_sb(K, es, "sb_e%d" % i, [128, 512], F32) for i in range(2)]
    sps = [_sb(K, es, "sb_sp%d" % i, [128, 512], BF16) for i in range(4)]
    ws = [_sb(K, es, "sb_w%d" % i, [128, 512], BF16) for i in range(3)]
    oaccs = [_sb(K, es, "sb_oa%d" % i, [128, 4, 64], F32) for i in range(2)]
    raccs = [_sb(K, es, "sb_ra%d" % i, [128, 4], F32) for i in range(2)]
    eRs = [_sb(K, es, "sb_eR%d" % i, [128, 4], F32) for i in range(2)]
    tmps = [_sb(K, es, "sb_tmp%d" % i, [128, 4, 64], F32) for i in range(2)]
    osbs = [_sb(K, es, "sb_os%d" % i, [128, 4, 128], BF16) for i in range(2)]
    ss = _sb(K, es, "sb_ss", [128, 4], F32)
    zb = K.psum[0:3]
    pb = K.psum[3:5]
    NG = (NT + 3) // 4
    gi = 0
    for hp in range(4):
        S.dma("sp", qT[:, :], K.d["qT"][hp], R=[K.sbuf_q], W=[qT])
        S.dma("sp", kT[:, :], K.d["kT"][hp], R=[K.sbuf_k], W=[kT])
        S.dma("sp", Vt[:, :, :], K.d["V"][:, hp * 128:(hp + 1) * 128].rearrange("(t p) c -> p t c", p=128), R=[K.sbuf_v], W=[Vt])
        its = []
        for qg in range(NG):
            nt = min(4, NT - 4 * qg)
            for h2 in range(2):
                kbs = list(range(4 * qg + nt - 1, -1, -1))
                for j, kb in enumerate(kbs):
                    its.append((qg, nt, h2, kb, j == 0, j == len(kbs) - 1, gi))
                gi += 1
        N = len(its)

        def geom(it):
            qg, nt, h2, kb = it[0], it[1], it[2], it[3]
            rel = kb - 4 * qg
            if rel >= 0:
                mi = 4 if kb == 0 else rel
            elif kb == 0:
                mi = 5
            else:
                mi = None
            lo = max(rel, 0)
            return rel, mi, lo, lo * 128, nt * 128, qg * 512

        def stA(i):
            it = its[i]
            qg, nt, h2, kb = it[0], it[1], it[2], it[3]
            rel, mi, lo, c0, n, q0 = geom(it)
            z, e, sp = zb[i % 3], es_[i % 2], sps[i % 4]
            r0 = h2 * 64
            kk = kT[r0:r0 + 64, kb * 128:(kb + 1) * 128]
            qq = qT[r0:r0 + 64, q0 + c0:q0 + n]
            S.op("pe", lambda: nc.tensor.matmul(z[:, c0:n], lhsT=kk, rhs=qq, start=True, stop=(mi is None)), R=[kT, qT], W=[z])
            if mi is not None:
                S.op("pe", lambda: nc.tensor.matmul(z[:, c0:n], lhsT=K.identb[:], rhs=K.masks[:, mi, c0:n], start=False, stop=True),
                     R=[K.identb, K.masks], W=[z])
            S.op("act", lambda: nc.scalar.activation(out=e[:, c0:n], in_=z[:, c0:n], func=AF.Exp), R=[z], W=[e])
            S.op("act", lambda: nc.scalar.activation(out=sp[:, c0:n], in_=e[:, c0:n], func=AF.Ln, bias=K.one_c[:, 0:1], scale=1.0),
                 R=[e, K.one_c], W=[sp])

        def stB(i):
            it = its[i]
            qg, nt, h2, kb = it[0], it[1], it[2], it[3]
            rel, mi, lo, c0, n, q0 = geom(it)
            z, sp, w = zb[i % 3], sps[i % 4], ws[i % 3]
            r0 = h2 * 64
            kk = kT[r0:r0 + 64, kb * 128:(kb + 1) * 128]
            qq = qT[r0:r0 + 64, q0 + c0:q0 + n]
            S.op("pe", lambda: nc.tensor.matmul(z[:, c0:n], lhsT=kk, rhs=qq, start=True, stop=False), R=[kT, qT], W=[z])
            if mi is not None:
                S.op("pe", lambda: nc.tensor.matmul(z[:, c0:n], lhsT=K.identb[:], rhs=K.masks[:, mi, c0:n], start=False, stop=False),
                     R=[K.identb, K.masks], W=[z])
            S.op("pe", lambda: nc.tensor.matmul(z[:, c0:n], lhsT=K.negtri[:], rhs=sp[:, c0:n], start=False, stop=True),
                 R=[K.negtri, sp], W=[z])
            S.op("act", lambda: nc.scalar.activation(out=w[:, c0:n], in_=z[:, c0:n], func=AF.Exp), R=[z], W=[w])

        def stC(i):
            it = its[i]
            qg, nt, h2, kb, first, last, g_ = it
            rel, mi, lo, c0, n, q0 = geom(it)
            sp, w = sps[i % 4], ws[i % 3]
            oacc, racc = oaccs[g_ % 2], raccs[g_ % 2]
            eR, tmp = eRs[i % 2], tmps[i % 2]
            osb = osbs[qg % 2]
            if first:
                S.op("pool", lambda: nc.gpsimd.memset(oacc[:], 0.0), W=[oacc])
                S.op("pool", lambda: nc.gpsimd.memset(racc[:], 0.0), W=[racc])
            po = pb[i % 2]
            pov = po.h[:, 0:260].rearrange("p (t c) -> p t c", c=65)
            for qt in range(lo, nt):
                S.op("pe", lambda: nc.tensor.matmul(pov[:, qt, 0:64], lhsT=w[:, qt * 128:(qt + 1) * 128],
                                                    rhs=Vt[:, kb, h2 * 64:(h2 + 1) * 64], start=True, stop=True),
                     R=[w, Vt], W=[po])
                S.op("pe", lambda: nc.tensor.matmul(pov[:, qt, 64:65], lhsT=sp[:, qt * 128:(qt + 1) * 128],
                                                    rhs=K.onesb[:, 0:1], start=True, stop=True),
                     R=[sp, K.onesb], W=[po])
            S.op("act", lambda: nc.scalar.activation(out=eR[:, lo:nt], in_=racc[:, lo:nt], func=AF.Exp, scale=-1.0),
                 R=[racc], W=[eR])
            S.op("dve", lambda: nc.vector.tensor_tensor(out=tmp[:, lo:nt, :], in0=pov[:, lo:nt, 0:64],
                                                        in1=eR[:, lo:nt].unsqueeze(2).to_broadcast([128, nt - lo, 64]), op=ALU.mult),
                 R=[po, eR], W=[tmp])
            S.op("pool", lambda: nc.gpsimd.tensor_tensor(out=oacc[:, lo:nt, :], in0=oacc[:, lo:nt, :], in1=tmp[:, lo:nt, :], op=ALU.add),
                 R=[oacc, tmp], W=[oacc])
            S.op("dve", lambda: nc.vector.tensor_tensor(out=racc[:, lo:nt], in0=racc[:, lo:nt], in1=pov[:, lo:nt, 64], op=ALU.add),
                 R=[racc, po], W=[racc])
            if last:
                tmp2 = tmps[(i + 1) % 2]
                S.op("dve", lambda: nc.vector.tensor_tensor(out=tmp2[:, 0:nt, :], in0=oacc[:, 0:nt, :], in1=oacc[:, 0:nt, :], op=ALU.mult),
                     R=[oacc], W=[tmp2])
                S.op("dve", lambda: nc.vector.tensor_reduce(out=ss[:, 0:nt], in_=tmp2[:, 0:nt, :], axis=AX.X, op=ALU.add), R=[tmp2], W=[ss])
                S.op("act", lambda: nc.scalar.activation(out=ss[:, 0:nt], in_=ss[:, 0:nt], func=AF.Ln, bias=K.eps_rms64[:, 0:1], scale=1.0),
                     R=[ss, K.eps_rms64], W=[ss])
                S.op("act", lambda: nc.scalar.activation(out=ss[:, 0:nt], in_=ss[:, 0:nt], func=AF.Exp, scale=-0.5), R=[ss], W=[ss])
                S.op("dve", lambda: nc.vector.scalar_tensor_tensor(out=tmp2[:, 0:nt, :], in0=oacc[:, 0:nt, :], scalar=8.0,
                                                                  in1=ss[:, 0:nt].unsqueeze(2).to_broadcast([128, nt, 64]),
                                                                  op0=ALU.mult, op1=ALU.mult),
                     R=[oacc, ss], W=[tmp2])
                S.op("dve", lambda: nc.vector.tensor_tensor(out=osb[:, 0:nt, h2 * 64:(h2 + 1) * 64], in0=tmp2[:, 0:nt, :],
                                                            in1=sbg[:].unsqueeze(1).to_broadcast([128, nt, 64]), op=ALU.mult),
                     R=[tmp2, sbg], W=[osb])
                if h2 == 1:
                    S.dma("sp", K.d["mixed"][q0:q0 + n, hp * 128:(hp + 1) * 128].rearrange("(t p) c -> p t c", p=128), osb[:, 0:nt, :],
                          R=[osb], W=[K.sbuf_mx_sb])

        for s in range(N + 2):
            if s < N:
                stA(s)
            if 0 <= s - 1 < N:
                stB(s - 1)
            if 0 <= s - 2 < N:
                stC(s - 2)
            yield


def gdn_master(K, l, es, rr, NSETS, YK):
    nc, S, NT = K.nc, K.S, K.NT
    NC_ = K.NC
    cw = _sb(K, es, "gd_cw", [128, 12, 4], F32)
    S.dma("sp", cw[:], K.d["conv_wT"][l].rearrange("(i d) t -> d i t", d=128), W=[cw])
    gng = _sb(K, es, "gd_gng", [64, 4, 128], F32)
    for h in range(4):
        S.dma("sp", gng[:, h, :], K.d["gdn_norm_g"][l].partition_broadcast(64), W=[gng])
    xin = [_sb(K, es, "gd_x%d" % i, [128, 12, 131], F32) for i in range(2)]
    cvs = [_sb(K, es, "gd_cv%d" % i, [128, 12, 128], F32) for i in range(2)]
    sq = [_sb(K, es, "gd_sq%d" % i, [128, 128], F32) for i in range(2)]
    rn = [_sb(K, es, "gd_rn%d" % i, [128, 128], F32) for i in range(2)]
    Sst = _sb(K, es, "gd_S", [128, 4, 128], F32)
    S.op("pool", lambda: nc.gpsimd.memset(Sst[:], 0.0), W=[Sst])
    tmpS = _sb(K, es, "gd_tmpS", [128, 4, 128], F32)

    def mk(name, shape, dt=F32):
        return [_sb(K, es, "gd_%s%d" % (name, i), shape, dt) for i in range(NSETS)]
    B = {}
    for name in ("egc", "egr", "ssq"):
        B[name] = mk(name, [64, 4])
    B["cdt"] = mk("cd", [128, 4])
    for name in ("gl", "dm", "dmT", "Nm", "Mm", "N2", "M2", "Pm", "qkT"):
        B[name] = mk(name, [64, 4, 64])
    for name in ("kbg", "vb", "ke", "gbc", "u", "vn", "ot", "o2"):
        B[name] = mk(name, [64, 4, 128])
    for name in ("egb", "qd", "wcT"):
        B[name] = mk(name, [128, 4, 64])
    B["ob"] = mk("ob", [64, 4, 128], BF16)
    B["bg"] = mk("bg", [64, 8])
    B["z"] = mk("z", [64, 512])
    st = {"inflight": 0, "rec_done": 0, "done": 0}
    assert NSETS == 2
    pbigs = [K.psum[5], K.psum[6]]
    psmls = pbigs
    psg1 = K.psum[7]
    n = 128

    def load_x(g):
        x = xin[g % 2]
        c0 = g * 128
        if g == 0:
            S.op("pool", lambda: nc.gpsimd.memset(x[:, :, 0:3], 0.0), W=[x])
            S.dma("sp", x[:, :, 3:3 + n], K.d["gT"][:, :, 0:n].rearrange("h p n -> p h n"), R=[K.sbuf_g], W=[x])
            S.op("pool", lambda: nc.gpsimd.memset(x[:, :, 3:3 + 48], 0.0), W=[x])
        else:
            S.dma("sp", x[:, :, 0:3 + n], K.d["gT"][:, :, c0 - 3:c0 + n].rearrange("h p n -> p h n"), R=[K.sbuf_g], W=[x])

    def G1(g):
        le = None
        x, cv = xin[g % 2], cvs[g % 2]
        k = 0
        for i in range(12):
            eng = "dve" if i % 2 == 0 else "pool"
            E = nc.vector if eng == "dve" else nc.gpsimd
            if le != eng:
                yield
            le = eng
            S.op(eng, lambda: E.tensor_scalar(out=cv[:, i, 0:n], in0=x[:, i, 0:n], scalar1=cw[:, i, 0:1], scalar2=None, op0=ALU.mult),
                 R=[x, cw], W=[cv])
            for tp in range(1, 4):
                if le != "dve":
                    yield
                le = "dve"
                S.op("dve", lambda: nc.vector.scalar_tensor_tensor(out=cv[:, i, 0:n], in0=x[:, i, tp:tp + n], scalar=cw[:, i, tp:tp + 1],
                                                                  in1=cv[:, i, 0:n], op0=ALU.mult, op1=ALU.add), R=[x, cw, cv], W=[cv])
            s_ = sq[i % 2]
            if le != "act":
                yield
            le = "act"
            S.op("act", lambda: nc.scalar.activation(out=s_[:, 0:n], in_=cv[:, i, 0:n], func=AF.Exp, scale=-1.0), R=[cv], W=[s_])
            if le != "pool":
                yield
            le = "pool"
            S.op("pool", lambda: nc.gpsimd.tensor_scalar(out=s_[:, 0:n], in0=s_[:, 0:n], scalar1=1.0, scalar2=None, op0=ALU.add),
                 R=[s_], W=[s_])
            if le != "dve":
                yield
            le = "dve"
            S.op("dve", lambda: nc.vector.reciprocal(out=s_[:, 0:n], in_=s_[:, 0:n]), R=[s_], W=[s_])
            S.op("dve", lambda: nc.vector.tensor_tensor(out=cv[:, i, 0:n], in0=cv[:, i, 0:n], in1=s_[:, 0:n], op=ALU.mult), R=[cv, s_], W=[cv])
        for i in range(8):
            s_, r_ = sq[i % 2], rn[i % 2]
            if le != "pool":
                yield
            le = "pool"
            S.op("pool", lambda: nc.gpsimd.tensor_tensor(out=s_[:, 0:n], in0=cv[:, i, 0:n], in1=cv[:, i, 0:n], op=ALU.mult), R=[cv], W=[s_])
            ps = psg1
            if le != "pe":
                yield
            le = "pe"
            S.op("pe", lambda: nc.tensor.matmul(ps[:, 0:n], lhsT=K.onesf[:, :], rhs=s_[:, 0:n], start=True, stop=True),
                 R=[K.onesf, s_], W=[ps])
            if le != "act":
                yield
            le = "act"
            S.op("act", lambda: nc.scalar.activation(out=r_[:, 0:n], in_=ps[:, 0:n], func=AF.Ln, bias=K.eps_rms[:, 0:1], scale=1.0),
                 R=[ps, K.eps_rms], W=[r_])
            S.op("act", lambda: nc.scalar.activation(out=r_[:, 0:n], in_=r_[:, 0:n], func=AF.Exp, scale=-0.5), R=[r_], W=[r_])
            if i < 4:
                if le != "dve":
                    yield
                le = "dve"
                S.op("dve", lambda: nc.vector.scalar_tensor_tensor(out=cv[:, i, 0:n], in0=cv[:, i, 0:n], scalar=float(128 ** -0.5),
                                                                  in1=r_[:, 0:n], op0=ALU.mult, op1=ALU.mult), R=[cv, r_], W=[cv])
            else:
                if le != "dve":
                    yield
                le = "dve"
                S.op("dve", lambda: nc.vector.tensor_tensor(out=cv[:, i, 0:n], in0=cv[:, i, 0:n], in1=r_[:, 0:n], op=ALU.mult),
                     R=[cv, r_], W=[cv])

    def chunk(g, ci, cidx):
        le = None
        b2 = cidx % NSETS
        pbig, psml = pbigs[b2], psmls[b2]
        cv = cvs[g % 2]
        egc, egr, cdt, ssq = B["egc"][b2], B["egr"][b2], B["cdt"][b2], B["ssq"][b2]
        gl, dm, dmT, Pm, qkT = B["gl"][b2], B["dm"][b2], B["dmT"][b2], B["Pm"][b2], B["qkT"][b2]
        kbg, vb, ke, gbc, u, vn, ot, o2 = (B[k_][b2] for k_ in ("kbg", "vb", "ke", "gbc", "u", "vn", "ot", "o2"))
        egb, qd, wcT, ob = B["egb"][b2], B["qd"][b2], B["wcT"][b2], B["ob"][b2]
        bgc, zc = B["bg"][b2], B["z"][b2]
        p0 = g * 128 + ci * 64
        if le != "sp":
            yield
        le = "sp"
        S.dma("sp", bgc[:, :], K.d["bg"][p0:p0 + 64, :], R=[K.sbuf_bg], W=[bgc])
        if le != "sp":
            yield
        le = "sp"
        S.dma("sp", zc[:, :], K.d["zs"][p0:p0 + 64, :], R=[K.sbuf_z], W=[zc])
        if cidx == 0:
            if le != "pool":
                yield
            le = "pool"
            S.op("pool", lambda: nc.gpsimd.memset(bgc[0:48, 4:8], 0.0), W=[bgc])
        for _ in range(DMA_PAD):
            yield
        cs = slice(ci * 64, ci * 64 + 64)
        gcol = bgc[:, 4:8]
        bcol = bgc[:, 0:4]
        pg = psml
        if le != "pe":
            yield
        le = "pe"
        S.op("pe", lambda: nc.tensor.matmul(pg[0:64, 0:4], lhsT=K.triu[:, 0, :], rhs=gcol, start=True, stop=True), R=[K.triu, bgc], W=[pg])
        if le != "pe":
            yield
        le = "pe"
        S.op("pe", lambda: nc.tensor.matmul(pg[0:64, 4:8], lhsT=K.sgt[:, :], rhs=gcol, start=True, stop=True), R=[K.sgt, bgc], W=[pg])
        if le != "pe":
            yield
        le = "pe"
        S.op("pe", lambda: nc.tensor.matmul(pg[:, 8:12], lhsT=K.ones64[:, :], rhs=gcol, start=True, stop=True), R=[K.ones64, bgc], W=[pg])
        if le != "act":
            yield
        le = "act"
        S.op("act", lambda: nc.scalar.activation(out=egc[:], in_=pg[0:64, 0:4], func=AF.Exp), R=[pg], W=[egc])
        if le != "act":
            yield
        le = "act"
        S.op("act", lambda: nc.scalar.activation(out=egr[:], in_=pg[0:64, 4:8], func=AF.Exp), R=[pg], W=[egr])
        if le != "act":
            yield
        le = "act"
        S.op("act", lambda: nc.scalar.activation(out=cdt[:], in_=pg[:, 8:12], func=AF.Exp), R=[pg], W=[cdt])
        if le != "dve":
            yield
        le = "dve"
        S.op("dve", lambda: nc.vector.tensor_tensor(out=gl[:], in0=K.triu[:], in1=gcol.unsqueeze(2).to_broadcast([64, 4, 64]), op=ALU.mult),
             R=[K.triu, bgc], W=[gl])
        pd = pbig
        pdv = pd.h[0:64, 0:512].rearrange("p (a h c) -> p a h c", a=2, h=4)
        for h in range(4):
            if le != "pe":
                yield
            le = "pe"
            S.op("pe", lambda: nc.tensor.matmul(pdv[:, 0, h, :], lhsT=gl[:, h, :], rhs=K.sgt[:, :], start=True, stop=True),
                 R=[gl, K.sgt], W=[pd])
        if le != "pe":
            yield
        le = "pe"
        S.op("pe", lambda: nc.tensor.matmul(pd[0:64, 256:512], lhsT=K.sgt[:, :], rhs=gl[:].rearrange("p h c -> p (h c)"), start=True, stop=True),
             R=[gl, K.sgt], W=[pd])
        if le != "act":
            yield
        le = "act"
        S.op("act", lambda: nc.scalar.activation(out=dm[:], in_=pdv[:, 0], func=AF.Exp), R=[pd], W=[dm])
        if le != "act":
            yield
        le = "act"
        S.op("act", lambda: nc.scalar.activation(out=dmT[:], in_=pdv[:, 1], func=AF.Exp), R=[pd], W=[dmT])
        if le != "pool":
            yield
        le = "pool"
        S.op("pool", lambda: nc.gpsimd.tensor_tensor(out=dm[:], in0=dm[:], in1=K.trilsn[:], op=ALU.mult), R=[dm, K.trilsn], W=[dm])
        if le != "pool":
            yield
        le = "pool"
        S.op("pool", lambda: nc.gpsimd.tensor_tensor(out=dmT[:], in0=dmT[:], in1=K.triu[:], op=ALU.mult), R=[dmT, K.triu], W=[dmT])
        pgq = pbig
        pgqv = pgq.h[0:64, 0:512].rearrange("p (a h c) -> p a h c", a=2, h=4)
        for h in range(4):
            if le != "pe":
                yield
            le = "pe"
            S.op("pe", lambda: nc.tensor.matmul(pgqv[:, 0, h, :], lhsT=cv[:, 4 + h, cs], rhs=cv[:, 4 + h, cs], start=True, stop=True), R=[cv], W=[pgq])
            if le != "pe":
                yield
            le = "pe"
            S.op("pe", lambda: nc.tensor.matmul(pgqv[:, 1, h, :], lhsT=cv[:, 4 + h, cs], rhs=cv[:, h, cs], start=True, stop=True), R=[cv], W=[pgq])
        Nm, Mm = B["Nm"][b2], B["Mm"][b2]
        if le != "dve":
            yield
        le = "dve"
        S.op("dve", lambda: nc.vector.tensor_tensor(out=Nm[:], in0=pgqv[:, 0], in1=bcol.unsqueeze(2).to_broadcast([64, 4, 64]), op=ALU.mult),
             R=[pgq, bgc], W=[Nm])
        if le != "dve":
            yield
        le = "dve"
        S.op("dve", lambda: nc.vector.tensor_tensor(out=Nm[:], in0=Nm[:], in1=dm[:], op=ALU.mult), R=[Nm, dm], W=[Nm])
        if le != "dve":
            yield
        le = "dve"
        S.op("dve", lambda: nc.vector.tensor_tensor(out=qkT[:], in0=pgqv[:, 1], in1=dmT[:], op=ALU.mult), R=[pgq, dmT], W=[qkT])
        pt = psml
        ptv = pt.h[0:64, 0:256].rearrange("p (h c) -> p h c", h=4)
        for h in range(4):
            if le != "pe":
                yield
            le = "pe"
            S.op("pe", lambda: nc.tensor.transpose(out=ptv[:, h, :], in_=Nm[:, h, :], identity=K.identf[0:64, 0:64]), R=[Nm, K.identf], W=[pt])
        if le != "act":
            yield
        le = "act"
        S.op("act", lambda: nc.scalar.copy(out=Mm[:], in_=ptv), R=[pt], W=[Mm])
        if le != "dve":
            yield
        le = "dve"
        S.op("dve", lambda: nc.vector.tensor_tensor(out=Pm[:], in0=Mm[:], in1=K.ident4[:], op=ALU.add), R=[Mm, K.ident4], W=[Pm])
        Nc, Mc, Nn, Mn = Nm, Mm, B["N2"][b2], B["M2"][b2]
        for r in range(5):
            pn = pbig
            pnv = pn.h[0:64, 0:512].rearrange("p (a h c) -> p a h c", a=2, h=4)
            for h in range(4):
                if le != "pe":
                    yield
                le = "pe"
                S.op("pe", lambda: nc.tensor.matmul(pnv[:, 0, h, :], lhsT=Mc[:, h, :], rhs=Nc[:, h, :], start=True, stop=True), R=[Mc, Nc], W=[pn])
                if r < 4:
                    if le != "pe":
                        yield
                    le = "pe"
                    S.op("pe", lambda: nc.tensor.matmul(pnv[:, 1, h, :], lhsT=Nc[:, h, :], rhs=Mc[:, h, :], start=True, stop=True), R=[Mc, Nc], W=[pn])
            if le != "act":
                yield
            le = "act"
            S.op("act", lambda: nc.scalar.copy(out=Nn[:], in_=pnv[:, 0]), R=[pn], W=[Nn])
            if r < 4:
                if le != "dve":
                    yield
                le = "dve"
                S.op("dve", lambda: nc.vector.tensor_copy(out=Mn[:], in_=pnv[:, 1]), R=[pn], W=[Mn])
            pp = psml
            ppv = pp.h[0:64, 0:256].rearrange("p (h c) -> p h c", h=4)
            for h in range(4):
                if le != "pe":
                    yield
                le = "pe"
                S.op("pe", lambda: nc.tensor.matmul(ppv[:, h, :], lhsT=Nn[:, h, :], rhs=Pm[:, h, :], start=True, stop=True), R=[Nn, Pm], W=[pp])
            if le != "dve":
                yield
            le = "dve"
            S.op("dve", lambda: nc.vector.tensor_tensor(out=Pm[:], in0=Pm[:], in1=ppv, op=ALU.add), R=[Pm, pp], W=[Pm])
            Nc, Mc, Nn, Mn = Nn, Mn, Nc, Mc
        pk = pbig
        pkv = pk.h[0:64, 0:512].rearrange("p (h c) -> p h c", h=4)
        for h in range(4):
            if le != "pe":
                yield
            le = "pe"
            S.op("pe", lambda: nc.tensor.transpose(out=pkv[:, h, :], in_=cv[:, 4 + h, cs], identity=K.identf[:]), R=[cv, K.identf], W=[pk])
        if le != "dve":
            yield
        le = "dve"
        S.op("dve", lambda: nc.vector.tensor_tensor(out=ke[:], in0=pkv, in1=egr[:].unsqueeze(2).to_broadcast([64, 4, 128]), op=ALU.mult),
             R=[pk, egr], W=[ke])
        if le != "dve":
            yield
        le = "dve"
        S.op("dve", lambda: nc.vector.tensor_tensor(out=kbg[:], in0=pkv, in1=bcol.unsqueeze(2).to_broadcast([64, 4, 128]), op=ALU.mult),
             R=[pk, bgc], W=[kbg])
        if le != "pool":
            yield
        le = "pool"
        S.op("pool", lambda: nc.gpsimd.tensor_tensor(out=kbg[:], in0=kbg[:], in1=egc[:].unsqueeze(2).to_broadcast([64, 4, 128]), op=ALU.mult),
             R=[kbg, egc], W=[kbg])
        pv = pbig
        pvv = pv.h[0:64, 0:512].rearrange("p (h c) -> p h c", h=4)
        for h in range(4):
            if le != "pe":
                yield
            le = "pe"
            S.op("pe", lambda: nc.tensor.transpose(out=pvv[:, h, :], in_=cv[:, 8 + h, cs], identity=K.identf[:]), R=[cv, K.identf], W=[pv])
        if le != "dve":
            yield
        le = "dve"
        S.op("dve", lambda: nc.vector.tensor_tensor(out=vb[:], in0=pvv, in1=bcol.unsqueeze(2).to_broadcast([64, 4, 128]), op=ALU.mult),
             R=[pv, bgc], W=[vb])
        if le != "pool":
            yield
        le = "pool"
        S.op("pool", lambda: nc.gpsimd.tensor_tensor(out=gbc[:], in0=K.ones4[:], in1=gcol.unsqueeze(2).to_broadcast([64, 4, 128]), op=ALU.mult),
             R=[K.ones4, bgc], W=[gbc])
        pe_ = psml
        pev = pe_.h[:, 0:256].rearrange("p (h c) -> p h c", h=4)
        for h in range(4):
            if le != "pe":
                yield
            le = "pe"
            S.op("pe", lambda: nc.tensor.matmul(pev[:, h, :], lhsT=gbc[:, h, :], rhs=K.triu[:, 0, :], start=True, stop=True), R=[gbc, K.triu], W=[pe_])
        if le != "act":
            yield
        le = "act"
        S.op("act", lambda: nc.scalar.activation(out=egb[:], in_=pev, func=AF.Exp), R=[pe_], W=[egb])
        if le != "dve":
            yield
        le = "dve"
        S.op("dve", lambda: nc.vector.tensor_tensor(out=qd[:], in0=cv[:, 0:4, cs], in1=egb[:], op=ALU.mult), R=[cv, egb], W=[qd])
        pw = psml
        pwv = pw.h[:, 0:256].rearrange("p (h c) -> p h c", h=4)
        for h in range(4):
            if le != "pe":
                yield
            le = "pe"
            S.op("pe", lambda: nc.tensor.matmul(pwv[:, h, :], lhsT=kbg[:, h, :], rhs=Pm[:, h, :], start=True, stop=True), R=[kbg, Pm], W=[pw])
        if le != "act":
            yield
        le = "act"
        S.op("act", lambda: nc.scalar.copy(out=wcT[:], in_=pwv), R=[pw], W=[wcT])
        pu = pbig
        puv = pu.h[0:64, 0:512].rearrange("p (h c) -> p h c", h=4)
        for h in range(4):
            if le != "pe":
                yield
            le = "pe"
            S.op("pe", lambda: nc.tensor.matmul(puv[:, h, :], lhsT=Pm[:, h, :], rhs=vb[:, h, :], start=True, stop=True), R=[vb, Pm], W=[pu])
        if le != "act":
            yield
        le = "act"
        S.op("act", lambda: nc.scalar.copy(out=u[:], in_=puv), R=[pu], W=[u])
        while st["rec_done"] < cidx:
            yield
        pws = pbig
        pwsv = pws.h[0:64, 0:512].rearrange("p (h c) -> p h c", h=4)
        for h in range(4):
            if le != "pe":
                yield
            le = "pe"
            S.op("pe", lambda: nc.tensor.matmul(pwsv[:, h, :], lhsT=wcT[:, h, :], rhs=Sst[:, h, :], start=True, stop=True), R=[wcT, Sst], W=[pws])
        if le != "dve":
            yield
        le = "dve"
        S.op("dve", lambda: nc.vector.tensor_tensor(out=vn[:], in0=u[:], in1=pwsv, op=ALU.subtract), R=[u, pws], W=[vn])
        po = pbig
        pov = po.h[0:64, 0:512].rearrange("p (h c) -> p h c", h=4)
        for h in range(4):
            if le != "pe":
                yield
            le = "pe"
            S.op("pe", lambda: nc.tensor.matmul(pov[:, h, :], lhsT=qd[:, h, :], rhs=Sst[:, h, :], start=True, stop=False), R=[qd, Sst], W=[po])
            if le != "pe":
                yield
            le = "pe"
            S.op("pe", lambda: nc.tensor.matmul(pov[:, h, :], lhsT=qkT[:, h, :], rhs=vn[:, h, :], start=False, stop=True), R=[qkT, vn], W=[po])
        if le != "act":
            yield
        le = "act"
        S.op("act", lambda: nc.scalar.copy(out=ot[:], in_=pov), R=[po], W=[ot])
        pS = pbig
        pSv = pS.h[:, 0:512].rearrange("p (h c) -> p h c", h=4)
        for h in range(4):
            if le != "pe":
                yield
            le = "pe"
            S.op("pe", lambda: nc.tensor.matmul(pSv[:, h, :], lhsT=ke[:, h, :], rhs=vn[:, h, :], start=True, stop=True), R=[ke, vn], W=[pS])
        if le != "pool":
            yield
        le = "pool"
        S.op("pool", lambda: nc.gpsimd.tensor_tensor(out=tmpS[:], in0=Sst[:], in1=cdt[:].unsqueeze(2).to_broadcast([128, 4, 128]), op=ALU.mult),
             R=[Sst, cdt], W=[tmpS])
        if le != "dve":
            yield
        le = "dve"
        S.op("dve", lambda: nc.vector.tensor_tensor(out=Sst[:], in0=tmpS[:], in1=pSv, op=ALU.add), R=[tmpS, pS], W=[Sst])
        st["rec_done"] = cidx + 1
        if le != "pool":
            yield
        le = "pool"
        S.op("pool", lambda: nc.gpsimd.tensor_tensor(out=o2[:], in0=ot[:], in1=ot[:], op=ALU.mult), R=[ot], W=[o2])
        if le != "dve":
            yield
        le = "dve"
        S.op("dve", lambda: nc.vector.tensor_reduce(out=ssq[:], in_=o2[:], axis=AX.X, op=ALU.add), R=[o2], W=[ssq])
        if le != "act":
            yield
        le = "act"
        S.op("act", lambda: nc.scalar.activation(out=ssq[:], in_=ssq[:], func=AF.Ln, bias=K.eps_rms128[0:64, 0:1], scale=1.0),
             R=[ssq, K.eps_rms128], W=[ssq])
        S.op("act", lambda: nc.scalar.activation(out=ssq[:], in_=ssq[:], func=AF.Exp, scale=-0.5), R=[ssq], W=[ssq])
        if le != "dve":
            yield
        le = "dve"
        S.op("dve", lambda: nc.vector.scalar_tensor_tensor(out=o2[:], in0=ot[:], scalar=float(128 ** 0.5),
                                                          in1=ssq[:].unsqueeze(2).to_broadcast([64, 4, 128]), op0=ALU.mult, op1=ALU.mult),
             R=[ot, ssq], W=[o2])
        if le != "pool":
            yield
        le = "pool"
        S.op("pool", lambda: nc.gpsimd.tensor_tensor(out=o2[:], in0=o2[:], in1=gng[:], op=ALU.mult), R=[o2, gng], W=[o2])
        if le != "dve":
            yield
        le = "dve"
        S.op("dve", lambda: nc.vector.tensor_tensor(out=ob[:], in0=o2[:], in1=zc[:, :].rearrange("p (h c) -> p h c", h=4), op=ALU.mult),
             R=[o2, zc], W=[ob])
        if le != "sp":
            yield
        le = "sp"
        S.dma("sp", K.d["mixed"][p0:p0 + 64, 512:1024], ob[:].rearrange("p h c -> p (h c)"), R=[ob], W=[K.sbuf_mx_gd])
        st["inflight"] -= 1
        st["done"] += 1

    NG = NT
    load_x(0)
    cidx = 0
    for g in range(NG):
        nch = min(2, NC_ - 2 * g)
        if nch <= 0:
            break
        if g + 1 < NG and NC_ - 2 * (g + 1) > 0:
            load_x(g + 1)
        while st["done"] < min(cidx, 2 * (g - 1)):
            yield
        yield from G1(g)
        for ci in range(nch):
            while st["inflight"] >= NSETS:
                yield
            st["inflight"] += 1
            rr.add(chunk(g, ci, cidx), YK)
            cidx += 1
            yield
    while st["done"] < cidx:
        yield


def phase_GDN_old(K, l):
    nc, S, NT = K.nc, K.S, K.NT
    NC_ = K.NC
    with contextlib.ExitStack() as es:
        cw = _sb(K, es, "gd_cw", [128, 12, 4], F32)
        S.dma("sp", cw[:], K.d["conv_wT"][l].rearrange("(i d) t -> d i t", d=128), W=[cw])
        gng = _sb(K, es, "gd_gng", [64, 4, 128], F32)
        for h in range(4):
            S.dma("sp", gng[:, h, :], K.d["gdn_norm_g"][l].partition_broadcast(64), W=[gng])
        xin = [_sb(K, es, "gd_x%d" % i, [128, 12, 259], F32) for i in range(2)]
        cv = _sb(K, es, "gd_cv", [128, 12, 256], F32)
        sq = [_sb(K, es, "gd_sq%d" % i, [128, 256], F32) for i in range(2)]
        rn = [_sb(K, es, "gd_rn%d" % i, [128, 256], F32) for i in range(2)]
        bgt = [_sb(K, es, "gd_bg%d" % i, [64, 4, 8], F32) for i in range(2)]
        zt = [_sb(K, es, "gd_z%d" % i, [64, 4, 512], F32) for i in range(2)]
        Sst = _sb(K, es, "gd_S", [128, 4, 128], F32)
        S.op("pool", lambda: nc.gpsimd.memset(Sst[:], 0.0), W=[Sst])
        def mk(name, shape, dt=F32):
            return [_sb(K, es, "gd_%s%d" % (name, i), shape, dt) for i in range(2)]
        egc, egr, cdt = mk("egc", [64, 4]), mk("egr", [64, 4]), mk("cd", [128, 4])
        gl, dm, dmT = mk("gl", [64, 4, 64]), mk("dm", [64, 4, 64]), mk("dmT", [64, 4, 64])
        Nm, Mm = mk("N", [64, 4, 64]), mk("M", [64, 4, 64])
        N2, M2 = mk("N2", [64, 4, 64]), mk("M2", [64, 4, 64])
        Pm = mk("P", [64, 4, 64])
        qkT = mk("qkT", [64, 4, 64])
        kbg, vb, ke = mk("kbg", [64, 4, 128]), mk("vb", [64, 4, 128]), mk("ke", [64, 4, 128])
        gbc, egb, qd = mk("gbc", [64, 4, 128]), mk("egb", [128, 4, 64]), mk("qd", [128, 4, 64])
        wcT, u, vn = mk("wcT", [128, 4, 64]), mk("u", [64, 4, 128]), mk("vn", [64, 4, 128])
        ot, o2 = mk("ot", [64, 4, 128]), mk("o2", [64, 4, 128])
        ssq, ob = mk("ssq", [64, 4]), mk("ob", [64, 4, 128], BF16)
        tmpS = _sb(K, es, "gd_tmpS", [128, 4, 128], F32)
        NG = (NT + 1) // 2
        ci_glob = 0
        for g in range(NG):
            nt = min(2, NT - 2 * g)
            n = nt * 128
            c0 = g * 256
            nch = min(n // 64, NC_ - c0 // 64)
            if nch <= 0:
                break
            x = xin[g % 2]
            if g == 0:
                S.op("pool", lambda: nc.gpsimd.memset(x[:, :, 0:3], 0.0), W=[x])
                S.dma("sp", x[:, :, 3:3 + n], K.d["gT"][:, :, 0:n].rearrange("h p n -> p h n"), R=[K.sbuf_g], W=[x])
                S.op("pool", lambda: nc.gpsimd.memset(x[:, :, 3:3 + 48], 0.0), W=[x])
            else:
                S.dma("sp", x[:, :, 0:3 + n], K.d["gT"][:, :, c0 - 3:c0 + n].rearrange("h p n -> p h n"), R=[K.sbuf_g], W=[x])
            for i in range(12):
                eng = "dve" if i % 2 == 0 else "pool"
                E = nc.vector if eng == "dve" else nc.gpsimd
                S.op(eng, lambda: E.tensor_scalar(out=cv[:, i, 0:n], in0=x[:, i, 0:n], scalar1=cw[:, i, 0:1], scalar2=None, op0=ALU.mult),
                     R=[x, cw], W=[cv])
                for tp in range(1, 4):
                    S.op("dve", lambda: nc.vector.scalar_tensor_tensor(out=cv[:, i, 0:n], in0=x[:, i, tp:tp + n], scalar=cw[:, i, tp:tp + 1],
                                                                      in1=cv[:, i, 0:n], op0=ALU.mult, op1=ALU.add), R=[x, cw, cv], W=[cv])
                S.op("act", lambda: nc.scalar.activation(out=cv[:, i, 0:n], in_=cv[:, i, 0:n], func=AF.Silu), R=[cv], W=[cv])
            for i in range(8):
                s_, r_ = sq[i % 2], rn[i % 2]
                S.op("pool", lambda: nc.gpsimd.tensor_tensor(out=s_[:, 0:n], in0=cv[:, i, 0:n], in1=cv[:, i, 0:n], op=ALU.mult), R=[cv], W=[s_])
                ps = _ps(K)
                S.op("pe", lambda: nc.tensor.matmul(ps[:, 0:n], lhsT=K.onesf[:, :], rhs=s_[:, 0:n], start=True, stop=True),
                     R=[K.onesf, s_], W=[ps])
                S.op("act", lambda: nc.scalar.activation(out=r_[:, 0:n], in_=ps[:, 0:n], func=AF.Sqrt, bias=K.eps_rms[:, 0:1], scale=1.0),
                     R=[ps, K.eps_rms], W=[r_])
                S.op("dve", lambda: nc.vector.reciprocal(out=r_[:, 0:n], in_=r_[:, 0:n]), R=[r_], W=[r_])
                if i < 4:
                    S.op("dve", lambda: nc.vector.scalar_tensor_tensor(out=cv[:, i, 0:n], in0=cv[:, i, 0:n], scalar=float(128 ** -0.5),
                                                                      in1=r_[:, 0:n], op0=ALU.mult, op1=ALU.mult), R=[cv, r_], W=[cv])
                else:
                    S.op("dve", lambda: nc.vector.tensor_tensor(out=cv[:, i, 0:n], in0=cv[:, i, 0:n], in1=r_[:, 0:n], op=ALU.mult),
                         R=[cv, r_], W=[cv])
            bgc, zc = bgt[g % 2], zt[g % 2]
            S.dma("sp", bgc[:, 0:nch, :], K.d["bg"][c0:c0 + nch * 64, :].rearrange("(n c) e -> c n e", c=64), R=[K.sbuf_bg], W=[bgc])
            S.dma("sp", zc[:, 0:nch, :], K.d["zs"][c0:c0 + nch * 64, :].rearrange("(n c) e -> c n e", c=64), R=[K.sbuf_z], W=[zc])
            if g == 0:
                S.op("pool", lambda: nc.gpsimd.memset(bgc[0:48, 0, 4:8], 0.0), W=[bgc])
            for ci in range(nch):
                b2 = ci_glob % 2
                ci_glob += 1
                cs = slice(ci * 64, ci * 64 + 64)
                gcol = bgc[:, ci, 4:8]
                bcol = bgc[:, ci, 0:4]
                pg = _ps(K)
                S.op("pe", lambda: nc.tensor.matmul(pg[0:64, 0:4], lhsT=K.triu[:, 0, :], rhs=gcol, start=True, stop=True), R=[K.triu, bgc], W=[pg])
                S.op("pe", lambda: nc.tensor.matmul(pg[0:64, 4:8], lhsT=K.sgt[:, :], rhs=gcol, start=True, stop=True), R=[K.sgt, bgc], W=[pg])
                S.op("pe", lambda: nc.tensor.matmul(pg[:, 8:12], lhsT=K.ones64[:, :], rhs=gcol, start=True, stop=True), R=[K.ones64, bgc], W=[pg])
                S.op("act", lambda: nc.scalar.activation(out=egc[b2][:], in_=pg[0:64, 0:4], func=AF.Exp), R=[pg], W=[egc[b2]])
                S.op("act", lambda: nc.scalar.activation(out=egr[b2][:], in_=pg[0:64, 4:8], func=AF.Exp), R=[pg], W=[egr[b2]])
                S.op("act", lambda: nc.scalar.activation(out=cdt[b2][:], in_=pg[:, 8:12], func=AF.Exp), R=[pg], W=[cdt[b2]])
                S.op("dve", lambda: nc.vector.tensor_tensor(out=gl[b2][:], in0=K.triu[:], in1=gcol.unsqueeze(2).to_broadcast([64, 4, 64]), op=ALU.mult),
                     R=[K.triu, bgc], W=[gl[b2]])
                pd = _ps(K)
                pdv = pd.h[0:64, 0:512].rearrange("p (a h c) -> p a h c", a=2, h=4)
                for h in range(4):
                    S.op("pe", lambda: nc.tensor.matmul(pdv[:, 0, h, :], lhsT=gl[b2][:, h, :], rhs=K.sgt[:, :], start=True, stop=True),
                         R=[gl[b2], K.sgt], W=[pd])
                S.op("pe", lambda: nc.tensor.matmul(pd[0:64, 256:512], lhsT=K.sgt[:, :], rhs=gl[b2][:].rearrange("p h c -> p (h c)"), start=True, stop=True),
                     R=[gl[b2], K.sgt], W=[pd])
                S.op("act", lambda: nc.scalar.activation(out=dm[b2][:], in_=pdv[:, 0], func=AF.Exp), R=[pd], W=[dm[b2]])
                S.op("act", lambda: nc.scalar.activation(out=dmT[b2][:], in_=pdv[:, 1], func=AF.Exp), R=[pd], W=[dmT[b2]])
                S.op("pool", lambda: nc.gpsimd.tensor_tensor(out=dm[b2][:], in0=dm[b2][:], in1=K.trilsn[:], op=ALU.mult), R=[dm[b2], K.trilsn], W=[dm[b2]])
                S.op("pool", lambda: nc.gpsimd.tensor_tensor(out=dmT[b2][:], in0=dmT[b2][:], in1=K.triu[:], op=ALU.mult), R=[dmT[b2], K.triu], W=[dmT[b2]])
                pgq = _ps(K)
                pgqv = pgq.h[0:64, 0:512].rearrange("p (a h c) -> p a h c", a=2, h=4)
                for h in range(4):
                    S.op("pe", lambda: nc.tensor.matmul(pgqv[:, 0, h, :], lhsT=cv[:, 4 + h, cs], rhs=cv[:, 4 + h, cs], start=True, stop=True), R=[cv], W=[pgq])
                    S.op("pe", lambda: nc.tensor.matmul(pgqv[:, 1, h, :], lhsT=cv[:, 4 + h, cs], rhs=cv[:, h, cs], start=True, stop=True), R=[cv], W=[pgq])
                S.op("dve", lambda: nc.vector.tensor_tensor(out=Nm[b2][:], in0=pgqv[:, 0], in1=bcol.unsqueeze(2).to_broadcast([64, 4, 64]), op=ALU.mult),
                     R=[pgq, bgc], W=[Nm[b2]])
                S.op("dve", lambda: nc.vector.tensor_tensor(out=Nm[b2][:], in0=Nm[b2][:], in1=dm[b2][:], op=ALU.mult), R=[Nm[b2], dm[b2]], W=[Nm[b2]])
                S.op("dve", lambda: nc.vector.tensor_tensor(out=qkT[b2][:], in0=pgqv[:, 1], in1=dmT[b2][:], op=ALU.mult), R=[pgq, dmT[b2]], W=[qkT[b2]])
                pt = _ps(K)
                ptv = pt.h[0:64, 0:256].rearrange("p (h c) -> p h c", h=4)
                for h in range(4):
                    S.op("pe", lambda: nc.tensor.transpose(out=ptv[:, h, :], in_=Nm[b2][:, h, :], identity=K.identf[0:64, 0:64]), R=[Nm[b2], K.identf], W=[pt])
                S.op("act", lambda: nc.scalar.copy(out=Mm[b2][:], in_=ptv), R=[pt], W=[Mm[b2]])
                S.op("dve", lambda: nc.vector.tensor_tensor(out=Pm[b2][:], in0=Mm[b2][:], in1=K.ident4[:], op=ALU.add), R=[Mm[b2], K.ident4], W=[Pm[b2]])
                Nc, Mc, Nn, Mn = Nm[b2], Mm[b2], N2[b2], M2[b2]
                for r in range(5):
                    pn = _ps(K)
                    pnv = pn.h[0:64, 0:512].rearrange("p (a h c) -> p a h c", a=2, h=4)
                    for h in range(4):
                        S.op("pe", lambda: nc.tensor.matmul(pnv[:, 0, h, :], lhsT=Mc[:, h, :], rhs=Nc[:, h, :], start=True, stop=True), R=[Mc, Nc], W=[pn])
                        if r < 4:
                            S.op("pe", lambda: nc.tensor.matmul(pnv[:, 1, h, :], lhsT=Nc[:, h, :], rhs=Mc[:, h, :], start=True, stop=True), R=[Mc, Nc], W=[pn])
                    S.op("act", lambda: nc.scalar.copy(out=Nn[:], in_=pnv[:, 0]), R=[pn], W=[Nn])
                    if r < 4:
                        S.op("dve", lambda: nc.vector.tensor_copy(out=Mn[:], in_=pnv[:, 1]), R=[pn], W=[Mn])
                    pp = _ps(K)
                    ppv = pp.h[0:64, 0:256].rearrange("p (h c) -> p h c", h=4)
                    for h in range(4):
                        S.op("pe", lambda: nc.tensor.matmul(ppv[:, h, :], lhsT=Nn[:, h, :], rhs=Pm[b2][:, h, :], start=True, stop=True), R=[Nn, Pm[b2]], W=[pp])
                    S.op("dve", lambda: nc.vector.tensor_tensor(out=Pm[b2][:], in0=Pm[b2][:], in1=ppv, op=ALU.add), R=[Pm[b2], pp], W=[Pm[b2]])
                    Nc, Mc, Nn, Mn = Nn, Mn, Nc, Mc
                pk = _ps(K)
                pkv = pk.h[0:64, 0:512].rearrange("p (h c) -> p h c", h=4)
                pv = _ps(K)
                pvv = pv.h[0:64, 0:512].rearrange("p (h c) -> p h c", h=4)
                for h in range(4):
                    S.op("pe", lambda: nc.tensor.transpose(out=pkv[:, h, :], in_=cv[:, 4 + h, cs], identity=K.identf[:]), R=[cv, K.identf], W=[pk])
                    S.op("pe", lambda: nc.tensor.transpose(out=pvv[:, h, :], in_=cv[:, 8 + h, cs], identity=K.identf[:]), R=[cv, K.identf], W=[pv])
                S.op("dve", lambda: nc.vector.tensor_tensor(out=ke[b2][:], in0=pkv, in1=egr[b2][:].unsqueeze(2).to_broadcast([64, 4, 128]), op=ALU.mult),
                     R=[pk, egr[b2]], W=[ke[b2]])
                S.op("dve", lambda: nc.vector.tensor_tensor(out=kbg[b2][:], in0=pkv, in1=bcol.unsqueeze(2).to_broadcast([64, 4, 128]), op=ALU.mult),
                     R=[pk, bgc], W=[kbg[b2]])
                S.op("pool", lambda: nc.gpsimd.tensor_tensor(out=kbg[b2][:], in0=kbg[b2][:], in1=egc[b2][:].unsqueeze(2).to_broadcast([64, 4, 128]), op=ALU.mult),
                     R=[kbg[b2], egc[b2]], W=[kbg[b2]])
                S.op("dve", lambda: nc.vector.tensor_tensor(out=vb[b2][:], in0=pvv, in1=bcol.unsqueeze(2).to_broadcast([64, 4, 128]), op=ALU.mult),
                     R=[pv, bgc], W=[vb[b2]])
                S.op("pool", lambda: nc.gpsimd.tensor_tensor(out=gbc[b2][:], in0=K.ones4[:], in1=gcol.unsqueeze(2).to_broadcast([64, 4, 128]), op=ALU.mult),
                     R=[K.ones4, bgc], W=[gbc[b2]])
                pe_ = _ps(K)
                pev = pe_.h[:, 0:256].rearrange("p (h c) -> p h c", h=4)
                for h in range(4):
                    S.op("pe", lambda: nc.tensor.matmul(pev[:, h, :], lhsT=gbc[b2][:, h, :], rhs=K.triu[:, 0, :], start=True, stop=True), R=[gbc[b2], K.triu], W=[pe_])
                S.op("act", lambda: nc.scalar.activation(out=egb[b2][:], in_=pev, func=AF.Exp), R=[pe_], W=[egb[b2]])
                S.op("dve", lambda: nc.vector.tensor_tensor(out=qd[b2][:], in0=cv[:, 0:4, cs], in1=egb[b2][:], op=ALU.mult), R=[cv, egb[b2]], W=[qd[b2]])
                pw = _ps(K)
                pwv = pw.h[:, 0:256].rearrange("p (h c) -> p h c", h=4)
                pu = _ps(K)
                puv = pu.h[0:64, 0:512].rearrange("p (h c) -> p h c", h=4)
                for h in range(4):
                    S.op("pe", lambda: nc.tensor.matmul(pwv[:, h, :], lhsT=kbg[b2][:, h, :], rhs=Pm[b2][:, h, :], start=True, stop=True), R=[kbg[b2], Pm[b2]], W=[pw])
                    S.op("pe", lambda: nc.tensor.matmul(puv[:, h, :], lhsT=Pm[b2][:, h, :], rhs=vb[b2][:, h, :], start=True, stop=True), R=[vb[b2], Pm[b2]], W=[pu])
                S.op("act", lambda: nc.scalar.copy(out=wcT[b2][:], in_=pwv), R=[pw], W=[wcT[b2]])
                S.op("act", lambda: nc.scalar.copy(out=u[b2][:], in_=puv), R=[pu], W=[u[b2]])
                pws = _ps(K)
                pwsv = pws.h[0:64, 0:512].rearrange("p (h c) -> p h c", h=4)
                for h in range(4):
                    S.op("pe", lambda: nc.tensor.matmul(pwsv[:, h, :], lhsT=wcT[b2][:, h, :], rhs=Sst[:, h, :], start=True, stop=True), R=[wcT[b2], Sst], W=[pws])
                S.op("dve", lambda: nc.vector.tensor_tensor(out=vn[b2][:], in0=u[b2][:], in1=pwsv, op=ALU.subtract), R=[u[b2], pws], W=[vn[b2]])
                po = _ps(K)
                pov = po.h[0:64, 0:512].rearrange("p (h c) -> p h c", h=4)
                for h in range(4):
                    S.op("pe", lambda: nc.tensor.matmul(pov[:, h, :], lhsT=qd[b2][:, h, :], rhs=Sst[:, h, :], start=True, stop=False), R=[qd[b2], Sst], W=[po])
                    S.op("pe", lambda: nc.tensor.matmul(pov[:, h, :], lhsT=qkT[b2][:, h, :], rhs=vn[b2][:, h, :], start=False, stop=True), R=[qkT[b2], vn[b2]], W=[po])
                pS = _ps(K)
                pSv = pS.h[:, 0:512].rearrange("p (h c) -> p h c", h=4)
                for h in range(4):
                    S.op("pe", lambda: nc.tensor.matmul(pSv[:, h, :], lhsT=ke[b2][:, h, :], rhs=vn[b2][:, h, :], start=True, stop=True), R=[ke[b2], vn[b2]], W=[pS])
                S.op("pool", lambda: nc.gpsimd.tensor_tensor(out=tmpS[:], in0=Sst[:], in1=cdt[b2][:].unsqueeze(2).to_broadcast([128, 4, 128]), op=ALU.mult),
                     R=[Sst, cdt[b2]], W=[tmpS])
                S.op("dve", lambda: nc.vector.tensor_tensor(out=Sst[:], in0=tmpS[:], in1=pSv, op=ALU.add), R=[tmpS, pS], W=[Sst])
                S.op("act", lambda: nc.scalar.copy(out=ot[b2][:], in_=pov), R=[po], W=[ot[b2]])
                S.op("pool", lambda: nc.gpsimd.tensor_tensor(out=o2[b2][:], in0=ot[b2][:], in1=ot[b2][:], op=ALU.mult), R=[ot[b2]], W=[o2[b2]])
                S.op("dve", lambda: nc.vector.tensor_reduce(out=ssq[b2][:], in_=o2[b2][:], axis=AX.X, op=ALU.add), R=[o2[b2]], W=[ssq[b2]])
                S.op("act", lambda: nc.scalar.activation(out=ssq[b2][:], in_=ssq[b2][:], func=AF.Sqrt, bias=K.eps_rms[0:64, 0:1], scale=1.0 / 128),
                     R=[ssq[b2], K.eps_rms], W=[ssq[b2]])
                S.op("dve", lambda: nc.vector.reciprocal(out=ssq[b2][:], in_=ssq[b2][:]), R=[ssq[b2]], W=[ssq[b2]])
                S.op("dve", lambda: nc.vector.tensor_tensor(out=o2[b2][:], in0=ot[b2][:], in1=ssq[b2][:].unsqueeze(2).to_broadcast([64, 4, 128]), op=ALU.mult),
                     R=[ot[b2], ssq[b2]], W=[o2[b2]])
                S.op("pool", lambda: nc.gpsimd.tensor_tensor(out=o2[b2][:], in0=o2[b2][:], in1=gng[:], op=ALU.mult), R=[o2[b2], gng], W=[o2[b2]])
                S.op("dve", lambda: nc.vector.tensor_tensor(out=ob[b2][:], in0=o2[b2][:], in1=zc[:, ci, :].rearrange("p (h c) -> p h c", h=4), op=ALU.mult),
                     R=[o2[b2], zc], W=[ob[b2]])
                p0 = c0 + ci * 64
                S.dma("sp", K.d["mixed"][p0:p0 + 64, 512:1024], ob[b2][:].rearrange("p h c -> p (h c)"), R=[ob[b2]], W=[K.sbuf_mx_gd])


def phase_SBGDN(K, l, NSETS=2, YK=None, run_sb=True, run_gdn=True):
    YK = YK or GDN_YK
    with contextlib.ExitStack() as es:
        K.psum_gd = K.psum[5:8]
        K.psgi = 0
        rr = RR()
        if run_sb:
            rr.add(sb_stream(K, l, es), 1)
        if run_gdn:
            rr.add(gdn_master(K, l, es, rr, NSETS, YK), YK)
        rr.run()


def phase_A3(K, l):
    nc, S, NT = K.nc, K.S, K.NT
    with contextlib.ExitStack() as es:
        K.stg = [_sb(K, es, "stg%d" % i, [128, IN_W], F32) for i in range(2)]
        Wo = _sb(K, es, "a3_W", [128, KC, D], BF16)
        wbufs = [Buf() for _ in range(KC)]
        wsrc = K.d["w_out"][l].rearrange("(k p) n -> p k n", p=128)
        for kc in range(KC):
            _load_cast(K, Wo, Wo[:, kc, :], wsrc[:, kc, :], [128, D], wbufs[kc])
        Wr = _sb(K, es, "a3_Wr", [128, KC, 36], F32)
        S.dma("sp", Wr[:, :, 0:4], K.d["w_group"][l].rearrange("(k p) n -> p k n", p=128), W=[Wr])
        S.dma("sp", Wr[:, :, 4:36], K.d["w_expert"][l].rearrange("(k p) n -> p k n", p=128), W=[Wr])
        br = _sb(K, es, "a3_br", [128, 36], F32)
        S.dma("sp", br[:, 0:4], K.d["b_group"][l].partition_broadcast(128), W=[br])
        S.dma("sp", br[:, 4:36], K.d["b_expert"][l].partition_broadcast(128), W=[br])
        g = _sb(K, es, "a3_g", [128, D], F32)
        b = _sb(K, es, "a3_b", [128, D], F32)
        S.dma("sp", g[:], K.d["ln1_g"][l].partition_broadcast(128), W=[g])
        S.dma("sp", b[:], K.d["ln1_b"][l].partition_broadcast(128), W=[b])
        mxs = [_sb(K, es, "a3_mx%d" % i, [128, D], BF16) for i in range(2)]
        mTs = [_sb(K, es, "a3_mT%d" % i, [128, KC, 128], BF16) for i in range(2)]
        hts = [_sb(K, es, "a3_h%d" % i, [128, D], F32) for i in range(2)]
        rs = [_sb(K, es, "a3_r%d" % i, [128, D], F32) for i in range(2)]
        h1s = [_sb(K, es, "a3_h1%d" % i, [128, D], F32) for i in range(2)]
        hTf = [_sb(K, es, "a3_hTf%d" % i, [128, KC, 128], F32) for i in range(2)]
        hTb = [_sb(K, es, "a3_hTb%d" % i, [128, KC, 128], BF16) for i in range(2)]
        sm = {"st": _sb(K, es, "a3_st", [128, 2, 6], F32), "mv": _sb(K, es, "a3_mv", [128, 2], F32),
              "rstd": _sb(K, es, "a3_rs", [128, 1], F32), "tmp": _sb(K, es, "a3_tmp", [128, D], F32)}
        lg = _sb(K, es, "a3_lg", [128, 36], F32)
        sc = {k: _sb(K, es, "a3_" + k, shp, F32) for k, shp in
              (("gm", [128, 1]), ("ge", [128, 4]), ("gs", [128, 1]), ("oh", [128, 4]), ("ig", [128, 8]), ("tmp8", [128, 4, 8]),
               ("m8", [128, 8]), ("sel", [128, 8]), ("ex", [128, 8]), ("dn", [128, 1]), ("wi", [128, 8]))}
        cbs = [_sb(K, es, "a3_cb%d" % i, [128, 4, 8], F32) for i in range(2)]
        for t in range(NT):
            mx, mT, ht, r, h1, hf, hb, cb = mxs[t % 2], mTs[t % 2], hts[t % 2], rs[t % 2], h1s[t % 2], hTf[t % 2], hTb[t % 2], cbs[t % 2]
            rows = slice(128 * t, 128 * (t + 1))
            S.dma("sp", mx[:], K.d["mixed"][rows, :], R=[K.sbuf_mx_sb, K.sbuf_mx_gd], W=[mx])
            S.dma("sp", ht[:], K.d["h"][rows, :], R=[K.hbuf[t]], W=[ht])
            for half in range(2):
                ps = _ps(K)
                psb = ps.h.bitcast(BF16)
                for j in range(4):
                    kc = half * 4 + j
                    S.op("pe", lambda: nc.tensor.transpose(out=psb[:, j * 128:(j + 1) * 128], in_=mx[:, kc * 128:(kc + 1) * 128], identity=K.identb[:]),
                         R=[mx, K.identb], W=[ps])
                _evac(K, S.alt(), mT[:, half * 4:half * 4 + 4, :], psb[:, 0:512].rearrange("p (a c) -> p a c", a=4), [ps], [mT])
            for half in range(2):
                ps = _ps(K)
                for kc in range(KC):
                    S.op("pe", lambda: nc.tensor.matmul(ps[:, :], lhsT=mT[:, kc, :], rhs=Wo[:, kc, half * 512:(half + 1) * 512],
                                                        start=(kc == 0), stop=(kc == KC - 1)), R=[mT, wbufs[kc]], W=[ps])
                S.op("dve", lambda: nc.vector.scalar_tensor_tensor(out=r[:, half * 512:(half + 1) * 512], in0=ht[:, half * 512:(half + 1) * 512],
                                                                  scalar=ALPHA, in1=ps[:, :], op0=ALU.mult, op1=ALU.add), R=[ht, ps], W=[r])
            _ln_tile(K, r, h1, g, b, sm)
            S.dma("sp", K.d["h1"][rows, :], h1[:], R=[h1], W=[K.h1buf[t]])
            for half in range(2):
                ps = _ps(K)
                for j in range(4):
                    kc = half * 4 + j
                    S.op("pe", lambda: nc.tensor.transpose(out=ps[:, j * 128:(j + 1) * 128], in_=h1[:, kc * 128:(kc + 1) * 128], identity=K.identf[:]),
                         R=[h1, K.identf], W=[ps])
                S.op("act", lambda: nc.scalar.copy(out=hf[:, half * 4:half * 4 + 4, :], in_=ps[:, :].rearrange("p (a c) -> p a c", a=4)), R=[ps], W=[hf])
                S.op("dve", lambda: nc.vector.tensor_copy(out=hb[:, half * 4:half * 4 + 4, :], in_=ps[:, :].rearrange("p (a c) -> p a c", a=4)), R=[ps], W=[hb])
            S.dma("sp", K.d["h1T"][:, :, rows].rearrange("k p n -> p k n"), hb[:], R=[hb], W=[K.sbuf_h1T])
            ps = _ps(K)
            for kc in range(KC):
                S.op("pe", lambda: nc.tensor.matmul(ps[:, 0:36], lhsT=hf[:, kc, :], rhs=Wr[:, kc, :], start=(kc == 0), stop=(kc == KC - 1)),
                     R=[hf, Wr], W=[ps])
            S.op("dve", lambda: nc.vector.tensor_tensor(out=lg[:], in0=ps[:, 0:36], in1=br[:], op=ALU.add), R=[ps, br], W=[lg])
            gm, ge, gs, oh, ig, tmp8, m8, sel, ex, dn, wi = (sc[k] for k in ("gm", "ge", "gs", "oh", "ig", "tmp8", "m8", "sel", "ex", "dn", "wi"))
            S.op("dve", lambda: nc.vector.tensor_reduce(out=gm[:], in_=lg[:, 0:4], axis=AX.X, op=ALU.max), R=[lg], W=[gm])
            S.op("dve", lambda: nc.vector.tensor_scalar(out=oh[:], in0=lg[:, 0:4], scalar1=gm[:, 0:1], scalar2=None, op0=ALU.is_equal), R=[lg, gm], W=[oh])
            S.op("dve", lambda: nc.vector.tensor_scalar(out=ge[:], in0=lg[:, 0:4], scalar1=gm[:, 0:1], scalar2=None, op0=ALU.subtract), R=[lg, gm], W=[ge])
            S.op("act", lambda: nc.scalar.activation(out=ge[:], in_=ge[:], func=AF.Exp), R=[ge], W=[ge])
            S.op("dve", lambda: nc.vector.tensor_reduce(out=gs[:], in_=ge[:], axis=AX.X, op=ALU.add), R=[ge], W=[gs])
            S.op("dve", lambda: nc.vector.tensor_tensor(out=tmp8[:], in0=lg[:, 4:36].rearrange("p (g e) -> p g e", g=4),
                                                        in1=oh[:].unsqueeze(2).to_broadcast([128, 4, 8]), op=ALU.mult), R=[lg, oh], W=[tmp8])
            S.op("dve", lambda: nc.vector.tensor_reduce(out=ig[:], in_=tmp8[:].rearrange("p g e -> p e g"), axis=AX.X, op=ALU.add), R=[tmp8], W=[ig])
            S.op("dve", lambda: nc.vector.max(out=m8[:], in_=ig[:]), R=[ig], W=[m8])
            S.op("dve", lambda: nc.vector.tensor_scalar(out=sel[:], in0=ig[:], scalar1=m8[:, 1:2], scalar2=None, op0=ALU.is_ge), R=[ig, m8], W=[sel])
            S.op("dve", lambda: nc.vector.tensor_scalar(out=ex[:], in0=ig[:], scalar1=m8[:, 0:1], scalar2=None, op0=ALU.subtract), R=[ig, m8], W=[ex])
            S.op("act", lambda: nc.scalar.activation(out=ex[:], in_=ex[:], func=AF.Exp), R=[ex], W=[ex])
            S.op("dve", lambda: nc.vector.tensor_tensor(out=ex[:], in0=ex[:], in1=sel[:], op=ALU.mult), R=[ex, sel], W=[ex])
            S.op("dve", lambda: nc.vector.tensor_reduce(out=dn[:], in_=ex[:], axis=AX.X, op=ALU.add), R=[ex], W=[dn])
            S.op("dve", lambda: nc.vector.tensor_tensor(out=dn[:], in0=dn[:], in1=gs[:], op=ALU.mult), R=[dn, gs], W=[dn])
            S.op("dve", lambda: nc.vector.reciprocal(out=dn[:], in_=dn[:]), R=[dn], W=[dn])
            S.op("dve", lambda: nc.vector.tensor_scalar(out=wi[:], in0=ex[:], scalar1=dn[:, 0:1], scalar2=None, op0=ALU.mult), R=[ex, dn], W=[wi])
            S.op("dve", lambda: nc.vector.tensor_tensor(out=cb[:], in0=oh[:].unsqueeze(2).to_broadcast([128, 4, 8]),
                                                        in1=wi[:].unsqueeze(1).to_broadcast([128, 4, 8]), op=ALU.mult), R=[oh, wi], W=[cb])
            S.dma("sp", K.d["comb"][rows, :], cb[:].rearrange("p g e -> p (g e)"), R=[cb], W=[K.sbuf_comb])


def phase_B(K, l, last):
    nc, S, NT = K.nc, K.S, K.NT
    NP = 4
    TH = (NT + NP - 1) // NP
    with contextlib.ExitStack() as es:
        K.stg = [_sb(K, es, "stg%d" % i, [128, IN_W], F32) for i in range(2)]
        g = _sb(K, es, "b_g", [128, D], F32)
        b = _sb(K, es, "b_b", [128, D], F32)
        S.dma("sp", g[:], K.d["ln2_g"][l].partition_broadcast(128), W=[g])
        S.dma("sp", b[:], K.d["ln2_b"][l].partition_broadcast(128), W=[b])
        xT = _sb(K, es, "b_xT", [128, KC, TH * 128], BF16)
        cbt = _sb(K, es, "b_cb", [128, TH, 32], F32)
        yacc = _sb(K, es, "b_y", [128, TH, D], F32)
        w1b = [_sb(K, es, "b_w1%d" % i, [128, KC, 256], BF16) for i in range(2)]
        w3b = [_sb(K, es, "b_w3%d" % i, [128, KC, 256], BF16) for i in range(2)]
        w2b = [_sb(K, es, "b_w2%d" % i, [128, 2, D], BF16) for i in range(2)]
        sil = [_sb(K, es, "b_sil%d" % i, [128, 512], F32) for i in range(2)]
        hid = [_sb(K, es, "b_hid%d" % i, [128, 2, 512], BF16) for i in range(2)]
        h1t = [_sb(K, es, "b_h1%d" % i, [128, D], F32) for i in range(1)]
        rr = [_sb(K, es, "b_r%d" % i, [128, D], F32) for i in range(1)]
        oo = [_sb(K, es, "b_o%d" % i, [128, D], F32) for i in range(2)]
        sm = {"st": _sb(K, es, "b_st", [128, 2, 6], F32), "mv": _sb(K, es, "b_mv", [128, 2], F32),
              "rstd": _sb(K, es, "b_rs", [128, 1], F32), "tmp": _sb(K, es, "b_tmp", [128, D], F32)}
        it = 0
        for half in range(NP):
            t0 = half * TH
            nth = min(TH, NT - t0)
            if nth <= 0:
                break
            n_all = nth * 128
            S.dma("sp", xT[:, :, 0:n_all], K.d["h1T"][:, :, t0 * 128:t0 * 128 + n_all].rearrange("k p n -> p k n"), R=[K.sbuf_h1T], W=[xT])
            S.dma("sp", cbt[:, 0:nth, :], K.d["comb"][t0 * 128:t0 * 128 + n_all, :].rearrange("(t p) e -> p t e", p=128), R=[K.sbuf_comb], W=[cbt])
            S.op("pool", lambda: nc.gpsimd.memset(yacc[:, 0:nth, :], 0.0), W=[yacc])
            for e in range(32):
                wa, wc, wd = w1b[e % 2], w3b[e % 2], w2b[e % 2]
                _load_cast(K, wa, wa[:], K.d["w1"][l, e].rearrange("(k p) f -> p k f", p=128), [128, KC, 256], wa.b)
                _load_cast(K, wc, wc[:], K.d["w3"][l, e].rearrange("(k p) f -> p k f", p=128), [128, KC, 256], wc.b)
                _load_cast(K, wd, wd[:], K.d["w2"][l, e].rearrange("(k p) f -> p k f", p=128), [128, 2, D], wd.b)
                for gq in range((nth + 3) // 4):
                    nt = min(4, nth - 4 * gq)
                    n = nt * 128
                    c0 = gq * 512
                    hd = hid[it % 2]
                    it += 1
                    for f2 in range(2):
                        p1 = _ps(K)
                        for kc in range(KC):
                            S.op("pe", lambda: nc.tensor.matmul(p1[:, 0:n], lhsT=wa[:, kc, f2 * 128:(f2 + 1) * 128], rhs=xT[:, kc, c0:c0 + n],
                                                                start=(kc == 0), stop=(kc == KC - 1)), R=[wa, xT], W=[p1])
                        p3 = _ps(K)
                        for kc in range(KC):
                            S.op("pe", lambda: nc.tensor.matmul(p3[:, 0:n], lhsT=wc[:, kc, f2 * 128:(f2 + 1) * 128], rhs=xT[:, kc, c0:c0 + n],
                                                                start=(kc == 0), stop=(kc == KC - 1)), R=[wc, xT], W=[p3])
                        sl = sil[f2]
                        S.op("act", lambda: nc.scalar.activation(out=sl[:, 0:n], in_=p1[:, 0:n], func=AF.Silu), R=[p1], W=[sl])
                        S.op("dve", lambda: nc.vector.tensor_tensor(out=hd[:, f2, 0:n], in0=sl[:, 0:n], in1=p3[:, 0:n], op=ALU.mult), R=[sl, p3], W=[hd])
                    for t in range(nt):
                        tt = 4 * gq + t
                        for ch in range(2):
                            py = _ps(K)
                            for f2 in range(2):
                                S.op("pe", lambda: nc.tensor.matmul(py[:, :], lhsT=hd[:, f2, t * 128:(t + 1) * 128], rhs=wd[:, f2, ch * 512:(ch + 1) * 512],
                                                                    start=(f2 == 0), stop=(f2 == 1)), R=[hd, wd], W=[py])
                            S.op("dve", lambda: nc.vector.scalar_tensor_tensor(out=yacc[:, tt, ch * 512:(ch + 1) * 512], in0=py[:, :],
                                                                              scalar=cbt[:, tt, e:e + 1], in1=yacc[:, tt, ch * 512:(ch + 1) * 512],
                                                                              op0=ALU.mult, op1=ALU.add), R=[py, cbt, yacc], W=[yacc])
            for t in range(nth):
                tg = t0 + t
                rows = slice(128 * tg, 128 * (tg + 1))
                h1, r, o = h1t[0], rr[0], oo[t % 2]
                S.dma("sp", h1[:], K.d["h1"][rows, :], R=[K.h1buf[tg]], W=[h1])
                S.op("dve", lambda: nc.vector.scalar_tensor_tensor(out=r[:], in0=h1[:], scalar=ALPHA, in1=yacc[:, t, :], op0=ALU.mult, op1=ALU.add),
                     R=[h1, yacc], W=[r])
                _ln_tile(K, r, o, g, b, sm)
                if not last:
                    S.dma("sp", K.d["h"][rows, :], o[:], R=[o], W=[K.hbuf[tg]])
                else:
                    lo = 128 * tg - 64
                    r0, r1 = max(lo, 0), min(lo + 128, K.SEQ)
                    if r1 > r0:
                        S.dma("sp", K.d["out"][r0:r1, :], o[r0 - lo:r1 - lo, :], R=[o], W=[K.outbuf])


def make_consts():
    c = {}
    c["identf"] = np.eye(128, dtype=np.float32)
    c["identb"] = np.eye(128, dtype=np.float32).astype(ml_dtypes.bfloat16)
    s = np.arange(128)[:, None]
    masks = np.zeros((6, 128, 512), np.float32)
    for rel in range(4):
        for qt in range(4):
            blk = masks[rel, :, qt * 128:(qt + 1) * 128]
            if qt < rel:
                blk[:] = NEG
            elif qt == rel:
                blk[:] = np.where(s < np.arange(128)[None, :], 0.0, NEG)
    masks[4] = masks[0]
    masks[4, 0:48, :] = NEG
    masks[5, 0:48, :] = NEG
    c["masks"] = np.ascontiguousarray(masks.transpose(1, 0, 2)).astype(ml_dtypes.bfloat16)
    c["negtri"] = np.where(s >= np.arange(128)[None, :], -1.0, 0.0).astype(ml_dtypes.bfloat16)
    c["onesb"] = np.ones((128, 1), np.float32).astype(ml_dtypes.bfloat16)
    c["onesf"] = np.ones((128, 128), np.float32)
    m = np.arange(64)[:, None]
    i = np.arange(64)[None, :]
    triu = (m <= i).astype(np.float32)
    c["triu"] = np.ascontiguousarray(np.repeat(triu[:, None, :], 4, axis=1))
    c["sgt"] = (m > i).astype(np.float32)
    c["ones64"] = np.ones((64, 128), np.float32)
    c["ones4"] = np.ones((64, 4, 128), np.float32)
    c["trilsn"] = np.ascontiguousarray(np.repeat((-(m > i).astype(np.float32))[:, None, :], 4, axis=1))
    c["ident4"] = np.ascontiguousarray(np.repeat(np.eye(64, dtype=np.float32)[:, None, :], 4, axis=1))
    c["cvec"] = np.tile(np.array([[1.0, LN_EPS, RMS_EPS, 0.0, 64.0 * RMS_EPS, 128.0 * RMS_EPS]], np.float32), (128, 1))
    return c


CONST_DT = {"identb": BF16, "masks": BF16, "negtri": BF16, "onesb": BF16}

IN_SHAPES = lambda SEQ, depth: {
    "x": [SEQ, D], "meta": [16, D], "ln_in_g": [D], "ln_in_b": [D], "w_in": [depth, D, IN_W], "conv_wT": [depth, 1536, 4],
    "a_log": [depth, 4], "dt_bias": [depth, 4], "sb_norm_g": [depth, 64], "gdn_norm_g": [depth, 128], "w_out": [depth, D, D],
    "ln1_g": [depth, D], "ln1_b": [depth, D], "w_group": [depth, D, 4], "b_group": [depth, 4], "w_expert": [depth, D, 32],
    "b_expert": [depth, 32], "w1": [depth, 32, D, 256], "w3": [depth, 32, D, 256], "w2": [depth, 32, 256, D],
    "ln2_g": [depth, D], "ln2_b": [depth, D]}


def build(SEQ, depth, debug=False, same_eng=True, phases=None, max_ops=None, log=None):
    nc = bass.Bass("TRN2", target_bir_lowering=False)
    K = Ctx()
    K.nc = nc
    K.SEQ = SEQ
    PT = SEQ + 64
    K.NT = NT = (PT + 127) // 128
    K.NC = PT // 64
    P = NT * 128
    K.d = {}
    for name, shp in IN_SHAPES(SEQ, depth).items():
        K.d[name] = nc.dram_tensor(name, shp, F32, kind="ExternalInput").ap()
    consts = make_consts()
    for name, arr in consts.items():
        K.d["c_" + name] = nc.dram_tensor("c_" + name, list(arr.shape), CONST_DT.get(name, F32), kind="ExternalInput").ap()
    K.d["out"] = nc.dram_tensor("out", [SEQ, D], F32, kind="ExternalOutput").ap()
    kind = "ExternalOutput" if debug else "Internal"
    for name, shp, dt in (("h", [P, D], F32), ("qT", [4, 128, P], BF16), ("kT", [4, 128, P], BF16), ("V", [P, 512], BF16),
                          ("gT", [12, 128, P], F32), ("zs", [P, 512], F32), ("bg", [P, 8], F32), ("mixed", [P, D], BF16),
                          ("h1", [P, D], F32), ("h1T", [KC, 128, P], BF16), ("comb", [P, 32], F32)):
        K.d[name] = nc.dram_tensor("s_" + name, shp, dt, kind=kind).ap()
    K.hbuf = [Buf() for _ in range(NT)]
    K.h1buf = [Buf() for _ in range(NT)]
    for nm in ("sbuf_q", "sbuf_k", "sbuf_v", "sbuf_g", "sbuf_z", "sbuf_bg", "sbuf_mx_sb", "sbuf_mx_gd", "sbuf_h1T", "sbuf_comb", "outbuf"):
        setattr(K, nm, Buf())
    with contextlib.ExitStack() as es:
        K.S = S = Sched(nc, es, same_eng=same_eng)
        S.max_ops = max_ops
        S.log = log
        K.psum = [Tl(es.enter_context(nc.psum_tensor("ps%d" % i, [128, 512], F32)), "ps%d" % i) for i in range(8)]
        K.psi = 0
        for p_ in K.psum:
            p_.b.excl = True
        K.stgi = 0
        for name, arr in consts.items():
            if name == "cvec":
                continue
            tl = _sb(K, es, "k_" + name, list(arr.shape), CONST_DT.get(name, F32))
            setattr(K, name, tl)
            S.dma("sp", tl[:], K.d["c_" + name], W=[tl])
        cv = _sb(K, es, "k_cvec", [128, 6], F32)
        S.dma("sp", cv[:], K.d["c_cvec"], W=[cv])
        K.one_c = Tl(cv.h[:, 0:1]); K.one_c.b = cv.b
        K.eps_ln = Tl(cv.h[:, 1:2]); K.eps_ln.b = cv.b
        K.eps_rms = Tl(cv.h[:, 2:3]); K.eps_rms.b = cv.b
        K.eps_rms64 = Tl(cv.h[:, 4:5]); K.eps_rms64.b = cv.b
        K.eps_rms128 = Tl(cv.h[:, 5:6]); K.eps_rms128.b = cv.b
        ph = phases or ("in", "A1", "SB", "GDN", "A3", "B")
        if "in" in ph:
            phase_input(K)
            S.barrier()
        for l in range(depth):
            if "A1" in ph:
                phase_A1(K, l)
                S.barrier()
            if INTERLEAVE_GDN:
                if "SB" in ph or "GDN" in ph:
                    phase_SBGDN(K, l, run_sb=("SB" in ph), run_gdn=("GDN" in ph))
                    S.barrier()
            else:
                if "SB" in ph:
                    phase_SBGDN(K, l, run_sb=True, run_gdn=False)
                    S.barrier()
                if "GDN" in ph:
                    phase_GDN_old(K, l)
                    S.barrier()
            if "A3" in ph:
                phase_A3(K, l)
                S.barrier()
            if "B" in ph:
                phase_B(K, l, l == depth - 1)
                S.barrier()
        S.finish()
    K.consts = consts
    return nc, K


def make_in_maps(inputs, SEQ, depth, consts, n_cores=8):
    shared = {}
    for k in ("ln_in_g", "ln_in_b", "w_in", "a_log", "dt_bias", "sb_norm_g", "gdn_norm_g", "w_out", "ln1_g", "ln1_b",
              "w_group", "b_group", "w_expert", "b_expert", "w1", "w3", "w2", "ln2_g", "ln2_b"):
        shared[k] = np.ascontiguousarray(np.asarray(inputs[k], dtype=np.float32))
    shared["meta"] = np.ascontiguousarray(np.asarray(inputs["meta_tokens"], dtype=np.float32))
    shared["conv_wT"] = np.ascontiguousarray(np.asarray(inputs["conv_w"], dtype=np.float32).transpose(0, 2, 1))
    for k, v in consts.items():
        shared["c_" + k] = v
    x = np.asarray(inputs["x"], dtype=np.float32)
    B = x.shape[0]
    maps = []
    for c in range(n_cores):
        m = dict(shared)
        m["x"] = np.ascontiguousarray(x[c % B])
        maps.append(m)
    return maps


def kernel(**inputs):
    x = np.asarray(inputs["x"])
    B, SEQ, _ = x.shape
    depth = np.asarray(inputs["w_in"]).shape[0]
    nc, K = build(SEQ, depth)
    maps = make_in_maps(inputs, SEQ, depth, K.consts)
    res = run_bass_kernel_spmd(nc, maps, core_ids=list(range(8)))
    out = np.stack([np.asarray(res.results[b]["out"], dtype=np.float32) for b in range(B)], axis=0)
    return out
```

```python
import contextlib
import numpy as np
import ml_dtypes
import concourse.bass as bass
import concourse.mybir as mybir
from concourse.bass_utils import run_bass_kernel_spmd

F32 = mybir.dt.float32
BF16 = mybir.dt.bfloat16
AF = mybir.ActivationFunctionType
ALU = mybir.AluOpType
AX = mybir.AxisListType

D = 1024
KC = 8
DEPTH = 4
IN_W = 3592
ALPHA = float((2 * DEPTH) ** 0.25)
LN_EPS = 1e-5
RMS_EPS = 1e-6
NEG = -30000.0
INTERLEAVE_GDN = True
DMA_PAD = 3
GDN_YK = 1


class Ev:
    __slots__ = ("key", "val", "snap", "src")

    def __init__(self, key, val, snap, src):
        self.key, self.val, self.snap, self.src = key, val, snap, src


class Buf:
    __slots__ = ("w", "r", "name", "excl")

    def __init__(self, name=""):
        self.w = None
        self.r = {}
        self.name = name
        self.excl = False


class Tl:
    def __init__(self, h, name=""):
        self.h = h
        self.b = Buf(name)

    def __getitem__(self, idx):
        return self.h[idx]


ENG = ("pe", "act", "dve", "pool", "sp")


class Sched:
    def __init__(self, nc, es, ndma=8, same_eng=True):
        self.nc = nc
        self.e = {"pe": nc.tensor, "act": nc.scalar, "dve": nc.vector, "pool": nc.gpsimd, "sp": nc.sync}
        self.sem = {k: es.enter_context(nc.semaphore("sem_" + k)) for k in ENG}
        self.cnt = {k: 0 for k in ENG}
        self.seen = {k: {} for k in ENG}
        self.dq = {}
        self.semobj = {"c:" + k: self.sem[k] for k in ENG}
        for q in ("sp", "pool"):
            sems = [es.enter_context(nc.semaphore("dma_%s_%d" % (q, i))) for i in range(ndma)]
            self.dq[q] = {"sems": sems, "n": 0, "pending": [None] * ndma}
            for i, sm in enumerate(sems):
                self.semobj["d:%s:%d" % (q, i)] = sm
        self.same_eng = same_eng
        self.nwait = 0
        self.max_ops = None
        self.log = None
        self.nins = 0
        self.rr = 0

    def _wait(self, eng, ev):
        if ev is None:
            return
        seen = self.seen[eng]
        if seen.get(ev.key, 0) >= ev.val:
            return
        if ev.src == eng and (eng == "pe" or not self.same_eng):
            return
        self.e[eng].wait_ge(self.semobj[ev.key], ev.val)
        if self.log is not None:
            self.log.append((self.nins, eng, "WAIT %s >= %d" % (ev.key, ev.val)))
        self.nwait += 1
        new = dict(seen)
        for k, v in ev.snap.items():
            if new.get(k, 0) < v:
                new[k] = v
        if new.get(ev.key, 0) < ev.val:
            new[ev.key] = ev.val
        self.seen[eng] = new

    def _deps(self, eng, R, W):
        for b in R:
            self._wait(eng, b.w)
            if b.excl:
                for k, ev in list(b.r.items()):
                    if k != eng:
                        self._wait(eng, ev)
        for b in W:
            self._wait(eng, b.w)
            for ev in list(b.r.values()):
                self._wait(eng, ev)

    def op(self, eng, fn, R=(), W=()):
        if self.max_ops is not None and self.nins >= self.max_ops:
            return None
        R = [getattr(x, "b", x) for x in R]
        W = [getattr(x, "b", x) for x in W]
        self._deps(eng, R, W)
        ins = fn()
        if self.log is not None:
            self.log.append((self.nins, eng, str(ins)[:150]))
        self.cnt[eng] += 1
        self.nins += 1
        ins.then_inc(self.sem[eng], 1)
        ev = Ev("c:" + eng, self.cnt[eng], self.seen[eng], eng)
        for b in R:
            b.r[eng] = ev
        for b in W:
            b.w = ev
            b.r = {}
        return ev

    def dma(self, q, out, in_, R=(), W=(), **kw):
        if self.max_ops is not None and self.nins >= self.max_ops:
            return None
        R = [getattr(x, "b", x) for x in R]
        W = [getattr(x, "b", x) for x in W]
        dq = self.dq[q]
        ns = len(dq["sems"])
        i = dq["n"] % ns
        self._wait(q, dq["pending"][i])
        self._deps(q, R, W)
        ins = self.e[q].dma_start(out=out, in_=in_, **kw)
        val = 16 * (dq["n"] // ns + 1)
        ins.then_inc(dq["sems"][i], 16)
        ev = Ev("d:%s:%d" % (q, i), val, self.seen[q], "dma")
        dq["pending"][i] = ev
        dq["n"] += 1
        self.nins += 1
        for b in R:
            b.r[("dma", q, dq["n"])] = ev
        for b in W:
            b.w = ev
            b.r = {}
        return ev

    def barrier(self):
        evs = [Ev("c:" + k, self.cnt[k], {}, "x") for k in ENG if self.cnt[k] > 0]
        for dq in self.dq.values():
            evs += [p for p in dq["pending"] if p is not None]
        for eng in ENG:
            for ev in evs:
                self._wait(eng, ev)

    def finish(self):
        for dq in self.dq.values():
            for p in dq["pending"]:
                self._wait("sp", p)

    def alt(self):
        self.rr += 1
        return "act" if (self.rr & 1) else "dve"


class Ctx:
    pass


def _sb(K, es, name, shape, dt):
    K.uid = getattr(K, "uid", 0) + 1
    name = "%s_u%d" % (name, K.uid)
    return Tl(es.enter_context(K.nc.sbuf_tensor(name, list(shape), dt)), name)


def _evac(K, eng, out, in_, R, W, scale=None):
    nc, S = K.nc, K.S
    if eng == "act":
        if scale is None:
            S.op("act", lambda: nc.scalar.copy(out=out, in_=in_), R=R, W=W)
        else:
            S.op("act", lambda: nc.scalar.mul(out=out, in_=in_, mul=scale), R=R, W=W)
    else:
        if scale is None:
            S.op("dve", lambda: nc.vector.tensor_copy(out=out, in_=in_), R=R, W=W)
        else:
            S.op("dve", lambda: nc.vector.tensor_scalar_mul(out=out, in0=in_, scalar1=scale), R=R, W=W)


def _ps(K):
    K.psi = (K.psi + 1) % len(K.psum)
    return K.psum[K.psi]


def _ln_tile(K, x, out, g, b, sm):
    nc, S = K.nc, K.S
    st, mv, rstd, tmp = sm["st"], sm["mv"], sm["rstd"], sm["tmp"]
    S.op("dve", lambda: nc.vector.bn_stats(out=st[:, 0, :], in_=x[:, 0:512]), R=[x], W=[st])
    S.op("dve", lambda: nc.vector.bn_stats(out=st[:, 1, :], in_=x[:, 512:1024]), R=[x, st], W=[st])
    S.op("dve", lambda: nc.vector.bn_aggr(out=mv[:], in_=st[:].rearrange("p a b -> p (a b)")), R=[st], W=[mv])
    S.op("act", lambda: nc.scalar.activation(out=rstd[:], in_=mv[:, 1:2], func=AF.Sqrt, bias=K.eps_ln[:, 0:1], scale=1.0),
         R=[mv, K.eps_ln], W=[rstd])
    S.op("dve", lambda: nc.vector.reciprocal(out=rstd[:], in_=rstd[:]), R=[rstd], W=[rstd])
    S.op("dve", lambda: nc.vector.tensor_scalar(out=tmp[:], in0=x[:], scalar1=mv[:, 0:1], scalar2=rstd[:, 0:1],
                                                op0=ALU.subtract, op1=ALU.mult), R=[x, mv, rstd], W=[tmp])
    S.op("dve", lambda: nc.vector.tensor_tensor(out=tmp[:], in0=tmp[:], in1=g[:], op=ALU.mult), R=[tmp, g], W=[tmp])
    S.op("dve", lambda: nc.vector.tensor_tensor(out=out[:], in0=tmp[:], in1=b[:], op=ALU.add), R=[tmp, b], W=[out])


def _load_cast(K, dst_tl, dst_ap, src_ap, shape, Wb):
    nc, S = K.nc, K.S
    K.stgi = (K.stgi + 1) % len(K.stg)
    st = K.stg[K.stgi]
    n = int(np.prod(shape[1:]))
    if len(shape) == 3:
        v = st.h[:, 0:n].rearrange("p (a b) -> p a b", a=shape[1])
    else:
        v = st.h[:, 0:n]
    S.dma("sp", v, src_ap, W=[st])
    S.op("pool", lambda: nc.gpsimd.tensor_copy(out=dst_ap, in_=v), R=[st], W=[Wb])


def phase_input(K):
    nc, S, NT = K.nc, K.S, K.NT
    with contextlib.ExitStack() as es:
        g = _sb(K, es, "pi_g", [128, D], F32)
        b = _sb(K, es, "pi_b", [128, D], F32)
        S.dma("sp", g[:], K.d["ln_in_g"].partition_broadcast(128), W=[g])
        S.dma("sp", b[:], K.d["ln_in_b"].partition_broadcast(128), W=[b])
        xs = [_sb(K, es, "pi_x%d" % i, [128, D], F32) for i in range(2)]
        os_ = [_sb(K, es, "pi_o%d" % i, [128, D], F32) for i in range(2)]
        sm = {"st": _sb(K, es, "pi_st", [128, 2, 6], F32), "mv": _sb(K, es, "pi_mv", [128, 2], F32),
              "rstd": _sb(K, es, "pi_rs", [128, 1], F32), "tmp": _sb(K, es, "pi_tmp", [128, D], F32)}
        PT = K.SEQ + 64
        if NT * 128 > PT:
            zb = _sb(K, es, "pi_zb", [128, 512], BF16)
            S.op("pool", lambda: nc.gpsimd.memset(zb[:], 0.0), W=[zb])
            S.dma("sp", K.d["mixed"][PT:NT * 128, 512:1024], zb[0:NT * 128 - PT, :], R=[zb], W=[K.sbuf_mx_gd])
        for t in range(NT):
            x, o = xs[t % 2], os_[t % 2]
            lo = 128 * t - 64
            r0, r1 = max(lo, 0), min(lo + 128, K.SEQ)
            if t == 0 or r1 - lo < 128:
                S.op("pool", lambda: nc.gpsimd.memset(x[:], 0.0), W=[x])
            if t == 0:
                S.dma("sp", x[48:64, :], K.d["meta"][:, :], W=[x])
            if r1 > r0:
                S.dma("sp", x[r0 - lo:r1 - lo, :], K.d["x"][r0:r1, :], W=[x])
            _ln_tile(K, x, o, g, b, sm)
            S.dma("sp", K.d["h"][128 * t:128 * (t + 1), :], o[:], R=[o], W=[K.hbuf[t]])


def phase_A1(K, l):
    nc, S, NT = K.nc, K.S, K.NT
    with contextlib.ExitStack() as es:
        K.stg = [_sb(K, es, "stg%d" % i, [128, IN_W], F32) for i in range(2)]
        Wb = _sb(K, es, "a1_W", [128, KC, IN_W], BF16)
        wbufs = [Buf() for _ in range(KC)]
        wsrc = K.d["w_in"][l].rearrange("(k p) n -> p k n", p=128)
        for kc in range(KC):
            _load_cast(K, Wb, Wb[:, kc, :], wsrc[:, kc, :], [128, IN_W], wbufs[kc])
        dtb = _sb(K, es, "a1_dtb", [128, 4], F32)
        nea = _sb(K, es, "a1_nea", [128, 4], F32)
        S.dma("sp", dtb[:], K.d["dt_bias"][l].partition_broadcast(128), W=[dtb])
        S.dma("sp", nea[:], K.d["a_log"][l].partition_broadcast(128), W=[nea])
        S.op("act", lambda: nc.scalar.activation(out=nea[:], in_=nea[:], func=AF.Exp), R=[nea], W=[nea])
        S.op("dve", lambda: nc.vector.tensor_scalar_mul(out=nea[:], in0=nea[:], scalar1=-1.0), R=[nea], W=[nea])
        hts = [_sb(K, es, "a1_h%d" % i, [128, 4, D], F32) for i in range(1)]
        hTs = [_sb(K, es, "a1_hT%d" % i, [128, KC, 512], BF16) for i in range(2)]
        stq = [_sb(K, es, "a1_sq%d" % i, [128, 512], BF16) for i in range(3)]
        stg = [_sb(K, es, "a1_sg%d" % i, [128, 512], F32) for i in range(3)]
        stv = [_sb(K, es, "a1_sv%d" % i, [128, 4, 512], BF16) for i in range(2)]
        stz = [_sb(K, es, "a1_sz%d" % i, [128, 4, 512], F32) for i in range(2)]
        stb = [_sb(K, es, "a1_sb%d" % i, [128, 4, 8], F32) for i in range(2)]
        tb = _sb(K, es, "a1_tb", [128, 4, 4], F32)
        NG = (NT + 3) // 4
        for g in range(NG):
            nt = min(4, NT - 4 * g)
            n = nt * 128
            c0 = g * 512
            ht, hT = hts[0], hTs[g % 2]
            sv, sz, sbb = stv[g % 2], stz[g % 2], stb[g % 2]
            S.dma("sp", ht[:, 0:nt, :], K.d["h"][c0:c0 + n, :].rearrange("(t p) d -> p t d", p=128),
                  R=K.hbuf[4 * g:4 * g + nt], W=[ht])
            for kc in range(KC):
                ps = _ps(K)
                for t in range(nt):
                    S.op("pe", lambda: nc.tensor.transpose(out=ps[:, t * 128:(t + 1) * 128],
                                                           in_=ht[:, t, kc * 128:(kc + 1) * 128], identity=K.identf[:]),
                         R=[ht, K.identf], W=[ps])
                _evac(K, S.alt(), hT[:, kc, 0:n], ps[:, 0:n], [ps], [hT])
            for ci in range(20):
                if ci < 8:
                    col = ci * 128
                else:
                    col = 1536 + (ci - 8) * 128
                ps = _ps(K)
                for kc in range(KC):
                    S.op("pe", lambda: nc.tensor.matmul(ps[:, 0:n], lhsT=Wb[:, kc, col:col + 128], rhs=hT[:, kc, 0:n],
                                                        start=(kc == 0), stop=(kc == KC - 1)),
                         R=[wbufs[kc], hT], W=[ps])
                if ci < 8:
                    sq = stq[ci % 3]
                    _evac(K, S.alt(), sq[:, 0:n], ps[:, 0:n], [ps], [sq], scale=(0.125 if ci < 4 else None))
                    if ci < 4:
                        S.dma("sp", K.d["qT"][ci, :, c0:c0 + n], sq[:, 0:n], R=[sq], W=[K.sbuf_q])
                    else:
                        S.dma("sp", K.d["kT"][ci - 4, :, c0:c0 + n], sq[:, 0:n], R=[sq], W=[K.sbuf_k])
                else:
                    sg = stg[ci % 3]
                    _evac(K, S.alt(), sg[:, 0:n], ps[:, 0:n], [ps], [sg])
                    S.dma("sp", K.d["gT"][ci - 8, :, c0:c0 + n], sg[:, 0:n], R=[sg], W=[K.sbuf_g])
            for t in range(nt):
                ps = _ps(K)
                for kc in range(KC):
                    S.op("pe", lambda: nc.tensor.matmul(ps[:, :], lhsT=hT[:, kc, t * 128:(t + 1) * 128], rhs=Wb[:, kc, 1024:1536],
                                                        start=(kc == 0), stop=(kc == KC - 1)), R=[wbufs[kc], hT], W=[ps])
                _evac(K, S.alt(), sv[:, t, :], ps[:, :], [ps], [sv])
                ps = _ps(K)
                for kc in range(KC):
                    S.op("pe", lambda: nc.tensor.matmul(ps[:, :], lhsT=hT[:, kc, t * 128:(t + 1) * 128], rhs=Wb[:, kc, 3072:3584],
                                                        start=(kc == 0), stop=(kc == KC - 1)), R=[wbufs[kc], hT], W=[ps])
                S.op("act", lambda: nc.scalar.activation(out=sz[:, t, :], in_=ps[:, :], func=AF.Silu), R=[ps], W=[sz])
                ps = _ps(K)
                for kc in range(KC):
                    S.op("pe", lambda: nc.tensor.matmul(ps[:, 0:8], lhsT=hT[:, kc, t * 128:(t + 1) * 128], rhs=Wb[:, kc, 3584:3592],
                                                        start=(kc == 0), stop=(kc == KC - 1)), R=[wbufs[kc], hT], W=[ps])
                S.op("act", lambda: nc.scalar.activation(out=sbb[:, t, 0:4], in_=ps[:, 0:4], func=AF.Sigmoid), R=[ps], W=[sbb])
                S.op("dve", lambda: nc.vector.tensor_tensor(out=tb[:, t, :], in0=ps[:, 4:8], in1=dtb[:], op=ALU.add),
                     R=[ps, dtb], W=[tb])
            S.op("act", lambda: nc.scalar.activation(out=tb[:, 0:nt, :], in_=tb[:, 0:nt, :], func=AF.Exp), R=[tb], W=[tb])
            S.op("act", lambda: nc.scalar.activation(out=tb[:, 0:nt, :], in_=tb[:, 0:nt, :], func=AF.Ln, bias=K.one_c[:, 0:1], scale=1.0),
                 R=[tb, K.one_c], W=[tb])
            S.op("dve", lambda: nc.vector.tensor_tensor(out=sbb[:, 0:nt, 4:8], in0=tb[:, 0:nt, :],
                                                        in1=nea[:].unsqueeze(1).to_broadcast([128, nt, 4]), op=ALU.mult),
                 R=[tb, nea], W=[sbb])
            S.dma("sp", K.d["V"][c0:c0 + n, :].rearrange("(t p) c -> p t c", p=128), sv[:, 0:nt, :], R=[sv], W=[K.sbuf_v])
            S.dma("sp", K.d["zs"][c0:c0 + n, :].rearrange("(t p) c -> p t c", p=128), sz[:, 0:nt, :], R=[sz], W=[K.sbuf_z])
            S.dma("sp", K.d["bg"][c0:c0 + n, :].rearrange("(t p) c -> p t c", p=128), sbb[:, 0:nt, :], R=[sbb], W=[K.sbuf_bg])


class RR:
    def __init__(self):
        self.items = []

    def add(self, gen, w=1):
        self.items.append([gen, w])

    def run(self):
        while self.items:
            for item in list(self.items):
                for _ in range(item[1]):
                    try:
                        next(item[0])
                    except StopIteration:
                        self.items.remove(item)
                        break


def _psg(K):
    K.psgi = (K.psgi + 1) % len(K.psum_gd)
    return K.psum_gd[K.psgi]


def sb_stream(K, l, es):
    nc, S, NT = K.nc, K.S, K.NT
    P = NT * 128
    qT = _sb(K, es, "sb_q", [128, P], BF16)
    kT = _sb(K, es, "sb_k", [128, P], BF16)
    Vt = _sb(K, es, "sb_v", [128, NT, 128], BF16)
    sbg = _sb(K, es, "sb_g", [128, 64], F32)
    S.dma("sp", sbg[:], K.d["sb_norm_g"][l].partition_broadcast(128), W=[sbg])
    es_ = [_sb(K, es, "sb_e%d" % i, [128, 512], F32) for i in range(2)]
    sps = [_sb(K, es, "sb_sp%d" % i, [128, 512], BF16) for i in range(4)]
    ws = [_sb(K, es, "sb_w%d" % i, [128, 512], BF16) for i in range(3)]
    oaccs = [_sb(K, es, "sb_oa%d" % i, [128, 4, 64], F32) for i in range(2)]
    raccs = [_sb(K, es, "sb_ra%d" % i, [128, 4], F32) for i in range(2)]
    eRs = [_sb(K, es, "sb_eR%d" % i, [128, 4], F32) for i in range(2)]
    tmps = [_sb(K, es, "sb_tmp%d" % i, [128, 4, 64], F32) for i in range(2)]
    osbs = [_sb(K, es, "sb_os%d" % i, [128, 4, 128], BF16) for i in range(2)]
    ss = _sb(K, es, "sb_ss", [128, 4], F32)
    zb = K.psum[0:3]
    pb = K.psum[3:5]
    NG = (NT + 3) // 4
    gi = 0
    for hp in range(4):
        S.dma("sp", qT[:, :], K.d["qT"][hp], R=[K.sbuf_q], W=[qT])
        S.dma("sp", kT[:, :], K.d["kT"][hp], R=[K.sbuf_k], W=[kT])
        S.dma("sp", Vt[:, :, :], K.d["V"][:, hp * 128:(hp + 1) * 128].rearrange("(t p) c -> p t c", p=128), R=[K.sbuf_v], W=[Vt])
        its = []
        for qg in range(NG):
            nt = min(4, NT - 4 * qg)
            for h2 in range(2):
                kbs = list(range(4 * qg + nt - 1, -1, -1))
                for j, kb in enumerate(kbs):
                    its.append((qg, nt, h2, kb, j == 0, j == len(kbs) - 1, gi))
                gi += 1
        N = len(its)

        def geom(it):
            qg, nt, h2, kb = it[0], it[1], it[2], it[3]
            rel = kb - 4 * qg
            if rel >= 0:
                mi = 4 if kb == 0 else rel
            elif kb == 0:
                mi = 5
            else:
                mi = None
            lo = max(rel, 0)
            return rel, mi, lo, lo * 128, nt * 128, qg * 512

        def stA(i):
            it = its[i]
            qg, nt, h2, kb = it[0], it[1], it[2], it[3]
            rel, mi, lo, c0, n, q0 = geom(it)
            z, e, sp = zb[i % 3], es_[i % 2], sps[i % 4]
            r0 = h2 * 64
            kk = kT[r0:r0 + 64, kb * 128:(kb + 1) * 128]
            qq = qT[r0:r0 + 64, q0 + c0:q0 + n]
            S.op("pe", lambda: nc.tensor.matmul(z[:, c0:n], lhsT=kk, rhs=qq, start=True, stop=(mi is None)), R=[kT, qT], W=[z])
            if mi is not None:
                S.op("pe", lambda: nc.tensor.matmul(z[:, c0:n], lhsT=K.identb[:], rhs=K.masks[:, mi, c0:n], start=False, stop=True),
                     R=[K.identb, K.masks], W=[z])
            S.op("act", lambda: nc.scalar.activation(out=e[:, c0:n], in_=z[:, c0:n], func=AF.Exp), R=[z], W=[e])
            S.op("act", lambda: nc.scalar.activation(out=sp[:, c0:n], in_=e[:, c0:n], func=AF.Ln, bias=K.one_c[:, 0:1], scale=1.0),
                 R=[e, K.one_c], W=[sp])

        def stB(i):
            it = its[i]
            qg, nt, h2, kb = it[0], it[1], it[2], it[3]
            rel, mi, lo, c0, n, q0 = geom(it)
            z, sp, w = zb[i % 3], sps[i % 4], ws[i % 3]
            r0 = h2 * 64
            kk = kT[r0:r0 + 64, kb * 128:(kb + 1) * 128]
            qq = qT[r0:r0 + 64, q0 + c0:q0 + n]
            S.op("pe", lambda: nc.tensor.matmul(z[:, c0:n], lhsT=kk, rhs=qq, start=True, stop=False), R=[kT, qT], W=[z])
            if mi is not None:
                S.op("pe", lambda: nc.tensor.matmul(z[:, c0:n], lhsT=K.identb[:], rhs=K.masks[:, mi, c0:n], start=False, stop=False),
                     R=[K.identb, K.masks], W=[z])
            S.op("pe", lambda: nc.tensor.matmul(z[:, c0:n], lhsT=K.negtri[:], rhs=sp[:, c0:n], start=False, stop=True),
                 R=[K.negtri, sp], W=[z])
            S.op("act", lambda: nc.scalar.activation(out=w[:, c0:n], in_=z[:, c0:n], func=AF.Exp), R=[z], W=[w])

        def stC(i):
            it = its[i]
            qg, nt, h2, kb, first, last, g_ = it
            rel, mi, lo, c0, n, q0 = geom(it)
            sp, w = sps[i % 4], ws[i % 3]
            oacc, racc = oaccs[g_ % 2], raccs[g_ % 2]
            eR, tmp = eRs[i % 2], tmps[i % 2]
            osb = osbs[qg % 2]
            if first:
                S.op("pool", lambda: nc.gpsimd.memset(oacc[:], 0.0), W=[oacc])
                S.op("pool", lambda: nc.gpsimd.memset(racc[:], 0.0), W=[racc])
            po = pb[i % 2]
            pov = po.h[:, 0:260].rearrange("p (t c) -> p t c", c=65)
            for qt in range(lo, nt):
                S.op("pe", lambda: nc.tensor.matmul(pov[:, qt, 0:64], lhsT=w[:, qt * 128:(qt + 1) * 128],
                                                    rhs=Vt[:, kb, h2 * 64:(h2 + 1) * 64], start=True, stop=True),
                     R=[w, Vt], W=[po])
                S.op("pe", lambda: nc.tensor.matmul(pov[:, qt, 64:65], lhsT=sp[:, qt * 128:(qt + 1) * 128],
                                                    rhs=K.onesb[:, 0:1], start=True, stop=True),
                     R=[sp, K.onesb], W=[po])
            S.op("act", lambda: nc.scalar.activation(out=eR[:, lo:nt], in_=racc[:, lo:nt], func=AF.Exp, scale=-1.0),
                 R=[racc], W=[eR])
            S.op("dve", lambda: nc.vector.tensor_tensor(out=tmp[:, lo:nt, :], in0=pov[:, lo:nt, 0:64],
                                                        in1=eR[:, lo:nt].unsqueeze(2).to_broadcast([128, nt - lo, 64]), op=ALU.mult),
                 R=[po, eR], W=[tmp])
            S.op("pool", lambda: nc.gpsimd.tensor_tensor(out=oacc[:, lo:nt, :], in0=oacc[:, lo:nt, :], in1=tmp[:, lo:nt, :], op=ALU.add),
                 R=[oacc, tmp], W=[oacc])
            S.op("dve", lambda: nc.vector.tensor_tensor(out=racc[:, lo:nt], in0=racc[:, lo:nt], in1=pov[:, lo:nt, 64], op=ALU.add),
                 R=[racc, po], W=[racc])
            if last:
                tmp2 = tmps[(i + 1) % 2]
                S.op("dve", lambda: nc.vector.tensor_tensor(out=tmp2[:, 0:nt, :], in0=oacc[:, 0:nt, :], in1=oacc[:, 0:nt, :], op=ALU.mult),
                     R=[oacc], W=[tmp2])
                S.op("dve", lambda: nc.vector.tensor_reduce(out=ss[:, 0:nt], in_=tmp2[:, 0:nt, :], axis=AX.X, op=ALU.add), R=[tmp2], W=[ss])
                S.op("act", lambda: nc.scalar.activation(out=ss[:, 0:nt], in_=ss[:, 0:nt], func=AF.Ln, bias=K.eps_rms64[:, 0:1], scale=1.0),
                     R=[ss, K.eps_rms64], W=[ss])
                S.op("act", lambda: nc.scalar.activation(out=ss[:, 0:nt], in_=ss[:, 0:nt], func=AF.Exp, scale=-0.5), R=[ss], W=[ss])
                S.op("dve", lambda: nc.vector.scalar_tensor_tensor(out=tmp2[:, 0:nt, :], in0=oacc[:, 0:nt, :], scalar=8.0,
                                                                  in1=ss[:, 0:nt].unsqueeze(2).to_broadcast([128, nt, 64]),
                                                                  op0=ALU.mult, op1=ALU.mult),
                     R=[oacc, ss], W=[tmp2])
                S.op("dve", lambda: nc.vector.tensor_tensor(out=osb[:, 0:nt, h2 * 64:(h2 + 1) * 64], in0=tmp2[:, 0:nt, :],
                                                            in1=sbg[:].unsqueeze(1).to_broadcast([128, nt, 64]), op=ALU.mult),
                     R=[tmp2, sbg], W=[osb])
                if h2 == 1:
                    S.dma("sp", K.d["mixed"][q0:q0 + n, hp * 128:(hp + 1) * 128].rearrange("(t p) c -> p t c", p=128), osb[:, 0:nt, :],
                          R=[osb], W=[K.sbuf_mx_sb])

        for s in range(N + 2):
            if s < N:
                stA(s)
            if 0 <= s - 1 < N:
                stB(s - 1)
            if 0 <= s - 2 < N:
                stC(s - 2)
            yield


def gdn_master(K, l, es, rr, NSETS, YK):
    nc, S, NT = K.nc, K.S, K.NT
    NC_ = K.NC
    cw = _sb(K, es, "gd_cw", [128, 12, 4], F32)
    S.dma("sp", cw[:], K.d["conv_wT"][l].rearrange("(i d) t -> d i t", d=128), W=[cw])
    gng = _sb(K, es, "gd_gng", [64, 4, 128], F32)
    for h in range(4):
        S.dma("sp", gng[:, h, :], K.d["gdn_norm_g"][l].partition_broadcast(64), W=[gng])
    xin = [_sb(K, es, "gd_x%d" % i, [128, 12, 131], F32) for i in range(2)]
    cvs = [_sb(K, es, "gd_cv%d" % i, [128, 12, 128], F32) for i in range(2)]
    sq = [_sb(K, es, "gd_sq%d" % i, [128, 128], F32) for i in range(2)]
    rn = [_sb(K, es, "gd_rn%d" % i, [128, 128], F32) for i in range(2)]
    Sst = _sb(K, es, "gd_S", [128, 4, 128], F32)
    S.op("pool", lambda: nc.gpsimd.memset(Sst[:], 0.0), W=[Sst])
    tmpS = _sb(K, es, "gd_tmpS", [128, 4, 128], F32)

    def mk(name, shape, dt=F32):
        return [_sb(K, es, "gd_%s%d" % (name, i), shape, dt) for i in range(NSETS)]
    B = {}
    for name in ("egc", "egr", "ssq"):
        B[name] = mk(name, [64, 4])
    B["cdt"] = mk("cd", [128, 4])
    for name in ("gl", "dm", "dmT", "Nm", "Mm", "N2", "M2", "Pm", "qkT"):
        B[name] = mk(name, [64, 4, 64])
    for name in ("kbg", "vb", "ke", "gbc", "u", "vn", "ot", "o2"):
        B[name] = mk(name, [64, 4, 128])
    for name in ("egb", "qd", "wcT"):
        B[name] = mk(name, [128, 4, 64])
    B["ob"] = mk("ob", [64, 4, 128], BF16)
    B["bg"] = mk("bg", [64, 8])
    B["z"] = mk("z", [64, 512])
    st = {"inflight": 0, "rec_done": 0, "done": 0}
    assert NSETS == 2
    pbigs = [K.psum[5], K.psum[6]]
    psmls = pbigs
    psg1 = K.psum[7]
    n = 128

    def load_x(g):
        x = xin[g % 2]
        c0 = g * 128
        if g == 0:
            S.op("pool", lambda: nc.gpsimd.memset(x[:, :, 0:3], 0.0), W=[x])
            S.dma("sp", x[:, :, 3:3 + n], K.d["gT"][:, :, 0:n].rearrange("h p n -> p h n"), R=[K.sbuf_g], W=[x])
            S.op("pool", lambda: nc.gpsimd.memset(x[:, :, 3:3 + 48], 0.0), W=[x])
        else:
            S.dma("sp", x[:, :, 0:3 + n], K.d["gT"][:, :, c0 - 3:c0 + n].rearrange("h p n -> p h n"), R=[K.sbuf_g], W=[x])

    def G1(g):
        le = None
        x, cv = xin[g % 2], cvs[g % 2]
        k = 0
        for i in range(12):
            eng = "dve"
            E = nc.vector
            if le != eng:
                yield
            le = eng
            S.op(eng, lambda: E.tensor_scalar(out=cv[:, i, 0:n], in0=x[:, i, 0:n], scalar1=cw[:, i, 0:1], scalar2=None, op0=ALU.mult),
                 R=[x, cw], W=[cv])
            for tp in range(1, 4):
                if le != "dve":
                    yield
                le = "dve"
                S.op("dve", lambda: nc.vector.scalar_tensor_tensor(out=cv[:, i, 0:n], in0=x[:, i, tp:tp + n], scalar=cw[:, i, tp:tp + 1],
                                                                  in1=cv[:, i, 0:n], op0=ALU.mult, op1=ALU.add), R=[x, cw, cv], W=[cv])
            s_ = sq[i % 2]
            if le != "act":
                yield
            le = "act"
            S.op("act", lambda: nc.scalar.activation(out=s_[:, 0:n], in_=cv[:, i, 0:n], func=AF.Exp, scale=-1.0), R=[cv], W=[s_])
            S.op("act", lambda: nc.scalar.activation(out=s_[:, 0:n], in_=s_[:, 0:n], func=AF.Ln, bias=K.one_c[:, 0:1], scale=1.0),
                 R=[s_, K.one_c], W=[s_])
            S.op("act", lambda: nc.scalar.activation(out=s_[:, 0:n], in_=s_[:, 0:n], func=AF.Exp, scale=-1.0), R=[s_], W=[s_])
            if le != "dve":
                yield
            le = "dve"
            S.op("dve", lambda: nc.vector.tensor_tensor(out=cv[:, i, 0:n], in0=cv[:, i, 0:n], in1=s_[:, 0:n], op=ALU.mult), R=[cv, s_], W=[cv])
        for i in range(8):
            s_, r_ = sq[i % 2], rn[i % 2]
            if le != "pool":
                yield
            le = "pool"
            S.op("pool", lambda: nc.gpsimd.tensor_tensor(out=s_[:, 0:n], in0=cv[:, i, 0:n], in1=cv[:, i, 0:n], op=ALU.mult), R=[cv], W=[s_])
            ps = psg1
            if le != "pe":
                yield
            le = "pe"
            S.op("pe", lambda: nc.tensor.matmul(ps[:, 0:n], lhsT=K.onesf[:, :], rhs=s_[:, 0:n], start=True, stop=True),
                 R=[K.onesf, s_], W=[ps])
            if le != "act":
                yield
            le = "act"
            S.op("act", lambda: nc.scalar.activation(out=r_[:, 0:n], in_=ps[:, 0:n], func=AF.Ln, bias=K.eps_rms[:, 0:1], scale=1.0),
                 R=[ps, K.eps_rms], W=[r_])
            S.op("act", lambda: nc.scalar.activation(out=r_[:, 0:n], in_=r_[:, 0:n], func=AF.Exp, scale=-0.5), R=[r_], W=[r_])
            if i < 4:
                if le != "dve":
                    yield
                le = "dve"
                S.op("dve", lambda: nc.vector.scalar_tensor_tensor(out=cv[:, i, 0:n], in0=cv[:, i, 0:n], scalar=float(128 ** -0.5),
                                                                  in1=r_[:, 0:n], op0=ALU.mult, op1=ALU.mult), R=[cv, r_], W=[cv])
            else:
                if le != "dve":
                    yield
                le = "dve"
                S.op("dve", lambda: nc.vector.tensor_tensor(out=cv[:, i, 0:n], in0=cv[:, i, 0:n], in1=r_[:, 0:n], op=ALU.mult),
                     R=[cv, r_], W=[cv])

    def chunk(g, ci, cidx):
        le = None
        b2 = cidx % NSETS
        pbig, psml = pbigs[b2], psmls[b2]
        cv = cvs[g % 2]
        egc, egr, cdt, ssq = B["egc"][b2], B["egr"][b2], B["cdt"][b2], B["ssq"][b2]
        gl, dm, dmT, Pm, qkT = B["gl"][b2], B["dm"][b2], B["dmT"][b2], B["Pm"][b2], B["qkT"][b2]
        kbg, vb, ke, gbc, u, vn, ot, o2 = (B[k_][b2] for k_ in ("kbg", "vb", "ke", "gbc", "u", "vn", "ot", "o2"))
        egb, qd, wcT, ob = B["egb"][b2], B["qd"][b2], B["wcT"][b2], B["ob"][b2]
        bgc, zc = B["bg"][b2], B["z"][b2]
        p0 = g * 128 + ci * 64
        if le != "sp":
            yield
        le = "sp"
        S.dma("sp", bgc[:, :], K.d["bg"][p0:p0 + 64, :], R=[K.sbuf_bg], W=[bgc])
        if le != "sp":
            yield
        le = "sp"
        S.dma("sp", zc[:, :], K.d["zs"][p0:p0 + 64, :], R=[K.sbuf_z], W=[zc])
        if cidx == 0:
            if le != "pool":
                yield
            le = "pool"
            S.op("pool", lambda: nc.gpsimd.memset(bgc[0:48, 4:8], 0.0), W=[bgc])
        for _ in range(DMA_PAD):
            yield
        cs = slice(ci * 64, ci * 64 + 64)
        gcol = bgc[:, 4:8]
        bcol = bgc[:, 0:4]
        pg = psml
        if le != "pe":
            yield
        le = "pe"
        S.op("pe", lambda: nc.tensor.matmul(pg[0:64, 0:4], lhsT=K.triu[:, 0, :], rhs=gcol, start=True, stop=True), R=[K.triu, bgc], W=[pg])
        if le != "pe":
            yield
        le = "pe"
        S.op("pe", lambda: nc.tensor.matmul(pg[0:64, 4:8], lhsT=K.sgt[:, :], rhs=gcol, start=True, stop=True), R=[K.sgt, bgc], W=[pg])
        if le != "pe":
            yield
        le = "pe"
        S.op("pe", lambda: nc.tensor.matmul(pg[:, 8:12], lhsT=K.ones64[:, :], rhs=gcol, start=True, stop=True), R=[K.ones64, bgc], W=[pg])
        if le != "act":
            yield
        le = "act"
        S.op("act", lambda: nc.scalar.activation(out=egc[:], in_=pg[0:64, 0:4], func=AF.Exp), R=[pg], W=[egc])
        if le != "act":
            yield
        le = "act"
        S.op("act", lambda: nc.scalar.activation(out=egr[:], in_=pg[0:64, 4:8], func=AF.Exp), R=[pg], W=[egr])
        if le != "act":
            yield
        le = "act"
        S.op("act", lambda: nc.scalar.activation(out=cdt[:], in_=pg[:, 8:12], func=AF.Exp), R=[pg], W=[cdt])
        if le != "dve":
            yield
        le = "dve"
        S.op("dve", lambda: nc.vector.tensor_tensor(out=gl[:], in0=K.triu[:], in1=gcol.unsqueeze(2).to_broadcast([64, 4, 64]), op=ALU.mult),
             R=[K.triu, bgc], W=[gl])
        pd = pbig
        pdv = pd.h[0:64, 0:512].rearrange("p (a h c) -> p a h c", a=2, h=4)
        for h in range(4):
            if le != "pe":
                yield
            le = "pe"
            S.op("pe", lambda: nc.tensor.matmul(pdv[:, 0, h, :], lhsT=gl[:, h, :], rhs=K.sgt[:, :], start=True, stop=True),
                 R=[gl, K.sgt], W=[pd])
        if le != "pe":
            yield
        le = "pe"
        S.op("pe", lambda: nc.tensor.matmul(pd[0:64, 256:512], lhsT=K.sgt[:, :], rhs=gl[:].rearrange("p h c -> p (h c)"), start=True, stop=True),
             R=[gl, K.sgt], W=[pd])
        if le != "act":
            yield
        le = "act"
        S.op("act", lambda: nc.scalar.activation(out=dm[:], in_=pdv[:, 0], func=AF.Exp), R=[pd], W=[dm])
        if le != "act":
            yield
        le = "act"
        S.op("act", lambda: nc.scalar.activation(out=dmT[:], in_=pdv[:, 1], func=AF.Exp), R=[pd], W=[dmT])
        if le != "pool":
            yield
        le = "pool"
        S.op("pool", lambda: nc.gpsimd.tensor_tensor(out=dm[:], in0=dm[:], in1=K.trilsn[:], op=ALU.mult), R=[dm, K.trilsn], W=[dm])
        if le != "pool":
            yield
        le = "pool"
        S.op("pool", lambda: nc.gpsimd.tensor_tensor(out=dmT[:], in0=dmT[:], in1=K.triu[:], op=ALU.mult), R=[dmT, K.triu], W=[dmT])
        pgq = pbig
        pgqv = pgq.h[0:64, 0:512].rearrange("p (a h c) -> p a h c", a=2, h=4)
        for h in range(4):
            if le != "pe":
                yield
            le = "pe"
            S.op("pe", lambda: nc.tensor.matmul(pgqv[:, 0, h, :], lhsT=cv[:, 4 + h, cs], rhs=cv[:, 4 + h, cs], start=True, stop=True), R=[cv], W=[pgq])
            if le != "pe":
                yield
            le = "pe"
            S.op("pe", lambda: nc.tensor.matmul(pgqv[:, 1, h, :], lhsT=cv[:, 4 + h, cs], rhs=cv[:, h, cs], start=True, stop=True), R=[cv], W=[pgq])
        Nm, Mm = B["Nm"][b2], B["Mm"][b2]
        if le != "dve":
            yield
        le = "dve"
        S.op("dve", lambda: nc.vector.tensor_tensor(out=Nm[:], in0=pgqv[:, 0], in1=bcol.unsqueeze(2).to_broadcast([64, 4, 64]), op=ALU.mult),
             R=[pgq, bgc], W=[Nm])
        if le != "dve":
            yield
        le = "dve"
        S.op("dve", lambda: nc.vector.tensor_tensor(out=Nm[:], in0=Nm[:], in1=dm[:], op=ALU.mult), R=[Nm, dm], W=[Nm])
        if le != "dve":
            yield
        le = "dve"
        S.op("dve", lambda: nc.vector.tensor_tensor(out=qkT[:], in0=pgqv[:, 1], in1=dmT[:], op=ALU.mult), R=[pgq, dmT], W=[qkT])
        pt = psml
        ptv = pt.h[0:64, 0:256].rearrange("p (h c) -> p h c", h=4)
        for h in range(4):
            if le != "pe":
                yield
            le = "pe"
            S.op("pe", lambda: nc.tensor.transpose(out=ptv[:, h, :], in_=Nm[:, h, :], identity=K.identf[0:64, 0:64]), R=[Nm, K.identf], W=[pt])
        if le != "act":
            yield
        le = "act"
        S.op("act", lambda: nc.scalar.copy(out=Mm[:], in_=ptv), R=[pt], W=[Mm])
        if le != "dve":
            yield
        le = "dve"
        S.op("dve", lambda: nc.vector.tensor_tensor(out=Pm[:], in0=Mm[:], in1=K.ident4[:], op=ALU.add), R=[Mm, K.ident4], W=[Pm])
        Nc, Mc, Nn, Mn = Nm, Mm, B["N2"][b2], B["M2"][b2]
        for r in range(5):
            pn = pbig
            pnv = pn.h[0:64, 0:512].rearrange("p (a h c) -> p a h c", a=2, h=4)
            for h in range(4):
                if le != "pe":
                    yield
                le = "pe"
                S.op("pe", lambda: nc.tensor.matmul(pnv[:, 0, h, :], lhsT=Mc[:, h, :], rhs=Nc[:, h, :], start=True, stop=True), R=[Mc, Nc], W=[pn])
                if r < 4:
                    if le != "pe":
                        yield
                    le = "pe"
                    S.op("pe", lambda: nc.tensor.matmul(pnv[:, 1, h, :], lhsT=Nc[:, h, :], rhs=Mc[:, h, :], start=True, stop=True), R=[Mc, Nc], W=[pn])
            if le != "act":
                yield
            le = "act"
            S.op("act", lambda: nc.scalar.copy(out=Nn[:], in_=pnv[:, 0]), R=[pn], W=[Nn])
            if r < 4:
                if le != "dve":
                    yield
                le = "dve"
                S.op("dve", lambda: nc.vector.tensor_copy(out=Mn[:], in_=pnv[:, 1]), R=[pn], W=[Mn])
            pp = psml
            ppv = pp.h[0:64, 0:256].rearrange("p (h c) -> p h c", h=4)
            for h in range(4):
                if le != "pe":
                    yield
                le = "pe"
                S.op("pe", lambda: nc.tensor.matmul(ppv[:, h, :], lhsT=Nn[:, h, :], rhs=Pm[:, h, :], start=True, stop=True), R=[Nn, Pm], W=[pp])
            if le != "dve":
                yield
            le = "dve"
            S.op("dve", lambda: nc.vector.tensor_tensor(out=Pm[:], in0=Pm[:], in1=ppv, op=ALU.add), R=[Pm, pp], W=[Pm])
            Nc, Mc, Nn, Mn = Nn, Mn, Nc, Mc
        pk = pbig
        pkv = pk.h[0:64, 0:512].rearrange("p (h c) -> p h c", h=4)
        for h in range(4):
            if le != "pe":
                yield
            le = "pe"
            S.op("pe", lambda: nc.tensor.transpose(out=pkv[:, h, :], in_=cv[:, 4 + h, cs], identity=K.identf[:]), R=[cv, K.identf], W=[pk])
        if le != "dve":
            yield
        le = "dve"
        S.op("dve", lambda: nc.vector.tensor_tensor(out=ke[:], in0=pkv, in1=egr[:].unsqueeze(2).to_broadcast([64, 4, 128]), op=ALU.mult),
             R=[pk, egr], W=[ke])
        if le != "dve":
            yield
        le = "dve"
        S.op("dve", lambda: nc.vector.tensor_tensor(out=kbg[:], in0=pkv, in1=bcol.unsqueeze(2).to_broadcast([64, 4, 128]), op=ALU.mult),
             R=[pk, bgc], W=[kbg])
        if le != "pool":
            yield
        le = "pool"
        S.op("pool", lambda: nc.gpsimd.tensor_tensor(out=kbg[:], in0=kbg[:], in1=egc[:].unsqueeze(2).to_broadcast([64, 4, 128]), op=ALU.mult),
             R=[kbg, egc], W=[kbg])
        pv = pbig
        pvv = pv.h[0:64, 0:512].rearrange("p (h c) -> p h c", h=4)
        for h in range(4):
            if le != "pe":
                yield
            le = "pe"
            S.op("pe", lambda: nc.tensor.transpose(out=pvv[:, h, :], in_=cv[:, 8 + h, cs], identity=K.identf[:]), R=[cv, K.identf], W=[pv])
        if le != "dve":
            yield
        le = "dve"
        S.op("dve", lambda: nc.vector.tensor_tensor(out=vb[:], in0=pvv, in1=bcol.unsqueeze(2).to_broadcast([64, 4, 128]), op=ALU.mult),
             R=[pv, bgc], W=[vb])
        if le != "pool":
            yield
        le = "pool"
        S.op("pool", lambda: nc.gpsimd.tensor_tensor(out=gbc[:], in0=K.ones4[:], in1=gcol.unsqueeze(2).to_broadcast([64, 4, 128]), op=ALU.mult),
             R=[K.ones4, bgc], W=[gbc])
        pe_ = psml
        pev = pe_.h[:, 0:256].rearrange("p (h c) -> p h c", h=4)
        for h in range(4):
            if le != "pe":
                yield
            le = "pe"
            S.op("pe", lambda: nc.tensor.matmul(pev[:, h, :], lhsT=gbc[:, h, :], rhs=K.triu[:, 0, :], start=True, stop=True), R=[gbc, K.triu], W=[pe_])
        if le != "act":
            yield
        le = "act"
        S.op("act", lambda: nc.scalar.activation(out=egb[:], in_=pev, func=AF.Exp), R=[pe_], W=[egb])
        if le != "dve":
            yield
        le = "dve"
        S.op("dve", lambda: nc.vector.tensor_tensor(out=qd[:], in0=cv[:, 0:4, cs], in1=egb[:], op=ALU.mult), R=[cv, egb], W=[qd])
        pw = psml
        pwv = pw.h[:, 0:256].rearrange("p (h c) -> p h c", h=4)
        for h in range(4):
            if le != "pe":
                yield
            le = "pe"
            S.op("pe", lambda: nc.tensor.matmul(pwv[:, h, :], lhsT=kbg[:, h, :], rhs=Pm[:, h, :], start=True, stop=True), R=[kbg, Pm], W=[pw])
        if le != "act":
            yield
        le = "act"
        S.op("act", lambda: nc.scalar.copy(out=wcT[:], in_=pwv), R=[pw], W=[wcT])
        pu = pbig
        puv = pu.h[0:64, 0:512].rearrange("p (h c) -> p h c", h=4)
        for h in range(4):
            if le != "pe":
                yield
            le = "pe"
            S.op("pe", lambda: nc.tensor.matmul(puv[:, h, :], lhsT=Pm[:, h, :], rhs=vb[:, h, :], start=True, stop=True), R=[vb, Pm], W=[pu])
        if le != "act":
            yield
        le = "act"
        S.op("act", lambda: nc.scalar.copy(out=u[:], in_=puv), R=[pu], W=[u])
        while st["rec_done"] < cidx:
            yield
        pws = pbig
        pwsv = pws.h[0:64, 0:512].rearrange("p (h c) -> p h c", h=4)
        for h in range(4):
            if le != "pe":
                yield
            le = "pe"
            S.op("pe", lambda: nc.tensor.matmul(pwsv[:, h, :], lhsT=wcT[:, h, :], rhs=Sst[:, h, :], start=True, stop=True), R=[wcT, Sst], W=[pws])
        if le != "dve":
            yield
        le = "dve"
        S.op("dve", lambda: nc.vector.tensor_tensor(out=vn[:], in0=u[:], in1=pwsv, op=ALU.subtract), R=[u, pws], W=[vn])
        po = pbig
        pov = po.h[0:64, 0:512].rearrange("p (h c) -> p h c", h=4)
        for h in range(4):
            if le != "pe":
                yield
            le = "pe"
            S.op("pe", lambda: nc.tensor.matmul(pov[:, h, :], lhsT=qd[:, h, :], rhs=Sst[:, h, :], start=True, stop=False), R=[qd, Sst], W=[po])
            if le != "pe":
                yield
            le = "pe"
            S.op("pe", lambda: nc.tensor.matmul(pov[:, h, :], lhsT=qkT[:, h, :], rhs=vn[:, h, :], start=False, stop=True), R=[qkT, vn], W=[po])
        if le != "act":
            yield
        le = "act"
        S.op("act", lambda: nc.scalar.copy(out=ot[:], in_=pov), R=[po], W=[ot])
        pS = pbig
        pSv = pS.h[:, 0:512].rearrange("p (h c) -> p h c", h=4)
        for h in range(4):
            if le != "pe":
                yield
            le = "pe"
            S.op("pe", lambda: nc.tensor.matmul(pSv[:, h, :], lhsT=ke[:, h, :], rhs=vn[:, h, :], start=True, stop=True), R=[ke, vn], W=[pS])
        if le != "pool":
            yield
        le = "pool"
        S.op("pool", lambda: nc.gpsimd.tensor_tensor(out=tmpS[:], in0=Sst[:], in1=cdt[:].unsqueeze(2).to_broadcast([128, 4, 128]), op=ALU.mult),
             R=[Sst, cdt], W=[tmpS])
        if le != "dve":
            yield
        le = "dve"
        S.op("dve", lambda: nc.vector.tensor_tensor(out=Sst[:], in0=tmpS[:], in1=pSv, op=ALU.add), R=[tmpS, pS], W=[Sst])
        st["rec_done"] = cidx + 1
        if le != "pool":
            yield
        le = "pool"
        S.op("pool", lambda: nc.gpsimd.tensor_tensor(out=o2[:], in0=ot[:], in1=ot[:], op=ALU.mult), R=[ot], W=[o2])
        if le != "dve":
            yield
        le = "dve"
        S.op("dve", lambda: nc.vector.tensor_reduce(out=ssq[:], in_=o2[:], axis=AX.X, op=ALU.add), R=[o2], W=[ssq])
        if le != "act":
            yield
        le = "act"
        S.op("act", lambda: nc.scalar.activation(out=ssq[:], in_=ssq[:], func=AF.Ln, bias=K.eps_rms128[0:64, 0:1], scale=1.0),
             R=[ssq, K.eps_rms128], W=[ssq])
        S.op("act", lambda: nc.scalar.activation(out=ssq[:], in_=ssq[:], func=AF.Exp, scale=-0.5), R=[ssq], W=[ssq])
        if le != "dve":
            yield
        le = "dve"
        S.op("dve", lambda: nc.vector.scalar_tensor_tensor(out=o2[:], in0=ot[:], scalar=float(128 ** 0.5),
                                                          in1=ssq[:].unsqueeze(2).to_broadcast([64, 4, 128]), op0=ALU.mult, op1=ALU.mult),
             R=[ot, ssq], W=[o2])
        if le != "pool":
            yield
        le = "pool"
        S.op("pool", lambda: nc.gpsimd.tensor_tensor(out=o2[:], in0=o2[:], in1=gng[:], op=ALU.mult), R=[o2, gng], W=[o2])
        if le != "dve":
            yield
        le = "dve"
        S.op("dve", lambda: nc.vector.tensor_tensor(out=ob[:], in0=o2[:], in1=zc[:, :].rearrange("p (h c) -> p h c", h=4), op=ALU.mult),
             R=[o2, zc], W=[ob])
        if le != "sp":
            yield
        le = "sp"
        S.dma("sp", K.d["mixed"][p0:p0 + 64, 512:1024], ob[:].rearrange("p h c -> p (h c)"), R=[ob], W=[K.sbuf_mx_gd])
        st["inflight"] -= 1
        st["done"] += 1

    NG = NT
    load_x(0)
    cidx = 0
    for g in range(NG):
        nch = min(2, NC_ - 2 * g)
        if nch <= 0:
            break
        if g + 1 < NG and NC_ - 2 * (g + 1) > 0:
            load_x(g + 1)
        while st["done"] < min(cidx, 2 * (g - 1)):
            yield
        yield from G1(g)
        for ci in range(nch):
            while st["inflight"] >= NSETS:
                yield
            st["inflight"] += 1
            rr.add(chunk(g, ci, cidx), YK)
            cidx += 1
            yield
    while st["done"] < cidx:
        yield


def phase_GDN_old(K, l):
    nc, S, NT = K.nc, K.S, K.NT
    NC_ = K.NC
    with contextlib.ExitStack() as es:
        cw = _sb(K, es, "gd_cw", [128, 12, 4], F32)
        S.dma("sp", cw[:], K.d["conv_wT"][l].rearrange("(i d) t -> d i t", d=128), W=[cw])
        gng = _sb(K, es, "gd_gng", [64, 4, 128], F32)
        for h in range(4):
            S.dma("sp", gng[:, h, :], K.d["gdn_norm_g"][l].partition_broadcast(64), W=[gng])
        xin = [_sb(K, es, "gd_x%d" % i, [128, 12, 259], F32) for i in range(2)]
        cv = _sb(K, es, "gd_cv", [128, 12, 256], F32)
        sq = [_sb(K, es, "gd_sq%d" % i, [128, 256], F32) for i in range(2)]
        rn = [_sb(K, es, "gd_rn%d" % i, [128, 256], F32) for i in range(2)]
        bgt = [_sb(K, es, "gd_bg%d" % i, [64, 4, 8], F32) for i in range(2)]
        zt = [_sb(K, es, "gd_z%d" % i, [64, 4, 512], F32) for i in range(2)]
        Sst = _sb(K, es, "gd_S", [128, 4, 128], F32)
        S.op("pool", lambda: nc.gpsimd.memset(Sst[:], 0.0), W=[Sst])
        def mk(name, shape, dt=F32):
            return [_sb(K, es, "gd_%s%d" % (name, i), shape, dt) for i in range(2)]
        egc, egr, cdt = mk("egc", [64, 4]), mk("egr", [64, 4]), mk("cd", [128, 4])
        gl, dm, dmT = mk("gl", [64, 4, 64]), mk("dm", [64, 4, 64]), mk("dmT", [64, 4, 64])
        Nm, Mm = mk("N", [64, 4, 64]), mk("M", [64, 4, 64])
        N2, M2 = mk("N2", [64, 4, 64]), mk("M2", [64, 4, 64])
        Pm = mk("P", [64, 4, 64])
        qkT = mk("qkT", [64, 4, 64])
        kbg, vb, ke = mk("kbg", [64, 4, 128]), mk("vb", [64, 4, 128]), mk("ke", [64, 4, 128])
        gbc, egb, qd = mk("gbc", [64, 4, 128]), mk("egb", [128, 4, 64]), mk("qd", [128, 4, 64])
        wcT, u, vn = mk("wcT", [128, 4, 64]), mk("u", [64, 4, 128]), mk("vn", [64, 4, 128])
        ot, o2 = mk("ot", [64, 4, 128]), mk("o2", [64, 4, 128])
        ssq, ob = mk("ssq", [64, 4]), mk("ob", [64, 4, 128], BF16)
        tmpS = _sb(K, es, "gd_tmpS", [128, 4, 128], F32)
        NG = (NT + 1) // 2
        ci_glob = 0
        for g in range(NG):
            nt = min(2, NT - 2 * g)
            n = nt * 128
            c0 = g * 256
            nch = min(n // 64, NC_ - c0 // 64)
            if nch <= 0:
                break
            x = xin[g % 2]
            if g == 0:
                S.op("pool", lambda: nc.gpsimd.memset(x[:, :, 0:3], 0.0), W=[x])
                S.dma("sp", x[:, :, 3:3 + n], K.d["gT"][:, :, 0:n].rearrange("h p n -> p h n"), R=[K.sbuf_g], W=[x])
                S.op("pool", lambda: nc.gpsimd.memset(x[:, :, 3:3 + 48], 0.0), W=[x])
            else:
                S.dma("sp", x[:, :, 0:3 + n], K.d["gT"][:, :, c0 - 3:c0 + n].rearrange("h p n -> p h n"), R=[K.sbuf_g], W=[x])
            for i in range(12):
                eng = "dve" if i % 2 == 0 else "pool"
                E = nc.vector if eng == "dve" else nc.gpsimd
                S.op(eng, lambda: E.tensor_scalar(out=cv[:, i, 0:n], in0=x[:, i, 0:n], scalar1=cw[:, i, 0:1], scalar2=None, op0=ALU.mult),
                     R=[x, cw], W=[cv])
                for tp in range(1, 4):
                    S.op("dve", lambda: nc.vector.scalar_tensor_tensor(out=cv[:, i, 0:n], in0=x[:, i, tp:tp + n], scalar=cw[:, i, tp:tp + 1],
                                                                      in1=cv[:, i, 0:n], op0=ALU.mult, op1=ALU.add), R=[x, cw, cv], W=[cv])
                S.op("act", lambda: nc.scalar.activation(out=cv[:, i, 0:n], in_=cv[:, i, 0:n], func=AF.Silu), R=[cv], W=[cv])
            for i in range(8):
                s_, r_ = sq[i % 2], rn[i % 2]
                S.op("pool", lambda: nc.gpsimd.tensor_tensor(out=s_[:, 0:n], in0=cv[:, i, 0:n], in1=cv[:, i, 0:n], op=ALU.mult), R=[cv], W=[s_])
                ps = _ps(K)
                S.op("pe", lambda: nc.tensor.matmul(ps[:, 0:n], lhsT=K.onesf[:, :], rhs=s_[:, 0:n], start=True, stop=True),
                     R=[K.onesf, s_], W=[ps])
                S.op("act", lambda: nc.scalar.activation(out=r_[:, 0:n], in_=ps[:, 0:n], func=AF.Sqrt, bias=K.eps_rms[:, 0:1], scale=1.0),
                     R=[ps, K.eps_rms], W=[r_])
                S.op("dve", lambda: nc.vector.reciprocal(out=r_[:, 0:n], in_=r_[:, 0:n]), R=[r_], W=[r_])
                if i < 4:
                    S.op("dve", lambda: nc.vector.scalar_tensor_tensor(out=cv[:, i, 0:n], in0=cv[:, i, 0:n], scalar=float(128 ** -0.5),
                                                                      in1=r_[:, 0:n], op0=ALU.mult, op1=ALU.mult), R=[cv, r_], W=[cv])
                else:
                    S.op("dve", lambda: nc.vector.tensor_tensor(out=cv[:, i, 0:n], in0=cv[:, i, 0:n], in1=r_[:, 0:n], op=ALU.mult),
                         R=[cv, r_], W=[cv])
            bgc, zc = bgt[g % 2], zt[g % 2]
            S.dma("sp", bgc[:, 0:nch, :], K.d["bg"][c0:c0 + nch * 64, :].rearrange("(n c) e -> c n e", c=64), R=[K.sbuf_bg], W=[bgc])
            S.dma("sp", zc[:, 0:nch, :], K.d["zs"][c0:c0 + nch * 64, :].rearrange("(n c) e -> c n e", c=64), R=[K.sbuf_z], W=[zc])
            if g == 0:
                S.op("pool", lambda: nc.gpsimd.memset(bgc[0:48, 0, 4:8], 0.0), W=[bgc])
            for ci in range(nch):
                b2 = ci_glob % 2
                ci_glob += 1
                cs = slice(ci * 64, ci * 64 + 64)
                gcol = bgc[:, ci, 4:8]
                bcol = bgc[:, ci, 0:4]
                pg = _ps(K)
                S.op("pe", lambda: nc.tensor.matmul(pg[0:64, 0:4], lhsT=K.triu[:, 0, :], rhs=gcol, start=True, stop=True), R=[K.triu, bgc], W=[pg])
                S.op("pe", lambda: nc.tensor.matmul(pg[0:64, 4:8], lhsT=K.sgt[:, :], rhs=gcol, start=True, stop=True), R=[K.sgt, bgc], W=[pg])
                S.op("pe", lambda: nc.tensor.matmul(pg[:, 8:12], lhsT=K.ones64[:, :], rhs=gcol, start=True, stop=True), R=[K.ones64, bgc], W=[pg])
                S.op("act", lambda: nc.scalar.activation(out=egc[b2][:], in_=pg[0:64, 0:4], func=AF.Exp), R=[pg], W=[egc[b2]])
                S.op("act", lambda: nc.scalar.activation(out=egr[b2][:], in_=pg[0:64, 4:8], func=AF.Exp), R=[pg], W=[egr[b2]])
                S.op("act", lambda: nc.scalar.activation(out=cdt[b2][:], in_=pg[:, 8:12], func=AF.Exp), R=[pg], W=[cdt[b2]])
                S.op("dve", lambda: nc.vector.tensor_tensor(out=gl[b2][:], in0=K.triu[:], in1=gcol.unsqueeze(2).to_broadcast([64, 4, 64]), op=ALU.mult),
                     R=[K.triu, bgc], W=[gl[b2]])
                pd = _ps(K)
                pdv = pd.h[0:64, 0:512].rearrange("p (a h c) -> p a h c", a=2, h=4)
                for h in range(4):
                    S.op("pe", lambda: nc.tensor.matmul(pdv[:, 0, h, :], lhsT=gl[b2][:, h, :], rhs=K.sgt[:, :], start=True, stop=True),
                         R=[gl[b2], K.sgt], W=[pd])
                S.op("pe", lambda: nc.tensor.matmul(pd[0:64, 256:512], lhsT=K.sgt[:, :], rhs=gl[b2][:].rearrange("p h c -> p (h c)"), start=True, stop=True),
                     R=[gl[b2], K.sgt], W=[pd])
                S.op("act", lambda: nc.scalar.activation(out=dm[b2][:], in_=pdv[:, 0], func=AF.Exp), R=[pd], W=[dm[b2]])
                S.op("act", lambda: nc.scalar.activation(out=dmT[b2][:], in_=pdv[:, 1], func=AF.Exp), R=[pd], W=[dmT[b2]])
                S.op("pool", lambda: nc.gpsimd.tensor_tensor(out=dm[b2][:], in0=dm[b2][:], in1=K.trilsn[:], op=ALU.mult), R=[dm[b2], K.trilsn], W=[dm[b2]])
                S.op("pool", lambda: nc.gpsimd.tensor_tensor(out=dmT[b2][:], in0=dmT[b2][:], in1=K.triu[:], op=ALU.mult), R=[dmT[b2], K.triu], W=[dmT[b2]])
                pgq = _ps(K)
                pgqv = pgq.h[0:64, 0:512].rearrange("p (a h c) -> p a h c", a=2, h=4)
                for h in range(4):
                    S.op("pe", lambda: nc.tensor.matmul(pgqv[:, 0, h, :], lhsT=cv[:, 4 + h, cs], rhs=cv[:, 4 + h, cs], start=True, stop=True), R=[cv], W=[pgq])
                    S.op("pe", lambda: nc.tensor.matmul(pgqv[:, 1, h, :], lhsT=cv[:, 4 + h, cs], rhs=cv[:, h, cs], start=True, stop=True), R=[cv], W=[pgq])
                S.op("dve", lambda: nc.vector.tensor_tensor(out=Nm[b2][:], in0=pgqv[:, 0], in1=bcol.unsqueeze(2).to_broadcast([64, 4, 64]), op=ALU.mult),
                     R=[pgq, bgc], W=[Nm[b2]])
                S.op("dve", lambda: nc.vector.tensor_tensor(out=Nm[b2][:], in0=Nm[b2][:], in1=dm[b2][:], op=ALU.mult), R=[Nm[b2], dm[b2]], W=[Nm[b2]])
                S.op("dve", lambda: nc.vector.tensor_tensor(out=qkT[b2][:], in0=pgqv[:, 1], in1=dmT[b2][:], op=ALU.mult), R=[pgq, dmT[b2]], W=[qkT[b2]])
                pt = _ps(K)
                ptv = pt.h[0:64, 0:256].rearrange("p (h c) -> p h c", h=4)
                for h in range(4):
                    S.op("pe", lambda: nc.tensor.transpose(out=ptv[:, h, :], in_=Nm[b2][:, h, :], identity=K.identf[0:64, 0:64]), R=[Nm[b2], K.identf], W=[pt])
                S.op("act", lambda: nc.scalar.copy(out=Mm[b2][:], in_=ptv), R=[pt], W=[Mm[b2]])
                S.op("dve", lambda: nc.vector.tensor_tensor(out=Pm[b2][:], in0=Mm[b2][:], in1=K.ident4[:], op=ALU.add), R=[Mm[b2], K.ident4], W=[Pm[b2]])
                Nc, Mc, Nn, Mn = Nm[b2], Mm[b2], N2[b2], M2[b2]
                for r in range(5):
                    pn = _ps(K)
                    pnv = pn.h[0:64, 0:512].rearrange("p (a h c) -> p a h c", a=2, h=4)
                    for h in range(4):
                        S.op("pe", lambda: nc.tensor.matmul(pnv[:, 0, h, :], lhsT=Mc[:, h, :], rhs=Nc[:, h, :], start=True, stop=True), R=[Mc, Nc], W=[pn])
                        if r < 4:
                            S.op("pe", lambda: nc.tensor.matmul(pnv[:, 1, h, :], lhsT=Nc[:, h, :], rhs=Mc[:, h, :], start=True, stop=True), R=[Mc, Nc], W=[pn])
                    S.op("act", lambda: nc.scalar.copy(out=Nn[:], in_=pnv[:, 0]), R=[pn], W=[Nn])
                    if r < 4:
                        S.op("dve", lambda: nc.vector.tensor_copy(out=Mn[:], in_=pnv[:, 1]), R=[pn], W=[Mn])
                    pp = _ps(K)
                    ppv = pp.h[0:64, 0:256].rearrange("p (h c) -> p h c", h=4)
                    for h in range(4):
                        S.op("pe", lambda: nc.tensor.matmul(ppv[:, h, :], lhsT=Nn[:, h, :], rhs=Pm[b2][:, h, :], start=True, stop=True), R=[Nn, Pm[b2]], W=[pp])
                    S.op("dve", lambda: nc.vector.tensor_tensor(out=Pm[b2][:], in0=Pm[b2][:], in1=ppv, op=ALU.add), R=[Pm[b2], pp], W=[Pm[b2]])
                    Nc, Mc, Nn, Mn = Nn, Mn, Nc, Mc
                pk = _ps(K)
                pkv = pk.h[0:64, 0:512].rearrange("p (h c) -> p h c", h=4)
                pv = _ps(K)
                pvv = pv.h[0:64, 0:512].rearrange("p (h c) -> p h c", h=4)
                for h in range(4):
                    S.op("pe", lambda: nc.tensor.transpose(out=pkv[:, h, :], in_=cv[:, 4 + h, cs], identity=K.identf[:]), R=[cv, K.identf], W=[pk])
                    S.op("pe", lambda: nc.tensor.transpose(out=pvv[:, h, :], in_=cv[:, 8 + h, cs], identity=K.identf[:]), R=[cv, K.identf], W=[pv])
                S.op("dve", lambda: nc.vector.tensor_tensor(out=ke[b2][:], in0=pkv, in1=egr[b2][:].unsqueeze(2).to_broadcast([64, 4, 128]), op=ALU.mult),
                     R=[pk, egr[b2]], W=[ke[b2]])
                S.op("dve", lambda: nc.vector.tensor_tensor(out=kbg[b2][:], in0=pkv, in1=bcol.unsqueeze(2).to_broadcast([64, 4, 128]), op=ALU.mult),
                     R=[pk, bgc], W=[kbg[b2]])
                S.op("pool", lambda: nc.gpsimd.tensor_tensor(out=kbg[b2][:], in0=kbg[b2][:], in1=egc[b2][:].unsqueeze(2).to_broadcast([64, 4, 128]), op=ALU.mult),
                     R=[kbg[b2], egc[b2]], W=[kbg[b2]])
                S.op("dve", lambda: nc.vector.tensor_tensor(out=vb[b2][:], in0=pvv, in1=bcol.unsqueeze(2).to_broadcast([64, 4, 128]), op=ALU.mult),
                     R=[pv, bgc], W=[vb[b2]])
                S.op("pool", lambda: nc.gpsimd.tensor_tensor(out=gbc[b2][:], in0=K.ones4[:], in1=gcol.unsqueeze(2).to_broadcast([64, 4, 128]), op=ALU.mult),
                     R=[K.ones4, bgc], W=[gbc[b2]])
                pe_ = _ps(K)
                pev = pe_.h[:, 0:256].rearrange("p (h c) -> p h c", h=4)
                for h in range(4):
                    S.op("pe", lambda: nc.tensor.matmul(pev[:, h, :], lhsT=gbc[b2][:, h, :], rhs=K.triu[:, 0, :], start=True, stop=True), R=[gbc[b2], K.triu], W=[pe_])
                S.op("act", lambda: nc.scalar.activation(out=egb[b2][:], in_=pev, func=AF.Exp), R=[pe_], W=[egb[b2]])
                S.op("dve", lambda: nc.vector.tensor_tensor(out=qd[b2][:], in0=cv[:, 0:4, cs], in1=egb[b2][:], op=ALU.mult), R=[cv, egb[b2]], W=[qd[b2]])
                pw = _ps(K)
                pwv = pw.h[:, 0:256].rearrange("p (h c) -> p h c", h=4)
                pu = _ps(K)
                puv = pu.h[0:64, 0:512].rearrange("p (h c) -> p h c", h=4)
                for h in range(4):
                    S.op("pe", lambda: nc.tensor.matmul(pwv[:, h, :], lhsT=kbg[b2][:, h, :], rhs=Pm[b2][:, h, :], start=True, stop=True), R=[kbg[b2], Pm[b2]], W=[pw])
                    S.op("pe", lambda: nc.tensor.matmul(puv[:, h, :], lhsT=Pm[b2][:, h, :], rhs=vb[b2][:, h, :], start=True, stop=True), R=[vb[b2], Pm[b2]], W=[pu])
                S.op("act", lambda: nc.scalar.copy(out=wcT[b2][:], in_=pwv), R=[pw], W=[wcT[b2]])
                S.op("act", lambda: nc.scalar.copy(out=u[b2][:], in_=puv), R=[pu], W=[u[b2]])
                pws = _ps(K)
                pwsv = pws.h[0:64, 0:512].rearrange("p (h c) -> p h c", h=4)
                for h in range(4):
                    S.op("pe", lambda: nc.tensor.matmul(pwsv[:, h, :], lhsT=wcT[b2][:, h, :], rhs=Sst[:, h, :], start=True, stop=True), R=[wcT[b2], Sst], W=[pws])
                S.op("dve", lambda: nc.vector.tensor_tensor(out=vn[b2][:], in0=u[b2][:], in1=pwsv, op=ALU.subtract), R=[u[b2], pws], W=[vn[b2]])
                po = _ps(K)
                pov = po.h[0:64, 0:512].rearrange("p (h c) -> p h c", h=4)
                for h in range(4):
                    S.op("pe", lambda: nc.tensor.matmul(pov[:, h, :], lhsT=qd[b2][:, h, :], rhs=Sst[:, h, :], start=True, stop=False), R=[qd[b2], Sst], W=[po])
                    S.op("pe", lambda: nc.tensor.matmul(pov[:, h, :], lhsT=qkT[b2][:, h, :], rhs=vn[b2][:, h, :], start=False, stop=True), R=[qkT[b2], vn[b2]], W=[po])
                pS = _ps(K)
                pSv = pS.h[:, 0:512].rearrange("p (h c) -> p h c", h=4)
                for h in range(4):
                    S.op("pe", lambda: nc.tensor.matmul(pSv[:, h, :], lhsT=ke[b2][:, h, :], rhs=vn[b2][:, h, :], start=True, stop=True), R=[ke[b2], vn[b2]], W=[pS])
                S.op("pool", lambda: nc.gpsimd.tensor_tensor(out=tmpS[:], in0=Sst[:], in1=cdt[b2][:].unsqueeze(2).to_broadcast([128, 4, 128]), op=ALU.mult),
                     R=[Sst, cdt[b2]], W=[tmpS])
                S.op("dve", lambda: nc.vector.tensor_tensor(out=Sst[:], in0=tmpS[:], in1=pSv, op=ALU.add), R=[tmpS, pS], W=[Sst])
                S.op("act", lambda: nc.scalar.copy(out=ot[b2][:], in_=pov), R=[po], W=[ot[b2]])
                S.op("pool", lambda: nc.gpsimd.tensor_tensor(out=o2[b2][:], in0=ot[b2][:], in1=ot[b2][:], op=ALU.mult), R=[ot[b2]], W=[o2[b2]])
                S.op("dve", lambda: nc.vector.tensor_reduce(out=ssq[b2][:], in_=o2[b2][:], axis=AX.X, op=ALU.add), R=[o2[b2]], W=[ssq[b2]])
                S.op("act", lambda: nc.scalar.activation(out=ssq[b2][:], in_=ssq[b2][:], func=AF.Sqrt, bias=K.eps_rms[0:64, 0:1], scale=1.0 / 128),
                     R=[ssq[b2], K.eps_rms], W=[ssq[b2]])
                S.op("dve", lambda: nc.vector.reciprocal(out=ssq[b2][:], in_=ssq[b2][:]), R=[ssq[b2]], W=[ssq[b2]])
                S.op("dve", lambda: nc.vector.tensor_tensor(out=o2[b2][:], in0=ot[b2][:], in1=ssq[b2][:].unsqueeze(2).to_broadcast([64, 4, 128]), op=ALU.mult),
                     R=[ot[b2], ssq[b2]], W=[o2[b2]])
                S.op("pool", lambda: nc.gpsimd.tensor_tensor(out=o2[b2][:], in0=o2[b2][:], in1=gng[:], op=ALU.mult), R=[o2[b2], gng], W=[o2[b2]])
                S.op("dve", lambda: nc.vector.tensor_tensor(out=ob[b2][:], in0=o2[b2][:], in1=zc[:, ci, :].rearrange("p (h c) -> p h c", h=4), op=ALU.mult),
                     R=[o2[b2], zc], W=[ob[b2]])
                p0 = c0 + ci * 64
                S.dma("sp", K.d["mixed"][p0:p0 + 64, 512:1024], ob[b2][:].rearrange("p h c -> p (h c)"), R=[ob[b2]], W=[K.sbuf_mx_gd])


def phase_SBGDN(K, l, NSETS=2, YK=None, run_sb=True, run_gdn=True):
    YK = YK or GDN_YK
    with contextlib.ExitStack() as es:
        K.psum_gd = K.psum[5:8]
        K.psgi = 0
        rr = RR()
        if run_sb:
            rr.add(sb_stream(K, l, es), 1)
        if run_gdn:
            rr.add(gdn_master(K, l, es, rr, NSETS, YK), YK)
        rr.run()


def phase_A3(K, l):
    nc, S, NT = K.nc, K.S, K.NT
    with contextlib.ExitStack() as es:
        K.stg = [_sb(K, es, "stg%d" % i, [128, IN_W], F32) for i in range(2)]
        Wo = _sb(K, es, "a3_W", [128, KC, D], BF16)
        wbufs = [Buf() for _ in range(KC)]
        wsrc = K.d["w_out"][l].rearrange("(k p) n -> p k n", p=128)
        for kc in range(KC):
            _load_cast(K, Wo, Wo[:, kc, :], wsrc[:, kc, :], [128, D], wbufs[kc])
        Wr = _sb(K, es, "a3_Wr", [128, KC, 36], F32)
        S.dma("sp", Wr[:, :, 0:4], K.d["w_group"][l].rearrange("(k p) n -> p k n", p=128), W=[Wr])
        S.dma("sp", Wr[:, :, 4:36], K.d["w_expert"][l].rearrange("(k p) n -> p k n", p=128), W=[Wr])
        br = _sb(K, es, "a3_br", [128, 36], F32)
        S.dma("sp", br[:, 0:4], K.d["b_group"][l].partition_broadcast(128), W=[br])
        S.dma("sp", br[:, 4:36], K.d["b_expert"][l].partition_broadcast(128), W=[br])
        g = _sb(K, es, "a3_g", [128, D], F32)
        b = _sb(K, es, "a3_b", [128, D], F32)
        S.dma("sp", g[:], K.d["ln1_g"][l].partition_broadcast(128), W=[g])
        S.dma("sp", b[:], K.d["ln1_b"][l].partition_broadcast(128), W=[b])
        mxs = [_sb(K, es, "a3_mx%d" % i, [128, D], BF16) for i in range(2)]
        mTs = [_sb(K, es, "a3_mT%d" % i, [128, KC, 128], BF16) for i in range(2)]
        hts = [_sb(K, es, "a3_h%d" % i, [128, D], F32) for i in range(2)]
        rs = [_sb(K, es, "a3_r%d" % i, [128, D], F32) for i in range(2)]
        h1s = [_sb(K, es, "a3_h1%d" % i, [128, D], F32) for i in range(2)]
        hTf = [_sb(K, es, "a3_hTf%d" % i, [128, KC, 128], F32) for i in range(2)]
        hTb = [_sb(K, es, "a3_hTb%d" % i, [128, KC, 128], BF16) for i in range(2)]
        sm = {"st": _sb(K, es, "a3_st", [128, 2, 6], F32), "mv": _sb(K, es, "a3_mv", [128, 2], F32),
              "rstd": _sb(K, es, "a3_rs", [128, 1], F32), "tmp": _sb(K, es, "a3_tmp", [128, D], F32)}
        lg = _sb(K, es, "a3_lg", [128, 36], F32)
        sc = {k: _sb(K, es, "a3_" + k, shp, F32) for k, shp in
              (("gm", [128, 1]), ("ge", [128, 4]), ("gs", [128, 1]), ("oh", [128, 4]), ("ig", [128, 8]), ("tmp8", [128, 4, 8]),
               ("m8", [128, 8]), ("sel", [128, 8]), ("ex", [128, 8]), ("dn", [128, 1]), ("wi", [128, 8]))}
        cbs = [_sb(K, es, "a3_cb%d" % i, [128, 4, 8], F32) for i in range(2)]
        for t in range(NT):
            mx, mT, ht, r, h1, hf, hb, cb = mxs[t % 2], mTs[t % 2], hts[t % 2], rs[t % 2], h1s[t % 2], hTf[t % 2], hTb[t % 2], cbs[t % 2]
            rows = slice(128 * t, 128 * (t + 1))
            S.dma("sp", mx[:], K.d["mixed"][rows, :], R=[K.sbuf_mx_sb, K.sbuf_mx_gd], W=[mx])
            S.dma("sp", ht[:], K.d["h"][rows, :], R=[K.hbuf[t]], W=[ht])
            for half in range(2):
                ps = _ps(K)
                psb = ps.h.bitcast(BF16)
                for j in range(4):
                    kc = half * 4 + j
                    S.op("pe", lambda: nc.tensor.transpose(out=psb[:, j * 128:(j + 1) * 128], in_=mx[:, kc * 128:(kc + 1) * 128], identity=K.identb[:]),
                         R=[mx, K.identb], W=[ps])
                _evac(K, S.alt(), mT[:, half * 4:half * 4 + 4, :], psb[:, 0:512].rearrange("p (a c) -> p a c", a=4), [ps], [mT])
            for half in range(2):
                ps = _ps(K)
                for kc in range(KC):
                    S.op("pe", lambda: nc.tensor.matmul(ps[:, :], lhsT=mT[:, kc, :], rhs=Wo[:, kc, half * 512:(half + 1) * 512],
                                                        start=(kc == 0), stop=(kc == KC - 1)), R=[mT, wbufs[kc]], W=[ps])
                S.op("dve", lambda: nc.vector.scalar_tensor_tensor(out=r[:, half * 512:(half + 1) * 512], in0=ht[:, half * 512:(half + 1) * 512],
                                                                  scalar=ALPHA, in1=ps[:, :], op0=ALU.mult, op1=ALU.add), R=[ht, ps], W=[r])
            _ln_tile(K, r, h1, g, b, sm)
            S.dma("sp", K.d["h1"][rows, :], h1[:], R=[h1], W=[K.h1buf[t]])
            for half in range(2):
                ps = _ps(K)
                for j in range(4):
                    kc = half * 4 + j
                    S.op("pe", lambda: nc.tensor.transpose(out=ps[:, j * 128:(j + 1) * 128], in_=h1[:, kc * 128:(kc + 1) * 128], identity=K.identf[:]),
                         R=[h1, K.identf], W=[ps])
                S.op("act", lambda: nc.scalar.copy(out=hf[:, half * 4:half * 4 + 4, :], in_=ps[:, :].rearrange("p (a c) -> p a c", a=4)), R=[ps], W=[hf])
                S.op("dve", lambda: nc.vector.tensor_copy(out=hb[:, half * 4:half * 4 + 4, :], in_=ps[:, :].rearrange("p (a c) -> p a c", a=4)), R=[ps], W=[hb])
            S.dma("sp", K.d["h1T"][:, :, rows].rearrange("k p n -> p k n"), hb[:], R=[hb], W=[K.sbuf_h1T])
            ps = _ps(K)
            for kc in range(KC):
                S.op("pe", lambda: nc.tensor.matmul(ps[:, 0:36], lhsT=hf[:, kc, :], rhs=Wr[:, kc, :], start=(kc == 0), stop=(kc == KC - 1)),
                     R=[hf, Wr], W=[ps])
            S.op("dve", lambda: nc.vector.tensor_tensor(out=lg[:], in0=ps[:, 0:36], in1=br[:], op=ALU.add), R=[ps, br], W=[lg])
            gm, ge, gs, oh, ig, tmp8, m8, sel, ex, dn, wi = (sc[k] for k in ("gm", "ge", "gs", "oh", "ig", "tmp8", "m8", "sel", "ex", "dn", "wi"))
            S.op("dve", lambda: nc.vector.tensor_reduce(out=gm[:], in_=lg[:, 0:4], axis=AX.X, op=ALU.max), R=[lg], W=[gm])
            S.op("dve", lambda: nc.vector.tensor_scalar(out=oh[:], in0=lg[:, 0:4], scalar1=gm[:, 0:1], scalar2=None, op0=ALU.is_equal), R=[lg, gm], W=[oh])
            S.op("dve", lambda: nc.vector.tensor_scalar(out=ge[:], in0=lg[:, 0:4], scalar1=gm[:, 0:1], scalar2=None, op0=ALU.subtract), R=[lg, gm], W=[ge])
            S.op("act", lambda: nc.scalar.activation(out=ge[:], in_=ge[:], func=AF.Exp), R=[ge], W=[ge])
            S.op("dve", lambda: nc.vector.tensor_reduce(out=gs[:], in_=ge[:], axis=AX.X, op=ALU.add), R=[ge], W=[gs])
            S.op("dve", lambda: nc.vector.tensor_tensor(out=tmp8[:], in0=lg[:, 4:36].rearrange("p (g e) -> p g e", g=4),
                                                        in1=oh[:].unsqueeze(2).to_broadcast([128, 4, 8]), op=ALU.mult), R=[lg, oh], W=[tmp8])
            S.op("dve", lambda: nc.vector.tensor_reduce(out=ig[:], in_=tmp8[:].rearrange("p g e -> p e g"), axis=AX.X, op=ALU.add), R=[tmp8], W=[ig])
            S.op("dve", lambda: nc.vector.max(out=m8[:], in_=ig[:]), R=[ig], W=[m8])
            S.op("dve", lambda: nc.vector.tensor_scalar(out=sel[:], in0=ig[:], scalar1=m8[:, 1:2], scalar2=None, op0=ALU.is_ge), R=[ig, m8], W=[sel])
            S.op("dve", lambda: nc.vector.tensor_scalar(out=ex[:], in0=ig[:], scalar1=m8[:, 0:1], scalar2=None, op0=ALU.subtract), R=[ig, m8], W=[ex])
            S.op("act", lambda: nc.scalar.activation(out=ex[:], in_=ex[:], func=AF.Exp), R=[ex], W=[ex])
            S.op("dve", lambda: nc.vector.tensor_tensor(out=ex[:], in0=ex[:], in1=sel[:], op=ALU.mult), R=[ex, sel], W=[ex])
            S.op("dve", lambda: nc.vector.tensor_reduce(out=dn[:], in_=ex[:], axis=AX.X, op=ALU.add), R=[ex], W=[dn])
            S.op("dve", lambda: nc.vector.tensor_tensor(out=dn[:], in0=dn[:], in1=gs[:], op=ALU.mult), R=[dn, gs], W=[dn])
            S.op("dve", lambda: nc.vector.reciprocal(out=dn[:], in_=dn[:]), R=[dn], W=[dn])
            S.op("dve", lambda: nc.vector.tensor_scalar(out=wi[:], in0=ex[:], scalar1=dn[:, 0:1], scalar2=None, op0=ALU.mult), R=[ex, dn], W=[wi])
            S.op("dve", lambda: nc.vector.tensor_tensor(out=cb[:], in0=oh[:].unsqueeze(2).to_broadcast([128, 4, 8]),
                                                        in1=wi[:].unsqueeze(1).to_broadcast([128, 4, 8]), op=ALU.mult), R=[oh, wi], W=[cb])
            S.dma("sp", K.d["comb"][rows, :], cb[:].rearrange("p g e -> p (g e)"), R=[cb], W=[K.sbuf_comb])


def phase_B(K, l, last):
    nc, S, NT = K.nc, K.S, K.NT
    NP = 4
    TH = (NT + NP - 1) // NP
    with contextlib.ExitStack() as es:
        K.stg = [_sb(K, es, "stg%d" % i, [128, IN_W], F32) for i in range(2)]
        g = _sb(K, es, "b_g", [128, D], F32)
        b = _sb(K, es, "b_b", [128, D], F32)
        S.dma("sp", g[:], K.d["ln2_g"][l].partition_broadcast(128), W=[g])
        S.dma("sp", b[:], K.d["ln2_b"][l].partition_broadcast(128), W=[b])
        xT = _sb(K, es, "b_xT", [128, KC, TH * 128], BF16)
        cbt = _sb(K, es, "b_cb", [128, TH, 32], F32)
        yacc = _sb(K, es, "b_y", [128, TH, D], F32)
        w1b = [_sb(K, es, "b_w1%d" % i, [128, KC, 256], BF16) for i in range(2)]
        w3b = [_sb(K, es, "b_w3%d" % i, [128, KC, 256], BF16) for i in range(2)]
        w2b = [_sb(K, es, "b_w2%d" % i, [128, 2, D], BF16) for i in range(2)]
        sil = [_sb(K, es, "b_sil%d" % i, [128, 512], F32) for i in range(2)]
        hid = [_sb(K, es, "b_hid%d" % i, [128, 2, 512], BF16) for i in range(2)]
        h1t = [_sb(K, es, "b_h1%d" % i, [128, D], F32) for i in range(1)]
        rr = [_sb(K, es, "b_r%d" % i, [128, D], F32) for i in range(1)]
        oo = [_sb(K, es, "b_o%d" % i, [128, D], F32) for i in range(2)]
        sm = {"st": _sb(K, es, "b_st", [128, 2, 6], F32), "mv": _sb(K, es, "b_mv", [128, 2], F32),
              "rstd": _sb(K, es, "b_rs", [128, 1], F32), "tmp": _sb(K, es, "b_tmp", [128, D], F32)}
        it = 0
        for half in range(NP):
            t0 = half * TH
            nth = min(TH, NT - t0)
            if nth <= 0:
                break
            n_all = nth * 128
            S.dma("sp", xT[:, :, 0:n_all], K.d["h1T"][:, :, t0 * 128:t0 * 128 + n_all].rearrange("k p n -> p k n"), R=[K.sbuf_h1T], W=[xT])
            S.dma("sp", cbt[:, 0:nth, :], K.d["comb"][t0 * 128:t0 * 128 + n_all, :].rearrange("(t p) e -> p t e", p=128), R=[K.sbuf_comb], W=[cbt])
            S.op("pool", lambda: nc.gpsimd.memset(yacc[:, 0:nth, :], 0.0), W=[yacc])
            for e in range(32):
                wa, wc, wd = w1b[e % 2], w3b[e % 2], w2b[e % 2]
                _load_cast(K, wa, wa[:], K.d["w1"][l, e].rearrange("(k p) f -> p k f", p=128), [128, KC, 256], wa.b)
                _load_cast(K, wc, wc[:], K.d["w3"][l, e].rearrange("(k p) f -> p k f", p=128), [128, KC, 256], wc.b)
                _load_cast(K, wd, wd[:], K.d["w2"][l, e].rearrange("(k p) f -> p k f", p=128), [128, 2, D], wd.b)
                for gq in range((nth + 3) // 4):
                    nt = min(4, nth - 4 * gq)
                    n = nt * 128
                    c0 = gq * 512
                    hd = hid[it % 2]
                    it += 1
                    for f2 in range(2):
                        p1 = _ps(K)
                        for kc in range(KC):
                            S.op("pe", lambda: nc.tensor.matmul(p1[:, 0:n], lhsT=wa[:, kc, f2 * 128:(f2 + 1) * 128], rhs=xT[:, kc, c0:c0 + n],
                                                                start=(kc == 0), stop=(kc == KC - 1)), R=[wa, xT], W=[p1])
                        p3 = _ps(K)
                        for kc in range(KC):
                            S.op("pe", lambda: nc.tensor.matmul(p3[:, 0:n], lhsT=wc[:, kc, f2 * 128:(f2 + 1) * 128], rhs=xT[:, kc, c0:c0 + n],
                                                                start=(kc == 0), stop=(kc == KC - 1)), R=[wc, xT], W=[p3])
                        sl = sil[f2]
                        S.op("act", lambda: nc.scalar.activation(out=sl[:, 0:n], in_=p1[:, 0:n], func=AF.Silu), R=[p1], W=[sl])
                        S.op("dve", lambda: nc.vector.tensor_tensor(out=hd[:, f2, 0:n], in0=sl[:, 0:n], in1=p3[:, 0:n], op=ALU.mult), R=[sl, p3], W=[hd])
                    for t in range(nt):
                        tt = 4 * gq + t
                        for ch in range(2):
                            py = _ps(K)
                            for f2 in range(2):
                                S.op("pe", lambda: nc.tensor.matmul(py[:, :], lhsT=hd[:, f2, t * 128:(t + 1) * 128], rhs=wd[:, f2, ch * 512:(ch + 1) * 512],
                                                                    start=(f2 == 0), stop=(f2 == 1)), R=[hd, wd], W=[py])
                            S.op("dve", lambda: nc.vector.scalar_tensor_tensor(out=yacc[:, tt, ch * 512:(ch + 1) * 512], in0=py[:, :],
                                                                              scalar=cbt[:, tt, e:e + 1], in1=yacc[:, tt, ch * 512:(ch + 1) * 512],
                                                                              op0=ALU.mult, op1=ALU.add), R=[py, cbt, yacc], W=[yacc])
            for t in range(nth):
                tg = t0 + t
                rows = slice(128 * tg, 128 * (tg + 1))
                h1, r, o = h1t[0], rr[0], oo[t % 2]
                S.dma("sp", h1[:], K.d["h1"][rows, :], R=[K.h1buf[tg]], W=[h1])
                S.op("dve", lambda: nc.vector.scalar_tensor_tensor(out=r[:], in0=h1[:], scalar=ALPHA, in1=yacc[:, t, :], op0=ALU.mult, op1=ALU.add),
                     R=[h1, yacc], W=[r])
                _ln_tile(K, r, o, g, b, sm)
                if not last:
                    S.dma("sp", K.d["h"][rows, :], o[:], R=[o], W=[K.hbuf[tg]])
                else:
                    lo = 128 * tg - 64
                    r0, r1 = max(lo, 0), min(lo + 128, K.SEQ)
                    if r1 > r0:
                        S.dma("sp", K.d["out"][r0:r1, :], o[r0 - lo:r1 - lo, :], R=[o], W=[K.outbuf])


def make_consts():
    c = {}
    c["identf"] = np.eye(128, dtype=np.float32)
    c["identb"] = np.eye(128, dtype=np.float32).astype(ml_dtypes.bfloat16)
    s = np.arange(128)[:, None]
    masks = np.zeros((6, 128, 512), np.float32)
    for rel in range(4):
        for qt in range(4):
            blk = masks[rel, :, qt * 128:(qt + 1) * 128]
            if qt < rel:
                blk[:] = NEG
            elif qt == rel:
                blk[:] = np.where(s < np.arange(128)[None, :], 0.0, NEG)
    masks[4] = masks[0]
    masks[4, 0:48, :] = NEG
    masks[5, 0:48, :] = NEG
    c["masks"] = np.ascontiguousarray(masks.transpose(1, 0, 2)).astype(ml_dtypes.bfloat16)
    c["negtri"] = np.where(s >= np.arange(128)[None, :], -1.0, 0.0).astype(ml_dtypes.bfloat16)
    c["onesb"] = np.ones((128, 1), np.float32).astype(ml_dtypes.bfloat16)
    c["onesf"] = np.ones((128, 128), np.float32)
    m = np.arange(64)[:, None]
    i = np.arange(64)[None, :]
    triu = (m <= i).astype(np.float32)
    c["triu"] = np.ascontiguousarray(np.repeat(triu[:, None, :], 4, axis=1))
    c["sgt"] = (m > i).astype(np.float32)
    c["ones64"] = np.ones((64, 128), np.float32)
    c["ones4"] = np.ones((64, 4, 128), np.float32)
    c["trilsn"] = np.ascontiguousarray(np.repeat((-(m > i).astype(np.float32))[:, None, :], 4, axis=1))
    c["ident4"] = np.ascontiguousarray(np.repeat(np.eye(64, dtype=np.float32)[:, None, :], 4, axis=1))
    c["cvec"] = np.tile(np.array([[1.0, LN_EPS, RMS_EPS, 0.0, 64.0 * RMS_EPS, 128.0 * RMS_EPS]], np.float32), (128, 1))
    return c


CONST_DT = {"identb": BF16, "masks": BF16, "negtri": BF16, "onesb": BF16}

IN_SHAPES = lambda SEQ, depth: {
    "x": [SEQ, D], "meta": [16, D], "ln_in_g": [D], "ln_in_b": [D], "w_in": [depth, D, IN_W], "conv_wT": [depth, 1536, 4],
    "a_log": [depth, 4], "dt_bias": [depth, 4], "sb_norm_g": [depth, 64], "gdn_norm_g": [depth, 128], "w_out": [depth, D, D],
    "ln1_g": [depth, D], "ln1_b": [depth, D], "w_group": [depth, D, 4], "b_group": [depth, 4], "w_expert": [depth, D, 32],
    "b_expert": [depth, 32], "w1": [depth, 32, D, 256], "w3": [depth, 32, D, 256], "w2": [depth, 32, 256, D],
    "ln2_g": [depth, D], "ln2_b": [depth, D]}


def build(SEQ, depth, debug=False, same_eng=True, phases=None, max_ops=None, log=None):
    nc = bass.Bass("TRN2", target_bir_lowering=False)
    K = Ctx()
    K.nc = nc
    K.SEQ = SEQ
    PT = SEQ + 64
    K.NT = NT = (PT + 127) // 128
    K.NC = PT // 64
    P = NT * 128
    K.d = {}
    for name, shp in IN_SHAPES(SEQ, depth).items():
        K.d[name] = nc.dram_tensor(name, shp, F32, kind="ExternalInput").ap()
    consts = make_consts()
    for name, arr in consts.items():
        K.d["c_" + name] = nc.dram_tensor("c_" + name, list(arr.shape), CONST_DT.get(name, F32), kind="ExternalInput").ap()
    K.d["out"] = nc.dram_tensor("out", [SEQ, D], F32, kind="ExternalOutput").ap()
    kind = "ExternalOutput" if debug else "Internal"
    for name, shp, dt in (("h", [P, D], F32), ("qT", [4, 128, P], BF16), ("kT", [4, 128, P], BF16), ("V", [P, 512], BF16),
                          ("gT", [12, 128, P], F32), ("zs", [P, 512], F32), ("bg", [P, 8], F32), ("mixed", [P, D], BF16),
                          ("h1", [P, D], F32), ("h1T", [KC, 128, P], BF16), ("comb", [P, 32], F32)):
        K.d[name] = nc.dram_tensor("s_" + name, shp, dt, kind=kind).ap()
    K.hbuf = [Buf() for _ in range(NT)]
    K.h1buf = [Buf() for _ in range(NT)]
    for nm in ("sbuf_q", "sbuf_k", "sbuf_v", "sbuf_g", "sbuf_z", "sbuf_bg", "sbuf_mx_sb", "sbuf_mx_gd", "sbuf_h1T", "sbuf_comb", "outbuf"):
        setattr(K, nm, Buf())
    with contextlib.ExitStack() as es:
        K.S = S = Sched(nc, es, same_eng=same_eng)
        S.max_ops = max_ops
        S.log = log
        K.psum = [Tl(es.enter_context(nc.psum_tensor("ps%d" % i, [128, 512], F32)), "ps%d" % i) for i in range(8)]
        K.psi = 0
        for p_ in K.psum:
            p_.b.excl = True
        K.stgi = 0
        for name, arr in consts.items():
            if name == "cvec":
                continue
            tl = _sb(K, es, "k_" + name, list(arr.shape), CONST_DT.get(name, F32))
            setattr(K, name, tl)
            S.dma("sp", tl[:], K.d["c_" + name], W=[tl])
        cv = _sb(K, es, "k_cvec", [128, 6], F32)
        S.dma("sp", cv[:], K.d["c_cvec"], W=[cv])
        K.one_c = Tl(cv.h[:, 0:1]); K.one_c.b = cv.b
        K.eps_ln = Tl(cv.h[:, 1:2]); K.eps_ln.b = cv.b
        K.eps_rms = Tl(cv.h[:, 2:3]); K.eps_rms.b = cv.b
        K.eps_rms64 = Tl(cv.h[:, 4:5]); K.eps_rms64.b = cv.b
        K.eps_rms128 = Tl(cv.h[:, 5:6]); K.eps_rms128.b = cv.b
        ph = phases or ("in", "A1", "SB", "GDN", "A3", "B")
        if "in" in ph:
            phase_input(K)
            S.barrier()
        for l in range(depth):
            if "A1" in ph:
                phase_A1(K, l)
                S.barrier()
            if INTERLEAVE_GDN:
                if "SB" in ph or "GDN" in ph:
                    phase_SBGDN(K, l, run_sb=("SB" in ph), run_gdn=("GDN" in ph))
                    S.barrier()
            else:
                if "SB" in ph:
                    phase_SBGDN(K, l, run_sb=True, run_gdn=False)
                    S.barrier()
                if "GDN" in ph:
                    phase_GDN_old(K, l)
                    S.barrier()
            if "A3" in ph:
                phase_A3(K, l)
                S.barrier()
            if "B" in ph:
                phase_B(K, l, l == depth - 1)
                S.barrier()
        S.finish()
    K.consts = consts
    return nc, K


def make_in_maps(inputs, SEQ, depth, consts, n_cores=8):
    shared = {}
    for k in ("ln_in_g", "ln_in_b", "w_in", "a_log", "dt_bias", "sb_norm_g", "gdn_norm_g", "w_out", "ln1_g", "ln1_b",
              "w_group", "b_group", "w_expert", "b_expert", "w1", "w3", "w2", "ln2_g", "ln2_b"):
        shared[k] = np.ascontiguousarray(np.asarray(inputs[k], dtype=np.float32))
    shared["meta"] = np.ascontiguousarray(np.asarray(inputs["meta_tokens"], dtype=np.float32))
    shared["conv_wT"] = np.ascontiguousarray(np.asarray(inputs["conv_w"], dtype=np.float32).transpose(0, 2, 1))
    for k, v in consts.items():
        shared["c_" + k] = v
    x = np.asarray(inputs["x"], dtype=np.float32)
    B = x.shape[0]
    maps = []
    for c in range(n_cores):
        m = dict(shared)
        m["x"] = np.ascontiguousarray(x[c % B])
        maps.append(m)
    return maps


def kernel(**inputs):
    x = np.asarray(inputs["x"])
    B, SEQ, _ = x.shape
    depth = np.asarray(inputs["w_in"]).shape[0]
    nc, K = build(SEQ, depth)
    maps = make_in_maps(inputs, SEQ, depth, K.consts)
    res = run_bass_kernel_spmd(nc, maps, core_ids=list(range(8)))
    out = np.stack([np.asarray(res.results[b]["out"], dtype=np.float32) for b in range(B)], axis=0)
    return out
```

```python
import contextlib
import numpy as np
import ml_dtypes
import concourse.bass as bass
import concourse.mybir as mybir
from concourse.bass_utils import run_bass_kernel_spmd

F32 = mybir.dt.float32
BF16 = mybir.dt.bfloat16
AF = mybir.ActivationFunctionType
ALU = mybir.AluOpType
AX = mybir.AxisListType

D = 1024
KC = 8
DEPTH = 4
IN_W = 3592
ALPHA = float((2 * DEPTH) ** 0.25)
LN_EPS = 1e-5
RMS_EPS = 1e-6
NEG = -30000.0
INTERLEAVE_GDN = True
DMA_PAD = 3
GDN_YK = 1


class Ev:
    __slots__ = ("key", "val", "snap", "src")

    def __init__(self, key, val, snap, src):
        self.key, self.val, self.snap, self.src = key, val, snap, src


class Buf:
    __slots__ = ("w", "r", "name", "excl")

    def __init__(self, name=""):
        self.w = None
        self.r = {}
        self.name = name
        self.excl = False


class Tl:
    def __init__(self, h, name=""):
        self.h = h
        self.b = Buf(name)

    def __getitem__(self, idx):
        return self.h[idx]


ENG = ("pe", "act", "dve", "pool", "sp")


class Sched:
    def __init__(self, nc, es, ndma=8, same_eng=True):
        self.nc = nc
        self.e = {"pe": nc.tensor, "act": nc.scalar, "dve": nc.vector, "pool": nc.gpsimd, "sp": nc.sync}
        self.sem = {k: es.enter_context(nc.semaphore("sem_" + k)) for k in ENG}
        self.cnt = {k: 0 for k in ENG}
        self.seen = {k: {} for k in ENG}
        self.dq = {}
        self.semobj = {"c:" + k: self.sem[k] for k in ENG}
        for q in ("sp", "pool"):
            sems = [es.enter_context(nc.semaphore("dma_%s_%d" % (q, i))) for i in range(ndma)]
            self.dq[q] = {"sems": sems, "n": 0, "pending": [None] * ndma}
            for i, sm in enumerate(sems):
                self.semobj["d:%s:%d" % (q, i)] = sm
        self.same_eng = same_eng
        self.nwait = 0
        self.max_ops = None
        self.log = None
        self.nins = 0
        self.rr = 0

    def _wait(self, eng, ev):
        if ev is None:
            return
        seen = self.seen[eng]
        if seen.get(ev.key, 0) >= ev.val:
            return
        if ev.src == eng and (eng == "pe" or not self.same_eng):
            return
        self.e[eng].wait_ge(self.semobj[ev.key], ev.val)
        if self.log is not None:
            self.log.append((self.nins, eng, "WAIT %s >= %d" % (ev.key, ev.val)))
        self.nwait += 1
        new = dict(seen)
        for k, v in ev.snap.items():
            if new.get(k, 0) < v:
                new[k] = v
        if new.get(ev.key, 0) < ev.val:
            new[ev.key] = ev.val
        self.seen[eng] = new

    def _deps(self, eng, R, W):
        for b in R:
            self._wait(eng, b.w)
            if b.excl:
                for k, ev in list(b.r.items()):
                    if k != eng:
                        self._wait(eng, ev)
        for b in W:
            self._wait(eng, b.w)
            for ev in list(b.r.values()):
                self._wait(eng, ev)

    def op(self, eng, fn, R=(), W=()):
        if self.max_ops is not None and self.nins >= self.max_ops:
            return None
        R = [getattr(x, "b", x) for x in R]
        W = [getattr(x, "b", x) for x in W]
        self._deps(eng, R, W)
        ins = fn()
        if self.log is not None:
            self.log.append((self.nins, eng, str(ins)[:150]))
        self.cnt[eng] += 1
        self.nins += 1
        ins.then_inc(self.sem[eng], 1)
        ev = Ev("c:" + eng, self.cnt[eng], self.seen[eng], eng)
        for b in R:
            b.r[eng] = ev
        for b in W:
            b.w = ev
            b.r = {}
        return ev

    def dma(self, q, out, in_, R=(), W=(), **kw):
        if self.max_ops is not None and self.nins >= self.max_ops:
            return None
        R = [getattr(x, "b", x) for x in R]
        W = [getattr(x, "b", x) for x in W]
        dq = self.dq[q]
        ns = len(dq["sems"])
        i = dq["n"] % ns
        self._wait(q, dq["pending"][i])
        self._deps(q, R, W)
        ins = self.e[q].dma_start(out=out, in_=in_, **kw)
        val = 16 * (dq["n"] // ns + 1)
        ins.then_inc(dq["sems"][i], 16)
        ev = Ev("d:%s:%d" % (q, i), val, self.seen[q], "dma")
        dq["pending"][i] = ev
        dq["n"] += 1
        self.nins += 1
        for b in R:
            b.r[("dma", q, dq["n"])] = ev
        for b in W:
            b.w = ev
            b.r = {}
        return ev

    def barrier(self):
        evs = [Ev("c:" + k, self.cnt[k], {}, "x") for k in ENG if self.cnt[k] > 0]
        for dq in self.dq.values():
            evs += [p for p in dq["pending"] if p is not None]
        for eng in ENG:
            for ev in evs:
                self._wait(eng, ev)

    def finish(self):
        for dq in self.dq.values():
            for p in dq["pending"]:
                self._wait("sp", p)

    def alt(self):
        self.rr += 1
        return "act" if (self.rr & 1) else "dve"


class Ctx:
    pass


def _sb(K, es, name, shape, dt):
    K.uid = getattr(K, "uid", 0) + 1
    name = "%s_u%d" % (name, K.uid)
    return Tl(es.enter_context(K.nc.sbuf_tensor(name, list(shape), dt)), name)


def _evac(K, eng, out, in_, R, W, scale=None):
    nc, S = K.nc, K.S
    if eng == "act":
        if scale is None:
            S.op("act", lambda: nc.scalar.copy(out=out, in_=in_), R=R, W=W)
        else:
            S.op("act", lambda: nc.scalar.mul(out=out, in_=in_, mul=scale), R=R, W=W)
    else:
        if scale is None:
            S.op("dve", lambda: nc.vector.tensor_copy(out=out, in_=in_), R=R, W=W)
        else:
            S.op("dve", lambda: nc.vector.tensor_scalar_mul(out=out, in0=in_, scalar1=scale), R=R, W=W)


def _ps(K):
    K.psi = (K.psi + 1) % len(K.psum)
    return K.psum[K.psi]


def _ln_tile(K, x, out, g, b, sm):
    nc, S = K.nc, K.S
    st, mv, rstd, tmp = sm["st"], sm["mv"], sm["rstd"], sm["tmp"]
    S.op("dve", lambda: nc.vector.bn_stats(out=st[:, 0, :], in_=x[:, 0:512]), R=[x], W=[st])
    S.op("dve", lambda: nc.vector.bn_stats(out=st[:, 1, :], in_=x[:, 512:1024]), R=[x, st], W=[st])
    S.op("dve", lambda: nc.vector.bn_aggr(out=mv[:], in_=st[:].rearrange("p a b -> p (a b)")), R=[st], W=[mv])
    S.op("act", lambda: nc.scalar.activation(out=rstd[:], in_=mv[:, 1:2], func=AF.Sqrt, bias=K.eps_ln[:, 0:1], scale=1.0),
         R=[mv, K.eps_ln], W=[rstd])
    S.op("dve", lambda: nc.vector.reciprocal(out=rstd[:], in_=rstd[:]), R=[rstd], W=[rstd])
    S.op("dve", lambda: nc.vector.tensor_scalar(out=tmp[:], in0=x[:], scalar1=mv[:, 0:1], scalar2=rstd[:, 0:1],
                                                op0=ALU.subtract, op1=ALU.mult), R=[x, mv, rstd], W=[tmp])
    S.op("dve", lambda: nc.vector.tensor_tensor(out=tmp[:], in0=tmp[:], in1=g[:], op=ALU.mult), R=[tmp, g], W=[tmp])
    S.op("dve", lambda: nc.vector.tensor_tensor(out=out[:], in0=tmp[:], in1=b[:], op=ALU.add), R=[tmp, b], W=[out])


def _load_cast(K, dst_tl, dst_ap, src_ap, shape, Wb):
    nc, S = K.nc, K.S
    K.stgi = (K.stgi + 1) % len(K.stg)
    st = K.stg[K.stgi]
    n = int(np.prod(shape[1:]))
    if len(shape) == 3:
        v = st.h[:, 0:n].rearrange("p (a b) -> p a b", a=shape[1])
    else:
        v = st.h[:, 0:n]
    S.dma("sp", v, src_ap, W=[st])
    S.op("pool", lambda: nc.gpsimd.tensor_copy(out=dst_ap, in_=v), R=[st], W=[Wb])


def phase_input(K):
    nc, S, NT = K.nc, K.S, K.NT
    with contextlib.ExitStack() as es:
        g = _sb(K, es, "pi_g", [128, D], F32)
        b = _sb(K, es, "pi_b", [128, D], F32)
        S.dma("sp", g[:], K.d["ln_in_g"].partition_broadcast(128), W=[g])
        S.dma("sp", b[:], K.d["ln_in_b"].partition_broadcast(128), W=[b])
        xs = [_sb(K, es, "pi_x%d" % i, [128, D], F32) for i in range(2)]
        os_ = [_sb(K, es, "pi_o%d" % i, [128, D], F32) for i in range(2)]
        sm = {"st": _sb(K, es, "pi_st", [128, 2, 6], F32), "mv": _sb(K, es, "pi_mv", [128, 2], F32),
              "rstd": _sb(K, es, "pi_rs", [128, 1], F32), "tmp": _sb(K, es, "pi_tmp", [128, D], F32)}
        PT = K.SEQ + 64
        if NT * 128 > PT:
            zb = _sb(K, es, "pi_zb", [128, 512], BF16)
            S.op("pool", lambda: nc.gpsimd.memset(zb[:], 0.0), W=[zb])
            S.dma("sp", K.d["mixed"][PT:NT * 128, 512:1024], zb[0:NT * 128 - PT, :], R=[zb], W=[K.sbuf_mx_gd])
        for t in range(NT):
            x, o = xs[t % 2], os_[t % 2]
            lo = 128 * t - 64
            r0, r1 = max(lo, 0), min(lo + 128, K.SEQ)
            if t == 0 or r1 - lo < 128:
                S.op("pool", lambda: nc.gpsimd.memset(x[:], 0.0), W=[x])
            if t == 0:
                S.dma("sp", x[48:64, :], K.d["meta"][:, :], W=[x])
            if r1 > r0:
                S.dma("sp", x[r0 - lo:r1 - lo, :], K.d["x"][r0:r1, :], W=[x])
            _ln_tile(K, x, o, g, b, sm)
            S.dma("sp", K.d["h"][128 * t:128 * (t + 1), :], o[:], R=[o], W=[K.hbuf[t]])


def phase_A1(K, l):
    nc, S, NT = K.nc, K.S, K.NT
    with contextlib.ExitStack() as es:
        K.stg = [_sb(K, es, "stg%d" % i, [128, IN_W], F32) for i in range(2)]
        Wb = _sb(K, es, "a1_W", [128, KC, IN_W], BF16)
        wbufs = [Buf() for _ in range(KC)]
        wsrc = K.d["w_in"][l].rearrange("(k p) n -> p k n", p=128)
        for kc in range(KC):
            _load_cast(K, Wb, Wb[:, kc, :], wsrc[:, kc, :], [128, IN_W], wbufs[kc])
        dtb = _sb(K, es, "a1_dtb", [128, 4], F32)
        nea = _sb(K, es, "a1_nea", [128, 4], F32)
        S.dma("sp", dtb[:], K.d["dt_bias"][l].partition_broadcast(128), W=[dtb])
        S.dma("sp", nea[:], K.d["a_log"][l].partition_broadcast(128), W=[nea])
        S.op("act", lambda: nc.scalar.activation(out=nea[:], in_=nea[:], func=AF.Exp), R=[nea], W=[nea])
        S.op("dve", lambda: nc.vector.tensor_scalar_mul(out=nea[:], in0=nea[:], scalar1=-1.0), R=[nea], W=[nea])
        hts = [_sb(K, es, "a1_h%d" % i, [128, 4, D], F32) for i in range(1)]
        hTs = [_sb(K, es, "a1_hT%d" % i, [128, KC, 512], BF16) for i in range(2)]
        stq = [_sb(K, es, "a1_sq%d" % i, [128, 512], BF16) for i in range(3)]
        stg = [_sb(K, es, "a1_sg%d" % i, [128, 512], F32) for i in range(3)]
        stv = [_sb(K, es, "a1_sv%d" % i, [128, 4, 512], BF16) for i in range(2)]
        stz = [_sb(K, es, "a1_sz%d" % i, [128, 4, 512], F32) for i in range(2)]
        stb = [_sb(K, es, "a1_sb%d" % i, [128, 4, 8], F32) for i in range(2)]
        tb = _sb(K, es, "a1_tb", [128, 4, 4], F32)
        NG = (NT + 3) // 4
        for g in range(NG):
            nt = min(4, NT - 4 * g)
            n = nt * 128
            c0 = g * 512
            ht, hT = hts[0], hTs[g % 2]
            sv, sz, sbb = stv[g % 2], stz[g % 2], stb[g % 2]
            S.dma("sp", ht[:, 0:nt, :], K.d["h"][c0:c0 + n, :].rearrange("(t p) d -> p t d", p=128),
                  R=K.hbuf[4 * g:4 * g + nt], W=[ht])
            for kc in range(KC):
                ps = _ps(K)
                for t in range(nt):
                    S.op("pe", lambda: nc.tensor.transpose(out=ps[:, t * 128:(t + 1) * 128],
                                                           in_=ht[:, t, kc * 128:(kc + 1) * 128], identity=K.identf[:]),
                         R=[ht, K.identf], W=[ps])
                _evac(K, S.alt(), hT[:, kc, 0:n], ps[:, 0:n], [ps], [hT])
            for ci in range(20):
                if ci < 8:
                    col = ci * 128
                else:
                    col = 1536 + (ci - 8) * 128
                ps = _ps(K)
                for kc in range(KC):
                    S.op("pe", lambda: nc.tensor.matmul(ps[:, 0:n], lhsT=Wb[:, kc, col:col + 128], rhs=hT[:, kc, 0:n],
                                                        start=(kc == 0), stop=(kc == KC - 1)),
                         R=[wbufs[kc], hT], W=[ps])
                if ci < 8:
                    sq = stq[ci % 3]
                    _evac(K, S.alt(), sq[:, 0:n], ps[:, 0:n], [ps], [sq], scale=(0.125 if ci < 4 else None))
                    if ci < 4:
                        S.dma("sp", K.d["qT"][ci, :, c0:c0 + n], sq[:, 0:n], R=[sq], W=[K.sbuf_q])
                    else:
                        S.dma("sp", K.d["kT"][ci - 4, :, c0:c0 + n], sq[:, 0:n], R=[sq], W=[K.sbuf_k])
                else:
                    sg = stg[ci % 3]
                    _evac(K, S.alt(), sg[:, 0:n], ps[:, 0:n], [ps], [sg])
                    S.dma("sp", K.d["gT"][ci - 8, :, c0:c0 + n], sg[:, 0:n], R=[sg], W=[K.sbuf_g])
            for t in range(nt):
                ps = _ps(K)
                for kc in range(KC):
                    S.op("pe", lambda: nc.tensor.matmul(ps[:, :], lhsT=hT[:, kc, t * 128:(t + 1) * 128], rhs=Wb[:, kc, 1024:1536],
                                                        start=(kc == 0), stop=(kc == KC - 1)), R=[wbufs[kc], hT], W=[ps])
                _evac(K, S.alt(), sv[:, t, :], ps[:, :], [ps], [sv])
                ps = _ps(K)
                for kc in range(KC):
                    S.op("pe", lambda: nc.tensor.matmul(ps[:, :], lhsT=hT[:, kc, t * 128:(t + 1) * 128], rhs=Wb[:, kc, 3072:3584],
                                                        start=(kc == 0), stop=(kc == KC - 1)), R=[wbufs[kc], hT], W=[ps])
                S.op("act", lambda: nc.scalar.activation(out=sz[:, t, :], in_=ps[:, :], func=AF.Silu), R=[ps], W=[sz])
                ps = _ps(K)
                for kc in range(KC):
                    S.op("pe", lambda: nc.tensor.matmul(ps[:, 0:8], lhsT=hT[:, kc, t * 128:(t + 1) * 128], rhs=Wb[:, kc, 3584:3592],
                                                        start=(kc == 0), stop=(kc == KC - 1)), R=[wbufs[kc], hT], W=[ps])
                S.op("act", lambda: nc.scalar.activation(out=sbb[:, t, 0:4], in_=ps[:, 0:4], func=AF.Sigmoid), R=[ps], W=[sbb])
                S.op("dve", lambda: nc.vector.tensor_tensor(out=tb[:, t, :], in0=ps[:, 4:8], in1=dtb[:], op=ALU.add),
                     R=[ps, dtb], W=[tb])
            S.op("act", lambda: nc.scalar.activation(out=tb[:, 0:nt, :], in_=tb[:, 0:nt, :], func=AF.Exp), R=[tb], W=[tb])
            S.op("act", lambda: nc.scalar.activation(out=tb[:, 0:nt, :], in_=tb[:, 0:nt, :], func=AF.Ln, bias=K.one_c[:, 0:1], scale=1.0),
                 R=[tb, K.one_c], W=[tb])
            S.op("dve", lambda: nc.vector.tensor_tensor(out=sbb[:, 0:nt, 4:8], in0=tb[:, 0:nt, :],
                                                        in1=nea[:].unsqueeze(1).to_broadcast([128, nt, 4]), op=ALU.mult),
                 R=[tb, nea], W=[sbb])
            S.dma("sp", K.d["V"][c0:c0 + n, :].rearrange("(t p) c -> p t c", p=128), sv[:, 0:nt, :], R=[sv], W=[K.sbuf_v])
            S.dma("sp", K.d["zs"][c0:c0 + n, :].rearrange("(t p) c -> p t c", p=128), sz[:, 0:nt, :], R=[sz], W=[K.sbuf_z])
            S.dma("sp", K.d["bg"][c0:c0 + n, :].rearrange("(t p) c -> p t c", p=128), sbb[:, 0:nt, :], R=[sbb], W=[K.sbuf_bg])


class RR:
    def __init__(self):
        self.items = []

    def add(self, gen, w=1):
        self.items.append([gen, w])

    def run(self):
        while self.items:
            for item in list(self.items):
                for _ in range(item[1]):
                    try:
                        next(item[0])
                    except StopIteration:
                        self.items.remove(item)
                        break


def _psg(K):
    K.psgi = (K.psgi + 1) % len(K.psum_gd)
    return K.psum_gd[K.psgi]


def sb_stream(K, l, es):
    nc, S, NT = K.nc, K.S, K.NT
    P = NT * 128
    qT = _sb(K, es, "sb_q", [128, P], BF16)
    kT = _sb(K, es, "sb_k", [128, P], BF16)
    Vt = _sb(K, es, "sb_v", [128, NT, 128], BF16)
    sbg = _sb(K, es, "sb_g", [128, 64], F32)
    S.dma("sp", sbg[:], K.d["sb_norm_g"][l].partition_broadcast(128), W=[sbg])
    es_ = [_sb(K, es, "sb_e%d" % i, [128, 512], F32) for i in range(2)]
    sps = [_sb(K, es, "sb_sp%d" % i, [128, 512], BF16) for i in range(4)]
    ws = [_sb(K, es, "sb_w%d" % i, [128, 512], BF16) for i in range(3)]
    oaccs = [_sb(K, es, "sb_oa%d" % i, [128, 4, 64], F32) for i in range(2)]
    raccs = [_sb(K, es, "sb_ra%d" % i, [128, 4], F32) for i in range(2)]
    eRs = [_sb(K, es, "sb_eR%d" % i, [128, 4], F32) for i in range(2)]
    tmps = [_sb(K, es, "sb_tmp%d" % i, [128, 4, 64], F32) for i in range(2)]
    osbs = [_sb(K, es, "sb_os%d" % i, [128, 4, 128], BF16) for i in range(2)]
    ss = _sb(K, es, "sb_ss", [128, 4], F32)
    zb = K.psum[0:3]
    pb = K.psum[3:5]
    NG = (NT + 3) // 4
    gi = 0
    for hp in range(4):
        S.dma("sp", qT[:, :], K.d["qT"][hp], R=[K.sbuf_q], W=[qT])
        S.dma("sp", kT[:, :], K.d["kT"][hp], R=[K.sbuf_k], W=[kT])
        S.dma("sp", Vt[:, :, :], K.d["V"][:, hp * 128:(hp + 1) * 128].rearrange("(t p) c -> p t c", p=128), R=[K.sbuf_v], W=[Vt])
        its = []
        for qg in range(NG):
            nt = min(4, NT - 4 * qg)
            for h2 in range(2):
                kbs = list(range(4 * qg + nt - 1, -1, -1))
                for j, kb in enumerate(kbs):
                    its.append((qg, nt, h2, kb, j == 0, j == len(kbs) - 1, gi))
                gi += 1
        N = len(its)

        def geom(it):
            qg, nt, h2, kb = it[0], it[1], it[2], it[3]
            rel = kb - 4 * qg
            if rel >= 0:
                mi = 4 if kb == 0 else rel
            elif kb == 0:
                mi = 5
            else:
                mi = None
            lo = max(rel, 0)
            return rel, mi, lo, lo * 128, nt * 128, qg * 512

        def stA(i):
            it = its[i]
            qg, nt, h2, kb = it[0], it[1], it[2], it[3]
            rel, mi, lo, c0, n, q0 = geom(it)
            z, e, sp = zb[i % 3], es_[i % 2], sps[i % 4]
            r0 = h2 * 64
            kk = kT[r0:r0 + 64, kb * 128:(kb + 1) * 128]
            qq = qT[r0:r0 + 64, q0 + c0:q0 + n]
            S.op("pe", lambda: nc.tensor.matmul(z[:, c0:n], lhsT=kk, rhs=qq, start=True, stop=(mi is None)), R=[kT, qT], W=[z])
            if mi is not None:
                S.op("pe", lambda: nc.tensor.matmul(z[:, c0:n], lhsT=K.identb[:], rhs=K.masks[:, mi, c0:n], start=False, stop=True),
                     R=[K.identb, K.masks], W=[z])
            S.op("act", lambda: nc.scalar.activation(out=e[:, c0:n], in_=z[:, c0:n], func=AF.Exp), R=[z], W=[e])
            S.op("act", lambda: nc.scalar.activation(out=sp[:, c0:n], in_=e[:, c0:n], func=AF.Ln, bias=K.one_c[:, 0:1], scale=1.0),
                 R=[e, K.one_c], W=[sp])

        def stB(i):
            it = its[i]
            qg, nt, h2, kb = it[0], it[1], it[2], it[3]
            rel, mi, lo, c0, n, q0 = geom(it)
            z, sp, w = zb[i % 3], sps[i % 4], ws[i % 3]
            r0 = h2 * 64
            kk = kT[r0:r0 + 64, kb * 128:(kb + 1) * 128]
            qq = qT[r0:r0 + 64, q0 + c0:q0 + n]
            S.op("pe", lambda: nc.tensor.matmul(z[:, c0:n], lhsT=kk, rhs=qq, start=True, stop=False), R=[kT, qT], W=[z])
            if mi is not None:
                S.op("pe", lambda: nc.tensor.matmul(z[:, c0:n], lhsT=K.identb[:], rhs=K.masks[:, mi, c0:n], start=False, stop=False),
                     R=[K.identb, K.masks], W=[z])
            S.op("pe", lambda: nc.tensor.matmul(z[:, c0:n], lhsT=K.negtri[:], rhs=sp[:, c0:n], start=False, stop=True),
                 R=[K.negtri, sp], W=[z])
            S.op("act", lambda: nc.scalar.activation(out=w[:, c0:n], in_=z[:, c0:n], func=AF.Exp), R=[z], W=[w])

        def stC(i):
            it = its[i]
            qg, nt, h2, kb, first, last, g_ = it
            rel, mi, lo, c0, n, q0 = geom(it)
            sp, w = sps[i % 4], ws[i % 3]
            oacc, racc = oaccs[g_ % 2], raccs[g_ % 2]
            eR, tmp = eRs[i % 2], tmps[i % 2]
            osb = osbs[qg % 2]
            if first:
                S.op("pool", lambda: nc.gpsimd.memset(oacc[:], 0.0), W=[oacc])
                S.op("pool", lambda: nc.gpsimd.memset(racc[:], 0.0), W=[racc])
            po = pb[i % 2]
            pov = po.h[:, 0:260].rearrange("p (t c) -> p t c", c=65)
            for qt in range(lo, nt):
                S.op("pe", lambda: nc.tensor.matmul(pov[:, qt, 0:64], lhsT=w[:, qt * 128:(qt + 1) * 128],
                                                    rhs=Vt[:, kb, h2 * 64:(h2 + 1) * 64], start=True, stop=True),
                     R=[w, Vt], W=[po])
                S.op("pe", lambda: nc.tensor.matmul(pov[:, qt, 64:65], lhsT=sp[:, qt * 128:(qt + 1) * 128],
                                                    rhs=K.onesb[:, 0:1], start=True, stop=True),
                     R=[sp, K.onesb], W=[po])
            S.op("act", lambda: nc.scalar.activation(out=eR[:, lo:nt], in_=racc[:, lo:nt], func=AF.Exp, scale=-1.0),
                 R=[racc], W=[eR])
            S.op("dve", lambda: nc.vector.tensor_tensor(out=tmp[:, lo:nt, :], in0=pov[:, lo:nt, 0:64],
                                                        in1=eR[:, lo:nt].unsqueeze(2).to_broadcast([128, nt - lo, 64]), op=ALU.mult),
                 R=[po, eR], W=[tmp])
            S.op("pool", lambda: nc.gpsimd.tensor_tensor(out=oacc[:, lo:nt, :], in0=oacc[:, lo:nt, :], in1=tmp[:, lo:nt, :], op=ALU.add),
                 R=[oacc, tmp], W=[oacc])
            S.op("dve", lambda: nc.vector.tensor_tensor(out=racc[:, lo:nt], in0=racc[:, lo:nt], in1=pov[:, lo:nt, 64], op=ALU.add),
                 R=[racc, po], W=[racc])
            if last:
                tmp2 = tmps[(i + 1) % 2]
                S.op("dve", lambda: nc.vector.tensor_tensor(out=tmp2[:, 0:nt, :], in0=oacc[:, 0:nt, :], in1=oacc[:, 0:nt, :], op=ALU.mult),
                     R=[oacc], W=[tmp2])
                S.op("dve", lambda: nc.vector.tensor_reduce(out=ss[:, 0:nt], in_=tmp2[:, 0:nt, :], axis=AX.X, op=ALU.add), R=[tmp2], W=[ss])
                S.op("act", lambda: nc.scalar.activation(out=ss[:, 0:nt], in_=ss[:, 0:nt], func=AF.Ln, bias=K.eps_rms64[:, 0:1], scale=1.0),
                     R=[ss, K.eps_rms64], W=[ss])
                S.op("act", lambda: nc.scalar.activation(out=ss[:, 0:nt], in_=ss[:, 0:nt], func=AF.Exp, scale=-0.5), R=[ss], W=[ss])
                S.op("dve", lambda: nc.vector.scalar_tensor_tensor(out=tmp2[:, 0:nt, :], in0=oacc[:, 0:nt, :], scalar=8.0,
                                                                  in1=ss[:, 0:nt].unsqueeze(2).to_broadcast([128, nt, 64]),
                                                                  op0=ALU.mult, op1=ALU.mult),
                     R=[oacc, ss], W=[tmp2])
                S.op("dve", lambda: nc.vector.tensor_tensor(out=osb[:, 0:nt, h2 * 64:(h2 + 1) * 64], in0=tmp2[:, 0:nt, :],
                                                            in1=sbg[:].unsqueeze(1).to_broadcast([128, nt, 64]), op=ALU.mult),
                     R=[tmp2, sbg], W=[osb])
                if h2 == 1:
                    S.dma("sp", K.d["mixed"][q0:q0 + n, hp * 128:(hp + 1) * 128].rearrange("(t p) c -> p t c", p=128), osb[:, 0:nt, :],
                          R=[osb], W=[K.sbuf_mx_sb])

        for s in range(N + 2):
            if s < N:
                stA(s)
            if 0 <= s - 1 < N:
                stB(s - 1)
            if 0 <= s - 2 < N:
                stC(s - 2)
            yield


def gdn_master(K, l, es, rr, NSETS, YK):
    nc, S, NT = K.nc, K.S, K.NT
    NC_ = K.NC
    cw = _sb(K, es, "gd_cw", [128, 12, 4], F32)
    S.dma("sp", cw[:], K.d["conv_wT"][l].rearrange("(i d) t -> d i t", d=128), W=[cw])
    gng = _sb(K, es, "gd_gng", [64, 4, 128], F32)
    for h in range(4):
        S.dma("sp", gng[:, h, :], K.d["gdn_norm_g"][l].partition_broadcast(64), W=[gng])
    xin = [_sb(K, es, "gd_x%d" % i, [128, 12, 131], F32) for i in range(2)]
    cvs = [_sb(K, es, "gd_cv%d" % i, [128, 12, 128], F32) for i in range(2)]
    sq = [_sb(K, es, "gd_sq%d" % i, [128, 128], F32) for i in range(2)]
    rn = [_sb(K, es, "gd_rn%d" % i, [128, 128], F32) for i in range(2)]
    Sst = _sb(K, es, "gd_S", [128, 4, 128], F32)
    S.op("pool", lambda: nc.gpsimd.memset(Sst[:], 0.0), W=[Sst])
    tmpS = _sb(K, es, "gd_tmpS", [128, 4, 128], F32)

    def mk(name, shape, dt=F32):
        return [_sb(K, es, "gd_%s%d" % (name, i), shape, dt) for i in range(NSETS)]
    B = {}
    for name in ("egc", "egr", "ssq"):
        B[name] = mk(name, [64, 4])
    B["cdt"] = mk("cd", [128, 4])
    for name in ("gl", "dm", "dmT", "Nm", "Mm", "N2", "M2", "Pm", "qkT"):
        B[name] = mk(name, [64, 4, 64])
    for name in ("kbg", "vb", "ke", "gbc", "u", "vn", "ot", "o2"):
        B[name] = mk(name, [64, 4, 128])
    for name in ("egb", "qd", "wcT"):
        B[name] = mk(name, [128, 4, 64])
    B["ob"] = mk("ob", [64, 4, 128], BF16)
    B["bg"] = mk("bg", [64, 8])
    B["z"] = mk("z", [64, 512])
    st = {"inflight": 0, "rec_done": 0, "done": 0}
    assert NSETS == 2
    pbigs = [K.psum[5], K.psum[6]]
    psmls = pbigs
    psg1 = K.psum[7]
    n = 128

    def load_x(g):
        x = xin[g % 2]
        c0 = g * 128
        if g == 0:
            S.op("pool", lambda: nc.gpsimd.memset(x[:, :, 0:3], 0.0), W=[x])
            S.dma("sp", x[:, :, 3:3 + n], K.d["gT"][:, :, 0:n].rearrange("h p n -> p h n"), R=[K.sbuf_g], W=[x])
            S.op("pool", lambda: nc.gpsimd.memset(x[:, :, 3:3 + 48], 0.0), W=[x])
        else:
            S.dma("sp", x[:, :, 0:3 + n], K.d["gT"][:, :, c0 - 3:c0 + n].rearrange("h p n -> p h n"), R=[K.sbuf_g], W=[x])

    def G1(g):
        le = None
        x, cv = xin[g % 2], cvs[g % 2]
        k = 0
        for i in range(12):
            eng = "dve"
            E = nc.vector
            if le != eng:
                yield
            le = eng
            S.op(eng, lambda: E.tensor_scalar(out=cv[:, i, 0:n], in0=x[:, i, 0:n], scalar1=cw[:, i, 0:1], scalar2=None, op0=ALU.mult),
                 R=[x, cw], W=[cv])
            for tp in range(1, 4):
                if le != "dve":
                    yield
                le = "dve"
                S.op("dve", lambda: nc.vector.scalar_tensor_tensor(out=cv[:, i, 0:n], in0=x[:, i, tp:tp + n], scalar=cw[:, i, tp:tp + 1],
                                                                  in1=cv[:, i, 0:n], op0=ALU.mult, op1=ALU.add), R=[x, cw, cv], W=[cv])
            s_ = sq[i % 2]
            if le != "act":
                yield
            le = "act"
            S.op("act", lambda: nc.scalar.activation(out=s_[:, 0:n], in_=cv[:, i, 0:n], func=AF.Exp, scale=-1.0), R=[cv], W=[s_])
            S.op("act", lambda: nc.scalar.activation(out=s_[:, 0:n], in_=s_[:, 0:n], func=AF.Ln, bias=K.one_c[:, 0:1], scale=1.0),
                 R=[s_, K.one_c], W=[s_])
            S.op("act", lambda: nc.scalar.activation(out=s_[:, 0:n], in_=s_[:, 0:n], func=AF.Exp, scale=-1.0), R=[s_], W=[s_])
            if le != "dve":
                yield
            le = "dve"
            S.op("dve", lambda: nc.vector.tensor_tensor(out=cv[:, i, 0:n], in0=cv[:, i, 0:n], in1=s_[:, 0:n], op=ALU.mult), R=[cv, s_], W=[cv])
        for i in range(8):
            s_, r_ = sq[i % 2], rn[i % 2]
            if le != "pool":
                yield
            le = "pool"
            S.op("pool", lambda: nc.gpsimd.tensor_tensor(out=s_[:, 0:n], in0=cv[:, i, 0:n], in1=cv[:, i, 0:n], op=ALU.mult), R=[cv], W=[s_])
            ps = psg1
            if le != "pe":
                yield
            le = "pe"
            S.op("pe", lambda: nc.tensor.matmul(ps[:, 0:n], lhsT=K.onesf[:, :], rhs=s_[:, 0:n], start=True, stop=True),
                 R=[K.onesf, s_], W=[ps])
            if le != "act":
                yield
            le = "act"
            S.op("act", lambda: nc.scalar.activation(out=r_[:, 0:n], in_=ps[:, 0:n], func=AF.Ln, bias=K.eps_rms[:, 0:1], scale=1.0),
                 R=[ps, K.eps_rms], W=[r_])
            S.op("act", lambda: nc.scalar.activation(out=r_[:, 0:n], in_=r_[:, 0:n], func=AF.Exp, scale=-0.5), R=[r_], W=[r_])
            if i < 4:
                if le != "dve":
                    yield
                le = "dve"
                S.op("dve", lambda: nc.vector.scalar_tensor_tensor(out=cv[:, i, 0:n], in0=cv[:, i, 0:n], scalar=float(128 ** -0.5),
                                                                  in1=r_[:, 0:n], op0=ALU.mult, op1=ALU.mult), R=[cv, r_], W=[cv])
            else:
                if le != "dve":
                    yield
                le = "dve"
                S.op("dve", lambda: nc.vector.tensor_tensor(out=cv[:, i, 0:n], in0=cv[:, i, 0:n], in1=r_[:, 0:n], op=ALU.mult),
                     R=[cv, r_], W=[cv])

    def chunk(g, ci, cidx):
        le = None
        b2 = cidx % NSETS
        pbig, psml = pbigs[b2], psmls[b2]
        cv = cvs[g % 2]
        egc, egr, cdt, ssq = B["egc"][b2], B["egr"][b2], B["cdt"][b2], B["ssq"][b2]
        gl, dm, dmT, Pm, qkT = B["gl"][b2], B["dm"][b2], B["dmT"][b2], B["Pm"][b2], B["qkT"][b2]
        kbg, vb, ke, gbc, u, vn, ot, o2 = (B[k_][b2] for k_ in ("kbg", "vb", "ke", "gbc", "u", "vn", "ot", "o2"))
        egb, qd, wcT, ob = B["egb"][b2], B["qd"][b2], B["wcT"][b2], B["ob"][b2]
        bgc, zc = B["bg"][b2], B["z"][b2]
        p0 = g * 128 + ci * 64
        if le != "sp":
            yield
        le = "sp"
        S.dma("sp", bgc[:, :], K.d["bg"][p0:p0 + 64, :], R=[K.sbuf_bg], W=[bgc])
        if le != "sp":
            yield
        le = "sp"
        S.dma("sp", zc[:, :], K.d["zs"][p0:p0 + 64, :], R=[K.sbuf_z], W=[zc])
        if cidx == 0:
            if le != "pool":
                yield
            le = "pool"
            S.op("pool", lambda: nc.gpsimd.memset(bgc[0:48, 4:8], 0.0), W=[bgc])
        for _ in range(DMA_PAD):
            yield
        cs = slice(ci * 64, ci * 64 + 64)
        gcol = bgc[:, 4:8]
        bcol = bgc[:, 0:4]
        pg = psml
        if le != "pe":
            yield
        le = "pe"
        S.op("pe", lambda: nc.tensor.matmul(pg[0:64, 0:4], lhsT=K.triu[:, 0, :], rhs=gcol, start=True, stop=True), R=[K.triu, bgc], W=[pg])
        if le != "pe":
            yield
        le = "pe"
        S.op("pe", lambda: nc.tensor.matmul(pg[0:64, 4:8], lhsT=K.sgt[:, :], rhs=gcol, start=True, stop=True), R=[K.sgt, bgc], W=[pg])
        if le != "pe":
            yield
        le = "pe"
        S.op("pe", lambda: nc.tensor.matmul(pg[:, 8:12], lhsT=K.ones64[:, :], rhs=gcol, start=True, stop=True), R=[K.ones64, bgc], W=[pg])
        if le != "act":
            yield
        le = "act"
        S.op("act", lambda: nc.scalar.activation(out=egc[:], in_=pg[0:64, 0:4], func=AF.Exp), R=[pg], W=[egc])
        if le != "act":
            yield
        le = "act"
        S.op("act", lambda: nc.scalar.activation(out=egr[:], in_=pg[0:64, 4:8], func=AF.Exp), R=[pg], W=[egr])
        if le != "act":
            yield
        le = "act"
        S.op("act", lambda: nc.scalar.activation(out=cdt[:], in_=pg[:, 8:12], func=AF.Exp), R=[pg], W=[cdt])
        if le != "dve":
            yield
        le = "dve"
        S.op("dve", lambda: nc.vector.tensor_tensor(out=gl[:], in0=K.triu[:], in1=gcol.unsqueeze(2).to_broadcast([64, 4, 64]), op=ALU.mult),
             R=[K.triu, bgc], W=[gl])
        pd = pbig
        pdv = pd.h[0:64, 0:512].rearrange("p (a h c) -> p a h c", a=2, h=4)
        for h in range(4):
            if le != "pe":
                yield
            le = "pe"
            S.op("pe", lambda: nc.tensor.matmul(pdv[:, 0, h, :], lhsT=gl[:, h, :], rhs=K.sgt[:, :], start=True, stop=True),
                 R=[gl, K.sgt], W=[pd])
        if le != "pe":
            yield
        le = "pe"
        S.op("pe", lambda: nc.tensor.matmul(pd[0:64, 256:512], lhsT=K.sgt[:, :], rhs=gl[:].rearrange("p h c -> p (h c)"), start=True, stop=True),
             R=[gl, K.sgt], W=[pd])
        if le != "act":
            yield
        le = "act"
        S.op("act", lambda: nc.scalar.activation(out=dm[:], in_=pdv[:, 0], func=AF.Exp), R=[pd], W=[dm])
        if le != "act":
            yield
        le = "act"
        S.op("act", lambda: nc.scalar.activation(out=dmT[:], in_=pdv[:, 1], func=AF.Exp), R=[pd], W=[dmT])
        if le != "pool":
            yield
        le = "pool"
        S.op("pool", lambda: nc.gpsimd.tensor_tensor(out=dm[:], in0=dm[:], in1=K.trilsn[:], op=ALU.mult), R=[dm, K.trilsn], W=[dm])
        if le != "pool":
            yield
        le = "pool"
        S.op("pool", lambda: nc.gpsimd.tensor_tensor(out=dmT[:], in0=dmT[:], in1=K.triu[:], op=ALU.mult), R=[dmT, K.triu], W=[dmT])
        pgq = pbig
        pgqv = pgq.h[0:64, 0:512].rearrange("p (a h c) -> p a h c", a=2, h=4)
        for h in range(4):
            if le != "pe":
                yield
            le = "pe"
            S.op("pe", lambda: nc.tensor.matmul(pgqv[:, 0, h, :], lhsT=cv[:, 4 + h, cs], rhs=cv[:, 4 + h, cs], start=True, stop=True), R=[cv], W=[pgq])
            if le != "pe":
                yield
            le = "pe"
            S.op("pe", lambda: nc.tensor.matmul(pgqv[:, 1, h, :], lhsT=cv[:, 4 + h, cs], rhs=cv[:, h, cs], start=True, stop=True), R=[cv], W=[pgq])
        Nm, Mm = B["Nm"][b2], B["Mm"][b2]
        if le != "dve":
            yield
        le = "dve"
        S.op("dve", lambda: nc.vector.tensor_tensor(out=Nm[:], in0=pgqv[:, 0], in1=bcol.unsqueeze(2).to_broadcast([64, 4, 64]), op=ALU.mult),
             R=[pgq, bgc], W=[Nm])
        if le != "dve":
            yield
        le = "dve"
        S.op("dve", lambda: nc.vector.tensor_tensor(out=Nm[:], in0=Nm[:], in1=dm[:], op=ALU.mult), R=[Nm, dm], W=[Nm])
        if le != "dve":
            yield
        le = "dve"
        S.op("dve", lambda: nc.vector.tensor_tensor(out=qkT[:], in0=pgqv[:, 1], in1=dmT[:], op=ALU.mult), R=[pgq, dmT], W=[qkT])
        pt = psml
        ptv = pt.h[0:64, 0:256].rearrange("p (h c) -> p h c", h=4)
        for h in range(4):
            if le != "pe":
                yield
            le = "pe"
            S.op("pe", lambda: nc.tensor.transpose(out=ptv[:, h, :], in_=Nm[:, h, :], identity=K.identf[0:64, 0:64]), R=[Nm, K.identf], W=[pt])
        if le != "act":
            yield
        le = "act"
        S.op("act", lambda: nc.scalar.copy(out=Mm[:], in_=ptv), R=[pt], W=[Mm])
        if le != "dve":
            yield
        le = "dve"
        S.op("dve", lambda: nc.vector.tensor_tensor(out=Pm[:], in0=Mm[:], in1=K.ident4[:], op=ALU.add), R=[Mm, K.ident4], W=[Pm])
        Nc, Mc, Nn, Mn = Nm, Mm, B["N2"][b2], B["M2"][b2]
        for r in range(5):
            pn = pbig
            pnv = pn.h[0:64, 0:512].rearrange("p (a h c) -> p a h c", a=2, h=4)
            for h in range(4):
                if le != "pe":
                    yield
                le = "pe"
                S.op("pe", lambda: nc.tensor.matmul(pnv[:, 0, h, :], lhsT=Mc[:, h, :], rhs=Nc[:, h, :], start=True, stop=True), R=[Mc, Nc], W=[pn])
                if r < 4:
                    if le != "pe":
                        yield
                    le = "pe"
                    S.op("pe", lambda: nc.tensor.matmul(pnv[:, 1, h, :], lhsT=Nc[:, h, :], rhs=Mc[:, h, :], start=True, stop=True), R=[Mc, Nc], W=[pn])
            if le != "act":
                yield
            le = "act"
            S.op("act", lambda: nc.scalar.copy(out=Nn[:], in_=pnv[:, 0]), R=[pn], W=[Nn])
            if r < 4:
                if le != "dve":
                    yield
                le = "dve"
                S.op("dve", lambda: nc.vector.tensor_copy(out=Mn[:], in_=pnv[:, 1]), R=[pn], W=[Mn])
            pp = psml
            ppv = pp.h[0:64, 0:256].rearrange("p (h c) -> p h c", h=4)
            for h in range(4):
                if le != "pe":
                    yield
                le = "pe"
                S.op("pe", lambda: nc.tensor.matmul(ppv[:, h, :], lhsT=Nn[:, h, :], rhs=Pm[:, h, :], start=True, stop=True), R=[Nn, Pm], W=[pp])
            if le != "dve":
                yield
            le = "dve"
            S.op("dve", lambda: nc.vector.tensor_tensor(out=Pm[:], in0=Pm[:], in1=ppv, op=ALU.add), R=[Pm, pp], W=[Pm])
            Nc, Mc, Nn, Mn = Nn, Mn, Nc, Mc
        pk = pbig
        pkv = pk.h[0:64, 0:512].rearrange("p (h c) -> p h c", h=4)
        for h in range(4):
            if le != "pe":
                yield
            le = "pe"
            S.op("pe", lambda: nc.tensor.transpose(out=pkv[:, h, :], in_=cv[:, 4 + h, cs], identity=K.identf[:]), R=[cv, K.identf], W=[pk])
        if le != "dve":
            yield
        le = "dve"
        S.op("dve", lambda: nc.vector.tensor_tensor(out=ke[:], in0=pkv, in1=egr[:].unsqueeze(2).to_broadcast([64, 4, 128]), op=ALU.mult),
             R=[pk, egr], W=[ke])
        if le != "dve":
            yield
        le = "dve"
        S.op("dve", lambda: nc.vector.tensor_tensor(out=kbg[:], in0=pkv, in1=bcol.unsqueeze(2).to_broadcast([64, 4, 128]), op=ALU.mult),
             R=[pk, bgc], W=[kbg])
        if le != "pool":
            yield
        le = "pool"
        S.op("pool", lambda: nc.gpsimd.tensor_tensor(out=kbg[:], in0=kbg[:], in1=egc[:].unsqueeze(2).to_broadcast([64, 4, 128]), op=ALU.mult),
             R=[kbg, egc], W=[kbg])
        pv = pbig
        pvv = pv.h[0:64, 0:512].rearrange("p (h c) -> p h c", h=4)
        for h in range(4):
            if le != "pe":
                yield
            le = "pe"
            S.op("pe", lambda: nc.tensor.transpose(out=pvv[:, h, :], in_=cv[:, 8 + h, cs], identity=K.identf[:]), R=[cv, K.identf], W=[pv])
        if le != "dve":
            yield
        le = "dve"
        S.op("dve", lambda: nc.vector.tensor_tensor(out=vb[:], in0=pvv, in1=bcol.unsqueeze(2).to_broadcast([64, 4, 128]), op=ALU.mult),
             R=[pv, bgc], W=[vb])
        if le != "pool":
            yield
        le = "pool"
        S.op("pool", lambda: nc.gpsimd.tensor_tensor(out=gbc[:], in0=K.ones4[:], in1=gcol.unsqueeze(2).to_broadcast([64, 4, 128]), op=ALU.mult),
             R=[K.ones4, bgc], W=[gbc])
        pe_ = psml
        pev = pe_.h[:, 0:256].rearrange("p (h c) -> p h c", h=4)
        for h in range(4):
            if le != "pe":
                yield
            le = "pe"
            S.op("pe", lambda: nc.tensor.matmul(pev[:, h, :], lhsT=gbc[:, h, :], rhs=K.triu[:, 0, :], start=True, stop=True), R=[gbc, K.triu], W=[pe_])
        if le != "act":
            yield
        le = "act"
        S.op("act", lambda: nc.scalar.activation(out=egb[:], in_=pev, func=AF.Exp), R=[pe_], W=[egb])
        if le != "dve":
            yield
        le = "dve"
        S.op("dve", lambda: nc.vector.tensor_tensor(out=qd[:], in0=cv[:, 0:4, cs], in1=egb[:], op=ALU.mult), R=[cv, egb], W=[qd])
        pw = psml
        pwv = pw.h[:, 0:256].rearrange("p (h c) -> p h c", h=4)
        for h in range(4):
            if le != "pe":
                yield
            le = "pe"
            S.op("pe", lambda: nc.tensor.matmul(pwv[:, h, :], lhsT=kbg[:, h, :], rhs=Pm[:, h, :], start=True, stop=True), R=[kbg, Pm], W=[pw])
        if le != "act":
            yield
        le = "act"
        S.op("act", lambda: nc.scalar.copy(out=wcT[:], in_=pwv), R=[pw], W=[wcT])
        pu = pbig
        puv = pu.h[0:64, 0:512].rearrange("p (h c) -> p h c", h=4)
        for h in range(4):
            if le != "pe":
                yield
            le = "pe"
            S.op("pe", lambda: nc.tensor.matmul(puv[:, h, :], lhsT=Pm[:, h, :], rhs=vb[:, h, :], start=True, stop=True), R=[vb, Pm], W=[pu])
        if le != "act":
            yield
        le = "act"
        S.op("act", lambda: nc.scalar.copy(out=u[:], in_=puv), R=[pu], W=[u])
        while st["rec_done"] < cidx:
            yield
        pws = pbig
        pwsv = pws.h[0:64, 0:512].rearrange("p (h c) -> p h c", h=4)
        for h in range(4):
            if le != "pe":
                yield
            le = "pe"
            S.op("pe", lambda: nc.tensor.matmul(pwsv[:, h, :], lhsT=wcT[:, h, :], rhs=Sst[:, h, :], start=True, stop=True), R=[wcT, Sst], W=[pws])
        if le != "dve":
            yield
        le = "dve"
        S.op("dve", lambda: nc.vector.tensor_tensor(out=vn[:], in0=u[:], in1=pwsv, op=ALU.subtract), R=[u, pws], W=[vn])
        po = pbig
        pov = po.h[0:64, 0:512].rearrange("p (h c) -> p h c", h=4)
        for h in range(4):
            if le != "pe":
                yield
            le = "pe"
            S.op("pe", lambda: nc.tensor.matmul(pov[:, h, :], lhsT=qd[:, h, :], rhs=Sst[:, h, :], start=True, stop=False), R=[qd, Sst], W=[po])
            if le != "pe":
                yield
            le = "pe"
            S.op("pe", lambda: nc.tensor.matmul(pov[:, h, :], lhsT=qkT[:, h, :], rhs=vn[:, h, :], start=False, stop=True), R=[qkT, vn], W=[po])
        if le != "act":
            yield
        le = "act"
        S.op("act", lambda: nc.scalar.copy(out=ot[:], in_=pov), R=[po], W=[ot])
        pS = pbig
        pSv = pS.h[:, 0:512].rearrange("p (h c) -> p h c", h=4)
        for h in range(4):
            if le != "pe":
                yield
            le = "pe"
            S.op("pe", lambda: nc.tensor.matmul(pSv[:, h, :], lhsT=ke[:, h, :], rhs=vn[:, h, :], start=True, stop=True), R=[ke, vn], W=[pS])
        if le != "pool":
            yield
        le = "pool"
        S.op("pool", lambda: nc.gpsimd.tensor_tensor(out=tmpS[:], in0=Sst[:], in1=cdt[:].unsqueeze(2).to_broadcast([128, 4, 128]), op=ALU.mult),
             R=[Sst, cdt], W=[tmpS])
        if le != "dve":
            yield
        le = "dve"
        S.op("dve", lambda: nc.vector.tensor_tensor(out=Sst[:], in0=tmpS[:], in1=pSv, op=ALU.add), R=[tmpS, pS], W=[Sst])
        st["rec_done"] = cidx + 1
        if le != "pool":
            yield
        le = "pool"
        S.op("pool", lambda: nc.gpsimd.tensor_tensor(out=o2[:], in0=ot[:], in1=ot[:], op=ALU.mult), R=[ot], W=[o2])
        if le != "dve":
            yield
        le = "dve"
        S.op("dve", lambda: nc.vector.tensor_reduce(out=ssq[:], in_=o2[:], axis=AX.X, op=ALU.add), R=[o2], W=[ssq])
        if le != "act":
            yield
        le = "act"
        S.op("act", lambda: nc.scalar.activation(out=ssq[:], in_=ssq[:], func=AF.Ln, bias=K.eps_rms128[0:64, 0:1], scale=1.0),
             R=[ssq, K.eps_rms128], W=[ssq])
        S.op("act", lambda: nc.scalar.activation(out=ssq[:], in_=ssq[:], func=AF.Exp, scale=-0.5), R=[ssq], W=[ssq])
        if le != "dve":
            yield
        le = "dve"
        S.op("dve", lambda: nc.vector.scalar_tensor_tensor(out=o2[:], in0=ot[:], scalar=float(128 ** 0.5),
                                                          in1=ssq[:].unsqueeze(2).to_broadcast([64, 4, 128]), op0=ALU.mult, op1=ALU.mult),
             R=[ot, ssq], W=[o2])
        if le != "pool":
            yield
        le = "pool"
        S.op("pool", lambda: nc.gpsimd.tensor_tensor(out=o2[:], in0=o2[:], in1=gng[:], op=ALU.mult), R=[o2, gng], W=[o2])
        if le != "dve":
            yield
        le = "dve"
        S.op("dve", lambda: nc.vector.tensor_tensor(out=ob[:], in0=o2[:], in1=zc[:, :].rearrange("p (h c) -> p h c", h=4), op=ALU.mult),
             R=[o2, zc], W=[ob])
        if le != "sp":
            yield
        le = "sp"
        S.dma("sp", K.d["mixed"][p0:p0 + 64, 512:1024], ob[:].rearrange("p h c -> p (h c)"), R=[ob], W=[K.sbuf_mx_gd])
        st["inflight"] -= 1
        st["done"] += 1

    NG = NT
    load_x(0)
    cidx = 0
    for g in range(NG):
        nch = min(2, NC_ - 2 * g)
        if nch <= 0:
            break
        if g + 1 < NG and NC_ - 2 * (g + 1) > 0:
            load_x(g + 1)
        while st["done"] < min(cidx, 2 * (g - 1)):
            yield
        yield from G1(g)
        for ci in range(nch):
            while st["inflight"] >= NSETS:
                yield
            st["inflight"] += 1
            rr.add(chunk(g, ci, cidx), YK)
            cidx += 1
            yield
    while st["done"] < cidx:
        yield


def phase_GDN_old(K, l):
    nc, S, NT = K.nc, K.S, K.NT
    NC_ = K.NC
    with contextlib.ExitStack() as es:
        cw = _sb(K, es, "gd_cw", [128, 12, 4], F32)
        S.dma("sp", cw[:], K.d["conv_wT"][l].rearrange("(i d) t -> d i t", d=128), W=[cw])
        gng = _sb(K, es, "gd_gng", [64, 4, 128], F32)
        for h in range(4):
            S.dma("sp", gng[:, h, :], K.d["gdn_norm_g"][l].partition_broadcast(64), W=[gng])
        xin = [_sb(K, es, "gd_x%d" % i, [128, 12, 259], F32) for i in range(2)]
        cv = _sb(K, es, "gd_cv", [128, 12, 256], F32)
        sq = [_sb(K, es, "gd_sq%d" % i, [128, 256], F32) for i in range(2)]
        rn = [_sb(K, es, "gd_rn%d" % i, [128, 256], F32) for i in range(2)]
        bgt = [_sb(K, es, "gd_bg%d" % i, [64, 4, 8], F32) for i in range(2)]
        zt = [_sb(K, es, "gd_z%d" % i, [64, 4, 512], F32) for i in range(2)]
        Sst = _sb(K, es, "gd_S", [128, 4, 128], F32)
        S.op("pool", lambda: nc.gpsimd.memset(Sst[:], 0.0), W=[Sst])
        def mk(name, shape, dt=F32):
            return [_sb(K, es, "gd_%s%d" % (name, i), shape, dt) for i in range(2)]
        egc, egr, cdt = mk("egc", [64, 4]), mk("egr", [64, 4]), mk("cd", [128, 4])
        gl, dm, dmT = mk("gl", [64, 4, 64]), mk("dm", [64, 4, 64]), mk("dmT", [64, 4, 64])
        Nm, Mm = mk("N", [64, 4, 64]), mk("M", [64, 4, 64])
        N2, M2 = mk("N2", [64, 4, 64]), mk("M2", [64, 4, 64])
        Pm = mk("P", [64, 4, 64])
        qkT = mk("qkT", [64, 4, 64])
        kbg, vb, ke = mk("kbg", [64, 4, 128]), mk("vb", [64, 4, 128]), mk("ke", [64, 4, 128])
        gbc, egb, qd = mk("gbc", [64, 4, 128]), mk("egb", [128, 4, 64]), mk("qd", [128, 4, 64])
        wcT, u, vn = mk("wcT", [128, 4, 64]), mk("u", [64, 4, 128]), mk("vn", [64, 4, 128])
        ot, o2 = mk("ot", [64, 4, 128]), mk("o2", [64, 4, 128])
        ssq, ob = mk("ssq", [64, 4]), mk("ob", [64, 4, 128], BF16)
        tmpS = _sb(K, es, "gd_tmpS", [128, 4, 128], F32)
        NG = (NT + 1) // 2
        ci_glob = 0
        for g in range(NG):
            nt = min(2, NT - 2 * g)
            n = nt * 128
            c0 = g * 256
            nch = min(n // 64, NC_ - c0 // 64)
            if nch <= 0:
                break
            x = xin[g % 2]
            if g == 0:
                S.op("pool", lambda: nc.gpsimd.memset(x[:, :, 0:3], 0.0), W=[x])
                S.dma("sp", x[:, :, 3:3 + n], K.d["gT"][:, :, 0:n].rearrange("h p n -> p h n"), R=[K.sbuf_g], W=[x])
                S.op("pool", lambda: nc.gpsimd.memset(x[:, :, 3:3 + 48], 0.0), W=[x])
            else:
                S.dma("sp", x[:, :, 0:3 + n], K.d["gT"][:, :, c0 - 3:c0 + n].rearrange("h p n -> p h n"), R=[K.sbuf_g], W=[x])
            for i in range(12):
                eng = "dve" if i % 2 == 0 else "pool"
                E = nc.vector if eng == "dve" else nc.gpsimd
                S.op(eng, lambda: E.tensor_scalar(out=cv[:, i, 0:n], in0=x[:, i, 0:n], scalar1=cw[:, i, 0:1], scalar2=None, op0=ALU.mult),
                     R=[x, cw], W=[cv])
                for tp in range(1, 4):
                    S.op("dve", lambda: nc.vector.scalar_tensor_tensor(out=cv[:, i, 0:n], in0=x[:, i, tp:tp + n], scalar=cw[:, i, tp:tp + 1],
                                                                      in1=cv[:, i, 0:n], op0=ALU.mult, op1=ALU.add), R=[x, cw, cv], W=[cv])
                S.op("act", lambda: nc.scalar.activation(out=cv[:, i, 0:n], in_=cv[:, i, 0:n], func=AF.Silu), R=[cv], W=[cv])
            for i in range(8):
                s_, r_ = sq[i % 2], rn[i % 2]
                S.op("pool", lambda: nc.gpsimd.tensor_tensor(out=s_[:, 0:n], in0=cv[:, i, 0:n], in1=cv[:, i, 0:n], op=ALU.mult), R=[cv], W=[s_])
                ps = _ps(K)
                S.op("pe", lambda: nc.tensor.matmul(ps[:, 0:n], lhsT=K.onesf[:, :], rhs=s_[:, 0:n], start=True, stop=True),
                     R=[K.onesf, s_], W=[ps])
                S.op("act", lambda: nc.scalar.activation(out=r_[:, 0:n], in_=ps[:, 0:n], func=AF.Sqrt, bias=K.eps_rms[:, 0:1], scale=1.0),
                     R=[ps, K.eps_rms], W=[r_])
                S.op("dve", lambda: nc.vector.reciprocal(out=r_[:, 0:n], in_=r_[:, 0:n]), R=[r_], W=[r_])
                if i < 4:
                    S.op("dve", lambda: nc.vector.scalar_tensor_tensor(out=cv[:, i, 0:n], in0=cv[:, i, 0:n], scalar=float(128 ** -0.5),
                                                                      in1=r_[:, 0:n], op0=ALU.mult, op1=ALU.mult), R=[cv, r_], W=[cv])
                else:
                    S.op("dve", lambda: nc.vector.tensor_tensor(out=cv[:, i, 0:n], in0=cv[:, i, 0:n], in1=r_[:, 0:n], op=ALU.mult),
                         R=[cv, r_], W=[cv])
            bgc, zc = bgt[g % 2], zt[g % 2]
            S.dma("sp", bgc[:, 0:nch, :], K.d["bg"][c0:c0 + nch * 64, :].rearrange("(n c) e -> c n e", c=64), R=[K.sbuf_bg], W=[bgc])
            S.dma("sp", zc[:, 0:nch, :], K.d["zs"][c0:c0 + nch * 64, :].rearrange("(n c) e -> c n e", c=64), R=[K.sbuf_z], W=[zc])
            if g == 0:
                S.op("pool", lambda: nc.gpsimd.memset(bgc[0:48, 0, 4:8], 0.0), W=[bgc])
            for ci in range(nch):
                b2 = ci_glob % 2
                ci_glob += 1
                cs = slice(ci * 64, ci * 64 + 64)
                gcol = bgc[:, ci, 4:8]
                bcol = bgc[:, ci, 0:4]
                pg = _ps(K)
                S.op("pe", lambda: nc.tensor.matmul(pg[0:64, 0:4], lhsT=K.triu[:, 0, :], rhs=gcol, start=True, stop=True), R=[K.triu, bgc], W=[pg])
                S.op("pe", lambda: nc.tensor.matmul(pg[0:64, 4:8], lhsT=K.sgt[:, :], rhs=gcol, start=True, stop=True), R=[K.sgt, bgc], W=[pg])
                S.op("pe", lambda: nc.tensor.matmul(pg[:, 8:12], lhsT=K.ones64[:, :], rhs=gcol, start=True, stop=True), R=[K.ones64, bgc], W=[pg])
                S.op("act", lambda: nc.scalar.activation(out=egc[b2][:], in_=pg[0:64, 0:4], func=AF.Exp), R=[pg], W=[egc[b2]])
                S.op("act", lambda: nc.scalar.activation(out=egr[b2][:], in_=pg[0:64, 4:8], func=AF.Exp), R=[pg], W=[egr[b2]])
                S.op("act", lambda: nc.scalar.activation(out=cdt[b2][:], in_=pg[:, 8:12], func=AF.Exp), R=[pg], W=[cdt[b2]])
                S.op("dve", lambda: nc.vector.tensor_tensor(out=gl[b2][:], in0=K.triu[:], in1=gcol.unsqueeze(2).to_broadcast([64, 4, 64]), op=ALU.mult),
                     R=[K.triu, bgc], W=[gl[b2]])
                pd = _ps(K)
                pdv = pd.h[0:64, 0:512].rearrange("p (a h c) -> p a h c", a=2, h=4)
                for h in range(4):
                    S.op("pe", lambda: nc.tensor.matmul(pdv[:, 0, h, :], lhsT=gl[b2][:, h, :], rhs=K.sgt[:, :], start=True, stop=True),
                         R=[gl[b2], K.sgt], W=[pd])
                S.op("pe", lambda: nc.tensor.matmul(pd[0:64, 256:512], lhsT=K.sgt[:, :], rhs=gl[b2][:].rearrange("p h c -> p (h c)"), start=True, stop=True),
                     R=[gl[b2], K.sgt], W=[pd])
                S.op("act", lambda: nc.scalar.activation(out=dm[b2][:], in_=pdv[:, 0], func=AF.Exp), R=[pd], W=[dm[b2]])
                S.op("act", lambda: nc.scalar.activation(out=dmT[b2][:], in_=pdv[:, 1], func=AF.Exp), R=[pd], W=[dmT[b2]])
                S.op("pool", lambda: nc.gpsimd.tensor_tensor(out=dm[b2][:], in0=dm[b2][:], in1=K.trilsn[:], op=ALU.mult), R=[dm[b2], K.trilsn], W=[dm[b2]])
                S.op("pool", lambda: nc.gpsimd.tensor_tensor(out=dmT[b2][:], in0=dmT[b2][:], in1=K.triu[:], op=ALU.mult), R=[dmT[b2], K.triu], W=[dmT[b2]])
                pgq = _ps(K)
                pgqv = pgq.h[0:64, 0:512].rearrange("p (a h c) -> p a h c", a=2, h=4)
                for h in range(4):
                    S.op("pe", lambda: nc.tensor.matmul(pgqv[:, 0, h, :], lhsT=cv[:, 4 + h, cs], rhs=cv[:, 4 + h, cs], start=True, stop=True), R=[cv], W=[pgq])
                    S.op("pe", lambda: nc.tensor.matmul(pgqv[:, 1, h, :], lhsT=cv[:, 4 + h, cs], rhs=cv[:, h, cs], start=True, stop=True), R=[cv], W=[pgq])
                S.op("dve", lambda: nc.vector.tensor_tensor(out=Nm[b2][:], in0=pgqv[:, 0], in1=bcol.unsqueeze(2).to_broadcast([64, 4, 64]), op=ALU.mult),
                     R=[pgq, bgc], W=[Nm[b2]])
                S.op("dve", lambda: nc.vector.tensor_tensor(out=Nm[b2][:], in0=Nm[b2][:], in1=dm[b2][:], op=ALU.mult), R=[Nm[b2], dm[b2]], W=[Nm[b2]])
                S.op("dve", lambda: nc.vector.tensor_tensor(out=qkT[b2][:], in0=pgqv[:, 1], in1=dmT[b2][:], op=ALU.mult), R=[pgq, dmT[b2]], W=[qkT[b2]])
                pt = _ps(K)
                ptv = pt.h[0:64, 0:256].rearrange("p (h c) -> p h c", h=4)
                for h in range(4):
                    S.op("pe", lambda: nc.tensor.transpose(out=ptv[:, h, :], in_=Nm[b2][:, h, :], identity=K.identf[0:64, 0:64]), R=[Nm[b2], K.identf], W=[pt])
                S.op("act", lambda: nc.scalar.copy(out=Mm[b2][:], in_=ptv), R=[pt], W=[Mm[b2]])
                S.op("dve", lambda: nc.vector.tensor_tensor(out=Pm[b2][:], in0=Mm[b2][:], in1=K.ident4[:], op=ALU.add), R=[Mm[b2], K.ident4], W=[Pm[b2]])
                Nc, Mc, Nn, Mn = Nm[b2], Mm[b2], N2[b2], M2[b2]
                for r in range(5):
                    pn = _ps(K)
                    pnv = pn.h[0:64, 0:512].rearrange("p (a h c) -> p a h c", a=2, h=4)
                    for h in range(4):
                        S.op("pe", lambda: nc.tensor.matmul(pnv[:, 0, h, :], lhsT=Mc[:, h, :], rhs=Nc[:, h, :], start=True, stop=True), R=[Mc, Nc], W=[pn])
                        if r < 4:
                            S.op("pe", lambda: nc.tensor.matmul(pnv[:, 1, h, :], lhsT=Nc[:, h, :], rhs=Mc[:, h, :], start=True, stop=True), R=[Mc, Nc], W=[pn])
                    S.op("act", lambda: nc.scalar.copy(out=Nn[:], in_=pnv[:, 0]), R=[pn], W=[Nn])
                    if r < 4:
                        S.op("dve", lambda: nc.vector.tensor_copy(out=Mn[:], in_=pnv[:, 1]), R=[pn], W=[Mn])
                    pp = _ps(K)
                    ppv = pp.h[0:64, 0:256].rearrange("p (h c) -> p h c", h=4)
                    for h in range(4):
                        S.op("pe", lambda: nc.tensor.matmul(ppv[:, h, :], lhsT=Nn[:, h, :], rhs=Pm[b2][:, h, :], start=True, stop=True), R=[Nn, Pm[b2]], W=[pp])
                    S.op("dve", lambda: nc.vector.tensor_tensor(out=Pm[b2][:], in0=Pm[b2][:], in1=ppv, op=ALU.add), R=[Pm[b2], pp], W=[Pm[b2]])
                    Nc, Mc, Nn, Mn = Nn, Mn, Nc, Mc
                pk = _ps(K)
                pkv = pk.h[0:64, 0:512].rearrange("p (h c) -> p h c", h=4)
                pv = _ps(K)
                pvv = pv.h[0:64, 0:512].rearrange("p (h c) -> p h c", h=4)
                for h in range(4):
                    S.op("pe", lambda: nc.tensor.transpose(out=pkv[:, h, :], in_=cv[:, 4 + h, cs], identity=K.identf[:]), R=[cv, K.identf], W=[pk])
                    S.op("pe", lambda: nc.tensor.transpose(out=pvv[:, h, :], in_=cv[:, 8 + h, cs], identity=K.identf[:]), R=[cv, K.identf], W=[pv])
                S.op("dve", lambda: nc.vector.tensor_tensor(out=ke[b2][:], in0=pkv, in1=egr[b2][:].unsqueeze(2).to_broadcast([64, 4, 128]), op=ALU.mult),
                     R=[pk, egr[b2]], W=[ke[b2]])
                S.op("dve", lambda: nc.vector.tensor_tensor(out=kbg[b2][:], in0=pkv, in1=bcol.unsqueeze(2).to_broadcast([64, 4, 128]), op=ALU.mult),
                     R=[pk, bgc], W=[kbg[b2]])
                S.op("pool", lambda: nc.gpsimd.tensor_tensor(out=kbg[b2][:], in0=kbg[b2][:], in1=egc[b2][:].unsqueeze(2).to_broadcast([64, 4, 128]), op=ALU.mult),
                     R=[kbg[b2], egc[b2]], W=[kbg[b2]])
                S.op("dve", lambda: nc.vector.tensor_tensor(out=vb[b2][:], in0=pvv, in1=bcol.unsqueeze(2).to_broadcast([64, 4, 128]), op=ALU.mult),
                     R=[pv, bgc], W=[vb[b2]])
                S.op("pool", lambda: nc.gpsimd.tensor_tensor(out=gbc[b2][:], in0=K.ones4[:], in1=gcol.unsqueeze(2).to_broadcast([64, 4, 128]), op=ALU.mult),
                     R=[K.ones4, bgc], W=[gbc[b2]])
                pe_ = _ps(K)
                pev = pe_.h[:, 0:256].rearrange("p (h c) -> p h c", h=4)
                for h in range(4):
                    S.op("pe", lambda: nc.tensor.matmul(pev[:, h, :], lhsT=gbc[b2][:, h, :], rhs=K.triu[:, 0, :], start=True, stop=True), R=[gbc[b2], K.triu], W=[pe_])
                S.op("act", lambda: nc.scalar.activation(out=egb[b2][:], in_=pev, func=AF.Exp), R=[pe_], W=[egb[b2]])
                S.op("dve", lambda: nc.vector.tensor_tensor(out=qd[b2][:], in0=cv[:, 0:4, cs], in1=egb[b2][:], op=ALU.mult), R=[cv, egb[b2]], W=[qd[b2]])
                pw = _ps(K)
                pwv = pw.h[:, 0:256].rearrange("p (h c) -> p h c", h=4)
                pu = _ps(K)
                puv = pu.h[0:64, 0:512].rearrange("p (h c) -> p h c", h=4)
                for h in range(4):
                    S.op("pe", lambda: nc.tensor.matmul(pwv[:, h, :], lhsT=kbg[b2][:, h, :], rhs=Pm[b2][:, h, :], start=True, stop=True), R=[kbg[b2], Pm[b2]], W=[pw])
                    S.op("pe", lambda: nc.tensor.matmul(puv[:, h, :], lhsT=Pm[b2][:, h, :], rhs=vb[b2][:, h, :], start=True, stop=True), R=[vb[b2], Pm[b2]], W=[pu])
                S.op("act", lambda: nc.scalar.copy(out=wcT[b2][:], in_=pwv), R=[pw], W=[wcT[b2]])
                S.op("act", lambda: nc.scalar.copy(out=u[b2][:], in_=puv), R=[pu], W=[u[b2]])
                pws = _ps(K)
                pwsv = pws.h[0:64, 0:512].rearrange("p (h c) -> p h c", h=4)
                for h in range(4):
                    S.op("pe", lambda: nc.tensor.matmul(pwsv[:, h, :], lhsT=wcT[b2][:, h, :], rhs=Sst[:, h, :], start=True, stop=True), R=[wcT[b2], Sst], W=[pws])
                S.op("dve", lambda: nc.vector.tensor_tensor(out=vn[b2][:], in0=u[b2][:], in1=pwsv, op=ALU.subtract), R=[u[b2], pws], W=[vn[b2]])
                po = _ps(K)
                pov = po.h[0:64, 0:512].rearrange("p (h c) -> p h c", h=4)
                for h in range(4):
                    S.op("pe", lambda: nc.tensor.matmul(pov[:, h, :], lhsT=qd[b2][:, h, :], rhs=Sst[:, h, :], start=True, stop=False), R=[qd[b2], Sst], W=[po])
                    S.op("pe", lambda: nc.tensor.matmul(pov[:, h, :], lhsT=qkT[b2][:, h, :], rhs=vn[b2][:, h, :], start=False, stop=True), R=[qkT[b2], vn[b2]], W=[po])
                pS = _ps(K)
                pSv = pS.h[:, 0:512].rearrange("p (h c) -> p h c", h=4)
                for h in range(4):
                    S.op("pe", lambda: nc.tensor.matmul(pSv[:, h, :], lhsT=ke[b2][:, h, :], rhs=vn[b2][:, h, :], start=True, stop=True), R=[ke[b2], vn[b2]], W=[pS])
                S.op("pool", lambda: nc.gpsimd.tensor_tensor(out=tmpS[:], in0=Sst[:], in1=cdt[b2][:].unsqueeze(2).to_broadcast([128, 4, 128]), op=ALU.mult),
                     R=[Sst, cdt[b2]], W=[tmpS])
                S.op("dve", lambda: nc.vector.tensor_tensor(out=Sst[:], in0=tmpS[:], in1=pSv, op=ALU.add), R=[tmpS, pS], W=[Sst])
                S.op("act", lambda: nc.scalar.copy(out=ot[b2][:], in_=pov), R=[po], W=[ot[b2]])
                S.op("pool", lambda: nc.gpsimd.tensor_tensor(out=o2[b2][:], in0=ot[b2][:], in1=ot[b2][:], op=ALU.mult), R=[ot[b2]], W=[o2[b2]])
                S.op("dve", lambda: nc.vector.tensor_reduce(out=ssq[b2][:], in_=o2[b2][:], axis=AX.X, op=ALU.add), R=[o2[b2]], W=[ssq[b2]])
                S.op("act", lambda: nc.scalar.activation(out=ssq[b2][:], in_=ssq[b2][:], func=AF.Sqrt, bias=K.eps_rms[0:64, 0:1], scale=1.0 / 128),
                     R=[ssq[b2], K.eps_rms], W=[ssq[b2]])
                S.op("dve", lambda: nc.vector.reciprocal(out=ssq[b2][:], in_=ssq[b2][:]), R=[ssq[b2]], W=[ssq[b2]])
                S.op("dve", lambda: nc.vector.tensor_tensor(out=o2[b2][:], in0=ot[b2][:], in1=ssq[b2][:].unsqueeze(2).to_broadcast([64, 4, 128]), op=ALU.mult),
                     R=[ot[b2], ssq[b2]], W=[o2[b2]])
                S.op("pool", lambda: nc.gpsimd.tensor_tensor(out=o2[b2][:], in0=o2[b2][:], in1=gng[:], op=ALU.mult), R=[o2[b2], gng], W=[o2[b2]])
                S.op("dve", lambda: nc.vector.tensor_tensor(out=ob[b2][:], in0=o2[b2][:], in1=zc[:, ci, :].rearrange("p (h c) -> p h c", h=4), op=ALU.mult),
                     R=[o2[b2], zc], W=[ob[b2]])
                p0 = c0 + ci * 64
                S.dma("sp", K.d["mixed"][p0:p0 + 64, 512:1024], ob[b2][:].rearrange("p h c -> p (h c)"), R=[ob[b2]], W=[K.sbuf_mx_gd])


def phase_SBGDN(K, l, NSETS=2, YK=None, run_sb=True, run_gdn=True):
    YK = YK or GDN_YK
    with contextlib.ExitStack() as es:
        K.psum_gd = K.psum[5:8]
        K.psgi = 0
        rr = RR()
        if run_sb:
            rr.add(sb_stream(K, l, es), 1)
        if run_gdn:
            rr.add(gdn_master(K, l, es, rr, NSETS, YK), YK)
        rr.run()


def phase_A3(K, l):
    nc, S, NT = K.nc, K.S, K.NT
    with contextlib.ExitStack() as es:
        K.stg = [_sb(K, es, "stg%d" % i, [128, IN_W], F32) for i in range(2)]
        Wo = _sb(K, es, "a3_W", [128, KC, D], BF16)
        wbufs = [Buf() for _ in range(KC)]
        wsrc = K.d["w_out"][l].rearrange("(k p) n -> p k n", p=128)
        for kc in range(KC):
            _load_cast(K, Wo, Wo[:, kc, :], wsrc[:, kc, :], [128, D], wbufs[kc])
        Wr = _sb(K, es, "a3_Wr", [128, KC, 36], F32)
        S.dma("sp", Wr[:, :, 0:4], K.d["w_group"][l].rearrange("(k p) n -> p k n", p=128), W=[Wr])
        S.dma("sp", Wr[:, :, 4:36], K.d["w_expert"][l].rearrange("(k p) n -> p k n", p=128), W=[Wr])
        br = _sb(K, es, "a3_br", [128, 36], F32)
        S.dma("sp", br[:, 0:4], K.d["b_group"][l].partition_broadcast(128), W=[br])
        S.dma("sp", br[:, 4:36], K.d["b_expert"][l].partition_broadcast(128), W=[br])
        g = _sb(K, es, "a3_g", [128, D], F32)
        b = _sb(K, es, "a3_b", [128, D], F32)
        S.dma("sp", g[:], K.d["ln1_g"][l].partition_broadcast(128), W=[g])
        S.dma("sp", b[:], K.d["ln1_b"][l].partition_broadcast(128), W=[b])
        mxs = [_sb(K, es, "a3_mx%d" % i, [128, D], BF16) for i in range(2)]
        mTs = [_sb(K, es, "a3_mT%d" % i, [128, KC, 128], BF16) for i in range(2)]
        hts = [_sb(K, es, "a3_h%d" % i, [128, D], F32) for i in range(2)]
        rs = [_sb(K, es, "a3_r%d" % i, [128, D], F32) for i in range(2)]
        h1s = [_sb(K, es, "a3_h1%d" % i, [128, D], F32) for i in range(2)]
        hTf = [_sb(K, es, "a3_hTf%d" % i, [128, KC, 128], F32) for i in range(2)]
        hTb = [_sb(K, es, "a3_hTb%d" % i, [128, KC, 128], BF16) for i in range(2)]
        sm = {"st": _sb(K, es, "a3_st", [128, 2, 6], F32), "mv": _sb(K, es, "a3_mv", [128, 2], F32),
              "rstd": _sb(K, es, "a3_rs", [128, 1], F32), "tmp": _sb(K, es, "a3_tmp", [128, D], F32)}
        lg = _sb(K, es, "a3_lg", [128, 36], F32)
        sc = {k: _sb(K, es, "a3_" + k, shp, F32) for k, shp in
              (("gm", [128, 1]), ("ge", [128, 4]), ("gs", [128, 1]), ("oh", [128, 4]), ("ig", [128, 8]), ("tmp8", [128, 4, 8]),
               ("m8", [128, 8]), ("sel", [128, 8]), ("ex", [128, 8]), ("dn", [128, 1]), ("wi", [128, 8]))}
        cbs = [_sb(K, es, "a3_cb%d" % i, [128, 4, 8], F32) for i in range(2)]
        def stage1(t):
            mx, mT, ht, r, h1, hf, hb, cb = mxs[t % 2], mTs[t % 2], hts[t % 2], rs[t % 2], h1s[t % 2], hTf[t % 2], hTb[t % 2], cbs[t % 2]
            rows = slice(128 * t, 128 * (t + 1))
            S.dma("sp", mx[:], K.d["mixed"][rows, :], R=[K.sbuf_mx_sb, K.sbuf_mx_gd], W=[mx])
            S.dma("sp", ht[:], K.d["h"][rows, :], R=[K.hbuf[t]], W=[ht])
            for half in range(2):
                ps = _ps(K)
                psb = ps.h.bitcast(BF16)
                for j in range(4):
                    kc = half * 4 + j
                    S.op("pe", lambda: nc.tensor.transpose(out=psb[:, j * 128:(j + 1) * 128], in_=mx[:, kc * 128:(kc + 1) * 128], identity=K.identb[:]),
                         R=[mx, K.identb], W=[ps])
                _evac(K, S.alt(), mT[:, half * 4:half * 4 + 4, :], psb[:, 0:512].rearrange("p (a c) -> p a c", a=4), [ps], [mT])
            for half in range(2):
                ps = _ps(K)
                for kc in range(KC):
                    S.op("pe", lambda: nc.tensor.matmul(ps[:, :], lhsT=mT[:, kc, :], rhs=Wo[:, kc, half * 512:(half + 1) * 512],
                                                        start=(kc == 0), stop=(kc == KC - 1)), R=[mT, wbufs[kc]], W=[ps])
                S.op("dve", lambda: nc.vector.scalar_tensor_tensor(out=r[:, half * 512:(half + 1) * 512], in0=ht[:, half * 512:(half + 1) * 512],
                                                                  scalar=ALPHA, in1=ps[:, :], op0=ALU.mult, op1=ALU.add), R=[ht, ps], W=[r])
            _ln_tile(K, r, h1, g, b, sm)

        def stage2(t):
            mx, mT, ht, r, h1, hf, hb, cb = mxs[t % 2], mTs[t % 2], hts[t % 2], rs[t % 2], h1s[t % 2], hTf[t % 2], hTb[t % 2], cbs[t % 2]
            rows = slice(128 * t, 128 * (t + 1))
            S.dma("sp", K.d["h1"][rows, :], h1[:], R=[h1], W=[K.h1buf[t]])
            for half in range(2):
                ps = _ps(K)
                for j in range(4):
                    kc = half * 4 + j
                    S.op("pe", lambda: nc.tensor.transpose(out=ps[:, j * 128:(j + 1) * 128], in_=h1[:, kc * 128:(kc + 1) * 128], identity=K.identf[:]),
                         R=[h1, K.identf], W=[ps])
                S.op("act", lambda: nc.scalar.copy(out=hf[:, half * 4:half * 4 + 4, :], in_=ps[:, :].rearrange("p (a c) -> p a c", a=4)), R=[ps], W=[hf])
                S.op("dve", lambda: nc.vector.tensor_copy(out=hb[:, half * 4:half * 4 + 4, :], in_=ps[:, :].rearrange("p (a c) -> p a c", a=4)), R=[ps], W=[hb])
            S.dma("sp", K.d["h1T"][:, :, rows].rearrange("k p n -> p k n"), hb[:], R=[hb], W=[K.sbuf_h1T])
            ps = _ps(K)
            for kc in range(KC):
                S.op("pe", lambda: nc.tensor.matmul(ps[:, 0:36], lhsT=hf[:, kc, :], rhs=Wr[:, kc, :], start=(kc == 0), stop=(kc == KC - 1)),
                     R=[hf, Wr], W=[ps])
            S.op("dve", lambda: nc.vector.tensor_tensor(out=lg[:], in0=ps[:, 0:36], in1=br[:], op=ALU.add), R=[ps, br], W=[lg])
            gm, ge, gs, oh, ig, tmp8, m8, sel, ex, dn, wi = (sc[k] for k in ("gm", "ge", "gs", "oh", "ig", "tmp8", "m8", "sel", "ex", "dn", "wi"))
            S.op("dve", lambda: nc.vector.tensor_reduce(out=gm[:], in_=lg[:, 0:4], axis=AX.X, op=ALU.max), R=[lg], W=[gm])
            S.op("dve", lambda: nc.vector.tensor_scalar(out=oh[:], in0=lg[:, 0:4], scalar1=gm[:, 0:1], scalar2=None, op0=ALU.is_equal), R=[lg, gm], W=[oh])
            S.op("dve", lambda: nc.vector.tensor_scalar(out=ge[:], in0=lg[:, 0:4], scalar1=gm[:, 0:1], scalar2=None, op0=ALU.subtract), R=[lg, gm], W=[ge])
            S.op("act", lambda: nc.scalar.activation(out=ge[:], in_=ge[:], func=AF.Exp), R=[ge], W=[ge])
            S.op("dve", lambda: nc.vector.tensor_reduce(out=gs[:], in_=ge[:], axis=AX.X, op=ALU.add), R=[ge], W=[gs])
            S.op("dve", lambda: nc.vector.tensor_tensor(out=tmp8[:], in0=lg[:, 4:36].rearrange("p (g e) -> p g e", g=4),
                                                        in1=oh[:].unsqueeze(2).to_broadcast([128, 4, 8]), op=ALU.mult), R=[lg, oh], W=[tmp8])
            S.op("dve", lambda: nc.vector.tensor_reduce(out=ig[:], in_=tmp8[:].rearrange("p g e -> p e g"), axis=AX.X, op=ALU.add), R=[tmp8], W=[ig])
            S.op("dve", lambda: nc.vector.max(out=m8[:], in_=ig[:]), R=[ig], W=[m8])
            S.op("dve", lambda: nc.vector.tensor_scalar(out=sel[:], in0=ig[:], scalar1=m8[:, 1:2], scalar2=None, op0=ALU.is_ge), R=[ig, m8], W=[sel])
            S.op("dve", lambda: nc.vector.tensor_scalar(out=ex[:], in0=ig[:], scalar1=m8[:, 0:1], scalar2=None, op0=ALU.subtract), R=[ig, m8], W=[ex])
            S.op("act", lambda: nc.scalar.activation(out=ex[:], in_=ex[:], func=AF.Exp), R=[ex], W=[ex])
            S.op("dve", lambda: nc.vector.tensor_tensor(out=ex[:], in0=ex[:], in1=sel[:], op=ALU.mult), R=[ex, sel], W=[ex])
            S.op("dve", lambda: nc.vector.tensor_reduce(out=dn[:], in_=ex[:], axis=AX.X, op=ALU.add), R=[ex], W=[dn])
            S.op("dve", lambda: nc.vector.tensor_tensor(out=dn[:], in0=dn[:], in1=gs[:], op=ALU.mult), R=[dn, gs], W=[dn])
            S.op("dve", lambda: nc.vector.reciprocal(out=dn[:], in_=dn[:]), R=[dn], W=[dn])
            S.op("dve", lambda: nc.vector.tensor_scalar(out=wi[:], in0=ex[:], scalar1=dn[:, 0:1], scalar2=None, op0=ALU.mult), R=[ex, dn], W=[wi])
            S.op("dve", lambda: nc.vector.tensor_tensor(out=cb[:], in0=oh[:].unsqueeze(2).to_broadcast([128, 4, 8]),
                                                        in1=wi[:].unsqueeze(1).to_broadcast([128, 4, 8]), op=ALU.mult), R=[oh, wi], W=[cb])
            S.dma("sp", K.d["comb"][rows, :], cb[:].rearrange("p g e -> p (g e)"), R=[cb], W=[K.sbuf_comb])

        for t in range(NT + 1):
            if t < NT:
                stage1(t)
            if t >= 1:
                stage2(t - 1)


def phase_B(K, l, last):
    nc, S, NT = K.nc, K.S, K.NT
    NP = 4
    TH = (NT + NP - 1) // NP
    with contextlib.ExitStack() as es:
        g = _sb(K, es, "b_g", [128, D], F32)
        b = _sb(K, es, "b_b", [128, D], F32)
        S.dma("sp", g[:], K.d["ln2_g"][l].partition_broadcast(128), W=[g])
        S.dma("sp", b[:], K.d["ln2_b"][l].partition_broadcast(128), W=[b])
        xT = _sb(K, es, "b_xT", [128, KC, TH * 128], BF16)
        cbt = _sb(K, es, "b_cb", [128, TH, 32], F32)
        yacc = _sb(K, es, "b_y", [128, TH, D], F32)
        w1b = [_sb(K, es, "b_w1%d" % i, [128, KC, 256], BF16) for i in range(2)]
        w3b = [_sb(K, es, "b_w3%d" % i, [128, KC, 256], BF16) for i in range(2)]
        w2b = [_sb(K, es, "b_w2%d" % i, [128, 2, D], BF16) for i in range(2)]
        sil = [_sb(K, es, "b_sil%d" % i, [128, 512], F32) for i in range(2)]
        hid = [_sb(K, es, "b_hid%d" % i, [128, 2, 512], BF16) for i in range(2)]
        h1t = [_sb(K, es, "b_h1%d" % i, [128, D], F32) for i in range(1)]
        rr = [_sb(K, es, "b_r%d" % i, [128, D], F32) for i in range(1)]
        oo = [_sb(K, es, "b_o%d" % i, [128, D], F32) for i in range(2)]
        sm = {"st": _sb(K, es, "b_st", [128, 2, 6], F32), "mv": _sb(K, es, "b_mv", [128, 2], F32),
              "rstd": _sb(K, es, "b_rs", [128, 1], F32), "tmp": _sb(K, es, "b_tmp", [128, D], F32)}
        stgB = [_sb(K, es, "stgB%d" % i, [128, 2048], F32) for i in range(6)]
        ld = {"n": 0}

        def load_w(dst, src_ap, a_):
            k = ld["n"]
            ld["n"] += 1
            st = stgB[k % 6]
            v = st.h[:, 0:2048].rearrange("p (a b) -> p a b", a=a_)
            S.dma("sp" if k % 2 == 0 else "pool", v, src_ap, W=[st])
            if k % 3 == 2:
                S.op("pool", lambda: nc.gpsimd.tensor_copy(out=dst[:], in_=v), R=[st], W=[dst])
            else:
                S.op("act", lambda: nc.scalar.copy(out=dst[:], in_=v), R=[st], W=[dst])

        def load_expert(e):
            load_w(w1b[e % 2], K.d["w1"][l, e].rearrange("(k p) f -> p k f", p=128), KC)
            load_w(w3b[e % 2], K.d["w3"][l, e].rearrange("(k p) f -> p k f", p=128), KC)
            load_w(w2b[e % 2], K.d["w2"][l, e].rearrange("(k p) f -> p k f", p=128), 2)
        it = 0
        load_expert(0)
        for half in range(NP):
            t0 = half * TH
            nth = min(TH, NT - t0)
            if nth <= 0:
                break
            n_all = nth * 128
            S.dma("sp", xT[:, :, 0:n_all], K.d["h1T"][:, :, t0 * 128:t0 * 128 + n_all].rearrange("k p n -> p k n"), R=[K.sbuf_h1T], W=[xT])
            S.dma("sp", cbt[:, 0:nth, :], K.d["comb"][t0 * 128:t0 * 128 + n_all, :].rearrange("(t p) e -> p t e", p=128), R=[K.sbuf_comb], W=[cbt])
            S.op("pool", lambda: nc.gpsimd.memset(yacc[:, 0:nth, :], 0.0), W=[yacc])
            for e in range(32):
                wa, wc, wd = w1b[e % 2], w3b[e % 2], w2b[e % 2]
                if e + 1 < 32:
                    load_expert(e + 1)
                elif half + 1 < NP and (half + 1) * TH < NT:
                    load_expert(0)
                for gq in range((nth + 3) // 4):
                    nt = min(4, nth - 4 * gq)
                    n = nt * 128
                    c0 = gq * 512
                    hd = hid[it % 2]
                    it += 1
                    for f2 in range(2):
                        p1 = _ps(K)
                        for kc in range(KC):
                            S.op("pe", lambda: nc.tensor.matmul(p1[:, 0:n], lhsT=wa[:, kc, f2 * 128:(f2 + 1) * 128], rhs=xT[:, kc, c0:c0 + n],
                                                                start=(kc == 0), stop=(kc == KC - 1)), R=[wa, xT], W=[p1])
                        p3 = _ps(K)
                        for kc in range(KC):
                            S.op("pe", lambda: nc.tensor.matmul(p3[:, 0:n], lhsT=wc[:, kc, f2 * 128:(f2 + 1) * 128], rhs=xT[:, kc, c0:c0 + n],
                                                                start=(kc == 0), stop=(kc == KC - 1)), R=[wc, xT], W=[p3])
                        sl = sil[f2]
                        S.op("act", lambda: nc.scalar.activation(out=sl[:, 0:n], in_=p1[:, 0:n], func=AF.Silu), R=[p1], W=[sl])
                        S.op("dve", lambda: nc.vector.tensor_tensor(out=hd[:, f2, 0:n], in0=sl[:, 0:n], in1=p3[:, 0:n], op=ALU.mult), R=[sl, p3], W=[hd])
                    for t in range(nt):
                        tt = 4 * gq + t
                        for ch in range(2):
                            py = _ps(K)
                            for f2 in range(2):
                                S.op("pe", lambda: nc.tensor.matmul(py[:, :], lhsT=hd[:, f2, t * 128:(t + 1) * 128], rhs=wd[:, f2, ch * 512:(ch + 1) * 512],
                                                                    start=(f2 == 0), stop=(f2 == 1)), R=[hd, wd], W=[py])
                            S.op("dve", lambda: nc.vector.scalar_tensor_tensor(out=yacc[:, tt, ch * 512:(ch + 1) * 512], in0=py[:, :],
                                                                              scalar=cbt[:, tt, e:e + 1], in1=yacc[:, tt, ch * 512:(ch + 1) * 512],
                                                                              op0=ALU.mult, op1=ALU.add), R=[py, cbt, yacc], W=[yacc])
            for t in range(nth):
                tg = t0 + t
                rows = slice(128 * tg, 128 * (tg + 1))
                h1, r, o = h1t[0], rr[0], oo[t % 2]
                S.dma("sp", h1[:], K.d["h1"][rows, :], R=[K.h1buf[tg]], W=[h1])
                S.op("dve", lambda: nc.vector.scalar_tensor_tensor(out=r[:], in0=h1[:], scalar=ALPHA, in1=yacc[:, t, :], op0=ALU.mult, op1=ALU.add),
                     R=[h1, yacc], W=[r])
                _ln_tile(K, r, o, g, b, sm)
                if not last:
                    S.dma("sp", K.d["h"][rows, :], o[:], R=[o], W=[K.hbuf[tg]])
                else:
                    lo = 128 * tg - 64
                    r0, r1 = max(lo, 0), min(lo + 128, K.SEQ)
                    if r1 > r0:
                        S.dma("sp", K.d["out"][r0:r1, :], o[r0 - lo:r1 - lo, :], R=[o], W=[K.outbuf])


def make_consts():
    c = {}
    c["identf"] = np.eye(128, dtype=np.float32)
    c["identb"] = np.eye(128, dtype=np.float32).astype(ml_dtypes.bfloat16)
    s = np.arange(128)[:, None]
    masks = np.zeros((6, 128, 512), np.float32)
    for rel in range(4):
        for qt in range(4):
            blk = masks[rel, :, qt * 128:(qt + 1) * 128]
            if qt < rel:
                blk[:] = NEG
            elif qt == rel:
                blk[:] = np.where(s < np.arange(128)[None, :], 0.0, NEG)
    masks[4] = masks[0]
    masks[4, 0:48, :] = NEG
    masks[5, 0:48, :] = NEG
    c["masks"] = np.ascontiguousarray(masks.transpose(1, 0, 2)).astype(ml_dtypes.bfloat16)
    c["negtri"] = np.where(s >= np.arange(128)[None, :], -1.0, 0.0).astype(ml_dtypes.bfloat16)
    c["onesb"] = np.ones((128, 1), np.float32).astype(ml_dtypes.bfloat16)
    c["onesf"] = np.ones((128, 128), np.float32)
    m = np.arange(64)[:, None]
    i = np.arange(64)[None, :]
    triu = (m <= i).astype(np.float32)
    c["triu"] = np.ascontiguousarray(np.repeat(triu[:, None, :], 4, axis=1))
    c["sgt"] = (m > i).astype(np.float32)
    c["ones64"] = np.ones((64, 128), np.float32)
    c["ones4"] = np.ones((64, 4, 128), np.float32)
    c["trilsn"] = np.ascontiguousarray(np.repeat((-(m > i).astype(np.float32))[:, None, :], 4, axis=1))
    c["ident4"] = np.ascontiguousarray(np.repeat(np.eye(64, dtype=np.float32)[:, None, :], 4, axis=1))
    c["cvec"] = np.tile(np.array([[1.0, LN_EPS, RMS_EPS, 0.0, 64.0 * RMS_EPS, 128.0 * RMS_EPS]], np.float32), (128, 1))
    return c


CONST_DT = {"identb": BF16, "masks": BF16, "negtri": BF16, "onesb": BF16}

IN_SHAPES = lambda SEQ, depth: {
    "x": [SEQ, D], "meta": [16, D], "ln_in_g": [D], "ln_in_b": [D], "w_in": [depth, D, IN_W], "conv_wT": [depth, 1536, 4],
    "a_log": [depth, 4], "dt_bias": [depth, 4], "sb_norm_g": [depth, 64], "gdn_norm_g": [depth, 128], "w_out": [depth, D, D],
    "ln1_g": [depth, D], "ln1_b": [depth, D], "w_group": [depth, D, 4], "b_group": [depth, 4], "w_expert": [depth, D, 32],
    "b_expert": [depth, 32], "w1": [depth, 32, D, 256], "w3": [depth, 32, D, 256], "w2": [depth, 32, 256, D],
    "ln2_g": [depth, D], "ln2_b": [depth, D]}


def build(SEQ, depth, debug=False, same_eng=True, phases=None, max_ops=None, log=None):
    nc = bass.Bass("TRN2", target_bir_lowering=False)
    K = Ctx()
    K.nc = nc
    K.SEQ = SEQ
    PT = SEQ + 64
    K.NT = NT = (PT + 127) // 128
    K.NC = PT // 64
    P = NT * 128
    K.d = {}
    for name, shp in IN_SHAPES(SEQ, depth).items():
        K.d[name] = nc.dram_tensor(name, shp, F32, kind="ExternalInput").ap()
    consts = make_consts()
    for name, arr in consts.items():
        K.d["c_" + name] = nc.dram_tensor("c_" + name, list(arr.shape), CONST_DT.get(name, F32), kind="ExternalInput").ap()
    K.d["out"] = nc.dram_tensor("out", [SEQ, D], F32, kind="ExternalOutput").ap()
    kind = "ExternalOutput" if debug else "Internal"
    for name, shp, dt in (("h", [P, D], F32), ("qT", [4, 128, P], BF16), ("kT", [4, 128, P], BF16), ("V", [P, 512], BF16),
                          ("gT", [12, 128, P], F32), ("zs", [P, 512], F32), ("bg", [P, 8], F32), ("mixed", [P, D], BF16),
                          ("h1", [P, D], F32), ("h1T", [KC, 128, P], BF16), ("comb", [P, 32], F32)):
        K.d[name] = nc.dram_tensor("s_" + name, shp, dt, kind=kind).ap()
    K.hbuf = [Buf() for _ in range(NT)]
    K.h1buf = [Buf() for _ in range(NT)]
    for nm in ("sbuf_q", "sbuf_k", "sbuf_v", "sbuf_g", "sbuf_z", "sbuf_bg", "sbuf_mx_sb", "sbuf_mx_gd", "sbuf_h1T", "sbuf_comb", "outbuf"):
        setattr(K, nm, Buf())
    with contextlib.ExitStack() as es:
        K.S = S = Sched(nc, es, same_eng=same_eng)
        S.max_ops = max_ops
        S.log = log
        K.psum = [Tl(es.enter_context(nc.psum_tensor("ps%d" % i, [128, 512], F32)), "ps%d" % i) for i in range(8)]
        K.psi = 0
        for p_ in K.psum:
            p_.b.excl = True
        K.stgi = 0
        for name, arr in consts.items():
            if name == "cvec":
                continue
            tl = _sb(K, es, "k_" + name, list(arr.shape), CONST_DT.get(name, F32))
            setattr(K, name, tl)
            S.dma("sp", tl[:], K.d["c_" + name], W=[tl])
        cv = _sb(K, es, "k_cvec", [128, 6], F32)
        S.dma("sp", cv[:], K.d["c_cvec"], W=[cv])
        K.one_c = Tl(cv.h[:, 0:1]); K.one_c.b = cv.b
        K.eps_ln = Tl(cv.h[:, 1:2]); K.eps_ln.b = cv.b
        K.eps_rms = Tl(cv.h[:, 2:3]); K.eps_rms.b = cv.b
        K.eps_rms64 = Tl(cv.h[:, 4:5]); K.eps_rms64.b = cv.b
        K.eps_rms128 = Tl(cv.h[:, 5:6]); K.eps_rms128.b = cv.b
        ph = phases or ("in", "A1", "SB", "GDN", "A3", "B")
        if "in" in ph:
            phase_input(K)
            S.barrier()
        for l in range(depth):
            if "A1" in ph:
                phase_A1(K, l)
                S.barrier()
            if INTERLEAVE_GDN:
                if "SB" in ph or "GDN" in ph:
                    phase_SBGDN(K, l, run_sb=("SB" in ph), run_gdn=("GDN" in ph))
                    S.barrier()
            else:
                if "SB" in ph:
                    phase_SBGDN(K, l, run_sb=True, run_gdn=False)
                    S.barrier()
                if "GDN" in ph:
                    phase_GDN_old(K, l)
                    S.barrier()
            if "A3" in ph:
                phase_A3(K, l)
                S.barrier()
            if "B" in ph:
                phase_B(K, l, l == depth - 1)
                S.barrier()
        S.finish()
    K.consts = consts
    return nc, K


def make_in_maps(inputs, SEQ, depth, consts, n_cores=8):
    shared = {}
    for k in ("ln_in_g", "ln_in_b", "w_in", "a_log", "dt_bias", "sb_norm_g", "gdn_norm_g", "w_out", "ln1_g", "ln1_b",
              "w_group", "b_group", "w_expert", "b_expert", "w1", "w3", "w2", "ln2_g", "ln2_b"):
        shared[k] = np.ascontiguousarray(np.asarray(inputs[k], dtype=np.float32))
    shared["meta"] = np.ascontiguousarray(np.asarray(inputs["meta_tokens"], dtype=np.float32))
    shared["conv_wT"] = np.ascontiguousarray(np.asarray(inputs["conv_w"], dtype=np.float32).transpose(0, 2, 1))
    for k, v in consts.items():
        shared["c_" + k] = v
    x = np.asarray(inputs["x"], dtype=np.float32)
    B = x.shape[0]
    maps = []
    for c in range(n_cores):
        m = dict(shared)
        m["x"] = np.ascontiguousarray(x[c % B])
        maps.append(m)
    return maps


def kernel(**inputs):
    x = np.asarray(inputs["x"])
    B, SEQ, _ = x.shape
    depth = np.asarray(inputs["w_in"]).shape[0]
    nc, K = build(SEQ, depth)
    maps = make_in_maps(inputs, SEQ, depth, K.consts)
    res = run_bass_kernel_spmd(nc, maps, core_ids=list(range(8)))
    out = np.stack([np.asarray(res.results[b]["out"], dtype=np.float32) for b in range(B)], axis=0)
    return out
```

```python
import contextlib
import numpy as np
import ml_dtypes
import concourse.bass as bass
import concourse.mybir as mybir
from concourse.bass_utils import run_bass_kernel_spmd

F32 = mybir.dt.float32
BF16 = mybir.dt.bfloat16
AF = mybir.ActivationFunctionType
ALU = mybir.AluOpType
AX = mybir.AxisListType

D = 1024
KC = 8
DEPTH = 4
IN_W = 3592
ALPHA = float((2 * DEPTH) ** 0.25)
LN_EPS = 1e-5
RMS_EPS = 1e-6
NEG = -30000.0
INTERLEAVE_GDN = True
DMA_PAD = 3
GDN_YK = 1


class Ev:
    __slots__ = ("key", "val", "snap", "src")

    def __init__(self, key, val, snap, src):
        self.key, self.val, self.snap, self.src = key, val, snap, src


class Buf:
    __slots__ = ("w", "r", "name", "excl")

    def __init__(self, name=""):
        self.w = None
        self.r = {}
        self.name = name
        self.excl = False


class Tl:
    def __init__(self, h, name=""):
        self.h = h
        self.b = Buf(name)

    def __getitem__(self, idx):
        return self.h[idx]


ENG = ("pe", "act", "dve", "pool", "sp")


class Sched:
    def __init__(self, nc, es, ndma=8, same_eng=True):
        self.nc = nc
        self.e = {"pe": nc.tensor, "act": nc.scalar, "dve": nc.vector, "pool": nc.gpsimd, "sp": nc.sync}
        self.sem = {k: es.enter_context(nc.semaphore("sem_" + k)) for k in ENG}
        self.cnt = {k: 0 for k in ENG}
        self.seen = {k: {} for k in ENG}
        self.dq = {}
        self.semobj = {"c:" + k: self.sem[k] for k in ENG}
        for q in ("sp", "pool"):
            sems = [es.enter_context(nc.semaphore("dma_%s_%d" % (q, i))) for i in range(ndma)]
            self.dq[q] = {"sems": sems, "n": 0, "pending": [None] * ndma}
            for i, sm in enumerate(sems):
                self.semobj["d:%s:%d" % (q, i)] = sm
        self.same_eng = same_eng
        self.nwait = 0
        self.max_ops = None
        self.log = None
        self.nins = 0
        self.rr = 0

    def _wait(self, eng, ev):
        if ev is None:
            return
        seen = self.seen[eng]
        if seen.get(ev.key, 0) >= ev.val:
            return
        if ev.src == eng and (eng == "pe" or not self.same_eng):
            return
        self.e[eng].wait_ge(self.semobj[ev.key], ev.val)
        if self.log is not None:
            self.log.append((self.nins, eng, "WAIT %s >= %d" % (ev.key, ev.val)))
        self.nwait += 1
        new = dict(seen)
        for k, v in ev.snap.items():
            if new.get(k, 0) < v:
                new[k] = v
        if new.get(ev.key, 0) < ev.val:
            new[ev.key] = ev.val
        self.seen[eng] = new

    def _deps(self, eng, R, W):
        for b in R:
            self._wait(eng, b.w)
            if b.excl:
                for k, ev in list(b.r.items()):
                    if k != eng:
                        self._wait(eng, ev)
        for b in W:
            self._wait(eng, b.w)
            for ev in list(b.r.values()):
                self._wait(eng, ev)

    def op(self, eng, fn, R=(), W=()):
        if self.max_ops is not None and self.nins >= self.max_ops:
            return None
        R = [getattr(x, "b", x) for x in R]
        W = [getattr(x, "b", x) for x in W]
        self._deps(eng, R, W)
        ins = fn()
        if self.log is not None:
            self.log.append((self.nins, eng, str(ins)[:150]))
        self.cnt[eng] += 1
        self.nins += 1
        ins.then_inc(self.sem[eng], 1)
        ev = Ev("c:" + eng, self.cnt[eng], self.seen[eng], eng)
        for b in R:
            b.r[eng] = ev
        for b in W:
            b.w = ev
            b.r = {}
        return ev

    def dma(self, q, out, in_, R=(), W=(), **kw):
        if self.max_ops is not None and self.nins >= self.max_ops:
            return None
        R = [getattr(x, "b", x) for x in R]
        W = [getattr(x, "b", x) for x in W]
        dq = self.dq[q]
        ns = len(dq["sems"])
        i = dq["n"] % ns
        self._wait(q, dq["pending"][i])
        self._deps(q, R, W)
        ins = self.e[q].dma_start(out=out, in_=in_, **kw)
        val = 16 * (dq["n"] // ns + 1)
        ins.then_inc(dq["sems"][i], 16)
        ev = Ev("d:%s:%d" % (q, i), val, self.seen[q], "dma")
        dq["pending"][i] = ev
        dq["n"] += 1
        self.nins += 1
        for b in R:
            b.r[("dma", q, dq["n"])] = ev
        for b in W:
            b.w = ev
            b.r = {}
        return ev

    def barrier(self):
        evs = [Ev("c:" + k, self.cnt[k], {}, "x") for k in ENG if self.cnt[k] > 0]
        for dq in self.dq.values():
            evs += [p for p in dq["pending"] if p is not None]
        for eng in ENG:
            for ev in evs:
                self._wait(eng, ev)

    def finish(self):
        for dq in self.dq.values():
            for p in dq["pending"]:
                self._wait("sp", p)

    def alt(self):
        self.rr += 1
        return "act" if (self.rr & 1) else "dve"


class Ctx:
    pass


def _sb(K, es, name, shape, dt):
    K.uid = getattr(K, "uid", 0) + 1
    name = "%s_u%d" % (name, K.uid)
    return Tl(es.enter_context(K.nc.sbuf_tensor(name, list(shape), dt)), name)


def _evac(K, eng, out, in_, R, W, scale=None):
    nc, S = K.nc, K.S
    if eng == "act":
        if scale is None:
            S.op("act", lambda: nc.scalar.copy(out=out, in_=in_), R=R, W=W)
        else:
            S.op("act", lambda: nc.scalar.mul(out=out, in_=in_, mul=scale), R=R, W=W)
    else:
        if scale is None:
            S.op("dve", lambda: nc.vector.tensor_copy(out=out, in_=in_), R=R, W=W)
        else:
            S.op("dve", lambda: nc.vector.tensor_scalar_mul(out=out, in0=in_, scalar1=scale), R=R, W=W)


def _ps(K):
    K.psi = (K.psi + 1) % len(K.psum)
    return K.psum[K.psi]


def _ln_tile(K, x, out, g, b, sm):
    nc, S = K.nc, K.S
    st, mv, rstd, tmp = sm["st"], sm["mv"], sm["rstd"], sm["tmp"]
    S.op("dve", lambda: nc.vector.bn_stats(out=st[:, 0, :], in_=x[:, 0:512]), R=[x], W=[st])
    S.op("dve", lambda: nc.vector.bn_stats(out=st[:, 1, :], in_=x[:, 512:1024]), R=[x, st], W=[st])
    S.op("dve", lambda: nc.vector.bn_aggr(out=mv[:], in_=st[:].rearrange("p a b -> p (a b)")), R=[st], W=[mv])
    S.op("act", lambda: nc.scalar.activation(out=rstd[:], in_=mv[:, 1:2], func=AF.Sqrt, bias=K.eps_ln[:, 0:1], scale=1.0),
         R=[mv, K.eps_ln], W=[rstd])
    S.op("dve", lambda: nc.vector.reciprocal(out=rstd[:], in_=rstd[:]), R=[rstd], W=[rstd])
    S.op("dve", lambda: nc.vector.tensor_scalar(out=tmp[:], in0=x[:], scalar1=mv[:, 0:1], scalar2=rstd[:, 0:1],
                                                op0=ALU.subtract, op1=ALU.mult), R=[x, mv, rstd], W=[tmp])
    S.op("dve", lambda: nc.vector.tensor_tensor(out=tmp[:], in0=tmp[:], in1=g[:], op=ALU.mult), R=[tmp, g], W=[tmp])
    S.op("dve", lambda: nc.vector.tensor_tensor(out=out[:], in0=tmp[:], in1=b[:], op=ALU.add), R=[tmp, b], W=[out])


def _load_cast(K, dst_tl, dst_ap, src_ap, shape, Wb):
    nc, S = K.nc, K.S
    K.stgi = (K.stgi + 1) % len(K.stg)
    st = K.stg[K.stgi]
    n = int(np.prod(shape[1:]))
    if len(shape) == 3:
        v = st.h[:, 0:n].rearrange("p (a b) -> p a b", a=shape[1])
    else:
        v = st.h[:, 0:n]
    S.dma("sp", v, src_ap, W=[st])
    S.op("pool", lambda: nc.gpsimd.tensor_copy(out=dst_ap, in_=v), R=[st], W=[Wb])


def phase_input(K):
    nc, S, NT = K.nc, K.S, K.NT
    with contextlib.ExitStack() as es:
        g = _sb(K, es, "pi_g", [128, D], F32)
        b = _sb(K, es, "pi_b", [128, D], F32)
        S.dma("sp", g[:], K.d["ln_in_g"].partition_broadcast(128), W=[g])
        S.dma("sp", b[:], K.d["ln_in_b"].partition_broadcast(128), W=[b])
        xs = [_sb(K, es, "pi_x%d" % i, [128, D], F32) for i in range(2)]
        os_ = [_sb(K, es, "pi_o%d" % i, [128, D], F32) for i in range(2)]
        sm = {"st": _sb(K, es, "pi_st", [128, 2, 6], F32), "mv": _sb(K, es, "pi_mv", [128, 2], F32),
              "rstd": _sb(K, es, "pi_rs", [128, 1], F32), "tmp": _sb(K, es, "pi_tmp", [128, D], F32)}
        PT = K.SEQ + 64
        if NT * 128 > PT:
            zb = _sb(K, es, "pi_zb", [128, 512], BF16)
            S.op("pool", lambda: nc.gpsimd.memset(zb[:], 0.0), W=[zb])
            S.dma("sp", K.d["mixed"][PT:NT * 128, 512:1024], zb[0:NT * 128 - PT, :], R=[zb], W=[K.sbuf_mx_gd])
        for t in range(NT):
            x, o = xs[t % 2], os_[t % 2]
            lo = 128 * t - 64
            r0, r1 = max(lo, 0), min(lo + 128, K.SEQ)
            if t == 0 or r1 - lo < 128:
                S.op("pool", lambda: nc.gpsimd.memset(x[:], 0.0), W=[x])
            if t == 0:
                S.dma("sp", x[48:64, :], K.d["meta"][:, :], W=[x])
            if r1 > r0:
                S.dma("sp", x[r0 - lo:r1 - lo, :], K.d["x"][r0:r1, :], W=[x])
            _ln_tile(K, x, o, g, b, sm)
            S.dma("sp", K.d["h"][128 * t:128 * (t + 1), :], o[:], R=[o], W=[K.hbuf[t]])


def phase_A1(K, l):
    nc, S, NT = K.nc, K.S, K.NT
    with contextlib.ExitStack() as es:
        K.stg = [_sb(K, es, "stg%d" % i, [128, IN_W], F32) for i in range(2)]
        Wb = _sb(K, es, "a1_W", [128, KC, IN_W], BF16)
        wbufs = [Buf() for _ in range(KC)]
        wsrc = K.d["w_in"][l].rearrange("(k p) n -> p k n", p=128)
        for kc in range(KC):
            _load_cast(K, Wb, Wb[:, kc, :], wsrc[:, kc, :], [128, IN_W], wbufs[kc])
        dtb = _sb(K, es, "a1_dtb", [128, 4], F32)
        nea = _sb(K, es, "a1_nea", [128, 4], F32)
        S.dma("sp", dtb[:], K.d["dt_bias"][l].partition_broadcast(128), W=[dtb])
        S.dma("sp", nea[:], K.d["a_log"][l].partition_broadcast(128), W=[nea])
        S.op("act", lambda: nc.scalar.activation(out=nea[:], in_=nea[:], func=AF.Exp), R=[nea], W=[nea])
        S.op("dve", lambda: nc.vector.tensor_scalar_mul(out=nea[:], in0=nea[:], scalar1=-1.0), R=[nea], W=[nea])
        hts = [_sb(K, es, "a1_h%d" % i, [128, 4, D], F32) for i in range(1)]
        hTs = [_sb(K, es, "a1_hT%d" % i, [128, KC, 512], BF16) for i in range(2)]
        stq = [_sb(K, es, "a1_sq%d" % i, [128, 512], BF16) for i in range(3)]
        stg = [_sb(K, es, "a1_sg%d" % i, [128, 512], F32) for i in range(3)]
        stv = [_sb(K, es, "a1_sv%d" % i, [128, 4, 512], BF16) for i in range(2)]
        stz = [_sb(K, es, "a1_sz%d" % i, [128, 4, 512], F32) for i in range(2)]
        stb = [_sb(K, es, "a1_sb%d" % i, [128, 4, 8], F32) for i in range(2)]
        tb = _sb(K, es, "a1_tb", [128, 4, 4], F32)
        NG = (NT + 3) // 4
        for g in range(NG):
            nt = min(4, NT - 4 * g)
            n = nt * 128
            c0 = g * 512
            ht, hT = hts[0], hTs[g % 2]
            sv, sz, sbb = stv[g % 2], stz[g % 2], stb[g % 2]
            S.dma("sp", ht[:, 0:nt, :], K.d["h"][c0:c0 + n, :].rearrange("(t p) d -> p t d", p=128),
                  R=K.hbuf[4 * g:4 * g + nt], W=[ht])
            for kc in range(KC):
                ps = _ps(K)
                for t in range(nt):
                    S.op("pe", lambda: nc.tensor.transpose(out=ps[:, t * 128:(t + 1) * 128],
                                                           in_=ht[:, t, kc * 128:(kc + 1) * 128], identity=K.identf[:]),
                         R=[ht, K.identf], W=[ps])
                _evac(K, S.alt(), hT[:, kc, 0:n], ps[:, 0:n], [ps], [hT])
            for ci in range(20):
                if ci < 8:
                    col = ci * 128
                else:
                    col = 1536 + (ci - 8) * 128
                ps = _ps(K)
                for kc in range(KC):
                    S.op("pe", lambda: nc.tensor.matmul(ps[:, 0:n], lhsT=Wb[:, kc, col:col + 128], rhs=hT[:, kc, 0:n],
                                                        start=(kc == 0), stop=(kc == KC - 1)),
                         R=[wbufs[kc], hT], W=[ps])
                if ci < 8:
                    sq = stq[ci % 3]
                    _evac(K, S.alt(), sq[:, 0:n], ps[:, 0:n], [ps], [sq], scale=(0.125 if ci < 4 else None))
                    if ci < 4:
                        S.dma("sp", K.d["qT"][ci, :, c0:c0 + n], sq[:, 0:n], R=[sq], W=[K.sbuf_q])
                    else:
                        S.dma("sp", K.d["kT"][ci - 4, :, c0:c0 + n], sq[:, 0:n], R=[sq], W=[K.sbuf_k])
                else:
                    sg = stg[ci % 3]
                    _evac(K, S.alt(), sg[:, 0:n], ps[:, 0:n], [ps], [sg])
                    S.dma("sp", K.d["gT"][ci - 8, :, c0:c0 + n], sg[:, 0:n], R=[sg], W=[K.sbuf_g])
            for t in range(nt):
                ps = _ps(K)
                for kc in range(KC):
                    S.op("pe", lambda: nc.tensor.matmul(ps[:, :], lhsT=hT[:, kc, t * 128:(t + 1) * 128], rhs=Wb[:, kc, 1024:1536],
                                                        start=(kc == 0), stop=(kc == KC - 1)), R=[wbufs[kc], hT], W=[ps])
                _evac(K, S.alt(), sv[:, t, :], ps[:, :], [ps], [sv])
                ps = _ps(K)
                for kc in range(KC):
                    S.op("pe", lambda: nc.tensor.matmul(ps[:, :], lhsT=hT[:, kc, t * 128:(t + 1) * 128], rhs=Wb[:, kc, 3072:3584],
                                                        start=(kc == 0), stop=(kc == KC - 1)), R=[wbufs[kc], hT], W=[ps])
                S.op("act", lambda: nc.scalar.activation(out=sz[:, t, :], in_=ps[:, :], func=AF.Silu), R=[ps], W=[sz])
                ps = _ps(K)
                for kc in range(KC):
                    S.op("pe", lambda: nc.tensor.matmul(ps[:, 0:8], lhsT=hT[:, kc, t * 128:(t + 1) * 128], rhs=Wb[:, kc, 3584:3592],
                                                        start=(kc == 0), stop=(kc == KC - 1)), R=[wbufs[kc], hT], W=[ps])
                S.op("act", lambda: nc.scalar.activation(out=sbb[:, t, 0:4], in_=ps[:, 0:4], func=AF.Sigmoid), R=[ps], W=[sbb])
                S.op("dve", lambda: nc.vector.tensor_tensor(out=tb[:, t, :], in0=ps[:, 4:8], in1=dtb[:], op=ALU.add),
                     R=[ps, dtb], W=[tb])
            S.op("act", lambda: nc.scalar.activation(out=tb[:, 0:nt, :], in_=tb[:, 0:nt, :], func=AF.Exp), R=[tb], W=[tb])
            S.op("act", lambda: nc.scalar.activation(out=tb[:, 0:nt, :], in_=tb[:, 0:nt, :], func=AF.Ln, bias=K.one_c[:, 0:1], scale=1.0),
                 R=[tb, K.one_c], W=[tb])
            S.op("dve", lambda: nc.vector.tensor_tensor(out=sbb[:, 0:nt, 4:8], in0=tb[:, 0:nt, :],
                                                        in1=nea[:].unsqueeze(1).to_broadcast([128, nt, 4]), op=ALU.mult),
                 R=[tb, nea], W=[sbb])
            S.dma("sp", K.d["V"][c0:c0 + n, :].rearrange("(t p) c -> p t c", p=128), sv[:, 0:nt, :], R=[sv], W=[K.sbuf_v])
            S.dma("sp", K.d["zs"][c0:c0 + n, :].rearrange("(t p) c -> p t c", p=128), sz[:, 0:nt, :], R=[sz], W=[K.sbuf_z])
            S.dma("sp", K.d["bg"][c0:c0 + n, :].rearrange("(t p) c -> p t c", p=128), sbb[:, 0:nt, :], R=[sbb], W=[K.sbuf_bg])


class RR:
    def __init__(self):
        self.items = []

    def add(self, gen, w=1):
        self.items.append([gen, w])

    def run(self):
        while self.items:
            for item in list(self.items):
                for _ in range(item[1]):
                    try:
                        next(item[0])
                    except StopIteration:
                        self.items.remove(item)
                        break


def _psg(K):
    K.psgi = (K.psgi + 1) % len(K.psum_gd)
    return K.psum_gd[K.psgi]


def sb_stream(K, l, es):
    nc, S, NT = K.nc, K.S, K.NT
    P = NT * 128
    qT = _sb(K, es, "sb_q", [128, P], BF16)
    kT = _sb(K, es, "sb_k", [128, P], BF16)
    Vt = _sb(K, es, "sb_v", [128, NT, 128], BF16)
    sbg = _sb(K, es, "sb_g", [128, 64], F32)
    S.dma("sp", sbg[:], K.d["sb_norm_g"][l].partition_broadcast(128), W=[sbg])
    es_ = [_sb(K, es, "sb_e%d" % i, [128, 512], F32) for i in range(2)]
    sps = [_sb(K, es, "sb_sp%d" % i, [128, 512], BF16) for i in range(4)]
    ws = [_sb(K, es, "sb_w%d" % i, [128, 512], BF16) for i in range(3)]
    oaccs = [_sb(K, es, "sb_oa%d" % i, [128, 4, 64], F32) for i in range(2)]
    raccs = [_sb(K, es, "sb_ra%d" % i, [128, 4], F32) for i in range(2)]
    eRs = [_sb(K, es, "sb_eR%d" % i, [128, 4], F32) for i in range(2)]
    tmps = [_sb(K, es, "sb_tmp%d" % i, [128, 4, 64], F32) for i in range(2)]
    osbs = [_sb(K, es, "sb_os%d" % i, [128, 4, 128], BF16) for i in range(2)]
    ss = _sb(K, es, "sb_ss", [128, 4], F32)
    zb = K.psum[0:3]
    pb = K.psum[3:5]
    NG = (NT + 3) // 4
    gi = 0
    for hp in range(4):
        S.dma("sp", qT[:, :], K.d["qT"][hp], R=[K.sbuf_q], W=[qT])
        S.dma("sp", kT[:, :], K.d["kT"][hp], R=[K.sbuf_k], W=[kT])
        S.dma("sp", Vt[:, :, :], K.d["V"][:, hp * 128:(hp + 1) * 128].rearrange("(t p) c -> p t c", p=128), R=[K.sbuf_v], W=[Vt])
        its = []
        for qg in range(NG):
            nt = min(4, NT - 4 * qg)
            for h2 in range(2):
                kbs = list(range(4 * qg + nt - 1, -1, -1))
                for j, kb in enumerate(kbs):
                    its.append((qg, nt, h2, kb, j == 0, j == len(kbs) - 1, gi))
                gi += 1
        N = len(its)

        def geom(it):
            qg, nt, h2, kb = it[0], it[1], it[2], it[3]
            rel = kb - 4 * qg
            if rel >= 0:
                mi = 4 if kb == 0 else rel
            elif kb == 0:
                mi = 5
            else:
                mi = None
            lo = max(rel, 0)
            return rel, mi, lo, lo * 128, nt * 128, qg * 512

        def stA(i):
            it = its[i]
            qg, nt, h2, kb = it[0], it[1], it[2], it[3]
            rel, mi, lo, c0, n, q0 = geom(it)
            z, e, sp = zb[i % 3], es_[i % 2], sps[i % 4]
            r0 = h2 * 64
            kk = kT[r0:r0 + 64, kb * 128:(kb + 1) * 128]
            qq = qT[r0:r0 + 64, q0 + c0:q0 + n]
            S.op("pe", lambda: nc.tensor.matmul(z[:, c0:n], lhsT=kk, rhs=qq, start=True, stop=(mi is None)), R=[kT, qT], W=[z])
            if mi is not None:
                S.op("pe", lambda: nc.tensor.matmul(z[:, c0:n], lhsT=K.identb[:], rhs=K.masks[:, mi, c0:n], start=False, stop=True),
                     R=[K.identb, K.masks], W=[z])
            S.op("act", lambda: nc.scalar.activation(out=e[:, c0:n], in_=z[:, c0:n], func=AF.Exp), R=[z], W=[e])
            S.op("act", lambda: nc.scalar.activation(out=sp[:, c0:n], in_=e[:, c0:n], func=AF.Ln, bias=K.one_c[:, 0:1], scale=1.0),
                 R=[e, K.one_c], W=[sp])

        def stB(i):
            it = its[i]
            qg, nt, h2, kb = it[0], it[1], it[2], it[3]
            rel, mi, lo, c0, n, q0 = geom(it)
            z, sp, w = zb[i % 3], sps[i % 4], ws[i % 3]
            r0 = h2 * 64
            kk = kT[r0:r0 + 64, kb * 128:(kb + 1) * 128]
            qq = qT[r0:r0 + 64, q0 + c0:q0 + n]
            S.op("pe", lambda: nc.tensor.matmul(z[:, c0:n], lhsT=kk, rhs=qq, start=True, stop=False), R=[kT, qT], W=[z])
            if mi is not None:
                S.op("pe", lambda: nc.tensor.matmul(z[:, c0:n], lhsT=K.identb[:], rhs=K.masks[:, mi, c0:n], start=False, stop=False),
                     R=[K.identb, K.masks], W=[z])
            S.op("pe", lambda: nc.tensor.matmul(z[:, c0:n], lhsT=K.negtri[:], rhs=sp[:, c0:n], start=False, stop=True),
                 R=[K.negtri, sp], W=[z])
            S.op("act", lambda: nc.scalar.activation(out=w[:, c0:n], in_=z[:, c0:n], func=AF.Exp), R=[z], W=[w])

        def stC(i):
            it = its[i]
            qg, nt, h2, kb, first, last, g_ = it
            rel, mi, lo, c0, n, q0 = geom(it)
            sp, w = sps[i % 4], ws[i % 3]
            oacc, racc = oaccs[g_ % 2], raccs[g_ % 2]
            eR, tmp = eRs[i % 2], tmps[i % 2]
            osb = osbs[qg % 2]
            if first:
                S.op("pool", lambda: nc.gpsimd.memset(oacc[:], 0.0), W=[oacc])
                S.op("pool", lambda: nc.gpsimd.memset(racc[:], 0.0), W=[racc])
            po = pb[i % 2]
            pov = po.h[:, 0:260].rearrange("p (t c) -> p t c", c=65)
            for qt in range(lo, nt):
                S.op("pe", lambda: nc.tensor.matmul(pov[:, qt, 0:64], lhsT=w[:, qt * 128:(qt + 1) * 128],
                                                    rhs=Vt[:, kb, h2 * 64:(h2 + 1) * 64], start=True, stop=True),
                     R=[w, Vt], W=[po])
                S.op("pe", lambda: nc.tensor.matmul(pov[:, qt, 64:65], lhsT=sp[:, qt * 128:(qt + 1) * 128],
                                                    rhs=K.onesb[:, 0:1], start=True, stop=True),
                     R=[sp, K.onesb], W=[po])
            S.op("act", lambda: nc.scalar.activation(out=eR[:, lo:nt], in_=racc[:, lo:nt], func=AF.Exp, scale=-1.0),
                 R=[racc], W=[eR])
            S.op("dve", lambda: nc.vector.tensor_tensor(out=tmp[:, lo:nt, :], in0=pov[:, lo:nt, 0:64],
                                                        in1=eR[:, lo:nt].unsqueeze(2).to_broadcast([128, nt - lo, 64]), op=ALU.mult),
                 R=[po, eR], W=[tmp])
            S.op("pool", lambda: nc.gpsimd.tensor_tensor(out=oacc[:, lo:nt, :], in0=oacc[:, lo:nt, :], in1=tmp[:, lo:nt, :], op=ALU.add),
                 R=[oacc, tmp], W=[oacc])
            S.op("dve", lambda: nc.vector.tensor_tensor(out=racc[:, lo:nt], in0=racc[:, lo:nt], in1=pov[:, lo:nt, 64], op=ALU.add),
                 R=[racc, po], W=[racc])
            if last:
                tmp2 = tmps[(i + 1) % 2]
                S.op("dve", lambda: nc.vector.tensor_tensor(out=tmp2[:, 0:nt, :], in0=oacc[:, 0:nt, :], in1=oacc[:, 0:nt, :], op=ALU.mult),
                     R=[oacc], W=[tmp2])
                S.op("dve", lambda: nc.vector.tensor_reduce(out=ss[:, 0:nt], in_=tmp2[:, 0:nt, :], axis=AX.X, op=ALU.add), R=[tmp2], W=[ss])
                S.op("act", lambda: nc.scalar.activation(out=ss[:, 0:nt], in_=ss[:, 0:nt], func=AF.Ln, bias=K.eps_rms64[:, 0:1], scale=1.0),
                     R=[ss, K.eps_rms64], W=[ss])
                S.op("act", lambda: nc.scalar.activation(out=ss[:, 0:nt], in_=ss[:, 0:nt], func=AF.Exp, scale=-0.5), R=[ss], W=[ss])
                S.op("dve", lambda: nc.vector.scalar_tensor_tensor(out=tmp2[:, 0:nt, :], in0=oacc[:, 0:nt, :], scalar=8.0,
                                                                  in1=ss[:, 0:nt].unsqueeze(2).to_broadcast([128, nt, 64]),
                                                                  op0=ALU.mult, op1=ALU.mult),
                     R=[oacc, ss], W=[tmp2])
                S.op("dve", lambda: nc.vector.tensor_tensor(out=osb[:, 0:nt, h2 * 64:(h2 + 1) * 64], in0=tmp2[:, 0:nt, :],
                                                            in1=sbg[:].unsqueeze(1).to_broadcast([128, nt, 64]), op=ALU.mult),
                     R=[tmp2, sbg], W=[osb])
                if h2 == 1:
                    S.dma("sp", K.d["mixed"][q0:q0 + n, hp * 128:(hp + 1) * 128].rearrange("(t p) c -> p t c", p=128), osb[:, 0:nt, :],
                          R=[osb], W=[K.sbuf_mx_sb])

        for s in range(N + 2):
            if s < N:
                stA(s)
            if 0 <= s - 1 < N:
                stB(s - 1)
            if 0 <= s - 2 < N:
                stC(s - 2)
            yield


def gdn_master(K, l, es, rr, NSETS, YK):
    nc, S, NT = K.nc, K.S, K.NT
    NC_ = K.NC
    cw = _sb(K, es, "gd_cw", [128, 12, 4], F32)
    S.dma("sp", cw[:], K.d["conv_wT"][l].rearrange("(i d) t -> d i t", d=128), W=[cw])
    gng = _sb(K, es, "gd_gng", [64, 4, 128], F32)
    for h in range(4):
        S.dma("sp", gng[:, h, :], K.d["gdn_norm_g"][l].partition_broadcast(64), W=[gng])
    xin = [_sb(K, es, "gd_x%d" % i, [128, 12, 131], F32) for i in range(2)]
    cvs = [_sb(K, es, "gd_cv%d" % i, [128, 12, 128], F32) for i in range(2)]
    sq = [_sb(K, es, "gd_sq%d" % i, [128, 128], F32) for i in range(2)]
    rn = [_sb(K, es, "gd_rn%d" % i, [128, 128], F32) for i in range(2)]
    Sst = _sb(K, es, "gd_S", [128, 4, 128], F32)
    S.op("pool", lambda: nc.gpsimd.memset(Sst[:], 0.0), W=[Sst])
    tmpS = _sb(K, es, "gd_tmpS", [128, 4, 128], F32)

    def mk(name, shape, dt=F32):
        return [_sb(K, es, "gd_%s%d" % (name, i), shape, dt) for i in range(NSETS)]
    B = {}
    for name in ("egc", "egr", "ssq"):
        B[name] = mk(name, [64, 4])
    B["cdt"] = mk("cd", [128, 4])
    for name in ("gl", "dm", "dmT", "Nm", "Mm", "N2", "M2", "Pm", "qkT"):
        B[name] = mk(name, [64, 4, 64])
    for name in ("kbg", "vb", "ke", "gbc", "u", "vn", "ot", "o2"):
        B[name] = mk(name, [64, 4, 128])
    for name in ("egb", "qd", "wcT"):
        B[name] = mk(name, [128, 4, 64])
    B["ob"] = mk("ob", [64, 4, 128], BF16)
    B["bg"] = mk("bg", [64, 8])
    B["z"] = mk("z", [64, 512])
    st = {"inflight": 0, "rec_done": 0, "done": 0}
    assert NSETS == 2
    pbigs = [K.psum[5], K.psum[6]]
    psmls = pbigs
    psg1 = K.psum[7]
    n = 128

    def load_x(g):
        x = xin[g % 2]
        c0 = g * 128
        if g == 0:
            S.op("pool", lambda: nc.gpsimd.memset(x[:, :, 0:3], 0.0), W=[x])
            S.dma("sp", x[:, :, 3:3 + n], K.d["gT"][:, :, 0:n].rearrange("h p n -> p h n"), R=[K.sbuf_g], W=[x])
            S.op("pool", lambda: nc.gpsimd.memset(x[:, :, 3:3 + 48], 0.0), W=[x])
        else:
            S.dma("sp", x[:, :, 0:3 + n], K.d["gT"][:, :, c0 - 3:c0 + n].rearrange("h p n -> p h n"), R=[K.sbuf_g], W=[x])

    def G1(g):
        le = None
        x, cv = xin[g % 2], cvs[g % 2]
        k = 0
        for i in range(12):
            eng = "dve"
            E = nc.vector
            if le != eng:
                yield
            le = eng
            S.op(eng, lambda: E.tensor_scalar(out=cv[:, i, 0:n], in0=x[:, i, 0:n], scalar1=cw[:, i, 0:1], scalar2=None, op0=ALU.mult),
                 R=[x, cw], W=[cv])
            for tp in range(1, 4):
                if le != "dve":
                    yield
                le = "dve"
                S.op("dve", lambda: nc.vector.scalar_tensor_tensor(out=cv[:, i, 0:n], in0=x[:, i, tp:tp + n], scalar=cw[:, i, tp:tp + 1],
                                                                  in1=cv[:, i, 0:n], op0=ALU.mult, op1=ALU.add), R=[x, cw, cv], W=[cv])
            s_ = sq[i % 2]
            if le != "act":
                yield
            le = "act"
            S.op("act", lambda: nc.scalar.activation(out=s_[:, 0:n], in_=cv[:, i, 0:n], func=AF.Exp, scale=-1.0), R=[cv], W=[s_])
            S.op("act", lambda: nc.scalar.activation(out=s_[:, 0:n], in_=s_[:, 0:n], func=AF.Ln, bias=K.one_c[:, 0:1], scale=1.0),
                 R=[s_, K.one_c], W=[s_])
            S.op("act", lambda: nc.scalar.activation(out=s_[:, 0:n], in_=s_[:, 0:n], func=AF.Exp, scale=-1.0), R=[s_], W=[s_])
            if le != "dve":
                yield
            le = "dve"
            S.op("dve", lambda: nc.vector.tensor_tensor(out=cv[:, i, 0:n], in0=cv[:, i, 0:n], in1=s_[:, 0:n], op=ALU.mult), R=[cv, s_], W=[cv])
        for i in range(8):
            s_, r_ = sq[i % 2], rn[i % 2]
            if le != "pool":
                yield
            le = "pool"
            S.op("pool", lambda: nc.gpsimd.tensor_tensor(out=s_[:, 0:n], in0=cv[:, i, 0:n], in1=cv[:, i, 0:n], op=ALU.mult), R=[cv], W=[s_])
            ps = psg1
            if le != "pe":
                yield
            le = "pe"
            S.op("pe", lambda: nc.tensor.matmul(ps[:, 0:n], lhsT=K.onesf[:, :], rhs=s_[:, 0:n], start=True, stop=True),
                 R=[K.onesf, s_], W=[ps])
            if le != "act":
                yield
            le = "act"
            S.op("act", lambda: nc.scalar.activation(out=r_[:, 0:n], in_=ps[:, 0:n], func=AF.Ln, bias=K.eps_rms[:, 0:1], scale=1.0),
                 R=[ps, K.eps_rms], W=[r_])
            S.op("act", lambda: nc.scalar.activation(out=r_[:, 0:n], in_=r_[:, 0:n], func=AF.Exp, scale=-0.5), R=[r_], W=[r_])
            if i < 4:
                if le != "dve":
                    yield
                le = "dve"
                S.op("dve", lambda: nc.vector.scalar_tensor_tensor(out=cv[:, i, 0:n], in0=cv[:, i, 0:n], scalar=float(128 ** -0.5),
                                                                  in1=r_[:, 0:n], op0=ALU.mult, op1=ALU.mult), R=[cv, r_], W=[cv])
            else:
                if le != "dve":
                    yield
                le = "dve"
                S.op("dve", lambda: nc.vector.tensor_tensor(out=cv[:, i, 0:n], in0=cv[:, i, 0:n], in1=r_[:, 0:n], op=ALU.mult),
                     R=[cv, r_], W=[cv])

    def chunk(g, ci, cidx):
        le = None
        b2 = cidx % NSETS
        pbig, psml = pbigs[b2], psmls[b2]
        cv = cvs[g % 2]
        egc, egr, cdt, ssq = B["egc"][b2], B["egr"][b2], B["cdt"][b2], B["ssq"][b2]
        gl, dm, dmT, Pm, qkT = B["gl"][b2], B["dm"][b2], B["dmT"][b2], B["Pm"][b2], B["qkT"][b2]
        kbg, vb, ke, gbc, u, vn, ot, o2 = (B[k_][b2] for k_ in ("kbg", "vb", "ke", "gbc", "u", "vn", "ot", "o2"))
        egb, qd, wcT, ob = B["egb"][b2], B["qd"][b2], B["wcT"][b2], B["ob"][b2]
        bgc, zc = B["bg"][b2], B["z"][b2]
        p0 = g * 128 + ci * 64
        if le != "sp":
            yield
        le = "sp"
        S.dma("sp", bgc[:, :], K.d["bg"][p0:p0 + 64, :], R=[K.sbuf_bg], W=[bgc])
        if le != "sp":
            yield
        le = "sp"
        S.dma("sp", zc[:, :], K.d["zs"][p0:p0 + 64, :], R=[K.sbuf_z], W=[zc])
        if cidx == 0:
            if le != "pool":
                yield
            le = "pool"
            S.op("pool", lambda: nc.gpsimd.memset(bgc[0:48, 4:8], 0.0), W=[bgc])
        for _ in range(DMA_PAD):
            yield
        cs = slice(ci * 64, ci * 64 + 64)
        gcol = bgc[:, 4:8]
        bcol = bgc[:, 0:4]
        pg = psml
        if le != "pe":
            yield
        le = "pe"
        S.op("pe", lambda: nc.tensor.matmul(pg[0:64, 0:4], lhsT=K.triu[:, 0, :], rhs=gcol, start=True, stop=True), R=[K.triu, bgc], W=[pg])
        if le != "pe":
            yield
        le = "pe"
        S.op("pe", lambda: nc.tensor.matmul(pg[0:64, 4:8], lhsT=K.sgt[:, :], rhs=gcol, start=True, stop=True), R=[K.sgt, bgc], W=[pg])
        if le != "pe":
            yield
        le = "pe"
        S.op("pe", lambda: nc.tensor.matmul(pg[:, 8:12], lhsT=K.ones64[:, :], rhs=gcol, start=True, stop=True), R=[K.ones64, bgc], W=[pg])
        if le != "act":
            yield
        le = "act"
        S.op("act", lambda: nc.scalar.activation(out=egc[:], in_=pg[0:64, 0:4], func=AF.Exp), R=[pg], W=[egc])
        if le != "act":
            yield
        le = "act"
        S.op("act", lambda: nc.scalar.activation(out=egr[:], in_=pg[0:64, 4:8], func=AF.Exp), R=[pg], W=[egr])
        if le != "act":
            yield
        le = "act"
        S.op("act", lambda: nc.scalar.activation(out=cdt[:], in_=pg[:, 8:12], func=AF.Exp), R=[pg], W=[cdt])
        if le != "dve":
            yield
        le = "dve"
        S.op("dve", lambda: nc.vector.tensor_tensor(out=gl[:], in0=K.triu[:], in1=gcol.unsqueeze(2).to_broadcast([64, 4, 64]), op=ALU.mult),
             R=[K.triu, bgc], W=[gl])
        pd = pbig
        pdv = pd.h[0:64, 0:512].rearrange("p (a h c) -> p a h c", a=2, h=4)
        for h in range(4):
            if le != "pe":
                yield
            le = "pe"
            S.op("pe", lambda: nc.tensor.matmul(pdv[:, 0, h, :], lhsT=gl[:, h, :], rhs=K.sgt[:, :], start=True, stop=True),
                 R=[gl, K.sgt], W=[pd])
        if le != "pe":
            yield
        le = "pe"
        S.op("pe", lambda: nc.tensor.matmul(pd[0:64, 256:512], lhsT=K.sgt[:, :], rhs=gl[:].rearrange("p h c -> p (h c)"), start=True, stop=True),
             R=[gl, K.sgt], W=[pd])
        if le != "act":
            yield
        le = "act"
        S.op("act", lambda: nc.scalar.activation(out=dm[:], in_=pdv[:, 0], func=AF.Exp), R=[pd], W=[dm])
        if le != "act":
            yield
        le = "act"
        S.op("act", lambda: nc.scalar.activation(out=dmT[:], in_=pdv[:, 1], func=AF.Exp), R=[pd], W=[dmT])
        if le != "pool":
            yield
        le = "pool"
        S.op("pool", lambda: nc.gpsimd.tensor_tensor(out=dm[:], in0=dm[:], in1=K.trilsn[:], op=ALU.mult), R=[dm, K.trilsn], W=[dm])
        if le != "pool":
            yield
        le = "pool"
        S.op("pool", lambda: nc.gpsimd.tensor_tensor(out=dmT[:], in0=dmT[:], in1=K.triu[:], op=ALU.mult), R=[dmT, K.triu], W=[dmT])
        pgq = pbig
        pgqv = pgq.h[0:64, 0:512].rearrange("p (a h c) -> p a h c", a=2, h=4)
        for h in range(4):
            if le != "pe":
                yield
            le = "pe"
            S.op("pe", lambda: nc.tensor.matmul(pgqv[:, 0, h, :], lhsT=cv[:, 4 + h, cs], rhs=cv[:, 4 + h, cs], start=True, stop=True), R=[cv], W=[pgq])
            if le != "pe":
                yield
            le = "pe"
            S.op("pe", lambda: nc.tensor.matmul(pgqv[:, 1, h, :], lhsT=cv[:, 4 + h, cs], rhs=cv[:, h, cs], start=True, stop=True), R=[cv], W=[pgq])
        Nm, Mm = B["Nm"][b2], B["Mm"][b2]
        if le != "dve":
            yield
        le = "dve"
        S.op("dve", lambda: nc.vector.tensor_tensor(out=Nm[:], in0=pgqv[:, 0], in1=bcol.unsqueeze(2).to_broadcast([64, 4, 64]), op=ALU.mult),
             R=[pgq, bgc], W=[Nm])
        if le != "dve":
            yield
        le = "dve"
        S.op("dve", lambda: nc.vector.tensor_tensor(out=Nm[:], in0=Nm[:], in1=dm[:], op=ALU.mult), R=[Nm, dm], W=[Nm])
        if le != "dve":
            yield
        le = "dve"
        S.op("dve", lambda: nc.vector.tensor_tensor(out=qkT[:], in0=pgqv[:, 1], in1=dmT[:], op=ALU.mult), R=[pgq, dmT], W=[qkT])
        pt = psml
        ptv = pt.h[0:64, 0:256].rearrange("p (h c) -> p h c", h=4)
        for h in range(4):
            if le != "pe":
                yield
            le = "pe"
            S.op("pe", lambda: nc.tensor.transpose(out=ptv[:, h, :], in_=Nm[:, h, :], identity=K.identf[0:64, 0:64]), R=[Nm, K.identf], W=[pt])
        if le != "act":
            yield
        le = "act"
        S.op("act", lambda: nc.scalar.copy(out=Mm[:], in_=ptv), R=[pt], W=[Mm])
        if le != "dve":
            yield
        le = "dve"
        S.op("dve", lambda: nc.vector.tensor_tensor(out=Pm[:], in0=Mm[:], in1=K.ident4[:], op=ALU.add), R=[Mm, K.ident4], W=[Pm])
        Nc, Mc, Nn, Mn = Nm, Mm, B["N2"][b2], B["M2"][b2]
        for r in range(5):
            pn = pbig
            pnv = pn.h[0:64, 0:512].rearrange("p (a h c) -> p a h c", a=2, h=4)
            for h in range(4):
                if le != "pe":
                    yield
                le = "pe"
                S.op("pe", lambda: nc.tensor.matmul(pnv[:, 0, h, :], lhsT=Mc[:, h, :], rhs=Nc[:, h, :], start=True, stop=True), R=[Mc, Nc], W=[pn])
                if r < 4:
                    if le != "pe":
                        yield
                    le = "pe"
                    S.op("pe", lambda: nc.tensor.matmul(pnv[:, 1, h, :], lhsT=Nc[:, h, :], rhs=Mc[:, h, :], start=True, stop=True), R=[Mc, Nc], W=[pn])
            if le != "act":
                yield
            le = "act"
            S.op("act", lambda: nc.scalar.copy(out=Nn[:], in_=pnv[:, 0]), R=[pn], W=[Nn])
            if r < 4:
                if le != "dve":
                    yield
                le = "dve"
                S.op("dve", lambda: nc.vector.tensor_copy(out=Mn[:], in_=pnv[:, 1]), R=[pn], W=[Mn])
            pp = psml
            ppv = pp.h[0:64, 0:256].rearrange("p (h c) -> p h c", h=4)
            for h in range(4):
                if le != "pe":
                    yield
                le = "pe"
                S.op("pe", lambda: nc.tensor.matmul(ppv[:, h, :], lhsT=Nn[:, h, :], rhs=Pm[:, h, :], start=True, stop=True), R=[Nn, Pm], W=[pp])
            if le != "dve":
                yield
            le = "dve"
            S.op("dve", lambda: nc.vector.tensor_tensor(out=Pm[:], in0=Pm[:], in1=ppv, op=ALU.add), R=[Pm, pp], W=[Pm])
            Nc, Mc, Nn, Mn = Nn, Mn, Nc, Mc
        pk = pbig
        pkv = pk.h[0:64, 0:512].rearrange("p (h c) -> p h c", h=4)
        for h in range(4):
            if le != "pe":
                yield
            le = "pe"
            S.op("pe", lambda: nc.tensor.transpose(out=pkv[:, h, :], in_=cv[:, 4 + h, cs], identity=K.identf[:]), R=[cv, K.identf], W=[pk])
        if le != "dve":
            yield
        le = "dve"
        S.op("dve", lambda: nc.vector.tensor_tensor(out=ke[:], in0=pkv, in1=egr[:].unsqueeze(2).to_broadcast([64, 4, 128]), op=ALU.mult),
             R=[pk, egr], W=[ke])
        if le != "dve":
            yield
        le = "dve"
        S.op("dve", lambda: nc.vector.tensor_tensor(out=kbg[:], in0=pkv, in1=bcol.unsqueeze(2).to_broadcast([64, 4, 128]), op=ALU.mult),
             R=[pk, bgc], W=[kbg])
        if le != "pool":
            yield
        le = "pool"
        S.op("pool", lambda: nc.gpsimd.tensor_tensor(out=kbg[:], in0=kbg[:], in1=egc[:].unsqueeze(2).to_broadcast([64, 4, 128]), op=ALU.mult),
             R=[kbg, egc], W=[kbg])
        pv = pbig
        pvv = pv.h[0:64, 0:512].rearrange("p (h c) -> p h c", h=4)
        for h in range(4):
            if le != "pe":
                yield
            le = "pe"
            S.op("pe", lambda: nc.tensor.transpose(out=pvv[:, h, :], in_=cv[:, 8 + h, cs], identity=K.identf[:]), R=[cv, K.identf], W=[pv])
        if le != "dve":
            yield
        le = "dve"
        S.op("dve", lambda: nc.vector.tensor_tensor(out=vb[:], in0=pvv, in1=bcol.unsqueeze(2).to_broadcast([64, 4, 128]), op=ALU.mult),
             R=[pv, bgc], W=[vb])
        if le != "pool":
            yield
        le = "pool"
        S.op("pool", lambda: nc.gpsimd.tensor_tensor(out=gbc[:], in0=K.ones4[:], in1=gcol.unsqueeze(2).to_broadcast([64, 4, 128]), op=ALU.mult),
             R=[K.ones4, bgc], W=[gbc])
        pe_ = psml
        pev = pe_.h[:, 0:256].rearrange("p (h c) -> p h c", h=4)
        for h in range(4):
            if le != "pe":
                yield
            le = "pe"
            S.op("pe", lambda: nc.tensor.matmul(pev[:, h, :], lhsT=gbc[:, h, :], rhs=K.triu[:, 0, :], start=True, stop=True), R=[gbc, K.triu], W=[pe_])
        if le != "act":
            yield
        le = "act"
        S.op("act", lambda: nc.scalar.activation(out=egb[:], in_=pev, func=AF.Exp), R=[pe_], W=[egb])
        if le != "dve":
            yield
        le = "dve"
        S.op("dve", lambda: nc.vector.tensor_tensor(out=qd[:], in0=cv[:, 0:4, cs], in1=egb[:], op=ALU.mult), R=[cv, egb], W=[qd])
        pw = psml
        pwv = pw.h[:, 0:256].rearrange("p (h c) -> p h c", h=4)
        for h in range(4):
            if le != "pe":
                yield
            le = "pe"
            S.op("pe", lambda: nc.tensor.matmul(pwv[:, h, :], lhsT=kbg[:, h, :], rhs=Pm[:, h, :], start=True, stop=True), R=[kbg, Pm], W=[pw])
        if le != "act":
            yield
        le = "act"
        S.op("act", lambda: nc.scalar.copy(out=wcT[:], in_=pwv), R=[pw], W=[wcT])
        pu = pbig
        puv = pu.h[0:64, 0:512].rearrange("p (h c) -> p h c", h=4)
        for h in range(4):
            if le != "pe":
                yield
            le = "pe"
            S.op("pe", lambda: nc.tensor.matmul(puv[:, h, :], lhsT=Pm[:, h, :], rhs=vb[:, h, :], start=True, stop=True), R=[vb, Pm], W=[pu])
        if le != "act":
            yield
        le = "act"
        S.op("act", lambda: nc.scalar.copy(out=u[:], in_=puv), R=[pu], W=[u])
        while st["rec_done"] < cidx:
            yield
        pws = pbig
        pwsv = pws.h[0:64, 0:512].rearrange("p (h c) -> p h c", h=4)
        for h in range(4):
            if le != "pe":
                yield
            le = "pe"
            S.op("pe", lambda: nc.tensor.matmul(pwsv[:, h, :], lhsT=wcT[:, h, :], rhs=Sst[:, h, :], start=True, stop=True), R=[wcT, Sst], W=[pws])
        if le != "dve":
            yield
        le = "dve"
        S.op("dve", lambda: nc.vector.tensor_tensor(out=vn[:], in0=u[:], in1=pwsv, op=ALU.subtract), R=[u, pws], W=[vn])
        po = pbig
        pov = po.h[0:64, 0:512].rearrange("p (h c) -> p h c", h=4)
        for h in range(4):
            if le != "pe":
                yield
            le = "pe"
            S.op("pe", lambda: nc.tensor.matmul(pov[:, h, :], lhsT=qd[:, h, :], rhs=Sst[:, h, :], start=True, stop=False), R=[qd, Sst], W=[po])
            if le != "pe":
                yield
            le = "pe"
            S.op("pe", lambda: nc.tensor.matmul(pov[:, h, :], lhsT=qkT[:, h, :], rhs=vn[:, h, :], start=False, stop=True), R=[qkT, vn], W=[po])
        if le != "act":
            yield
        le = "act"
        S.op("act", lambda: nc.scalar.copy(out=ot[:], in_=pov), R=[po], W=[ot])
        pS = pbig
        pSv = pS.h[:, 0:512].rearrange("p (h c) -> p h c", h=4)
        for h in range(4):
            if le != "pe":
                yield
            le = "pe"
            S.op("pe", lambda: nc.tensor.matmul(pSv[:, h, :], lhsT=ke[:, h, :], rhs=vn[:, h, :], start=True, stop=True), R=[ke, vn], W=[pS])
        if le != "pool":
            yield
        le = "pool"
        S.op("pool", lambda: nc.gpsimd.tensor_tensor(out=tmpS[:], in0=Sst[:], in1=cdt[:].unsqueeze(2).to_broadcast([128, 4, 128]), op=ALU.mult),
             R=[Sst, cdt], W=[tmpS])
        if le != "dve":
            yield
        le = "dve"
        S.op("dve", lambda: nc.vector.tensor_tensor(out=Sst[:], in0=tmpS[:], in1=pSv, op=ALU.add), R=[tmpS, pS], W=[Sst])
        st["rec_done"] = cidx + 1
        if le != "pool":
            yield
        le = "pool"
        S.op("pool", lambda: nc.gpsimd.tensor_tensor(out=o2[:], in0=ot[:], in1=ot[:], op=ALU.mult), R=[ot], W=[o2])
        if le != "dve":
            yield
        le = "dve"
        S.op("dve", lambda: nc.vector.tensor_reduce(out=ssq[:], in_=o2[:], axis=AX.X, op=ALU.add), R=[o2], W=[ssq])
        if le != "act":
            yield
        le = "act"
        S.op("act", lambda: nc.scalar.activation(out=ssq[:], in_=ssq[:], func=AF.Ln, bias=K.eps_rms128[0:64, 0:1], scale=1.0),
             R=[ssq, K.eps_rms128], W=[ssq])
        S.op("act", lambda: nc.scalar.activation(out=ssq[:], in_=ssq[:], func=AF.Exp, scale=-0.5), R=[ssq], W=[ssq])
        if le != "dve":
            yield
        le = "dve"
        S.op("dve", lambda: nc.vector.scalar_tensor_tensor(out=o2[:], in0=ot[:], scalar=float(128 ** 0.5),
                                                          in1=ssq[:].unsqueeze(2).to_broadcast([64, 4, 128]), op0=ALU.mult, op1=ALU.mult),
             R=[ot, ssq], W=[o2])
        if le != "pool":
            yield
        le = "pool"
        S.op("pool", lambda: nc.gpsimd.tensor_tensor(out=o2[:], in0=o2[:], in1=gng[:], op=ALU.mult), R=[o2, gng], W=[o2])
        if le != "dve":
            yield
        le = "dve"
        S.op("dve", lambda: nc.vector.tensor_tensor(out=ob[:], in0=o2[:], in1=zc[:, :].rearrange("p (h c) -> p h c", h=4), op=ALU.mult),
             R=[o2, zc], W=[ob])
        if le != "sp":
            yield
        le = "sp"
        S.dma("sp", K.d["mixed"][p0:p0 + 64, 512:1024], ob[:].rearrange("p h c -> p (h c)"), R=[ob], W=[K.sbuf_mx_gd])
        st["inflight"] -= 1
        st["done"] += 1

    NG = NT
    load_x(0)
    cidx = 0
    for g in range(NG):
        nch = min(2, NC_ - 2 * g)
        if nch <= 0:
            break
        if g + 1 < NG and NC_ - 2 * (g + 1) > 0:
            load_x(g + 1)
        while st["done"] < min(cidx, 2 * (g - 1)):
            yield
        yield from G1(g)
        for ci in range(nch):
            while st["inflight"] >= NSETS:
                yield
            st["inflight"] += 1
            rr.add(chunk(g, ci, cidx), YK)
            cidx += 1
            yield
    while st["done"] < cidx:
        yield


def phase_GDN_old(K, l):
    nc, S, NT = K.nc, K.S, K.NT
    NC_ = K.NC
    with contextlib.ExitStack() as es:
        cw = _sb(K, es, "gd_cw", [128, 12, 4], F32)
        S.dma("sp", cw[:], K.d["conv_wT"][l].rearrange("(i d) t -> d i t", d=128), W=[cw])
        gng = _sb(K, es, "gd_gng", [64, 4, 128], F32)
        for h in range(4):
            S.dma("sp", gng[:, h, :], K.d["gdn_norm_g"][l].partition_broadcast(64), W=[gng])
        xin = [_sb(K, es, "gd_x%d" % i, [128, 12, 259], F32) for i in range(2)]
        cv = _sb(K, es, "gd_cv", [128, 12, 256], F32)
        sq = [_sb(K, es, "gd_sq%d" % i, [128, 256], F32) for i in range(2)]
        rn = [_sb(K, es, "gd_rn%d" % i, [128, 256], F32) for i in range(2)]
        bgt = [_sb(K, es, "gd_bg%d" % i, [64, 4, 8], F32) for i in range(2)]
        zt = [_sb(K, es, "gd_z%d" % i, [64, 4, 512], F32) for i in range(2)]
        Sst = _sb(K, es, "gd_S", [128, 4, 128], F32)
        S.op("pool", lambda: nc.gpsimd.memset(Sst[:], 0.0), W=[Sst])
        def mk(name, shape, dt=F32):
            return [_sb(K, es, "gd_%s%d" % (name, i), shape, dt) for i in range(2)]
        egc, egr, cdt = mk("egc", [64, 4]), mk("egr", [64, 4]), mk("cd", [128, 4])
        gl, dm, dmT = mk("gl", [64, 4, 64]), mk("dm", [64, 4, 64]), mk("dmT", [64, 4, 64])
        Nm, Mm = mk("N", [64, 4, 64]), mk("M", [64, 4, 64])
        N2, M2 = mk("N2", [64, 4, 64]), mk("M2", [64, 4, 64])
        Pm = mk("P", [64, 4, 64])
        qkT = mk("qkT", [64, 4, 64])
        kbg, vb, ke = mk("kbg", [64, 4, 128]), mk("vb", [64, 4, 128]), mk("ke", [64, 4, 128])
        gbc, egb, qd = mk("gbc", [64, 4, 128]), mk("egb", [128, 4, 64]), mk("qd", [128, 4, 64])
        wcT, u, vn = mk("wcT", [128, 4, 64]), mk("u", [64, 4, 128]), mk("vn", [64, 4, 128])
        ot, o2 = mk("ot", [64, 4, 128]), mk("o2", [64, 4, 128])
        ssq, ob = mk("ssq", [64, 4]), mk("ob", [64, 4, 128], BF16)
        tmpS = _sb(K, es, "gd_tmpS", [128, 4, 128], F32)
        NG = (NT + 1) // 2
        ci_glob = 0
        for g in range(NG):
            nt = min(2, NT - 2 * g)
            n = nt * 128
            c0 = g * 256
            nch = min(n // 64, NC_ - c0 // 64)
            if nch <= 0:
                break
            x = xin[g % 2]
            if g == 0:
                S.op("pool", lambda: nc.gpsimd.memset(x[:, :, 0:3], 0.0), W=[x])
                S.dma("sp", x[:, :, 3:3 + n], K.d["gT"][:, :, 0:n].rearrange("h p n -> p h n"), R=[K.sbuf_g], W=[x])
                S.op("pool", lambda: nc.gpsimd.memset(x[:, :, 3:3 + 48], 0.0), W=[x])
            else:
                S.dma("sp", x[:, :, 0:3 + n], K.d["gT"][:, :, c0 - 3:c0 + n].rearrange("h p n -> p h n"), R=[K.sbuf_g], W=[x])
            for i in range(12):
                eng = "dve" if i % 2 == 0 else "pool"
                E = nc.vector if eng == "dve" else nc.gpsimd
                S.op(eng, lambda: E.tensor_scalar(out=cv[:, i, 0:n], in0=x[:, i, 0:n], scalar1=cw[:, i, 0:1], scalar2=None, op0=ALU.mult),
                     R=[x, cw], W=[cv])
                for tp in range(1, 4):
                    S.op("dve", lambda: nc.vector.scalar_tensor_tensor(out=cv[:, i, 0:n], in0=x[:, i, tp:tp + n], scalar=cw[:, i, tp:tp + 1],
                                                                      in1=cv[:, i, 0:n], op0=ALU.mult, op1=ALU.add), R=[x, cw, cv], W=[cv])
                S.op("act", lambda: nc.scalar.activation(out=cv[:, i, 0:n], in_=cv[:, i, 0:n], func=AF.Silu), R=[cv], W=[cv])
            for i in range(8):
                s_, r_ = sq[i % 2], rn[i % 2]
                S.op("pool", lambda: nc.gpsimd.tensor_tensor(out=s_[:, 0:n], in0=cv[:, i, 0:n], in1=cv[:, i, 0:n], op=ALU.mult), R=[cv], W=[s_])
                ps = _ps(K)
                S.op("pe", lambda: nc.tensor.matmul(ps[:, 0:n], lhsT=K.onesf[:, :], rhs=s_[:, 0:n], start=True, stop=True),
                     R=[K.onesf, s_], W=[ps])
                S.op("act", lambda: nc.scalar.activation(out=r_[:, 0:n], in_=ps[:, 0:n], func=AF.Sqrt, bias=K.eps_rms[:, 0:1], scale=1.0),
                     R=[ps, K.eps_rms], W=[r_])
                S.op("dve", lambda: nc.vector.reciprocal(out=r_[:, 0:n], in_=r_[:, 0:n]), R=[r_], W=[r_])
                if i < 4:
                    S.op("dve", lambda: nc.vector.scalar_tensor_tensor(out=cv[:, i, 0:n], in0=cv[:, i, 0:n], scalar=float(128 ** -0.5),
                                                                      in1=r_[:, 0:n], op0=ALU.mult, op1=ALU.mult), R=[cv, r_], W=[cv])
                else:
                    S.op("dve", lambda: nc.vector.tensor_tensor(out=cv[:, i, 0:n], in0=cv[:, i, 0:n], in1=r_[:, 0:n], op=ALU.mult),
                         R=[cv, r_], W=[cv])
            bgc, zc = bgt[g % 2], zt[g % 2]
            S.dma("sp", bgc[:, 0:nch, :], K.d["bg"][c0:c0 + nch * 64, :].rearrange("(n c) e -> c n e", c=64), R=[K.sbuf_bg], W=[bgc])
            S.dma("sp", zc[:, 0:nch, :], K.d["zs"][c0:c0 + nch * 64, :].rearrange("(n c) e -> c n e", c=64), R=[K.sbuf_z], W=[zc])
            if g == 0:
                S.op("pool", lambda: nc.gpsimd.memset(bgc[0:48, 0, 4:8], 0.0), W=[bgc])
            for ci in range(nch):
                b2 = ci_glob % 2
                ci_glob += 1
                cs = slice(ci * 64, ci * 64 + 64)
                gcol = bgc[:, ci, 4:8]
                bcol = bgc[:, ci, 0:4]
                pg = _ps(K)
                S.op("pe", lambda: nc.tensor.matmul(pg[0:64, 0:4], lhsT=K.triu[:, 0, :], rhs=gcol, start=True, stop=True), R=[K.triu, bgc], W=[pg])
                S.op("pe", lambda: nc.tensor.matmul(pg[0:64, 4:8], lhsT=K.sgt[:, :], rhs=gcol, start=True, stop=True), R=[K.sgt, bgc], W=[pg])
                S.op("pe", lambda: nc.tensor.matmul(pg[:, 8:12], lhsT=K.ones64[:, :], rhs=gcol, start=True, stop=True), R=[K.ones64, bgc], W=[pg])
                S.op("act", lambda: nc.scalar.activation(out=egc[b2][:], in_=pg[0:64, 0:4], func=AF.Exp), R=[pg], W=[egc[b2]])
                S.op("act", lambda: nc.scalar.activation(out=egr[b2][:], in_=pg[0:64, 4:8], func=AF.Exp), R=[pg], W=[egr[b2]])
                S.op("act", lambda: nc.scalar.activation(out=cdt[b2][:], in_=pg[:, 8:12], func=AF.Exp), R=[pg], W=[cdt[b2]])
                S.op("dve", lambda: nc.vector.tensor_tensor(out=gl[b2][:], in0=K.triu[:], in1=gcol.unsqueeze(2).to_broadcast([64, 4, 64]), op=ALU.mult),
                     R=[K.triu, bgc], W=[gl[b2]])
                pd = _ps(K)
                pdv = pd.h[0:64, 0:512].rearrange("p (a h c) -> p a h c", a=2, h=4)
                for h in range(4):
                    S.op("pe", lambda: nc.tensor.matmul(pdv[:, 0, h, :], lhsT=gl[b2][:, h, :], rhs=K.sgt[:, :], start=True, stop=True),
                         R=[gl[b2], K.sgt], W=[pd])
                S.op("pe", lambda: nc.tensor.matmul(pd[0:64, 256:512], lhsT=K.sgt[:, :], rhs=gl[b2][:].rearrange("p h c -> p (h c)"), start=True, stop=True),
                     R=[gl[b2], K.sgt], W=[pd])
                S.op("act", lambda: nc.scalar.activation(out=dm[b2][:], in_=pdv[:, 0], func=AF.Exp), R=[pd], W=[dm[b2]])
                S.op("act", lambda: nc.scalar.activation(out=dmT[b2][:], in_=pdv[:, 1], func=AF.Exp), R=[pd], W=[dmT[b2]])
                S.op("pool", lambda: nc.gpsimd.tensor_tensor(out=dm[b2][:], in0=dm[b2][:], in1=K.trilsn[:], op=ALU.mult), R=[dm[b2], K.trilsn], W=[dm[b2]])
                S.op("pool", lambda: nc.gpsimd.tensor_tensor(out=dmT[b2][:], in0=dmT[b2][:], in1=K.triu[:], op=ALU.mult), R=[dmT[b2], K.triu], W=[dmT[b2]])
                pgq = _ps(K)
                pgqv = pgq.h[0:64, 0:512].rearrange("p (a h c) -> p a h c", a=2, h=4)
                for h in range(4):
                    S.op("pe", lambda: nc.tensor.matmul(pgqv[:, 0, h, :], lhsT=cv[:, 4 + h, cs], rhs=cv[:, 4 + h, cs], start=True, stop=True), R=[cv], W=[pgq])
                    S.op("pe", lambda: nc.tensor.matmul(pgqv[:, 1, h, :], lhsT=cv[:, 4 + h, cs], rhs=cv[:, h, cs], start=True, stop=True), R=[cv], W=[pgq])
                S.op("dve", lambda: nc.vector.tensor_tensor(out=Nm[b2][:], in0=pgqv[:, 0], in1=bcol.unsqueeze(2).to_broadcast([64, 4, 64]), op=ALU.mult),
                     R=[pgq, bgc], W=[Nm[b2]])
                S.op("dve", lambda: nc.vector.tensor_tensor(out=Nm[b2][:], in0=Nm[b2][:], in1=dm[b2][:], op=ALU.mult), R=[Nm[b2], dm[b2]], W=[Nm[b2]])
                S.op("dve", lambda: nc.vector.tensor_tensor(out=qkT[b2][:], in0=pgqv[:, 1], in1=dmT[b2][:], op=ALU.mult), R=[pgq, dmT[b2]], W=[qkT[b2]])
                pt = _ps(K)
                ptv = pt.h[0:64, 0:256].rearrange("p (h c) -> p h c", h=4)
                for h in range(4):
                    S.op("pe", lambda: nc.tensor.transpose(out=ptv[:, h, :], in_=Nm[b2][:, h, :], identity=K.identf[0:64, 0:64]), R=[Nm[b2], K.identf], W=[pt])
                S.op("act", lambda: nc.scalar.copy(out=Mm[b2][:], in_=ptv), R=[pt], W=[Mm[b2]])
                S.op("dve", lambda: nc.vector.tensor_tensor(out=Pm[b2][:], in0=Mm[b2][:], in1=K.ident4[:], op=ALU.add), R=[Mm[b2], K.ident4], W=[Pm[b2]])
                Nc, Mc, Nn, Mn = Nm[b2], Mm[b2], N2[b2], M2[b2]
                for r in range(5):
                    pn = _ps(K)
                    pnv = pn.h[0:64, 0:512].rearrange("p (a h c) -> p a h c", a=2, h=4)
                    for h in range(4):
                        S.op("pe", lambda: nc.tensor.matmul(pnv[:, 0, h, :], lhsT=Mc[:, h, :], rhs=Nc[:, h, :], start=True, stop=True), R=[Mc, Nc], W=[pn])
                        if r < 4:
                            S.op("pe", lambda: nc.tensor.matmul(pnv[:, 1, h, :], lhsT=Nc[:, h, :], rhs=Mc[:, h, :], start=True, stop=True), R=[Mc, Nc], W=[pn])
                    S.op("act", lambda: nc.scalar.copy(out=Nn[:], in_=pnv[:, 0]), R=[pn], W=[Nn])
                    if r < 4:
                        S.op("dve", lambda: nc.vector.tensor_copy(out=Mn[:], in_=pnv[:, 1]), R=[pn], W=[Mn])
                    pp = _ps(K)
                    ppv = pp.h[0:64, 0:256].rearrange("p (h c) -> p h c", h=4)
                    for h in range(4):
                        S.op("pe", lambda: nc.tensor.matmul(ppv[:, h, :], lhsT=Nn[:, h, :], rhs=Pm[b2][:, h, :], start=True, stop=True), R=[Nn, Pm[b2]], W=[pp])
                    S.op("dve", lambda: nc.vector.tensor_tensor(out=Pm[b2][:], in0=Pm[b2][:], in1=ppv, op=ALU.add), R=[Pm[b2], pp], W=[Pm[b2]])
                    Nc, Mc, Nn, Mn = Nn, Mn, Nc, Mc
                pk = _ps(K)
                pkv = pk.h[0:64, 0:512].rearrange("p (h c) -> p h c", h=4)
                pv = _ps(K)
                pvv = pv.h[0:64, 0:512].rearrange("p (h c) -> p h c", h=4)
                for h in range(4):
                    S.op("pe", lambda: nc.tensor.transpose(out=pkv[:, h, :], in_=cv[:, 4 + h, cs], identity=K.identf[:]), R=[cv, K.identf], W=[pk])
                    S.op("pe", lambda: nc.tensor.transpose(out=pvv[:, h, :], in_=cv[:, 8 + h, cs], identity=K.identf[:]), R=[cv, K.identf], W=[pv])
                S.op("dve", lambda: nc.vector.tensor_tensor(out=ke[b2][:], in0=pkv, in1=egr[b2][:].unsqueeze(2).to_broadcast([64, 4, 128]), op=ALU.mult),
                     R=[pk, egr[b2]], W=[ke[b2]])
                S.op("dve", lambda: nc.vector.tensor_tensor(out=kbg[b2][:], in0=pkv, in1=bcol.unsqueeze(2).to_broadcast([64, 4, 128]), op=ALU.mult),
                     R=[pk, bgc], W=[kbg[b2]])
                S.op("pool", lambda: nc.gpsimd.tensor_tensor(out=kbg[b2][:], in0=kbg[b2][:], in1=egc[b2][:].unsqueeze(2).to_broadcast([64, 4, 128]), op=ALU.mult),
                     R=[kbg[b2], egc[b2]], W=[kbg[b2]])
                S.op("dve", lambda: nc.vector.tensor_tensor(out=vb[b2][:], in0=pvv, in1=bcol.unsqueeze(2).to_broadcast([64, 4, 128]), op=ALU.mult),
                     R=[pv, bgc], W=[vb[b2]])
                S.op("pool", lambda: nc.gpsimd.tensor_tensor(out=gbc[b2][:], in0=K.ones4[:], in1=gcol.unsqueeze(2).to_broadcast([64, 4, 128]), op=ALU.mult),
                     R=[K.ones4, bgc], W=[gbc[b2]])
                pe_ = _ps(K)
                pev = pe_.h[:, 0:256].rearrange("p (h c) -> p h c", h=4)
                for h in range(4):
                    S.op("pe", lambda: nc.tensor.matmul(pev[:, h, :], lhsT=gbc[b2][:, h, :], rhs=K.triu[:, 0, :], start=True, stop=True), R=[gbc[b2], K.triu], W=[pe_])
                S.op("act", lambda: nc.scalar.activation(out=egb[b2][:], in_=pev, func=AF.Exp), R=[pe_], W=[egb[b2]])
                S.op("dve", lambda: nc.vector.tensor_tensor(out=qd[b2][:], in0=cv[:, 0:4, cs], in1=egb[b2][:], op=ALU.mult), R=[cv, egb[b2]], W=[qd[b2]])
                pw = _ps(K)
                pwv = pw.h[:, 0:256].rearrange("p (h c) -> p h c", h=4)
                pu = _ps(K)
                puv = pu.h[0:64, 0:512].rearrange("p (h c) -> p h c", h=4)
                for h in range(4):
                    S.op("pe", lambda: nc.tensor.matmul(pwv[:, h, :], lhsT=kbg[b2][:, h, :], rhs=Pm[b2][:, h, :], start=True, stop=True), R=[kbg[b2], Pm[b2]], W=[pw])
                    S.op("pe", lambda: nc.tensor.matmul(puv[:, h, :], lhsT=Pm[b2][:, h, :], rhs=vb[b2][:, h, :], start=True, stop=True), R=[vb[b2], Pm[b2]], W=[pu])
                S.op("act", lambda: nc.scalar.copy(out=wcT[b2][:], in_=pwv), R=[pw], W=[wcT[b2]])
                S.op("act", lambda: nc.scalar.copy(out=u[b2][:], in_=puv), R=[pu], W=[u[b2]])
                pws = _ps(K)
                pwsv = pws.h[0:64, 0:512].rearrange("p (h c) -> p h c", h=4)
                for h in range(4):
                    S.op("pe", lambda: nc.tensor.matmul(pwsv[:, h, :], lhsT=wcT[b2][:, h, :], rhs=Sst[:, h, :], start=True, stop=True), R=[wcT[b2], Sst], W=[pws])
                S.op("dve", lambda: nc.vector.tensor_tensor(out=vn[b2][:], in0=u[b2][:], in1=pwsv, op=ALU.subtract), R=[u[b2], pws], W=[vn[b2]])
                po = _ps(K)
                pov = po.h[0:64, 0:512].rearrange("p (h c) -> p h c", h=4)
                for h in range(4):
                    S.op("pe", lambda: nc.tensor.matmul(pov[:, h, :], lhsT=qd[b2][:, h, :], rhs=Sst[:, h, :], start=True, stop=False), R=[qd[b2], Sst], W=[po])
                    S.op("pe", lambda: nc.tensor.matmul(pov[:, h, :], lhsT=qkT[b2][:, h, :], rhs=vn[b2][:, h, :], start=False, stop=True), R=[qkT[b2], vn[b2]], W=[po])
                pS = _ps(K)
                pSv = pS.h[:, 0:512].rearrange("p (h c) -> p h c", h=4)
                for h in range(4):
                    S.op("pe", lambda: nc.tensor.matmul(pSv[:, h, :], lhsT=ke[b2][:, h, :], rhs=vn[b2][:, h, :], start=True, stop=True), R=[ke[b2], vn[b2]], W=[pS])
                S.op("pool", lambda: nc.gpsimd.tensor_tensor(out=tmpS[:], in0=Sst[:], in1=cdt[b2][:].unsqueeze(2).to_broadcast([128, 4, 128]), op=ALU.mult),
                     R=[Sst, cdt[b2]], W=[tmpS])
                S.op("dve", lambda: nc.vector.tensor_tensor(out=Sst[:], in0=tmpS[:], in1=pSv, op=ALU.add), R=[tmpS, pS], W=[Sst])
                S.op("act", lambda: nc.scalar.copy(out=ot[b2][:], in_=pov), R=[po], W=[ot[b2]])
                S.op("pool", lambda: nc.gpsimd.tensor_tensor(out=o2[b2][:], in0=ot[b2][:], in1=ot[b2][:], op=ALU.mult), R=[ot[b2]], W=[o2[b2]])
                S.op("dve", lambda: nc.vector.tensor_reduce(out=ssq[b2][:], in_=o2[b2][:], axis=AX.X, op=ALU.add), R=[o2[b2]], W=[ssq[b2]])
                S.op("act", lambda: nc.scalar.activation(out=ssq[b2][:], in_=ssq[b2][:], func=AF.Sqrt, bias=K.eps_rms[0:64, 0:1], scale=1.0 / 128),
                     R=[ssq[b2], K.eps_rms], W=[ssq[b2]])
                S.op("dve", lambda: nc.vector.reciprocal(out=ssq[b2][:], in_=ssq[b2][:]), R=[ssq[b2]], W=[ssq[b2]])
                S.op("dve", lambda: nc.vector.tensor_tensor(out=o2[b2][:], in0=ot[b2][:], in1=ssq[b2][:].unsqueeze(2).to_broadcast([64, 4, 128]), op=ALU.mult),
                     R=[ot[b2], ssq[b2]], W=[o2[b2]])
                S.op("pool", lambda: nc.gpsimd.tensor_tensor(out=o2[b2][:], in0=o2[b2][:], in1=gng[:], op=ALU.mult), R=[o2[b2], gng], W=[o2[b2]])
                S.op("dve", lambda: nc.vector.tensor_tensor(out=ob[b2][:], in0=o2[b2][:], in1=zc[:, ci, :].rearrange("p (h c) -> p h c", h=4), op=ALU.mult),
                     R=[o2[b2], zc], W=[ob[b2]])
                p0 = c0 + ci * 64
                S.dma("sp", K.d["mixed"][p0:p0 + 64, 512:1024], ob[b2][:].rearrange("p h c -> p (h c)"), R=[ob[b2]], W=[K.sbuf_mx_gd])


def phase_SBGDN(K, l, NSETS=2, YK=None, run_sb=True, run_gdn=True):
    YK = YK or GDN_YK
    with contextlib.ExitStack() as es:
        K.psum_gd = K.psum[5:8]
        K.psgi = 0
        rr = RR()
        if run_sb:
            rr.add(sb_stream(K, l, es), 1)
        if run_gdn:
            rr.add(gdn_master(K, l, es, rr, NSETS, YK), YK)
        rr.run()


def phase_A3(K, l):
    nc, S, NT = K.nc, K.S, K.NT
    with contextlib.ExitStack() as es:
        K.stg = [_sb(K, es, "stg%d" % i, [128, IN_W], F32) for i in range(2)]
        Wo = _sb(K, es, "a3_W", [128, KC, D], BF16)
        wbufs = [Buf() for _ in range(KC)]
        wsrc = K.d["w_out"][l].rearrange("(k p) n -> p k n", p=128)
        for kc in range(KC):
            _load_cast(K, Wo, Wo[:, kc, :], wsrc[:, kc, :], [128, D], wbufs[kc])
        Wr = _sb(K, es, "a3_Wr", [128, KC, 36], F32)
        S.dma("sp", Wr[:, :, 0:4], K.d["w_group"][l].rearrange("(k p) n -> p k n", p=128), W=[Wr])
        S.dma("sp", Wr[:, :, 4:36], K.d["w_expert"][l].rearrange("(k p) n -> p k n", p=128), W=[Wr])
        br = _sb(K, es, "a3_br", [128, 36], F32)
        S.dma("sp", br[:, 0:4], K.d["b_group"][l].partition_broadcast(128), W=[br])
        S.dma("sp", br[:, 4:36], K.d["b_expert"][l].partition_broadcast(128), W=[br])
        g = _sb(K, es, "a3_g", [128, D], F32)
        b = _sb(K, es, "a3_b", [128, D], F32)
        S.dma("sp", g[:], K.d["ln1_g"][l].partition_broadcast(128), W=[g])
        S.dma("sp", b[:], K.d["ln1_b"][l].partition_broadcast(128), W=[b])
        mxs = [_sb(K, es, "a3_mx%d" % i, [128, D], BF16) for i in range(2)]
        mTs = [_sb(K, es, "a3_mT%d" % i, [128, KC, 128], BF16) for i in range(2)]
        hts = [_sb(K, es, "a3_h%d" % i, [128, D], F32) for i in range(2)]
        rs = [_sb(K, es, "a3_r%d" % i, [128, D], F32) for i in range(2)]
        h1s = [_sb(K, es, "a3_h1%d" % i, [128, D], F32) for i in range(2)]
        hTf = [_sb(K, es, "a3_hTf%d" % i, [128, KC, 128], F32) for i in range(2)]
        hTb = [_sb(K, es, "a3_hTb%d" % i, [128, KC, 128], BF16) for i in range(2)]
        sm = {"st": _sb(K, es, "a3_st", [128, 2, 6], F32), "mv": _sb(K, es, "a3_mv", [128, 2], F32),
              "rstd": _sb(K, es, "a3_rs", [128, 1], F32), "tmp": _sb(K, es, "a3_tmp", [128, D], F32)}
        lg = _sb(K, es, "a3_lg", [128, 36], F32)
        sc = {k: _sb(K, es, "a3_" + k, shp, F32) for k, shp in
              (("gm", [128, 1]), ("ge", [128, 4]), ("gs", [128, 1]), ("oh", [128, 4]), ("ig", [128, 8]), ("tmp8", [128, 4, 8]),
               ("m8", [128, 8]), ("sel", [128, 8]), ("ex", [128, 8]), ("dn", [128, 1]), ("wi", [128, 8]))}
        cbs = [_sb(K, es, "a3_cb%d" % i, [128, 4, 8], F32) for i in range(2)]
        def stage1(t):
            mx, mT, ht, r, h1, hf, hb, cb = mxs[t % 2], mTs[t % 2], hts[t % 2], rs[t % 2], h1s[t % 2], hTf[t % 2], hTb[t % 2], cbs[t % 2]
            rows = slice(128 * t, 128 * (t + 1))
            S.dma("sp", mx[:], K.d["mixed"][rows, :], R=[K.sbuf_mx_sb, K.sbuf_mx_gd], W=[mx])
            S.dma("sp", ht[:], K.d["h"][rows, :], R=[K.hbuf[t]], W=[ht])
            for half in range(2):
                ps = _ps(K)
                psb = ps.h.bitcast(BF16)
                for j in range(4):
                    kc = half * 4 + j
                    S.op("pe", lambda: nc.tensor.transpose(out=psb[:, j * 128:(j + 1) * 128], in_=mx[:, kc * 128:(kc + 1) * 128], identity=K.identb[:]),
                         R=[mx, K.identb], W=[ps])
                _evac(K, S.alt(), mT[:, half * 4:half * 4 + 4, :], psb[:, 0:512].rearrange("p (a c) -> p a c", a=4), [ps], [mT])
            for half in range(2):
                ps = _ps(K)
                for kc in range(KC):
                    S.op("pe", lambda: nc.tensor.matmul(ps[:, :], lhsT=mT[:, kc, :], rhs=Wo[:, kc, half * 512:(half + 1) * 512],
                                                        start=(kc == 0), stop=(kc == KC - 1)), R=[mT, wbufs[kc]], W=[ps])
                S.op("dve", lambda: nc.vector.scalar_tensor_tensor(out=r[:, half * 512:(half + 1) * 512], in0=ht[:, half * 512:(half + 1) * 512],
                                                                  scalar=ALPHA, in1=ps[:, :], op0=ALU.mult, op1=ALU.add), R=[ht, ps], W=[r])
            _ln_tile(K, r, h1, g, b, sm)

        def stage2(t):
            mx, mT, ht, r, h1, hf, hb, cb = mxs[t % 2], mTs[t % 2], hts[t % 2], rs[t % 2], h1s[t % 2], hTf[t % 2], hTb[t % 2], cbs[t % 2]
            rows = slice(128 * t, 128 * (t + 1))
            S.dma("sp", K.d["h1"][rows, :], h1[:], R=[h1], W=[K.h1buf[t]])
            for half in range(2):
                ps = _ps(K)
                for j in range(4):
                    kc = half * 4 + j
                    S.op("pe", lambda: nc.tensor.transpose(out=ps[:, j * 128:(j + 1) * 128], in_=h1[:, kc * 128:(kc + 1) * 128], identity=K.identf[:]),
                         R=[h1, K.identf], W=[ps])
                S.op("act", lambda: nc.scalar.copy(out=hf[:, half * 4:half * 4 + 4, :], in_=ps[:, :].rearrange("p (a c) -> p a c", a=4)), R=[ps], W=[hf])
                S.op("dve", lambda: nc.vector.tensor_copy(out=hb[:, half * 4:half * 4 + 4, :], in_=ps[:, :].rearrange("p (a c) -> p a c", a=4)), R=[ps], W=[hb])
            S.dma("sp", K.d["h1T"][:, :, rows].rearrange("k p n -> p k n"), hb[:], R=[hb], W=[K.sbuf_h1T])
            ps = _ps(K)
            for kc in range(KC):
                S.op("pe", lambda: nc.tensor.matmul(ps[:, 0:36], lhsT=hf[:, kc, :], rhs=Wr[:, kc, :], start=(kc == 0), stop=(kc == KC - 1)),
                     R=[hf, Wr], W=[ps])
            S.op("dve", lambda: nc.vector.tensor_tensor(out=lg[:], in0=ps[:, 0:36], in1=br[:], op=ALU.add), R=[ps, br], W=[lg])
            gm, ge, gs, oh, ig, tmp8, m8, sel, ex, dn, wi = (sc[k] for k in ("gm", "ge", "gs", "oh", "ig", "tmp8", "m8", "sel", "ex", "dn", "wi"))
            S.op("dve", lambda: nc.vector.tensor_reduce(out=gm[:], in_=lg[:, 0:4], axis=AX.X, op=ALU.max), R=[lg], W=[gm])
            S.op("dve", lambda: nc.vector.tensor_scalar(out=oh[:], in0=lg[:, 0:4], scalar1=gm[:, 0:1], scalar2=None, op0=ALU.is_equal), R=[lg, gm], W=[oh])
            S.op("dve", lambda: nc.vector.tensor_scalar(out=ge[:], in0=lg[:, 0:4], scalar1=gm[:, 0:1], scalar2=None, op0=ALU.subtract), R=[lg, gm], W=[ge])
            S.op("act", lambda: nc.scalar.activation(out=ge[:], in_=ge[:], func=AF.Exp), R=[ge], W=[ge])
            S.op("dve", lambda: nc.vector.tensor_reduce(out=gs[:], in_=ge[:], axis=AX.X, op=ALU.add), R=[ge], W=[gs])
            S.op("dve", lambda: nc.vector.tensor_tensor(out=tmp8[:], in0=lg[:, 4:36].rearrange("p (g e) -> p g e", g=4),
                                                        in1=oh[:].unsqueeze(2).to_broadcast([128, 4, 8]), op=ALU.mult), R=[lg, oh], W=[tmp8])
            S.op("dve", lambda: nc.vector.tensor_reduce(out=ig[:], in_=tmp8[:].rearrange("p g e -> p e g"), axis=AX.X, op=ALU.add), R=[tmp8], W=[ig])
            S.op("dve", lambda: nc.vector.max(out=m8[:], in_=ig[:]), R=[ig], W=[m8])
            S.op("dve", lambda: nc.vector.tensor_scalar(out=sel[:], in0=ig[:], scalar1=m8[:, 1:2], scalar2=None, op0=ALU.is_ge), R=[ig, m8], W=[sel])
            S.op("dve", lambda: nc.vector.tensor_scalar(out=ex[:], in0=ig[:], scalar1=m8[:, 0:1], scalar2=None, op0=ALU.subtract), R=[ig, m8], W=[ex])
            S.op("act", lambda: nc.scalar.activation(out=ex[:], in_=ex[:], func=AF.Exp), R=[ex], W=[ex])
            S.op("dve", lambda: nc.vector.tensor_tensor(out=ex[:], in0=ex[:], in1=sel[:], op=ALU.mult), R=[ex, sel], W=[ex])
            S.op("dve", lambda: nc.vector.tensor_reduce(out=dn[:], in_=ex[:], axis=AX.X, op=ALU.add), R=[ex], W=[dn])
            S.op("dve", lambda: nc.vector.tensor_tensor(out=dn[:], in0=dn[:], in1=gs[:], op=ALU.mult), R=[dn, gs], W=[dn])
            S.op("dve", lambda: nc.vector.reciprocal(out=dn[:], in_=dn[:]), R=[dn], W=[dn])
            S.op("dve", lambda: nc.vector.tensor_scalar(out=wi[:], in0=ex[:], scalar1=dn[:, 0:1], scalar2=None, op0=ALU.mult), R=[ex, dn], W=[wi])
            S.op("dve", lambda: nc.vector.tensor_tensor(out=cb[:], in0=oh[:].unsqueeze(2).to_broadcast([128, 4, 8]),
                                                        in1=wi[:].unsqueeze(1).to_broadcast([128, 4, 8]), op=ALU.mult), R=[oh, wi], W=[cb])
            S.dma("sp", K.d["comb"][rows, :], cb[:].rearrange("p g e -> p (g e)"), R=[cb], W=[K.sbuf_comb])

        for t in range(NT + 1):
            if t < NT:
                stage1(t)
            if t >= 1:
                stage2(t - 1)


def phase_B(K, l, last):
    nc, S, NT = K.nc, K.S, K.NT
    NP = 4
    TH = (NT + NP - 1) // NP
    with contextlib.ExitStack() as es:
        g = _sb(K, es, "b_g", [128, D], F32)
        b = _sb(K, es, "b_b", [128, D], F32)
        S.dma("sp", g[:], K.d["ln2_g"][l].partition_broadcast(128), W=[g])
        S.dma("sp", b[:], K.d["ln2_b"][l].partition_broadcast(128), W=[b])
        xT = _sb(K, es, "b_xT", [128, KC, TH * 128], BF16)
        cbt = _sb(K, es, "b_cb", [128, TH, 32], F32)
        yacc = _sb(K, es, "b_y", [128, TH, D], F32)
        w1b = [_sb(K, es, "b_w1%d" % i, [128, KC, 256], BF16) for i in range(2)]
        w3b = [_sb(K, es, "b_w3%d" % i, [128, KC, 256], BF16) for i in range(2)]
        w2b = [_sb(K, es, "b_w2%d" % i, [128, 2, D], BF16) for i in range(2)]
        sil = [_sb(K, es, "b_sil%d" % i, [128, 512], F32) for i in range(2)]
        hid = [_sb(K, es, "b_hid%d" % i, [128, 2, 512], BF16) for i in range(2)]
        h1t = [_sb(K, es, "b_h1%d" % i, [128, D], F32) for i in range(1)]
        rr = [_sb(K, es, "b_r%d" % i, [128, D], F32) for i in range(1)]
        oo = [_sb(K, es, "b_o%d" % i, [128, D], F32) for i in range(2)]
        sm = {"st": _sb(K, es, "b_st", [128, 2, 6], F32), "mv": _sb(K, es, "b_mv", [128, 2], F32),
              "rstd": _sb(K, es, "b_rs", [128, 1], F32), "tmp": _sb(K, es, "b_tmp", [128, D], F32)}
        stgB = [_sb(K, es, "stgB%d" % i, [128, 2048], F32) for i in range(6)]
        ld = {"n": 0}

        def load_w(dst, src_ap, a_):
            k = ld["n"]
            ld["n"] += 1
            st = stgB[k % 6]
            v = st.h[:, 0:2048].rearrange("p (a b) -> p a b", a=a_)
            S.dma("sp" if k % 2 == 0 else "pool", v, src_ap, W=[st])
            if k % 3 == 2:
                S.op("pool", lambda: nc.gpsimd.tensor_copy(out=dst[:], in_=v), R=[st], W=[dst])
            else:
                S.op("act", lambda: nc.scalar.copy(out=dst[:], in_=v), R=[st], W=[dst])

        def load_expert(e):
            load_w(w1b[e % 2], K.d["w1"][l, e].rearrange("(k p) f -> p k f", p=128), KC)
            load_w(w3b[e % 2], K.d["w3"][l, e].rearrange("(k p) f -> p k f", p=128), KC)
            load_w(w2b[e % 2], K.d["w2"][l, e].rearrange("(k p) f -> p k f", p=128), 2)
        it = 0
        npass = len([h_ for h_ in range(NP) if h_ * TH < NT])
        wl = {"next": 2, "total": 32 * npass}
        load_expert(0)
        load_expert(1)
        for half in range(NP):
            t0 = half * TH
            nth = min(TH, NT - t0)
            if nth <= 0:
                break
            n_all = nth * 128
            S.dma("sp", xT[:, :, 0:n_all], K.d["h1T"][:, :, t0 * 128:t0 * 128 + n_all].rearrange("k p n -> p k n"), R=[K.sbuf_h1T], W=[xT])
            S.dma("sp", cbt[:, 0:nth, :], K.d["comb"][t0 * 128:t0 * 128 + n_all, :].rearrange("(t p) e -> p t e", p=128), R=[K.sbuf_comb], W=[cbt])
            S.op("pool", lambda: nc.gpsimd.memset(yacc[:, 0:nth, :], 0.0), W=[yacc])
            def up(e, gq, hd):
                wa, wc = w1b[e % 2], w3b[e % 2]
                nt = min(4, nth - 4 * gq)
                n = nt * 128
                c0 = gq * 512
                for f2 in range(2):
                    p1 = _ps(K)
                    for kc in range(KC):
                        S.op("pe", lambda: nc.tensor.matmul(p1[:, 0:n], lhsT=wa[:, kc, f2 * 128:(f2 + 1) * 128], rhs=xT[:, kc, c0:c0 + n],
                                                            start=(kc == 0), stop=(kc == KC - 1)), R=[wa, xT], W=[p1])
                    p3 = _ps(K)
                    for kc in range(KC):
                        S.op("pe", lambda: nc.tensor.matmul(p3[:, 0:n], lhsT=wc[:, kc, f2 * 128:(f2 + 1) * 128], rhs=xT[:, kc, c0:c0 + n],
                                                            start=(kc == 0), stop=(kc == KC - 1)), R=[wc, xT], W=[p3])
                    sl = sil[f2]
                    S.op("act", lambda: nc.scalar.activation(out=sl[:, 0:n], in_=p1[:, 0:n], func=AF.Silu), R=[p1], W=[sl])
                    S.op("dve", lambda: nc.vector.tensor_tensor(out=hd[:, f2, 0:n], in0=sl[:, 0:n], in1=p3[:, 0:n], op=ALU.mult), R=[sl, p3], W=[hd])

            def down(e, gq, hd):
                wd = w2b[e % 2]
                nt = min(4, nth - 4 * gq)
                for t in range(nt):
                    tt = 4 * gq + t
                    for ch in range(2):
                        py = _ps(K)
                        for f2 in range(2):
                            S.op("pe", lambda: nc.tensor.matmul(py[:, :], lhsT=hd[:, f2, t * 128:(t + 1) * 128], rhs=wd[:, f2, ch * 512:(ch + 1) * 512],
                                                                start=(f2 == 0), stop=(f2 == 1)), R=[hd, wd], W=[py])
                        S.op("dve", lambda: nc.vector.scalar_tensor_tensor(out=yacc[:, tt, ch * 512:(ch + 1) * 512], in0=py[:, :],
                                                                          scalar=cbt[:, tt, e:e + 1], in1=yacc[:, tt, ch * 512:(ch + 1) * 512],
                                                                          op0=ALU.mult, op1=ALU.add), R=[py, cbt, yacc], W=[yacc])

            ngq = (nth + 3) // 4
            items = [(e, gq) for e in range(32) for gq in range(ngq)]
            up(items[0][0], items[0][1], hid[it % 2])
            for k, (e, gq) in enumerate(items):
                if k + 1 < len(items):
                    up(items[k + 1][0], items[k + 1][1], hid[(it + 1) % 2])
                down(e, gq, hid[it % 2])
                it += 1
                if gq == ngq - 1:
                    if wl["next"] < wl["total"]:
                        load_expert(wl["next"] % 32)
                        wl["next"] += 1
            for t in range(nth):
                tg = t0 + t
                rows = slice(128 * tg, 128 * (tg + 1))
                h1, r, o = h1t[0], rr[0], oo[t % 2]
                S.dma("sp", h1[:], K.d["h1"][rows, :], R=[K.h1buf[tg]], W=[h1])
                S.op("dve", lambda: nc.vector.scalar_tensor_tensor(out=r[:], in0=h1[:], scalar=ALPHA, in1=yacc[:, t, :], op0=ALU.mult, op1=ALU.add),
                     R=[h1, yacc], W=[r])
                _ln_tile(K, r, o, g, b, sm)
                if not last:
                    S.dma("sp", K.d["h"][rows, :], o[:], R=[o], W=[K.hbuf[tg]])
                else:
                    lo = 128 * tg - 64
                    r0, r1 = max(lo, 0), min(lo + 128, K.SEQ)
                    if r1 > r0:
                        S.dma("sp", K.d["out"][r0:r1, :], o[r0 - lo:r1 - lo, :], R=[o], W=[K.outbuf])


def make_consts():
    c = {}
    c["identf"] = np.eye(128, dtype=np.float32)
    c["identb"] = np.eye(128, dtype=np.float32).astype(ml_dtypes.bfloat16)
    s = np.arange(128)[:, None]
    masks = np.zeros((6, 128, 512), np.float32)
    for rel in range(4):
        for qt in range(4):
            blk = masks[rel, :, qt * 128:(qt + 1) * 128]
            if qt < rel:
                blk[:] = NEG
            elif qt == rel:
                blk[:] = np.where(s < np.arange(128)[None, :], 0.0, NEG)
    masks[4] = masks[0]
    masks[4, 0:48, :] = NEG
    masks[5, 0:48, :] = NEG
    c["masks"] = np.ascontiguousarray(masks.transpose(1, 0, 2)).astype(ml_dtypes.bfloat16)
    c["negtri"] = np.where(s >= np.arange(128)[None, :], -1.0, 0.0).astype(ml_dtypes.bfloat16)
    c["onesb"] = np.ones((128, 1), np.float32).astype(ml_dtypes.bfloat16)
    c["onesf"] = np.ones((128, 128), np.float32)
    m = np.arange(64)[:, None]
    i = np.arange(64)[None, :]
    triu = (m <= i).astype(np.float32)
    c["triu"] = np.ascontiguousarray(np.repeat(triu[:, None, :], 4, axis=1))
    c["sgt"] = (m > i).astype(np.float32)
    c["ones64"] = np.ones((64, 128), np.float32)
    c["ones4"] = np.ones((64, 4, 128), np.float32)
    c["trilsn"] = np.ascontiguousarray(np.repeat((-(m > i).astype(np.float32))[:, None, :], 4, axis=1))
    c["ident4"] = np.ascontiguousarray(np.repeat(np.eye(64, dtype=np.float32)[:, None, :], 4, axis=1))
    c["cvec"] = np.tile(np.array([[1.0, LN_EPS, RMS_EPS, 0.0, 64.0 * RMS_EPS, 128.0 * RMS_EPS]], np.float32), (128, 1))
    return c


CONST_DT = {"identb": BF16, "masks": BF16, "negtri": BF16, "onesb": BF16}

IN_SHAPES = lambda SEQ, depth: {
    "x": [SEQ, D], "meta": [16, D], "ln_in_g": [D], "ln_in_b": [D], "w_in": [depth, D, IN_W], "conv_wT": [depth, 1536, 4],
    "a_log": [depth, 4], "dt_bias": [depth, 4], "sb_norm_g": [depth, 64], "gdn_norm_g": [depth, 128], "w_out": [depth, D, D],
    "ln1_g": [depth, D], "ln1_b": [depth, D], "w_group": [depth, D, 4], "b_group": [depth, 4], "w_expert": [depth, D, 32],
    "b_expert": [depth, 32], "w1": [depth, 32, D, 256], "w3": [depth, 32, D, 256], "w2": [depth, 32, 256, D],
    "ln2_g": [depth, D], "ln2_b": [depth, D]}


def build(SEQ, depth, debug=False, same_eng=True, phases=None, max_ops=None, log=None):
    nc = bass.Bass("TRN2", target_bir_lowering=False)
    K = Ctx()
    K.nc = nc
    K.SEQ = SEQ
    PT = SEQ + 64
    K.NT = NT = (PT + 127) // 128
    K.NC = PT // 64
    P = NT * 128
    K.d = {}
    for name, shp in IN_SHAPES(SEQ, depth).items():
        K.d[name] = nc.dram_tensor(name, shp, F32, kind="ExternalInput").ap()
    consts = make_consts()
    for name, arr in consts.items():
        K.d["c_" + name] = nc.dram_tensor("c_" + name, list(arr.shape), CONST_DT.get(name, F32), kind="ExternalInput").ap()
    K.d["out"] = nc.dram_tensor("out", [SEQ, D], F32, kind="ExternalOutput").ap()
    kind = "ExternalOutput" if debug else "Internal"
    for name, shp, dt in (("h", [P, D], F32), ("qT", [4, 128, P], BF16), ("kT", [4, 128, P], BF16), ("V", [P, 512], BF16),
                          ("gT", [12, 128, P], F32), ("zs", [P, 512], F32), ("bg", [P, 8], F32), ("mixed", [P, D], BF16),
                          ("h1", [P, D], F32), ("h1T", [KC, 128, P], BF16), ("comb", [P, 32], F32)):
        K.d[name] = nc.dram_tensor("s_" + name, shp, dt, kind=kind).ap()
    K.hbuf = [Buf() for _ in range(NT)]
    K.h1buf = [Buf() for _ in range(NT)]
    for nm in ("sbuf_q", "sbuf_k", "sbuf_v", "sbuf_g", "sbuf_z", "sbuf_bg", "sbuf_mx_sb", "sbuf_mx_gd", "sbuf_h1T", "sbuf_comb", "outbuf"):
        setattr(K, nm, Buf())
    with contextlib.ExitStack() as es:
        K.S = S = Sched(nc, es, same_eng=same_eng)
        S.max_ops = max_ops
        S.log = log
        K.psum = [Tl(es.enter_context(nc.psum_tensor("ps%d" % i, [128, 512], F32)), "ps%d" % i) for i in range(8)]
        K.psi = 0
        for p_ in K.psum:
            p_.b.excl = True
        K.stgi = 0
        for name, arr in consts.items():
            if name == "cvec":
                continue
            tl = _sb(K, es, "k_" + name, list(arr.shape), CONST_DT.get(name, F32))
            setattr(K, name, tl)
            S.dma("sp", tl[:], K.d["c_" + name], W=[tl])
        cv = _sb(K, es, "k_cvec", [128, 6], F32)
        S.dma("sp", cv[:], K.d["c_cvec"], W=[cv])
        K.one_c = Tl(cv.h[:, 0:1]); K.one_c.b = cv.b
        K.eps_ln = Tl(cv.h[:, 1:2]); K.eps_ln.b = cv.b
        K.eps_rms = Tl(cv.h[:, 2:3]); K.eps_rms.b = cv.b
        K.eps_rms64 = Tl(cv.h[:, 4:5]); K.eps_rms64.b = cv.b
        K.eps_rms128 = Tl(cv.h[:, 5:6]); K.eps_rms128.b = cv.b
        ph = phases or ("in", "A1", "SB", "GDN", "A3", "B")
        if "in" in ph:
            phase_input(K)
            S.barrier()
        for l in range(depth):
            if "A1" in ph:
                phase_A1(K, l)
                S.barrier()
            if INTERLEAVE_GDN:
                if "SB" in ph or "GDN" in ph:
                    phase_SBGDN(K, l, run_sb=("SB" in ph), run_gdn=("GDN" in ph))
                    S.barrier()
            else:
                if "SB" in ph:
                    phase_SBGDN(K, l, run_sb=True, run_gdn=False)
                    S.barrier()
                if "GDN" in ph:
                    phase_GDN_old(K, l)
                    S.barrier()
            if "A3" in ph:
                phase_A3(K, l)
                S.barrier()
            if "B" in ph:
                phase_B(K, l, l == depth - 1)
                S.barrier()
        S.finish()
    K.consts = consts
    return nc, K


def make_in_maps(inputs, SEQ, depth, consts, n_cores=8):
    shared = {}
    for k in ("ln_in_g", "ln_in_b", "w_in", "a_log", "dt_bias", "sb_norm_g", "gdn_norm_g", "w_out", "ln1_g", "ln1_b",
              "w_group", "b_group", "w_expert", "b_expert", "w1", "w3", "w2", "ln2_g", "ln2_b"):
        shared[k] = np.ascontiguousarray(np.asarray(inputs[k], dtype=np.float32))
    shared["meta"] = np.ascontiguousarray(np.asarray(inputs["meta_tokens"], dtype=np.float32))
    shared["conv_wT"] = np.ascontiguousarray(np.asarray(inputs["conv_w"], dtype=np.float32).transpose(0, 2, 1))
    for k, v in consts.items():
        shared["c_" + k] = v
    x = np.asarray(inputs["x"], dtype=np.float32)
    B = x.shape[0]
    maps = []
    for c in range(n_cores):
        m = dict(shared)
        m["x"] = np.ascontiguousarray(x[c % B])
        maps.append(m)
    return maps


def kernel(**inputs):
    x = np.asarray(inputs["x"])
    B, SEQ, _ = x.shape
    depth = np.asarray(inputs["w_in"]).shape[0]
    nc, K = build(SEQ, depth)
    maps = make_in_maps(inputs, SEQ, depth, K.consts)
    res = run_bass_kernel_spmd(nc, maps, core_ids=list(range(8)))
    out = np.stack([np.asarray(res.results[b]["out"], dtype=np.float32) for b in range(B)], axis=0)
    return out
```

```python
import contextlib
import numpy as np
import ml_dtypes
import concourse.bass as bass
import concourse.mybir as mybir
from concourse.bass_utils import run_bass_kernel_spmd

F32 = mybir.dt.float32
BF16 = mybir.dt.bfloat16
AF = mybir.ActivationFunctionType
ALU = mybir.AluOpType
AX = mybir.AxisListType

D = 1024
KC = 8
DEPTH = 4
IN_W = 3592
ALPHA = float((2 * DEPTH) ** 0.25)
LN_EPS = 1e-5
RMS_EPS = 1e-6
NEG = -30000.0
INTERLEAVE_GDN = True
DMA_PAD = 3
GDN_YK = 1


class Ev:
    __slots__ = ("key", "val", "snap", "src")

    def __init__(self, key, val, snap, src):
        self.key, self.val, self.snap, self.src = key, val, snap, src


class Buf:
    __slots__ = ("w", "r", "name", "excl")

    def __init__(self, name=""):
        self.w = None
        self.r = {}
        self.name = name
        self.excl = False


class Tl:
    def __init__(self, h, name=""):
        self.h = h
        self.b = Buf(name)

    def __getitem__(self, idx):
        return self.h[idx]


ENG = ("pe", "act", "dve", "pool", "sp")


class Sched:
    def __init__(self, nc, es, ndma=8, same_eng=True):
        self.nc = nc
        self.e = {"pe": nc.tensor, "act": nc.scalar, "dve": nc.vector, "pool": nc.gpsimd, "sp": nc.sync}
        self.sem = {k: es.enter_context(nc.semaphore("sem_" + k)) for k in ENG}
        self.cnt = {k: 0 for k in ENG}
        self.seen = {k: {} for k in ENG}
        self.dq = {}
        self.semobj = {"c:" + k: self.sem[k] for k in ENG}
        for q in ("sp", "pool"):
            sems = [es.enter_context(nc.semaphore("dma_%s_%d" % (q, i))) for i in range(ndma)]
            self.dq[q] = {"sems": sems, "n": 0, "pending": [None] * ndma}
            for i, sm in enumerate(sems):
                self.semobj["d:%s:%d" % (q, i)] = sm
        self.same_eng = same_eng
        self.nwait = 0
        self.max_ops = None
        self.log = None
        self.nins = 0
        self.rr = 0

    def _wait(self, eng, ev):
        if ev is None:
            return
        seen = self.seen[eng]
        if seen.get(ev.key, 0) >= ev.val:
            return
        if ev.src == eng and (eng == "pe" or not self.same_eng):
            return
        self.e[eng].wait_ge(self.semobj[ev.key], ev.val)
        if self.log is not None:
            self.log.append((self.nins, eng, "WAIT %s >= %d" % (ev.key, ev.val)))
        self.nwait += 1
        new = dict(seen)
        for k, v in ev.snap.items():
            if new.get(k, 0) < v:
                new[k] = v
        if new.get(ev.key, 0) < ev.val:
            new[ev.key] = ev.val
        self.seen[eng] = new

    def _deps(self, eng, R, W):
        for b in R:
            self._wait(eng, b.w)
            if b.excl:
                for k, ev in list(b.r.items()):
                    if k != eng:
                        self._wait(eng, ev)
        for b in W:
            self._wait(eng, b.w)
            for ev in list(b.r.values()):
                self._wait(eng, ev)

    def op(self, eng, fn, R=(), W=()):
        if self.max_ops is not None and self.nins >= self.max_ops:
            return None
        R = [getattr(x, "b", x) for x in R]
        W = [getattr(x, "b", x) for x in W]
        self._deps(eng, R, W)
        ins = fn()
        if self.log is not None:
            self.log.append((self.nins, eng, str(ins)[:150]))
        self.cnt[eng] += 1
        self.nins += 1
        ins.then_inc(self.sem[eng], 1)
        ev = Ev("c:" + eng, self.cnt[eng], self.seen[eng], eng)
        for b in R:
            b.r[eng] = ev
        for b in W:
            b.w = ev
            b.r = {}
        return ev

    def dma(self, q, out, in_, R=(), W=(), **kw):
        if self.max_ops is not None and self.nins >= self.max_ops:
            return None
        R = [getattr(x, "b", x) for x in R]
        W = [getattr(x, "b", x) for x in W]
        dq = self.dq[q]
        ns = len(dq["sems"])
        i = dq["n"] % ns
        self._wait(q, dq["pending"][i])
        self._deps(q, R, W)
        ins = self.e[q].dma_start(out=out, in_=in_, **kw)
        val = 16 * (dq["n"] // ns + 1)
        ins.then_inc(dq["sems"][i], 16)
        ev = Ev("d:%s:%d" % (q, i), val, self.seen[q], "dma")
        dq["pending"][i] = ev
        dq["n"] += 1
        self.nins += 1
        for b in R:
            b.r[("dma", q, dq["n"])] = ev
        for b in W:
            b.w = ev
            b.r = {}
        return ev

    def barrier(self):
        evs = [Ev("c:" + k, self.cnt[k], {}, "x") for k in ENG if self.cnt[k] > 0]
        for dq in self.dq.values():
            evs += [p for p in dq["pending"] if p is not None]
        for eng in ENG:
            for ev in evs:
                self._wait(eng, ev)

    def finish(self):
        for dq in self.dq.values():
            for p in dq["pending"]:
                self._wait("sp", p)

    def alt(self):
        self.rr += 1
        return "act" if (self.rr & 1) else "dve"


class Ctx:
    pass


def _sb(K, es, name, shape, dt):
    K.uid = getattr(K, "uid", 0) + 1
    name = "%s_u%d" % (name, K.uid)
    return Tl(es.enter_context(K.nc.sbuf_tensor(name, list(shape), dt)), name)


def _evac(K, eng, out, in_, R, W, scale=None):
    nc, S = K.nc, K.S
    if eng == "act":
        if scale is None:
            S.op("act", lambda: nc.scalar.copy(out=out, in_=in_), R=R, W=W)
        else:
            S.op("act", lambda: nc.scalar.mul(out=out, in_=in_, mul=scale), R=R, W=W)
    else:
        if scale is None:
            S.op("dve", lambda: nc.vector.tensor_copy(out=out, in_=in_), R=R, W=W)
        else:
            S.op("dve", lambda: nc.vector.tensor_scalar_mul(out=out, in0=in_, scalar1=scale), R=R, W=W)


def _ps(K):
    K.psi = (K.psi + 1) % len(K.psum)
    return K.psum[K.psi]


def _ln_tile(K, x, out, g, b, sm):
    nc, S = K.nc, K.S
    st, mv, rstd, tmp = sm["st"], sm["mv"], sm["rstd"], sm["tmp"]
    S.op("dve", lambda: nc.vector.bn_stats(out=st[:, 0, :], in_=x[:, 0:512]), R=[x], W=[st])
    S.op("dve", lambda: nc.vector.bn_stats(out=st[:, 1, :], in_=x[:, 512:1024]), R=[x, st], W=[st])
    S.op("dve", lambda: nc.vector.bn_aggr(out=mv[:], in_=st[:].rearrange("p a b -> p (a b)")), R=[st], W=[mv])
    S.op("act", lambda: nc.scalar.activation(out=rstd[:], in_=mv[:, 1:2], func=AF.Sqrt, bias=K.eps_ln[:, 0:1], scale=1.0),
         R=[mv, K.eps_ln], W=[rstd])
    S.op("dve", lambda: nc.vector.reciprocal(out=rstd[:], in_=rstd[:]), R=[rstd], W=[rstd])
    S.op("dve", lambda: nc.vector.tensor_scalar(out=tmp[:], in0=x[:], scalar1=mv[:, 0:1], scalar2=rstd[:, 0:1],
                                                op0=ALU.subtract, op1=ALU.mult), R=[x, mv, rstd], W=[tmp])
    S.op("dve", lambda: nc.vector.tensor_tensor(out=tmp[:], in0=tmp[:], in1=g[:], op=ALU.mult), R=[tmp, g], W=[tmp])
    S.op("dve", lambda: nc.vector.tensor_tensor(out=out[:], in0=tmp[:], in1=b[:], op=ALU.add), R=[tmp, b], W=[out])


def _load_cast(K, dst_tl, dst_ap, src_ap, shape, Wb):
    nc, S = K.nc, K.S
    K.stgi = (K.stgi + 1) % len(K.stg)
    st = K.stg[K.stgi]
    n = int(np.prod(shape[1:]))
    if len(shape) == 3:
        v = st.h[:, 0:n].rearrange("p (a b) -> p a b", a=shape[1])
    else:
        v = st.h[:, 0:n]
    S.dma("sp", v, src_ap, W=[st])
    S.op("pool", lambda: nc.gpsimd.tensor_copy(out=dst_ap, in_=v), R=[st], W=[Wb])


def phase_input(K):
    nc, S, NT = K.nc, K.S, K.NT
    with contextlib.ExitStack() as es:
        g = _sb(K, es, "pi_g", [128, D], F32)
        b = _sb(K, es, "pi_b", [128, D], F32)
        S.dma("sp", g[:], K.d["ln_in_g"].partition_broadcast(128), W=[g])
        S.dma("sp", b[:], K.d["ln_in_b"].partition_broadcast(128), W=[b])
        xs = [_sb(K, es, "pi_x%d" % i, [128, D], F32) for i in range(2)]
        os_ = [_sb(K, es, "pi_o%d" % i, [128, D], F32) for i in range(2)]
        sm = {"st": _sb(K, es, "pi_st", [128, 2, 6], F32), "mv": _sb(K, es, "pi_mv", [128, 2], F32),
              "rstd": _sb(K, es, "pi_rs", [128, 1], F32), "tmp": _sb(K, es, "pi_tmp", [128, D], F32)}
        PT = K.SEQ + 64
        if NT * 128 > PT:
            zb = _sb(K, es, "pi_zb", [128, 512], BF16)
            S.op("pool", lambda: nc.gpsimd.memset(zb[:], 0.0), W=[zb])
            S.dma("sp", K.d["mixed"][PT:NT * 128, 512:1024], zb[0:NT * 128 - PT, :], R=[zb], W=[K.sbuf_mx_gd])
        for t in range(NT):
            x, o = xs[t % 2], os_[t % 2]
            lo = 128 * t - 64
            r0, r1 = max(lo, 0), min(lo + 128, K.SEQ)
            if t == 0 or r1 - lo < 128:
                S.op("pool", lambda: nc.gpsimd.memset(x[:], 0.0), W=[x])
            if t == 0:
                S.dma("sp", x[48:64, :], K.d["meta"][:, :], W=[x])
            if r1 > r0:
                S.dma("sp", x[r0 - lo:r1 - lo, :], K.d["x"][r0:r1, :], W=[x])
            _ln_tile(K, x, o, g, b, sm)
            S.dma("pool", K.d["h"][128 * t:128 * (t + 1), :], o[:], R=[o], W=[K.hbuf[t]])


def phase_A1(K, l):
    nc, S, NT = K.nc, K.S, K.NT
    with contextlib.ExitStack() as es:
        K.stg = [_sb(K, es, "stg%d" % i, [128, IN_W], F32) for i in range(2)]
        Wb = _sb(K, es, "a1_W", [128, KC, IN_W], BF16)
        wbufs = [Buf() for _ in range(KC)]
        wsrc = K.d["w_in"][l].rearrange("(k p) n -> p k n", p=128)
        for kc in range(KC):
            _load_cast(K, Wb, Wb[:, kc, :], wsrc[:, kc, :], [128, IN_W], wbufs[kc])
        dtb = _sb(K, es, "a1_dtb", [128, 4], F32)
        nea = _sb(K, es, "a1_nea", [128, 4], F32)
        S.dma("sp", dtb[:], K.d["dt_bias"][l].partition_broadcast(128), W=[dtb])
        S.dma("sp", nea[:], K.d["a_log"][l].partition_broadcast(128), W=[nea])
        S.op("act", lambda: nc.scalar.activation(out=nea[:], in_=nea[:], func=AF.Exp), R=[nea], W=[nea])
        S.op("dve", lambda: nc.vector.tensor_scalar_mul(out=nea[:], in0=nea[:], scalar1=-1.0), R=[nea], W=[nea])
        hts = [_sb(K, es, "a1_h%d" % i, [128, 4, D], F32) for i in range(1)]
        hTs = [_sb(K, es, "a1_hT%d" % i, [128, KC, 512], BF16) for i in range(2)]
        stq = [_sb(K, es, "a1_sq%d" % i, [128, 512], BF16) for i in range(3)]
        stg = [_sb(K, es, "a1_sg%d" % i, [128, 512], F32) for i in range(3)]
        stv = [_sb(K, es, "a1_sv%d" % i, [128, 4, 512], BF16) for i in range(2)]
        stz = [_sb(K, es, "a1_sz%d" % i, [128, 4, 512], F32) for i in range(2)]
        stb = [_sb(K, es, "a1_sb%d" % i, [128, 4, 8], F32) for i in range(2)]
        tb = _sb(K, es, "a1_tb", [128, 4, 4], F32)
        NG = (NT + 3) // 4
        for g in range(NG):
            nt = min(4, NT - 4 * g)
            n = nt * 128
            c0 = g * 512
            ht, hT = hts[0], hTs[g % 2]
            sv, sz, sbb = stv[g % 2], stz[g % 2], stb[g % 2]
            S.dma("sp", ht[:, 0:nt, :], K.d["h"][c0:c0 + n, :].rearrange("(t p) d -> p t d", p=128),
                  R=K.hbuf[4 * g:4 * g + nt], W=[ht])
            for kc in range(KC):
                ps = _ps(K)
                for t in range(nt):
                    S.op("pe", lambda: nc.tensor.transpose(out=ps[:, t * 128:(t + 1) * 128],
                                                           in_=ht[:, t, kc * 128:(kc + 1) * 128], identity=K.identf[:]),
                         R=[ht, K.identf], W=[ps])
                _evac(K, S.alt(), hT[:, kc, 0:n], ps[:, 0:n], [ps], [hT])
            for ci in range(20):
                if ci < 8:
                    col = ci * 128
                else:
                    col = 1536 + (ci - 8) * 128
                ps = _ps(K)
                for kc in range(KC):
                    S.op("pe", lambda: nc.tensor.matmul(ps[:, 0:n], lhsT=Wb[:, kc, col:col + 128], rhs=hT[:, kc, 0:n],
                                                        start=(kc == 0), stop=(kc == KC - 1)),
                         R=[wbufs[kc], hT], W=[ps])
                if ci < 8:
                    sq = stq[ci % 3]
                    _evac(K, S.alt(), sq[:, 0:n], ps[:, 0:n], [ps], [sq], scale=(0.125 if ci < 4 else None))
                    if ci < 4:
                        S.dma("pool", K.d["qT"][ci, :, c0:c0 + n], sq[:, 0:n], R=[sq], W=[K.sbuf_q])
                    else:
                        S.dma("pool", K.d["kT"][ci - 4, :, c0:c0 + n], sq[:, 0:n], R=[sq], W=[K.sbuf_k])
                else:
                    sg = stg[ci % 3]
                    _evac(K, S.alt(), sg[:, 0:n], ps[:, 0:n], [ps], [sg])
                    S.dma("pool", K.d["gT"][ci - 8, :, c0:c0 + n], sg[:, 0:n], R=[sg], W=[K.sbuf_g])
            for t in range(nt):
                ps = _ps(K)
                for kc in range(KC):
                    S.op("pe", lambda: nc.tensor.matmul(ps[:, :], lhsT=hT[:, kc, t * 128:(t + 1) * 128], rhs=Wb[:, kc, 1024:1536],
                                                        start=(kc == 0), stop=(kc == KC - 1)), R=[wbufs[kc], hT], W=[ps])
                _evac(K, S.alt(), sv[:, t, :], ps[:, :], [ps], [sv])
                ps = _ps(K)
                for kc in range(KC):
                    S.op("pe", lambda: nc.tensor.matmul(ps[:, :], lhsT=hT[:, kc, t * 128:(t + 1) * 128], rhs=Wb[:, kc, 3072:3584],
                                                        start=(kc == 0), stop=(kc == KC - 1)), R=[wbufs[kc], hT], W=[ps])
                S.op("act", lambda: nc.scalar.activation(out=sz[:, t, :], in_=ps[:, :], func=AF.Silu), R=[ps], W=[sz])
                ps = _ps(K)
                for kc in range(KC):
                    S.op("pe", lambda: nc.tensor.matmul(ps[:, 0:8], lhsT=hT[:, kc, t * 128:(t + 1) * 128], rhs=Wb[:, kc, 3584:3592],
                                                        start=(kc == 0), stop=(kc == KC - 1)), R=[wbufs[kc], hT], W=[ps])
                S.op("act", lambda: nc.scalar.activation(out=sbb[:, t, 0:4], in_=ps[:, 0:4], func=AF.Sigmoid), R=[ps], W=[sbb])
                S.op("dve", lambda: nc.vector.tensor_tensor(out=tb[:, t, :], in0=ps[:, 4:8], in1=dtb[:], op=ALU.add),
                     R=[ps, dtb], W=[tb])
            S.op("act", lambda: nc.scalar.activation(out=tb[:, 0:nt, :], in_=tb[:, 0:nt, :], func=AF.Exp), R=[tb], W=[tb])
            S.op("act", lambda: nc.scalar.activation(out=tb[:, 0:nt, :], in_=tb[:, 0:nt, :], func=AF.Ln, bias=K.one_c[:, 0:1], scale=1.0),
                 R=[tb, K.one_c], W=[tb])
            S.op("dve", lambda: nc.vector.tensor_tensor(out=sbb[:, 0:nt, 4:8], in0=tb[:, 0:nt, :],
                                                        in1=nea[:].unsqueeze(1).to_broadcast([128, nt, 4]), op=ALU.mult),
                 R=[tb, nea], W=[sbb])
            S.dma("pool", K.d["V"][c0:c0 + n, :].rearrange("(t p) c -> p t c", p=128), sv[:, 0:nt, :], R=[sv], W=[K.sbuf_v])
            S.dma("pool", K.d["zs"][c0:c0 + n, :].rearrange("(t p) c -> p t c", p=128), sz[:, 0:nt, :], R=[sz], W=[K.sbuf_z])
            S.dma("pool", K.d["bg"][c0:c0 + n, :].rearrange("(t p) c -> p t c", p=128), sbb[:, 0:nt, :], R=[sbb], W=[K.sbuf_bg])


class RR:
    def __init__(self):
        self.items = []

    def add(self, gen, w=1):
        self.items.append([gen, w])

    def run(self):
        while self.items:
            for item in list(self.items):
                for _ in range(item[1]):
                    try:
                        next(item[0])
                    except StopIteration:
                        self.items.remove(item)
                        break


def _psg(K):
    K.psgi = (K.psgi + 1) % len(K.psum_gd)
    return K.psum_gd[K.psgi]


def sb_stream(K, l, es):
    nc, S, NT = K.nc, K.S, K.NT
    P = NT * 128
    qT = _sb(K, es, "sb_q", [128, P], BF16)
    kT = _sb(K, es, "sb_k", [128, P], BF16)
    Vt = _sb(K, es, "sb_v", [128, NT, 128], BF16)
    sbg = _sb(K, es, "sb_g", [128, 64], F32)
    S.dma("sp", sbg[:], K.d["sb_norm_g"][l].partition_broadcast(128), W=[sbg])
    es_ = [_sb(K, es, "sb_e%d" % i, [128, 512], F32) for i in range(2)]
    sps = [_sb(K, es, "sb_sp%d" % i, [128, 512], BF16) for i in range(4)]
    ws = [_sb(K, es, "sb_w%d" % i, [128, 512], BF16) for i in range(3)]
    oaccs = [_sb(K, es, "sb_oa%d" % i, [128, 4, 64], F32) for i in range(2)]
    raccs = [_sb(K, es, "sb_ra%d" % i, [128, 4], F32) for i in range(2)]
    eRs = [_sb(K, es, "sb_eR%d" % i, [128, 4], F32) for i in range(2)]
    tmps = [_sb(K, es, "sb_tmp%d" % i, [128, 4, 64], F32) for i in range(2)]
    osbs = [_sb(K, es, "sb_os%d" % i, [128, 4, 128], BF16) for i in range(2)]
    ss = _sb(K, es, "sb_ss", [128, 4], F32)
    zb = K.psum[0:3]
    pb = K.psum[3:5]
    NG = (NT + 3) // 4
    gi = 0
    for hp in range(4):
        S.dma("sp", qT[:, :], K.d["qT"][hp], R=[K.sbuf_q], W=[qT])
        S.dma("sp", kT[:, :], K.d["kT"][hp], R=[K.sbuf_k], W=[kT])
        S.dma("sp", Vt[:, :, :], K.d["V"][:, hp * 128:(hp + 1) * 128].rearrange("(t p) c -> p t c", p=128), R=[K.sbuf_v], W=[Vt])
        its = []
        for qg in range(NG):
            nt = min(4, NT - 4 * qg)
            for h2 in range(2):
                kbs = list(range(4 * qg + nt - 1, -1, -1))
                for j, kb in enumerate(kbs):
                    its.append((qg, nt, h2, kb, j == 0, j == len(kbs) - 1, gi))
                gi += 1
        N = len(its)

        def geom(it):
            qg, nt, h2, kb = it[0], it[1], it[2], it[3]
            rel = kb - 4 * qg
            if rel >= 0:
                mi = 4 if kb == 0 else rel
            elif kb == 0:
                mi = 5
            else:
                mi = None
            lo = max(rel, 0)
            return rel, mi, lo, lo * 128, nt * 128, qg * 512

        def stA(i):
            it = its[i]
            qg, nt, h2, kb = it[0], it[1], it[2], it[3]
            rel, mi, lo, c0, n, q0 = geom(it)
            z, e, sp = zb[i % 3], es_[i % 2], sps[i % 4]
            r0 = h2 * 64
            kk = kT[r0:r0 + 64, kb * 128:(kb + 1) * 128]
            qq = qT[r0:r0 + 64, q0 + c0:q0 + n]
            S.op("pe", lambda: nc.tensor.matmul(z[:, c0:n], lhsT=kk, rhs=qq, start=True, stop=(mi is None)), R=[kT, qT], W=[z])
            if mi is not None:
                S.op("pe", lambda: nc.tensor.matmul(z[:, c0:n], lhsT=K.identb[:], rhs=K.masks[:, mi, c0:n], start=False, stop=True),
                     R=[K.identb, K.masks], W=[z])
            S.op("act", lambda: nc.scalar.activation(out=e[:, c0:n], in_=z[:, c0:n], func=AF.Exp), R=[z], W=[e])
            S.op("act", lambda: nc.scalar.activation(out=sp[:, c0:n], in_=e[:, c0:n], func=AF.Ln, bias=K.one_c[:, 0:1], scale=1.0),
                 R=[e, K.one_c], W=[sp])

        def stB(i):
            it = its[i]
            qg, nt, h2, kb = it[0], it[1], it[2], it[3]
            rel, mi, lo, c0, n, q0 = geom(it)
            z, sp, w = zb[i % 3], sps[i % 4], ws[i % 3]
            r0 = h2 * 64
            kk = kT[r0:r0 + 64, kb * 128:(kb + 1) * 128]
            qq = qT[r0:r0 + 64, q0 + c0:q0 + n]
            S.op("pe", lambda: nc.tensor.matmul(z[:, c0:n], lhsT=kk, rhs=qq, start=True, stop=False), R=[kT, qT], W=[z])
            if mi is not None:
                S.op("pe", lambda: nc.tensor.matmul(z[:, c0:n], lhsT=K.identb[:], rhs=K.masks[:, mi, c0:n], start=False, stop=False),
                     R=[K.identb, K.masks], W=[z])
            S.op("pe", lambda: nc.tensor.matmul(z[:, c0:n], lhsT=K.negtri[:], rhs=sp[:, c0:n], start=False, stop=True),
                 R=[K.negtri, sp], W=[z])
            S.op("act", lambda: nc.scalar.activation(out=w[:, c0:n], in_=z[:, c0:n], func=AF.Exp), R=[z], W=[w])

        def stC(i):
            it = its[i]
            qg, nt, h2, kb, first, last, g_ = it
            rel, mi, lo, c0, n, q0 = geom(it)
            sp, w = sps[i % 4], ws[i % 3]
            oacc, racc = oaccs[g_ % 2], raccs[g_ % 2]
            eR, tmp = eRs[i % 2], tmps[i % 2]
            osb = osbs[qg % 2]
            if first:
                S.op("pool", lambda: nc.gpsimd.memset(oacc[:], 0.0), W=[oacc])
                S.op("pool", lambda: nc.gpsimd.memset(racc[:], 0.0), W=[racc])
            po = pb[i % 2]
            pov = po.h[:, 0:260].rearrange("p (t c) -> p t c", c=65)
            for qt in range(lo, nt):
                S.op("pe", lambda: nc.tensor.matmul(pov[:, qt, 0:64], lhsT=w[:, qt * 128:(qt + 1) * 128],
                                                    rhs=Vt[:, kb, h2 * 64:(h2 + 1) * 64], start=True, stop=True),
                     R=[w, Vt], W=[po])
                S.op("pe", lambda: nc.tensor.matmul(pov[:, qt, 64:65], lhsT=sp[:, qt * 128:(qt + 1) * 128],
                                                    rhs=K.onesb[:, 0:1], start=True, stop=True),
                     R=[sp, K.onesb], W=[po])
            S.op("act", lambda: nc.scalar.activation(out=eR[:, lo:nt], in_=racc[:, lo:nt], func=AF.Exp, scale=-1.0),
                 R=[racc], W=[eR])
            S.op("dve", lambda: nc.vector.tensor_tensor(out=tmp[:, lo:nt, :], in0=pov[:, lo:nt, 0:64],
                                                        in1=eR[:, lo:nt].unsqueeze(2).to_broadcast([128, nt - lo, 64]), op=ALU.mult),
                 R=[po, eR], W=[tmp])
            S.op("pool", lambda: nc.gpsimd.tensor_tensor(out=oacc[:, lo:nt, :], in0=oacc[:, lo:nt, :], in1=tmp[:, lo:nt, :], op=ALU.add),
                 R=[oacc, tmp], W=[oacc])
            S.op("dve", lambda: nc.vector.tensor_tensor(out=racc[:, lo:nt], in0=racc[:, lo:nt], in1=pov[:, lo:nt, 64], op=ALU.add),
                 R=[racc, po], W=[racc])
            if last:
                tmp2 = tmps[(i + 1) % 2]
                S.op("dve", lambda: nc.vector.tensor_tensor(out=tmp2[:, 0:nt, :], in0=oacc[:, 0:nt, :], in1=oacc[:, 0:nt, :], op=ALU.mult),
                     R=[oacc], W=[tmp2])
                S.op("dve", lambda: nc.vector.tensor_reduce(out=ss[:, 0:nt], in_=tmp2[:, 0:nt, :], axis=AX.X, op=ALU.add), R=[tmp2], W=[ss])
                S.op("act", lambda: nc.scalar.activation(out=ss[:, 0:nt], in_=ss[:, 0:nt], func=AF.Ln, bias=K.eps_rms64[:, 0:1], scale=1.0),
                     R=[ss, K.eps_rms64], W=[ss])
                S.op("act", lambda: nc.scalar.activation(out=ss[:, 0:nt], in_=ss[:, 0:nt], func=AF.Exp, scale=-0.5), R=[ss], W=[ss])
                S.op("dve", lambda: nc.vector.scalar_tensor_tensor(out=tmp2[:, 0:nt, :], in0=oacc[:, 0:nt, :], scalar=8.0,
                                                                  in1=ss[:, 0:nt].unsqueeze(2).to_broadcast([128, nt, 64]),
                                                                  op0=ALU.mult, op1=ALU.mult),
                     R=[oacc, ss], W=[tmp2])
                S.op("dve", lambda: nc.vector.tensor_tensor(out=osb[:, 0:nt, h2 * 64:(h2 + 1) * 64], in0=tmp2[:, 0:nt, :],
                                                            in1=sbg[:].unsqueeze(1).to_broadcast([128, nt, 64]), op=ALU.mult),
                     R=[tmp2, sbg], W=[osb])
                if h2 == 1:
                    S.dma("sp", K.d["mixed"][q0:q0 + n, hp * 128:(hp + 1) * 128].rearrange("(t p) c -> p t c", p=128), osb[:, 0:nt, :],
                          R=[osb], W=[K.sbuf_mx_sb])

        for s in range(N + 2):
            if s < N:
                stA(s)
            if 0 <= s - 1 < N:
                stB(s - 1)
            if 0 <= s - 2 < N:
                stC(s - 2)
            yield


def gdn_master(K, l, es, rr, NSETS, YK):
    nc, S, NT = K.nc, K.S, K.NT
    NC_ = K.NC
    cw = _sb(K, es, "gd_cw", [128, 12, 4], F32)
    S.dma("sp", cw[:], K.d["conv_wT"][l].rearrange("(i d) t -> d i t", d=128), W=[cw])
    gng = _sb(K, es, "gd_gng", [64, 4, 128], F32)
    for h in range(4):
        S.dma("sp", gng[:, h, :], K.d["gdn_norm_g"][l].partition_broadcast(64), W=[gng])
    xin = [_sb(K, es, "gd_x%d" % i, [128, 12, 131], F32) for i in range(2)]
    cvs = [_sb(K, es, "gd_cv%d" % i, [128, 12, 128], F32) for i in range(2)]
    sq = [_sb(K, es, "gd_sq%d" % i, [128, 128], F32) for i in range(2)]
    rn = [_sb(K, es, "gd_rn%d" % i, [128, 128], F32) for i in range(2)]
    Sst = _sb(K, es, "gd_S", [128, 4, 128], F32)
    S.op("pool", lambda: nc.gpsimd.memset(Sst[:], 0.0), W=[Sst])
    tmpS = _sb(K, es, "gd_tmpS", [128, 4, 128], F32)

    def mk(name, shape, dt=F32):
        return [_sb(K, es, "gd_%s%d" % (name, i), shape, dt) for i in range(NSETS)]
    B = {}
    for name in ("egc", "egr", "ssq"):
        B[name] = mk(name, [64, 4])
    B["cdt"] = mk("cd", [128, 4])
    for name in ("gl", "dm", "dmT", "Nm", "Mm", "N2", "M2", "Pm", "qkT"):
        B[name] = mk(name, [64, 4, 64])
    for name in ("kbg", "vb", "ke", "gbc", "u", "vn", "ot", "o2"):
        B[name] = mk(name, [64, 4, 128])
    for name in ("egb", "qd", "wcT"):
        B[name] = mk(name, [128, 4, 64])
    B["ob"] = mk("ob", [64, 4, 128], BF16)
    B["bg"] = mk("bg", [64, 8])
    B["z"] = mk("z", [64, 512])
    st = {"inflight": 0, "rec_done": 0, "done": 0}
    assert NSETS == 2
    pbigs = [K.psum[5], K.psum[6]]
    psmls = pbigs
    psg1 = K.psum[7]
    n = 128

    def load_x(g):
        x = xin[g % 2]
        c0 = g * 128
        if g == 0:
            S.op("pool", lambda: nc.gpsimd.memset(x[:, :, 0:3], 0.0), W=[x])
            S.dma("sp", x[:, :, 3:3 + n], K.d["gT"][:, :, 0:n].rearrange("h p n -> p h n"), R=[K.sbuf_g], W=[x])
            S.op("pool", lambda: nc.gpsimd.memset(x[:, :, 3:3 + 48], 0.0), W=[x])
        else:
            S.dma("sp", x[:, :, 0:3 + n], K.d["gT"][:, :, c0 - 3:c0 + n].rearrange("h p n -> p h n"), R=[K.sbuf_g], W=[x])

    def G1(g):
        le = None
        x, cv = xin[g % 2], cvs[g % 2]
        k = 0
        for i in range(12):
            eng = "dve"
            E = nc.vector
            if le != eng:
                yield
            le = eng
            S.op(eng, lambda: E.tensor_scalar(out=cv[:, i, 0:n], in0=x[:, i, 0:n], scalar1=cw[:, i, 0:1], scalar2=None, op0=ALU.mult),
                 R=[x, cw], W=[cv])
            for tp in range(1, 4):
                if le != "dve":
                    yield
                le = "dve"
                S.op("dve", lambda: nc.vector.scalar_tensor_tensor(out=cv[:, i, 0:n], in0=x[:, i, tp:tp + n], scalar=cw[:, i, tp:tp + 1],
                                                                  in1=cv[:, i, 0:n], op0=ALU.mult, op1=ALU.add), R=[x, cw, cv], W=[cv])
            s_ = sq[i % 2]
            if le != "act":
                yield
            le = "act"
            S.op("act", lambda: nc.scalar.activation(out=s_[:, 0:n], in_=cv[:, i, 0:n], func=AF.Exp, scale=-1.0), R=[cv], W=[s_])
            S.op("act", lambda: nc.scalar.activation(out=s_[:, 0:n], in_=s_[:, 0:n], func=AF.Ln, bias=K.one_c[:, 0:1], scale=1.0),
                 R=[s_, K.one_c], W=[s_])
            S.op("act", lambda: nc.scalar.activation(out=s_[:, 0:n], in_=s_[:, 0:n], func=AF.Exp, scale=-1.0), R=[s_], W=[s_])
            if le != "dve":
                yield
            le = "dve"
            S.op("dve", lambda: nc.vector.tensor_tensor(out=cv[:, i, 0:n], in0=cv[:, i, 0:n], in1=s_[:, 0:n], op=ALU.mult), R=[cv, s_], W=[cv])
        for i in range(8):
            s_, r_ = sq[i % 2], rn[i % 2]
            if le != "pool":
                yield
            le = "pool"
            S.op("pool", lambda: nc.gpsimd.tensor_tensor(out=s_[:, 0:n], in0=cv[:, i, 0:n], in1=cv[:, i, 0:n], op=ALU.mult), R=[cv], W=[s_])
            ps = psg1
            if le != "pe":
                yield
            le = "pe"
            S.op("pe", lambda: nc.tensor.matmul(ps[:, 0:n], lhsT=K.onesf[:, :], rhs=s_[:, 0:n], start=True, stop=True),
                 R=[K.onesf, s_], W=[ps])
            if le != "act":
                yield
            le = "act"
            S.op("act", lambda: nc.scalar.activation(out=r_[:, 0:n], in_=ps[:, 0:n], func=AF.Ln, bias=K.eps_rms[:, 0:1], scale=1.0),
                 R=[ps, K.eps_rms], W=[r_])
            S.op("act", lambda: nc.scalar.activation(out=r_[:, 0:n], in_=r_[:, 0:n], func=AF.Exp, scale=-0.5), R=[r_], W=[r_])
            if i < 4:
                if le != "dve":
                    yield
                le = "dve"
                S.op("dve", lambda: nc.vector.scalar_tensor_tensor(out=cv[:, i, 0:n], in0=cv[:, i, 0:n], scalar=float(128 ** -0.5),
                                                                  in1=r_[:, 0:n], op0=ALU.mult, op1=ALU.mult), R=[cv, r_], W=[cv])
            else:
                if le != "dve":
                    yield
                le = "dve"
                S.op("dve", lambda: nc.vector.tensor_tensor(out=cv[:, i, 0:n], in0=cv[:, i, 0:n], in1=r_[:, 0:n], op=ALU.mult),
                     R=[cv, r_], W=[cv])

    def chunk(g, ci, cidx):
        le = None
        b2 = cidx % NSETS
        pbig, psml = pbigs[b2], psmls[b2]
        cv = cvs[g % 2]
        egc, egr, cdt, ssq = B["egc"][b2], B["egr"][b2], B["cdt"][b2], B["ssq"][b2]
        gl, dm, dmT, Pm, qkT = B["gl"][b2], B["dm"][b2], B["dmT"][b2], B["Pm"][b2], B["qkT"][b2]
        kbg, vb, ke, gbc, u, vn, ot, o2 = (B[k_][b2] for k_ in ("kbg", "vb", "ke", "gbc", "u", "vn", "ot", "o2"))
        egb, qd, wcT, ob = B["egb"][b2], B["qd"][b2], B["wcT"][b2], B["ob"][b2]
        bgc, zc = B["bg"][b2], B["z"][b2]
        p0 = g * 128 + ci * 64
        if le != "sp":
            yield
        le = "sp"
        S.dma("sp", bgc[:, :], K.d["bg"][p0:p0 + 64, :], R=[K.sbuf_bg], W=[bgc])
        if le != "sp":
            yield
        le = "sp"
        S.dma("sp", zc[:, :], K.d["zs"][p0:p0 + 64, :], R=[K.sbuf_z], W=[zc])
        if cidx == 0:
            if le != "pool":
                yield
            le = "pool"
            S.op("pool", lambda: nc.gpsimd.memset(bgc[0:48, 4:8], 0.0), W=[bgc])
        for _ in range(DMA_PAD):
            yield
        cs = slice(ci * 64, ci * 64 + 64)
        gcol = bgc[:, 4:8]
        bcol = bgc[:, 0:4]
        pg = psml
        if le != "pe":
            yield
        le = "pe"
        S.op("pe", lambda: nc.tensor.matmul(pg[0:64, 0:4], lhsT=K.triu[:, 0, :], rhs=gcol, start=True, stop=True), R=[K.triu, bgc], W=[pg])
        if le != "pe":
            yield
        le = "pe"
        S.op("pe", lambda: nc.tensor.matmul(pg[0:64, 4:8], lhsT=K.sgt[:, :], rhs=gcol, start=True, stop=True), R=[K.sgt, bgc], W=[pg])
        if le != "pe":
            yield
        le = "pe"
        S.op("pe", lambda: nc.tensor.matmul(pg[:, 8:12], lhsT=K.ones64[:, :], rhs=gcol, start=True, stop=True), R=[K.ones64, bgc], W=[pg])
        if le != "act":
            yield
        le = "act"
        S.op("act", lambda: nc.scalar.activation(out=egc[:], in_=pg[0:64, 0:4], func=AF.Exp), R=[pg], W=[egc])
        if le != "act":
            yield
        le = "act"
        S.op("act", lambda: nc.scalar.activation(out=egr[:], in_=pg[0:64, 4:8], func=AF.Exp), R=[pg], W=[egr])
        if le != "act":
            yield
        le = "act"
        S.op("act", lambda: nc.scalar.activation(out=cdt[:], in_=pg[:, 8:12], func=AF.Exp), R=[pg], W=[cdt])
        if le != "dve":
            yield
        le = "dve"
        S.op("dve", lambda: nc.vector.tensor_tensor(out=gl[:], in0=K.triu[:], in1=gcol.unsqueeze(2).to_broadcast([64, 4, 64]), op=ALU.mult),
             R=[K.triu, bgc], W=[gl])
        pd = pbig
        pdv = pd.h[0:64, 0:512].rearrange("p (a h c) -> p a h c", a=2, h=4)
        for h in range(4):
            if le != "pe":
                yield
            le = "pe"
            S.op("pe", lambda: nc.tensor.matmul(pdv[:, 0, h, :], lhsT=gl[:, h, :], rhs=K.sgt[:, :], start=True, stop=True),
                 R=[gl, K.sgt], W=[pd])
        if le != "pe":
            yield
        le = "pe"
        S.op("pe", lambda: nc.tensor.matmul(pd[0:64, 256:512], lhsT=K.sgt[:, :], rhs=gl[:].rearrange("p h c -> p (h c)"), start=True, stop=True),
             R=[gl, K.sgt], W=[pd])
        if le != "act":
            yield
        le = "act"
        S.op("act", lambda: nc.scalar.activation(out=dm[:], in_=pdv[:, 0], func=AF.Exp), R=[pd], W=[dm])
        if le != "act":
            yield
        le = "act"
        S.op("act", lambda: nc.scalar.activation(out=dmT[:], in_=pdv[:, 1], func=AF.Exp), R=[pd], W=[dmT])
        if le != "pool":
            yield
        le = "pool"
        S.op("pool", lambda: nc.gpsimd.tensor_tensor(out=dm[:], in0=dm[:], in1=K.trilsn[:], op=ALU.mult), R=[dm, K.trilsn], W=[dm])
        if le != "pool":
            yield
        le = "pool"
        S.op("pool", lambda: nc.gpsimd.tensor_tensor(out=dmT[:], in0=dmT[:], in1=K.triu[:], op=ALU.mult), R=[dmT, K.triu], W=[dmT])
        pgq = pbig
        pgqv = pgq.h[0:64, 0:512].rearrange("p (a h c) -> p a h c", a=2, h=4)
        for h in range(4):
            if le != "pe":
                yield
            le = "pe"
            S.op("pe", lambda: nc.tensor.matmul(pgqv[:, 0, h, :], lhsT=cv[:, 4 + h, cs], rhs=cv[:, 4 + h, cs], start=True, stop=True), R=[cv], W=[pgq])
            if le != "pe":
                yield
            le = "pe"
            S.op("pe", lambda: nc.tensor.matmul(pgqv[:, 1, h, :], lhsT=cv[:, 4 + h, cs], rhs=cv[:, h, cs], start=True, stop=True), R=[cv], W=[pgq])
        Nm, Mm = B["Nm"][b2], B["Mm"][b2]
        if le != "dve":
            yield
        le = "dve"
        S.op("dve", lambda: nc.vector.tensor_tensor(out=Nm[:], in0=pgqv[:, 0], in1=bcol.unsqueeze(2).to_broadcast([64, 4, 64]), op=ALU.mult),
             R=[pgq, bgc], W=[Nm])
        if le != "dve":
            yield
        le = "dve"
        S.op("dve", lambda: nc.vector.tensor_tensor(out=Nm[:], in0=Nm[:], in1=dm[:], op=ALU.mult), R=[Nm, dm], W=[Nm])
        if le != "dve":
            yield
        le = "dve"
        S.op("dve", lambda: nc.vector.tensor_tensor(out=qkT[:], in0=pgqv[:, 1], in1=dmT[:], op=ALU.mult), R=[pgq, dmT], W=[qkT])
        pt = psml
        ptv = pt.h[0:64, 0:256].rearrange("p (h c) -> p h c", h=4)
        for h in range(4):
            if le != "pe":
                yield
            le = "pe"
            S.op("pe", lambda: nc.tensor.transpose(out=ptv[:, h, :], in_=Nm[:, h, :], identity=K.identf[0:64, 0:64]), R=[Nm, K.identf], W=[pt])
        if le != "act":
            yield
        le = "act"
        S.op("act", lambda: nc.scalar.copy(out=Mm[:], in_=ptv), R=[pt], W=[Mm])
        if le != "dve":
            yield
        le = "dve"
        S.op("dve", lambda: nc.vector.tensor_tensor(out=Pm[:], in0=Mm[:], in1=K.ident4[:], op=ALU.add), R=[Mm, K.ident4], W=[Pm])
        Nc, Mc, Nn, Mn = Nm, Mm, B["N2"][b2], B["M2"][b2]
        for r in range(5):
            pn = pbig
            pnv = pn.h[0:64, 0:512].rearrange("p (a h c) -> p a h c", a=2, h=4)
            for h in range(4):
                if le != "pe":
                    yield
                le = "pe"
                S.op("pe", lambda: nc.tensor.matmul(pnv[:, 0, h, :], lhsT=Mc[:, h, :], rhs=Nc[:, h, :], start=True, stop=True), R=[Mc, Nc], W=[pn])
                if r < 4:
                    if le != "pe":
                        yield
                    le = "pe"
                    S.op("pe", lambda: nc.tensor.matmul(pnv[:, 1, h, :], lhsT=Nc[:, h, :], rhs=Mc[:, h, :], start=True, stop=True), R=[Mc, Nc], W=[pn])
            if le != "act":
                yield
            le = "act"
            S.op("act", lambda: nc.scalar.copy(out=Nn[:], in_=pnv[:, 0]), R=[pn], W=[Nn])
            if r < 4:
                if le != "dve":
                    yield
                le = "dve"
                S.op("dve", lambda: nc.vector.tensor_copy(out=Mn[:], in_=pnv[:, 1]), R=[pn], W=[Mn])
            pp = psml
            ppv = pp.h[0:64, 0:256].rearrange("p (h c) -> p h c", h=4)
            for h in range(4):
                if le != "pe":
                    yield
                le = "pe"
                S.op("pe", lambda: nc.tensor.matmul(ppv[:, h, :], lhsT=Nn[:, h, :], rhs=Pm[:, h, :], start=True, stop=True), R=[Nn, Pm], W=[pp])
            if le != "dve":
                yield
            le = "dve"
            S.op("dve", lambda: nc.vector.tensor_tensor(out=Pm[:], in0=Pm[:], in1=ppv, op=ALU.add), R=[Pm, pp], W=[Pm])
            Nc, Mc, Nn, Mn = Nn, Mn, Nc, Mc
        pk = pbig
        pkv = pk.h[0:64, 0:512].rearrange("p (h c) -> p h c", h=4)
        for h in range(4):
            if le != "pe":
                yield
            le = "pe"
            S.op("pe", lambda: nc.tensor.transpose(out=pkv[:, h, :], in_=cv[:, 4 + h, cs], identity=K.identf[:]), R=[cv, K.identf], W=[pk])
        if le != "dve":
            yield
        le = "dve"
        S.op("dve", lambda: nc.vector.tensor_tensor(out=ke[:], in0=pkv, in1=egr[:].unsqueeze(2).to_broadcast([64, 4, 128]), op=ALU.mult),
             R=[pk, egr], W=[ke])
        if le != "dve":
            yield
        le = "dve"
        S.op("dve", lambda: nc.vector.tensor_tensor(out=kbg[:], in0=pkv, in1=bcol.unsqueeze(2).to_broadcast([64, 4, 128]), op=ALU.mult),
             R=[pk, bgc], W=[kbg])
        if le != "pool":
            yield
        le = "pool"
        S.op("pool", lambda: nc.gpsimd.tensor_tensor(out=kbg[:], in0=kbg[:], in1=egc[:].unsqueeze(2).to_broadcast([64, 4, 128]), op=ALU.mult),
             R=[kbg, egc], W=[kbg])
        pv = pbig
        pvv = pv.h[0:64, 0:512].rearrange("p (h c) -> p h c", h=4)
        for h in range(4):
            if le != "pe":
                yield
            le = "pe"
            S.op("pe", lambda: nc.tensor.transpose(out=pvv[:, h, :], in_=cv[:, 8 + h, cs], identity=K.identf[:]), R=[cv, K.identf], W=[pv])
        if le != "dve":
            yield
        le = "dve"
        S.op("dve", lambda: nc.vector.tensor_tensor(out=vb[:], in0=pvv, in1=bcol.unsqueeze(2).to_broadcast([64, 4, 128]), op=ALU.mult),
             R=[pv, bgc], W=[vb])
        if le != "pool":
            yield
        le = "pool"
        S.op("pool", lambda: nc.gpsimd.tensor_tensor(out=gbc[:], in0=K.ones4[:], in1=gcol.unsqueeze(2).to_broadcast([64, 4, 128]), op=ALU.mult),
             R=[K.ones4, bgc], W=[gbc])
        pe_ = psml
        pev = pe_.h[:, 0:256].rearrange("p (h c) -> p h c", h=4)
        for h in range(4):
            if le != "pe":
                yield
            le = "pe"
            S.op("pe", lambda: nc.tensor.matmul(pev[:, h, :], lhsT=gbc[:, h, :], rhs=K.triu[:, 0, :], start=True, stop=True), R=[gbc, K.triu], W=[pe_])
        if le != "act":
            yield
        le = "act"
        S.op("act", lambda: nc.scalar.activation(out=egb[:], in_=pev, func=AF.Exp), R=[pe_], W=[egb])
        if le != "dve":
            yield
        le = "dve"
        S.op("dve", lambda: nc.vector.tensor_tensor(out=qd[:], in0=cv[:, 0:4, cs], in1=egb[:], op=ALU.mult), R=[cv, egb], W=[qd])
        pw = psml
        pwv = pw.h[:, 0:256].rearrange("p (h c) -> p h c", h=4)
        for h in range(4):
            if le != "pe":
                yield
            le = "pe"
            S.op("pe", lambda: nc.tensor.matmul(pwv[:, h, :], lhsT=kbg[:, h, :], rhs=Pm[:, h, :], start=True, stop=True), R=[kbg, Pm], W=[pw])
        if le != "act":
            yield
        le = "act"
        S.op("act", lambda: nc.scalar.copy(out=wcT[:], in_=pwv), R=[pw], W=[wcT])
        pu = pbig
        puv = pu.h[0:64, 0:512].rearrange("p (h c) -> p h c", h=4)
        for h in range(4):
            if le != "pe":
                yield
            le = "pe"
            S.op("pe", lambda: nc.tensor.matmul(puv[:, h, :], lhsT=Pm[:, h, :], rhs=vb[:, h, :], start=True, stop=True), R=[vb, Pm], W=[pu])
        if le != "act":
            yield
        le = "act"
        S.op("act", lambda: nc.scalar.copy(out=u[:], in_=puv), R=[pu], W=[u])
        while st["rec_done"] < cidx:
            yield
        pws = pbig
        pwsv = pws.h[0:64, 0:512].rearrange("p (h c) -> p h c", h=4)
        for h in range(4):
            if le != "pe":
                yield
            le = "pe"
            S.op("pe", lambda: nc.tensor.matmul(pwsv[:, h, :], lhsT=wcT[:, h, :], rhs=Sst[:, h, :], start=True, stop=True), R=[wcT, Sst], W=[pws])
        if le != "dve":
            yield
        le = "dve"
        S.op("dve", lambda: nc.vector.tensor_tensor(out=vn[:], in0=u[:], in1=pwsv, op=ALU.subtract), R=[u, pws], W=[vn])
        po = pbig
        pov = po.h[0:64, 0:512].rearrange("p (h c) -> p h c", h=4)
        for h in range(4):
            if le != "pe":
                yield
            le = "pe"
            S.op("pe", lambda: nc.tensor.matmul(pov[:, h, :], lhsT=qd[:, h, :], rhs=Sst[:, h, :], start=True, stop=False), R=[qd, Sst], W=[po])
            if le != "pe":
                yield
            le = "pe"
            S.op("pe", lambda: nc.tensor.matmul(pov[:, h, :], lhsT=qkT[:, h, :], rhs=vn[:, h, :], start=False, stop=True), R=[qkT, vn], W=[po])
        if le != "act":
            yield
        le = "act"
        S.op("act", lambda: nc.scalar.copy(out=ot[:], in_=pov), R=[po], W=[ot])
        pS = pbig
        pSv = pS.h[:, 0:512].rearrange("p (h c) -> p h c", h=4)
        for h in range(4):
            if le != "pe":
                yield
            le = "pe"
            S.op("pe", lambda: nc.tensor.matmul(pSv[:, h, :], lhsT=ke[:, h, :], rhs=vn[:, h, :], start=True, stop=True), R=[ke, vn], W=[pS])
        if le != "pool":
            yield
        le = "pool"
        S.op("pool", lambda: nc.gpsimd.tensor_tensor(out=tmpS[:], in0=Sst[:], in1=cdt[:].unsqueeze(2).to_broadcast([128, 4, 128]), op=ALU.mult),
             R=[Sst, cdt], W=[tmpS])
        if le != "dve":
            yield
        le = "dve"
        S.op("dve", lambda: nc.vector.tensor_tensor(out=Sst[:], in0=tmpS[:], in1=pSv, op=ALU.add), R=[tmpS, pS], W=[Sst])
        st["rec_done"] = cidx + 1
        if le != "pool":
            yield
        le = "pool"
        S.op("pool", lambda: nc.gpsimd.tensor_tensor(out=o2[:], in0=ot[:], in1=ot[:], op=ALU.mult), R=[ot], W=[o2])
        if le != "dve":
            yield
        le = "dve"
        S.op("dve", lambda: nc.vector.tensor_reduce(out=ssq[:], in_=o2[:], axis=AX.X, op=ALU.add), R=[o2], W=[ssq])
        if le != "act":
            yield
        le = "act"
        S.op("act", lambda: nc.scalar.activation(out=ssq[:], in_=ssq[:], func=AF.Ln, bias=K.eps_rms128[0:64, 0:1], scale=1.0),
             R=[ssq, K.eps_rms128], W=[ssq])
        S.op("act", lambda: nc.scalar.activation(out=ssq[:], in_=ssq[:], func=AF.Exp, scale=-0.5), R=[ssq], W=[ssq])
        if le != "dve":
            yield
        le = "dve"
        S.op("dve", lambda: nc.vector.scalar_tensor_tensor(out=o2[:], in0=ot[:], scalar=float(128 ** 0.5),
                                                          in1=ssq[:].unsqueeze(2).to_broadcast([64, 4, 128]), op0=ALU.mult, op1=ALU.mult),
             R=[ot, ssq], W=[o2])
        if le != "pool":
            yield
        le = "pool"
        S.op("pool", lambda: nc.gpsimd.tensor_tensor(out=o2[:], in0=o2[:], in1=gng[:], op=ALU.mult), R=[o2, gng], W=[o2])
        if le != "dve":
            yield
        le = "dve"
        S.op("dve", lambda: nc.vector.tensor_tensor(out=ob[:], in0=o2[:], in1=zc[:, :].rearrange("p (h c) -> p h c", h=4), op=ALU.mult),
             R=[o2, zc], W=[ob])
        if le != "sp":
            yield
        le = "sp"
        S.dma("sp", K.d["mixed"][p0:p0 + 64, 512:1024], ob[:].rearrange("p h c -> p (h c)"), R=[ob], W=[K.sbuf_mx_gd])
        st["inflight"] -= 1
        st["done"] += 1

    NG = NT
    load_x(0)
    cidx = 0
    for g in range(NG):
        nch = min(2, NC_ - 2 * g)
        if nch <= 0:
            break
        if g + 1 < NG and NC_ - 2 * (g + 1) > 0:
            load_x(g + 1)
        while st["done"] < min(cidx, 2 * (g - 1)):
            yield
        yield from G1(g)
        for ci in range(nch):
            while st["inflight"] >= NSETS:
                yield
            st["inflight"] += 1
            rr.add(chunk(g, ci, cidx), YK)
            cidx += 1
            yield
    while st["done"] < cidx:
        yield


def phase_GDN_old(K, l):
    nc, S, NT = K.nc, K.S, K.NT
    NC_ = K.NC
    with contextlib.ExitStack() as es:
        cw = _sb(K, es, "gd_cw", [128, 12, 4], F32)
        S.dma("sp", cw[:], K.d["conv_wT"][l].rearrange("(i d) t -> d i t", d=128), W=[cw])
        gng = _sb(K, es, "gd_gng", [64, 4, 128], F32)
        for h in range(4):
            S.dma("sp", gng[:, h, :], K.d["gdn_norm_g"][l].partition_broadcast(64), W=[gng])
        xin = [_sb(K, es, "gd_x%d" % i, [128, 12, 259], F32) for i in range(2)]
        cv = _sb(K, es, "gd_cv", [128, 12, 256], F32)
        sq = [_sb(K, es, "gd_sq%d" % i, [128, 256], F32) for i in range(2)]
        rn = [_sb(K, es, "gd_rn%d" % i, [128, 256], F32) for i in range(2)]
        bgt = [_sb(K, es, "gd_bg%d" % i, [64, 4, 8], F32) for i in range(2)]
        zt = [_sb(K, es, "gd_z%d" % i, [64, 4, 512], F32) for i in range(2)]
        Sst = _sb(K, es, "gd_S", [128, 4, 128], F32)
        S.op("pool", lambda: nc.gpsimd.memset(Sst[:], 0.0), W=[Sst])
        def mk(name, shape, dt=F32):
            return [_sb(K, es, "gd_%s%d" % (name, i), shape, dt) for i in range(2)]
        egc, egr, cdt = mk("egc", [64, 4]), mk("egr", [64, 4]), mk("cd", [128, 4])
        gl, dm, dmT = mk("gl", [64, 4, 64]), mk("dm", [64, 4, 64]), mk("dmT", [64, 4, 64])
        Nm, Mm = mk("N", [64, 4, 64]), mk("M", [64, 4, 64])
        N2, M2 = mk("N2", [64, 4, 64]), mk("M2", [64, 4, 64])
        Pm = mk("P", [64, 4, 64])
        qkT = mk("qkT", [64, 4, 64])
        kbg, vb, ke = mk("kbg", [64, 4, 128]), mk("vb", [64, 4, 128]), mk("ke", [64, 4, 128])
        gbc, egb, qd = mk("gbc", [64, 4, 128]), mk("egb", [128, 4, 64]), mk("qd", [128, 4, 64])
        wcT, u, vn = mk("wcT", [128, 4, 64]), mk("u", [64, 4, 128]), mk("vn", [64, 4, 128])
        ot, o2 = mk("ot", [64, 4, 128]), mk("o2", [64, 4, 128])
        ssq, ob = mk("ssq", [64, 4]), mk("ob", [64, 4, 128], BF16)
        tmpS = _sb(K, es, "gd_tmpS", [128, 4, 128], F32)
        NG = (NT + 1) // 2
        ci_glob = 0
        for g in range(NG):
            nt = min(2, NT - 2 * g)
            n = nt * 128
            c0 = g * 256
            nch = min(n // 64, NC_ - c0 // 64)
            if nch <= 0:
                break
            x = xin[g % 2]
            if g == 0:
                S.op("pool", lambda: nc.gpsimd.memset(x[:, :, 0:3], 0.0), W=[x])
                S.dma("sp", x[:, :, 3:3 + n], K.d["gT"][:, :, 0:n].rearrange("h p n -> p h n"), R=[K.sbuf_g], W=[x])
                S.op("pool", lambda: nc.gpsimd.memset(x[:, :, 3:3 + 48], 0.0), W=[x])
            else:
                S.dma("sp", x[:, :, 0:3 + n], K.d["gT"][:, :, c0 - 3:c0 + n].rearrange("h p n -> p h n"), R=[K.sbuf_g], W=[x])
            for i in range(12):
                eng = "dve" if i % 2 == 0 else "pool"
                E = nc.vector if eng == "dve" else nc.gpsimd
                S.op(eng, lambda: E.tensor_scalar(out=cv[:, i, 0:n], in0=x[:, i, 0:n], scalar1=cw[:, i, 0:1], scalar2=None, op0=ALU.mult),
                     R=[x, cw], W=[cv])
                for tp in range(1, 4):
                    S.op("dve", lambda: nc.vector.scalar_tensor_tensor(out=cv[:, i, 0:n], in0=x[:, i, tp:tp + n], scalar=cw[:, i, tp:tp + 1],
                                                                      in1=cv[:, i, 0:n], op0=ALU.mult, op1=ALU.add), R=[x, cw, cv], W=[cv])
                S.op("act", lambda: nc.scalar.activation(out=cv[:, i, 0:n], in_=cv[:, i, 0:n], func=AF.Silu), R=[cv], W=[cv])
            for i in range(8):
                s_, r_ = sq[i % 2], rn[i % 2]
                S.op("pool", lambda: nc.gpsimd.tensor_tensor(out=s_[:, 0:n], in0=cv[:, i, 0:n], in1=cv[:, i, 0:n], op=ALU.mult), R=[cv], W=[s_])
                ps = _ps(K)
                S.op("pe", lambda: nc.tensor.matmul(ps[:, 0:n], lhsT=K.onesf[:, :], rhs=s_[:, 0:n], start=True, stop=True),
                     R=[K.onesf, s_], W=[ps])
                S.op("act", lambda: nc.scalar.activation(out=r_[:, 0:n], in_=ps[:, 0:n], func=AF.Sqrt, bias=K.eps_rms[:, 0:1], scale=1.0),
                     R=[ps, K.eps_rms], W=[r_])
                S.op("dve", lambda: nc.vector.reciprocal(out=r_[:, 0:n], in_=r_[:, 0:n]), R=[r_], W=[r_])
                if i < 4:
                    S.op("dve", lambda: nc.vector.scalar_tensor_tensor(out=cv[:, i, 0:n], in0=cv[:, i, 0:n], scalar=float(128 ** -0.5),
                                                                      in1=r_[:, 0:n], op0=ALU.mult, op1=ALU.mult), R=[cv, r_], W=[cv])
                else:
                    S.op("dve", lambda: nc.vector.tensor_tensor(out=cv[:, i, 0:n], in0=cv[:, i, 0:n], in1=r_[:, 0:n], op=ALU.mult),
                         R=[cv, r_], W=[cv])
            bgc, zc = bgt[g % 2], zt[g % 2]
            S.dma("sp", bgc[:, 0:nch, :], K.d["bg"][c0:c0 + nch * 64, :].rearrange("(n c) e -> c n e", c=64), R=[K.sbuf_bg], W=[bgc])
            S.dma("sp", zc[:, 0:nch, :], K.d["zs"][c0:c0 + nch * 64, :].rearrange("(n c) e -> c n e", c=64), R=[K.sbuf_z], W=[zc])
            if g == 0:
                S.op("pool", lambda: nc.gpsimd.memset(bgc[0:48, 0, 4:8], 0.0), W=[bgc])
            for ci in range(nch):
                b2 = ci_glob % 2
                ci_glob += 1
                cs = slice(ci * 64, ci * 64 + 64)
                gcol = bgc[:, ci, 4:8]
                bcol = bgc[:, ci, 0:4]
                pg = _ps(K)
                S.op("pe", lambda: nc.tensor.matmul(pg[0:64, 0:4], lhsT=K.triu[:, 0, :], rhs=gcol, start=True, stop=True), R=[K.triu, bgc], W=[pg])
                S.op("pe", lambda: nc.tensor.matmul(pg[0:64, 4:8], lhsT=K.sgt[:, :], rhs=gcol, start=True, stop=True), R=[K.sgt, bgc], W=[pg])
                S.op("pe", lambda: nc.tensor.matmul(pg[:, 8:12], lhsT=K.ones64[:, :], rhs=gcol, start=True, stop=True), R=[K.ones64, bgc], W=[pg])
                S.op("act", lambda: nc.scalar.activation(out=egc[b2][:], in_=pg[0:64, 0:4], func=AF.Exp), R=[pg], W=[egc[b2]])
                S.op("act", lambda: nc.scalar.activation(out=egr[b2][:], in_=pg[0:64, 4:8], func=AF.Exp), R=[pg], W=[egr[b2]])
                S.op("act", lambda: nc.scalar.activation(out=cdt[b2][:], in_=pg[:, 8:12], func=AF.Exp), R=[pg], W=[cdt[b2]])
                S.op("dve", lambda: nc.vector.tensor_tensor(out=gl[b2][:], in0=K.triu[:], in1=gcol.unsqueeze(2).to_broadcast([64, 4, 64]), op=ALU.mult),
                     R=[K.triu, bgc], W=[gl[b2]])
                pd = _ps(K)
                pdv = pd.h[0:64, 0:512].rearrange("p (a h c) -> p a h c", a=2, h=4)
                for h in range(4):
                    S.op("pe", lambda: nc.tensor.matmul(pdv[:, 0, h, :], lhsT=gl[b2][:, h, :], rhs=K.sgt[:, :], start=True, stop=True),
                         R=[gl[b2], K.sgt], W=[pd])
                S.op("pe", lambda: nc.tensor.matmul(pd[0:64, 256:512], lhsT=K.sgt[:, :], rhs=gl[b2][:].rearrange("p h c -> p (h c)"), start=True, stop=True),
                     R=[gl[b2], K.sgt], W=[pd])
                S.op("act", lambda: nc.scalar.activation(out=dm[b2][:], in_=pdv[:, 0], func=AF.Exp), R=[pd], W=[dm[b2]])
                S.op("act", lambda: nc.scalar.activation(out=dmT[b2][:], in_=pdv[:, 1], func=AF.Exp), R=[pd], W=[dmT[b2]])
                S.op("pool", lambda: nc.gpsimd.tensor_tensor(out=dm[b2][:], in0=dm[b2][:], in1=K.trilsn[:], op=ALU.mult), R=[dm[b2], K.trilsn], W=[dm[b2]])
                S.op("pool", lambda: nc.gpsimd.tensor_tensor(out=dmT[b2][:], in0=dmT[b2][:], in1=K.triu[:], op=ALU.mult), R=[dmT[b2], K.triu], W=[dmT[b2]])
                pgq = _ps(K)
                pgqv = pgq.h[0:64, 0:512].rearrange("p (a h c) -> p a h c", a=2, h=4)
                for h in range(4):
                    S.op("pe", lambda: nc.tensor.matmul(pgqv[:, 0, h, :], lhsT=cv[:, 4 + h, cs], rhs=cv[:, 4 + h, cs], start=True, stop=True), R=[cv], W=[pgq])
                    S.op("pe", lambda: nc.tensor.matmul(pgqv[:, 1, h, :], lhsT=cv[:, 4 + h, cs], rhs=cv[:, h, cs], start=True, stop=True), R=[cv], W=[pgq])
                S.op("dve", lambda: nc.vector.tensor_tensor(out=Nm[b2][:], in0=pgqv[:, 0], in1=bcol.unsqueeze(2).to_broadcast([64, 4, 64]), op=ALU.mult),
                     R=[pgq, bgc], W=[Nm[b2]])
                S.op("dve", lambda: nc.vector.tensor_tensor(out=Nm[b2][:], in0=Nm[b2][:], in1=dm[b2][:], op=ALU.mult), R=[Nm[b2], dm[b2]], W=[Nm[b2]])
                S.op("dve", lambda: nc.vector.tensor_tensor(out=qkT[b2][:], in0=pgqv[:, 1], in1=dmT[b2][:], op=ALU.mult), R=[pgq, dmT[b2]], W=[qkT[b2]])
                pt = _ps(K)
                ptv = pt.h[0:64, 0:256].rearrange("p (h c) -> p h c", h=4)
                for h in range(4):
                    S.op("pe", lambda: nc.tensor.transpose(out=ptv[:, h, :], in_=Nm[b2][:, h, :], identity=K.identf[0:64, 0:64]), R=[Nm[b2], K.identf], W=[pt])
                S.op("act", lambda: nc.scalar.copy(out=Mm[b2][:], in_=ptv), R=[pt], W=[Mm[b2]])
                S.op("dve", lambda: nc.vector.tensor_tensor(out=Pm[b2][:], in0=Mm[b2][:], in1=K.ident4[:], op=ALU.add), R=[Mm[b2], K.ident4], W=[Pm[b2]])
                Nc, Mc, Nn, Mn = Nm[b2], Mm[b2], N2[b2], M2[b2]
                for r in range(5):
                    pn = _ps(K)
                    pnv = pn.h[0:64, 0:512].rearrange("p (a h c) -> p a h c", a=2, h=4)
                    for h in range(4):
                        S.op("pe", lambda: nc.tensor.matmul(pnv[:, 0, h, :], lhsT=Mc[:, h, :], rhs=Nc[:, h, :], start=True, stop=True), R=[Mc, Nc], W=[pn])
                        if r < 4:
                            S.op("pe", lambda: nc.tensor.matmul(pnv[:, 1, h, :], lhsT=Nc[:, h, :], rhs=Mc[:, h, :], start=True, stop=True), R=[Mc, Nc], W=[pn])
                    S.op("act", lambda: nc.scalar.copy(out=Nn[:], in_=pnv[:, 0]), R=[pn], W=[Nn])
                    if r < 4:
                        S.op("dve", lambda: nc.vector.tensor_copy(out=Mn[:], in_=pnv[:, 1]), R=[pn], W=[Mn])
                    pp = _ps(K)
                    ppv = pp.h[0:64, 0:256].rearrange("p (h c) -> p h c", h=4)
                    for h in range(4):
                        S.op("pe", lambda: nc.tensor.matmul(ppv[:, h, :], lhsT=Nn[:, h, :], rhs=Pm[b2][:, h, :], start=True, stop=True), R=[Nn, Pm[b2]], W=[pp])
                    S.op("dve", lambda: nc.vector.tensor_tensor(out=Pm[b2][:], in0=Pm[b2][:], in1=ppv, op=ALU.add), R=[Pm[b2], pp], W=[Pm[b2]])
                    Nc, Mc, Nn, Mn = Nn, Mn, Nc, Mc
                pk = _ps(K)
                pkv = pk.h[0:64, 0:512].rearrange("p (h c) -> p h c", h=4)
                pv = _ps(K)
                pvv = pv.h[0:64, 0:512].rearrange("p (h c) -> p h c", h=4)
                for h in range(4):
                    S.op("pe", lambda: nc.tensor.transpose(out=pkv[:, h, :], in_=cv[:, 4 + h, cs], identity=K.identf[:]), R=[cv, K.identf], W=[pk])
                    S.op("pe", lambda: nc.tensor.transpose(out=pvv[:, h, :], in_=cv[:, 8 + h, cs], identity=K.identf[:]), R=[cv, K.identf], W=[pv])
                S.op("dve", lambda: nc.vector.tensor_tensor(out=ke[b2][:], in0=pkv, in1=egr[b2][:].unsqueeze(2).to_broadcast([64, 4, 128]), op=ALU.mult),
                     R=[pk, egr[b2]], W=[ke[b2]])
                S.op("dve", lambda: nc.vector.tensor_tensor(out=kbg[b2][:], in0=pkv, in1=bcol.unsqueeze(2).to_broadcast([64, 4, 128]), op=ALU.mult),
                     R=[pk, bgc], W=[kbg[b2]])
                S.op("pool", lambda: nc.gpsimd.tensor_tensor(out=kbg[b2][:], in0=kbg[b2][:], in1=egc[b2][:].unsqueeze(2).to_broadcast([64, 4, 128]), op=ALU.mult),
                     R=[kbg[b2], egc[b2]], W=[kbg[b2]])
                S.op("dve", lambda: nc.vector.tensor_tensor(out=vb[b2][:], in0=pvv, in1=bcol.unsqueeze(2).to_broadcast([64, 4, 128]), op=ALU.mult),
                     R=[pv, bgc], W=[vb[b2]])
                S.op("pool", lambda: nc.gpsimd.tensor_tensor(out=gbc[b2][:], in0=K.ones4[:], in1=gcol.unsqueeze(2).to_broadcast([64, 4, 128]), op=ALU.mult),
                     R=[K.ones4, bgc], W=[gbc[b2]])
                pe_ = _ps(K)
                pev = pe_.h[:, 0:256].rearrange("p (h c) -> p h c", h=4)
                for h in range(4):
                    S.op("pe", lambda: nc.tensor.matmul(pev[:, h, :], lhsT=gbc[b2][:, h, :], rhs=K.triu[:, 0, :], start=True, stop=True), R=[gbc[b2], K.triu], W=[pe_])
                S.op("act", lambda: nc.scalar.activation(out=egb[b2][:], in_=pev, func=AF.Exp), R=[pe_], W=[egb[b2]])
                S.op("dve", lambda: nc.vector.tensor_tensor(out=qd[b2][:], in0=cv[:, 0:4, cs], in1=egb[b2][:], op=ALU.mult), R=[cv, egb[b2]], W=[qd[b2]])
                pw = _ps(K)
                pwv = pw.h[:, 0:256].rearrange("p (h c) -> p h c", h=4)
                pu = _ps(K)
                puv = pu.h[0:64, 0:512].rearrange("p (h c) -> p h c", h=4)
                for h in range(4):
                    S.op("pe", lambda: nc.tensor.matmul(pwv[:, h, :], lhsT=kbg[b2][:, h, :], rhs=Pm[b2][:, h, :], start=True, stop=True), R=[kbg[b2], Pm[b2]], W=[pw])
                    S.op("pe", lambda: nc.tensor.matmul(puv[:, h, :], lhsT=Pm[b2][:, h, :], rhs=vb[b2][:, h, :], start=True, stop=True), R=[vb[b2], Pm[b2]], W=[pu])
                S.op("act", lambda: nc.scalar.copy(out=wcT[b2][:], in_=pwv), R=[pw], W=[wcT[b2]])
                S.op("act", lambda: nc.scalar.copy(out=u[b2][:], in_=puv), R=[pu], W=[u[b2]])
                pws = _ps(K)
                pwsv = pws.h[0:64, 0:512].rearrange("p (h c) -> p h c", h=4)
                for h in range(4):
                    S.op("pe", lambda: nc.tensor.matmul(pwsv[:, h, :], lhsT=wcT[b2][:, h, :], rhs=Sst[:, h, :], start=True, stop=True), R=[wcT[b2], Sst], W=[pws])
                S.op("dve", lambda: nc.vector.tensor_tensor(out=vn[b2][:], in0=u[b2][:], in1=pwsv, op=ALU.subtract), R=[u[b2], pws], W=[vn[b2]])
                po = _ps(K)
                pov = po.h[0:64, 0:512].rearrange("p (h c) -> p h c", h=4)
                for h in range(4):
                    S.op("pe", lambda: nc.tensor.matmul(pov[:, h, :], lhsT=qd[b2][:, h, :], rhs=Sst[:, h, :], start=True, stop=False), R=[qd[b2], Sst], W=[po])
                    S.op("pe", lambda: nc.tensor.matmul(pov[:, h, :], lhsT=qkT[b2][:, h, :], rhs=vn[b2][:, h, :], start=False, stop=True), R=[qkT[b2], vn[b2]], W=[po])
                pS = _ps(K)
                pSv = pS.h[:, 0:512].rearrange("p (h c) -> p h c", h=4)
                for h in range(4):
                    S.op("pe", lambda: nc.tensor.matmul(pSv[:, h, :], lhsT=ke[b2][:, h, :], rhs=vn[b2][:, h, :], start=True, stop=True), R=[ke[b2], vn[b2]], W=[pS])
                S.op("pool", lambda: nc.gpsimd.tensor_tensor(out=tmpS[:], in0=Sst[:], in1=cdt[b2][:].unsqueeze(2).to_broadcast([128, 4, 128]), op=ALU.mult),
                     R=[Sst, cdt[b2]], W=[tmpS])
                S.op("dve", lambda: nc.vector.tensor_tensor(out=Sst[:], in0=tmpS[:], in1=pSv, op=ALU.add), R=[tmpS, pS], W=[Sst])
                S.op("act", lambda: nc.scalar.copy(out=ot[b2][:], in_=pov), R=[po], W=[ot[b2]])
                S.op("pool", lambda: nc.gpsimd.tensor_tensor(out=o2[b2][:], in0=ot[b2][:], in1=ot[b2][:], op=ALU.mult), R=[ot[b2]], W=[o2[b2]])
                S.op("dve", lambda: nc.vector.tensor_reduce(out=ssq[b2][:], in_=o2[b2][:], axis=AX.X, op=ALU.add), R=[o2[b2]], W=[ssq[b2]])
                S.op("act", lambda: nc.scalar.activation(out=ssq[b2][:], in_=ssq[b2][:], func=AF.Sqrt, bias=K.eps_rms[0:64, 0:1], scale=1.0 / 128),
                     R=[ssq[b2], K.eps_rms], W=[ssq[b2]])
                S.op("dve", lambda: nc.vector.reciprocal(out=ssq[b2][:], in_=ssq[b2][:]), R=[ssq[b2]], W=[ssq[b2]])
                S.op("dve", lambda: nc.vector.tensor_tensor(out=o2[b2][:], in0=ot[b2][:], in1=ssq[b2][:].unsqueeze(2).to_broadcast([64, 4, 128]), op=ALU.mult),
                     R=[ot[b2], ssq[b2]], W=[o2[b2]])
                S.op("pool", lambda: nc.gpsimd.tensor_tensor(out=o2[b2][:], in0=o2[b2][:], in1=gng[:], op=ALU.mult), R=[o2[b2], gng], W=[o2[b2]])
                S.op("dve", lambda: nc.vector.tensor_tensor(out=ob[b2][:], in0=o2[b2][:], in1=zc[:, ci, :].rearrange("p (h c) -> p h c", h=4), op=ALU.mult),
                     R=[o2[b2], zc], W=[ob[b2]])
                p0 = c0 + ci * 64
                S.dma("sp", K.d["mixed"][p0:p0 + 64, 512:1024], ob[b2][:].rearrange("p h c -> p (h c)"), R=[ob[b2]], W=[K.sbuf_mx_gd])


def phase_SBGDN(K, l, NSETS=2, YK=None, run_sb=True, run_gdn=True):
    YK = YK or GDN_YK
    with contextlib.ExitStack() as es:
        K.psum_gd = K.psum[5:8]
        K.psgi = 0
        rr = RR()
        if run_sb:
            rr.add(sb_stream(K, l, es), 1)
        if run_gdn:
            rr.add(gdn_master(K, l, es, rr, NSETS, YK), YK)
        rr.run()


def phase_A3(K, l):
    nc, S, NT = K.nc, K.S, K.NT
    with contextlib.ExitStack() as es:
        K.stg = [_sb(K, es, "stg%d" % i, [128, IN_W], F32) for i in range(2)]
        Wo = _sb(K, es, "a3_W", [128, KC, D], BF16)
        wbufs = [Buf() for _ in range(KC)]
        wsrc = K.d["w_out"][l].rearrange("(k p) n -> p k n", p=128)
        for kc in range(KC):
            _load_cast(K, Wo, Wo[:, kc, :], wsrc[:, kc, :], [128, D], wbufs[kc])
        Wr = _sb(K, es, "a3_Wr", [128, KC, 36], F32)
        S.dma("sp", Wr[:, :, 0:4], K.d["w_group"][l].rearrange("(k p) n -> p k n", p=128), W=[Wr])
        S.dma("sp", Wr[:, :, 4:36], K.d["w_expert"][l].rearrange("(k p) n -> p k n", p=128), W=[Wr])
        br = _sb(K, es, "a3_br", [128, 36], F32)
        S.dma("sp", br[:, 0:4], K.d["b_group"][l].partition_broadcast(128), W=[br])
        S.dma("sp", br[:, 4:36], K.d["b_expert"][l].partition_broadcast(128), W=[br])
        g = _sb(K, es, "a3_g", [128, D], F32)
        b = _sb(K, es, "a3_b", [128, D], F32)
        S.dma("sp", g[:], K.d["ln1_g"][l].partition_broadcast(128), W=[g])
        S.dma("sp", b[:], K.d["ln1_b"][l].partition_broadcast(128), W=[b])
        mxs = [_sb(K, es, "a3_mx%d" % i, [128, D], BF16) for i in range(2)]
        mTs = [_sb(K, es, "a3_mT%d" % i, [128, KC, 128], BF16) for i in range(2)]
        hts = [_sb(K, es, "a3_h%d" % i, [128, D], F32) for i in range(2)]
        rs = [_sb(K, es, "a3_r%d" % i, [128, D], F32) for i in range(2)]
        h1s = [_sb(K, es, "a3_h1%d" % i, [128, D], F32) for i in range(2)]
        hTf = [_sb(K, es, "a3_hTf%d" % i, [128, KC, 128], F32) for i in range(2)]
        hTb = [_sb(K, es, "a3_hTb%d" % i, [128, KC, 128], BF16) for i in range(2)]
        sm = {"st": _sb(K, es, "a3_st", [128, 2, 6], F32), "mv": _sb(K, es, "a3_mv", [128, 2], F32),
              "rstd": _sb(K, es, "a3_rs", [128, 1], F32), "tmp": _sb(K, es, "a3_tmp", [128, D], F32)}
        lg = _sb(K, es, "a3_lg", [128, 36], F32)
        sc = {k: _sb(K, es, "a3_" + k, shp, F32) for k, shp in
              (("gm", [128, 1]), ("ge", [128, 4]), ("gs", [128, 1]), ("oh", [128, 4]), ("ig", [128, 8]), ("tmp8", [128, 4, 8]),
               ("m8", [128, 8]), ("sel", [128, 8]), ("ex", [128, 8]), ("dn", [128, 1]), ("wi", [128, 8]))}
        cbs = [_sb(K, es, "a3_cb%d" % i, [128, 4, 8], F32) for i in range(2)]
        def stage1(t):
            mx, mT, ht, r, h1, hf, hb, cb = mxs[t % 2], mTs[t % 2], hts[t % 2], rs[t % 2], h1s[t % 2], hTf[t % 2], hTb[t % 2], cbs[t % 2]
            rows = slice(128 * t, 128 * (t + 1))
            S.dma("sp", mx[:], K.d["mixed"][rows, :], R=[K.sbuf_mx_sb, K.sbuf_mx_gd], W=[mx])
            S.dma("sp", ht[:], K.d["h"][rows, :], R=[K.hbuf[t]], W=[ht])
            for half in range(2):
                ps = _ps(K)
                psb = ps.h.bitcast(BF16)
                for j in range(4):
                    kc = half * 4 + j
                    S.op("pe", lambda: nc.tensor.transpose(out=psb[:, j * 128:(j + 1) * 128], in_=mx[:, kc * 128:(kc + 1) * 128], identity=K.identb[:]),
                         R=[mx, K.identb], W=[ps])
                _evac(K, S.alt(), mT[:, half * 4:half * 4 + 4, :], psb[:, 0:512].rearrange("p (a c) -> p a c", a=4), [ps], [mT])
            for half in range(2):
                ps = _ps(K)
                for kc in range(KC):
                    S.op("pe", lambda: nc.tensor.matmul(ps[:, :], lhsT=mT[:, kc, :], rhs=Wo[:, kc, half * 512:(half + 1) * 512],
                                                        start=(kc == 0), stop=(kc == KC - 1)), R=[mT, wbufs[kc]], W=[ps])
                S.op("dve", lambda: nc.vector.scalar_tensor_tensor(out=r[:, half * 512:(half + 1) * 512], in0=ht[:, half * 512:(half + 1) * 512],
                                                                  scalar=ALPHA, in1=ps[:, :], op0=ALU.mult, op1=ALU.add), R=[ht, ps], W=[r])

        def stage1b(t):
            r, h1 = rs[t % 2], h1s[t % 2]
            _ln_tile(K, r, h1, g, b, sm)

        def stage2(t):
            mx, mT, ht, r, h1, hf, hb, cb = mxs[t % 2], mTs[t % 2], hts[t % 2], rs[t % 2], h1s[t % 2], hTf[t % 2], hTb[t % 2], cbs[t % 2]
            rows = slice(128 * t, 128 * (t + 1))
            S.dma("pool", K.d["h1"][rows, :], h1[:], R=[h1], W=[K.h1buf[t]])
            for half in range(2):
                ps = _ps(K)
                for j in range(4):
                    kc = half * 4 + j
                    S.op("pe", lambda: nc.tensor.transpose(out=ps[:, j * 128:(j + 1) * 128], in_=h1[:, kc * 128:(kc + 1) * 128], identity=K.identf[:]),
                         R=[h1, K.identf], W=[ps])
                S.op("act", lambda: nc.scalar.copy(out=hf[:, half * 4:half * 4 + 4, :], in_=ps[:, :].rearrange("p (a c) -> p a c", a=4)), R=[ps], W=[hf])
                S.op("dve", lambda: nc.vector.tensor_copy(out=hb[:, half * 4:half * 4 + 4, :], in_=ps[:, :].rearrange("p (a c) -> p a c", a=4)), R=[ps], W=[hb])
            S.dma("pool", K.d["h1T"][:, :, rows].rearrange("k p n -> p k n"), hb[:], R=[hb], W=[K.sbuf_h1T])
            ps = _ps(K)
            for kc in range(KC):
                S.op("pe", lambda: nc.tensor.matmul(ps[:, 0:36], lhsT=hf[:, kc, :], rhs=Wr[:, kc, :], start=(kc == 0), stop=(kc == KC - 1)),
                     R=[hf, Wr], W=[ps])
            S.op("dve", lambda: nc.vector.tensor_tensor(out=lg[:], in0=ps[:, 0:36], in1=br[:], op=ALU.add), R=[ps, br], W=[lg])
            gm, ge, gs, oh, ig, tmp8, m8, sel, ex, dn, wi = (sc[k] for k in ("gm", "ge", "gs", "oh", "ig", "tmp8", "m8", "sel", "ex", "dn", "wi"))
            S.op("dve", lambda: nc.vector.tensor_reduce(out=gm[:], in_=lg[:, 0:4], axis=AX.X, op=ALU.max), R=[lg], W=[gm])
            S.op("dve", lambda: nc.vector.tensor_scalar(out=oh[:], in0=lg[:, 0:4], scalar1=gm[:, 0:1], scalar2=None, op0=ALU.is_equal), R=[lg, gm], W=[oh])
            S.op("dve", lambda: nc.vector.tensor_scalar(out=ge[:], in0=lg[:, 0:4], scalar1=gm[:, 0:1], scalar2=None, op0=ALU.subtract), R=[lg, gm], W=[ge])
            S.op("act", lambda: nc.scalar.activation(out=ge[:], in_=ge[:], func=AF.Exp), R=[ge], W=[ge])
            S.op("dve", lambda: nc.vector.tensor_reduce(out=gs[:], in_=ge[:], axis=AX.X, op=ALU.add), R=[ge], W=[gs])
            S.op("dve", lambda: nc.vector.tensor_tensor(out=tmp8[:], in0=lg[:, 4:36].rearrange("p (g e) -> p g e", g=4),
                                                        in1=oh[:].unsqueeze(2).to_broadcast([128, 4, 8]), op=ALU.mult), R=[lg, oh], W=[tmp8])
            S.op("dve", lambda: nc.vector.tensor_reduce(out=ig[:], in_=tmp8[:].rearrange("p g e -> p e g"), axis=AX.X, op=ALU.add), R=[tmp8], W=[ig])
            S.op("dve", lambda: nc.vector.max(out=m8[:], in_=ig[:]), R=[ig], W=[m8])
            S.op("dve", lambda: nc.vector.tensor_scalar(out=sel[:], in0=ig[:], scalar1=m8[:, 1:2], scalar2=None, op0=ALU.is_ge), R=[ig, m8], W=[sel])
            S.op("dve", lambda: nc.vector.tensor_scalar(out=ex[:], in0=ig[:], scalar1=m8[:, 0:1], scalar2=None, op0=ALU.subtract), R=[ig, m8], W=[ex])
            S.op("act", lambda: nc.scalar.activation(out=ex[:], in_=ex[:], func=AF.Exp), R=[ex], W=[ex])
            S.op("dve", lambda: nc.vector.tensor_tensor(out=ex[:], in0=ex[:], in1=sel[:], op=ALU.mult), R=[ex, sel], W=[ex])
            S.op("dve", lambda: nc.vector.tensor_reduce(out=dn[:], in_=ex[:], axis=AX.X, op=ALU.add), R=[ex], W=[dn])
            S.op("dve", lambda: nc.vector.tensor_tensor(out=dn[:], in0=dn[:], in1=gs[:], op=ALU.mult), R=[dn, gs], W=[dn])
            S.op("dve", lambda: nc.vector.reciprocal(out=dn[:], in_=dn[:]), R=[dn], W=[dn])
            S.op("dve", lambda: nc.vector.tensor_scalar(out=wi[:], in0=ex[:], scalar1=dn[:, 0:1], scalar2=None, op0=ALU.mult), R=[ex, dn], W=[wi])
            S.op("dve", lambda: nc.vector.tensor_tensor(out=cb[:], in0=oh[:].unsqueeze(2).to_broadcast([128, 4, 8]),
                                                        in1=wi[:].unsqueeze(1).to_broadcast([128, 4, 8]), op=ALU.mult), R=[oh, wi], W=[cb])
            S.dma("pool", K.d["comb"][rows, :], cb[:].rearrange("p g e -> p (g e)"), R=[cb], W=[K.sbuf_comb])

        for t in range(NT + 2):
            if t < NT:
                stage1(t)
            if 1 <= t <= NT:
                stage1b(t - 1)
            if t >= 2:
                stage2(t - 2)


def phase_B(K, l, last):
    nc, S, NT = K.nc, K.S, K.NT
    NP = 4
    TH = (NT + NP - 1) // NP
    with contextlib.ExitStack() as es:
        g = _sb(K, es, "b_g", [128, D], F32)
        b = _sb(K, es, "b_b", [128, D], F32)
        S.dma("sp", g[:], K.d["ln2_g"][l].partition_broadcast(128), W=[g])
        S.dma("sp", b[:], K.d["ln2_b"][l].partition_broadcast(128), W=[b])
        xT = _sb(K, es, "b_xT", [128, KC, TH * 128], BF16)
        cbt = _sb(K, es, "b_cb", [128, TH, 32], F32)
        yacc = _sb(K, es, "b_y", [128, TH, D], F32)
        w1b = [_sb(K, es, "b_w1%d" % i, [128, KC, 256], BF16) for i in range(2)]
        w3b = [_sb(K, es, "b_w3%d" % i, [128, KC, 256], BF16) for i in range(2)]
        w2b = [_sb(K, es, "b_w2%d" % i, [128, 2, D], BF16) for i in range(2)]
        sil = [_sb(K, es, "b_sil%d" % i, [128, 512], F32) for i in range(2)]
        hid = [_sb(K, es, "b_hid%d" % i, [128, 2, 512], BF16) for i in range(2)]
        h1t = [_sb(K, es, "b_h1%d" % i, [128, D], F32) for i in range(1)]
        rr = [_sb(K, es, "b_r%d" % i, [128, D], F32) for i in range(1)]
        oo = [_sb(K, es, "b_o%d" % i, [128, D], F32) for i in range(2)]
        sm = {"st": _sb(K, es, "b_st", [128, 2, 6], F32), "mv": _sb(K, es, "b_mv", [128, 2], F32),
              "rstd": _sb(K, es, "b_rs", [128, 1], F32), "tmp": _sb(K, es, "b_tmp", [128, D], F32)}
        stgB = [_sb(K, es, "stgB%d" % i, [128, 2048], F32) for i in range(6)]
        ld = {"n": 0}

        def load_w(dst, src_ap, a_):
            k = ld["n"]
            ld["n"] += 1
            st = stgB[k % 6]
            v = st.h[:, 0:2048].rearrange("p (a b) -> p a b", a=a_)
            S.dma("sp" if k % 2 == 0 else "pool", v, src_ap, W=[st])
            if k % 3 == 2:
                S.op("pool", lambda: nc.gpsimd.tensor_copy(out=dst[:], in_=v), R=[st], W=[dst])
            else:
                S.op("act", lambda: nc.scalar.copy(out=dst[:], in_=v), R=[st], W=[dst])

        def load_expert(e):
            load_w(w1b[e % 2], K.d["w1"][l, e].rearrange("(k p) f -> p k f", p=128), KC)
            load_w(w3b[e % 2], K.d["w3"][l, e].rearrange("(k p) f -> p k f", p=128), KC)
            load_w(w2b[e % 2], K.d["w2"][l, e].rearrange("(k p) f -> p k f", p=128), 2)
        it = 0
        npass = len([h_ for h_ in range(NP) if h_ * TH < NT])
        wl = {"next": 2, "total": 32 * npass}
        load_expert(0)
        load_expert(1)
        for half in range(NP):
            t0 = half * TH
            nth = min(TH, NT - t0)
            if nth <= 0:
                break
            n_all = nth * 128
            S.dma("sp", xT[:, :, 0:n_all], K.d["h1T"][:, :, t0 * 128:t0 * 128 + n_all].rearrange("k p n -> p k n"), R=[K.sbuf_h1T], W=[xT])
            S.dma("sp", cbt[:, 0:nth, :], K.d["comb"][t0 * 128:t0 * 128 + n_all, :].rearrange("(t p) e -> p t e", p=128), R=[K.sbuf_comb], W=[cbt])
            S.op("pool", lambda: nc.gpsimd.memset(yacc[:, 0:nth, :], 0.0), W=[yacc])
            def up(e, gq, hd):
                wa, wc = w1b[e % 2], w3b[e % 2]
                nt = min(4, nth - 4 * gq)
                n = nt * 128
                c0 = gq * 512
                for f2 in range(2):
                    p1 = _ps(K)
                    for kc in range(KC):
                        S.op("pe", lambda: nc.tensor.matmul(p1[:, 0:n], lhsT=wa[:, kc, f2 * 128:(f2 + 1) * 128], rhs=xT[:, kc, c0:c0 + n],
                                                            start=(kc == 0), stop=(kc == KC - 1)), R=[wa, xT], W=[p1])
                    p3 = _ps(K)
                    for kc in range(KC):
                        S.op("pe", lambda: nc.tensor.matmul(p3[:, 0:n], lhsT=wc[:, kc, f2 * 128:(f2 + 1) * 128], rhs=xT[:, kc, c0:c0 + n],
                                                            start=(kc == 0), stop=(kc == KC - 1)), R=[wc, xT], W=[p3])
                    sl = sil[f2]
                    S.op("act", lambda: nc.scalar.activation(out=sl[:, 0:n], in_=p1[:, 0:n], func=AF.Silu), R=[p1], W=[sl])
                    S.op("dve", lambda: nc.vector.tensor_tensor(out=hd[:, f2, 0:n], in0=sl[:, 0:n], in1=p3[:, 0:n], op=ALU.mult), R=[sl, p3], W=[hd])

            def down(e, gq, hd):
                wd = w2b[e % 2]
                nt = min(4, nth - 4 * gq)
                for t in range(nt):
                    tt = 4 * gq + t
                    for ch in range(2):
                        py = _ps(K)
                        for f2 in range(2):
                            S.op("pe", lambda: nc.tensor.matmul(py[:, :], lhsT=hd[:, f2, t * 128:(t + 1) * 128], rhs=wd[:, f2, ch * 512:(ch + 1) * 512],
                                                                start=(f2 == 0), stop=(f2 == 1)), R=[hd, wd], W=[py])
                        S.op("dve", lambda: nc.vector.scalar_tensor_tensor(out=yacc[:, tt, ch * 512:(ch + 1) * 512], in0=py[:, :],
                                                                          scalar=cbt[:, tt, e:e + 1], in1=yacc[:, tt, ch * 512:(ch + 1) * 512],
                                                                          op0=ALU.mult, op1=ALU.add), R=[py, cbt, yacc], W=[yacc])

            ngq = (nth + 3) // 4
            items = [(e, gq) for e in range(32) for gq in range(ngq)]
            up(items[0][0], items[0][1], hid[it % 2])
            for k, (e, gq) in enumerate(items):
                if k + 1 < len(items):
                    up(items[k + 1][0], items[k + 1][1], hid[(it + 1) % 2])
                down(e, gq, hid[it % 2])
                it += 1
                if gq == ngq - 1:
                    if wl["next"] < wl["total"]:
                        load_expert(wl["next"] % 32)
                        wl["next"] += 1
            for t in range(nth):
                tg = t0 + t
                rows = slice(128 * tg, 128 * (tg + 1))
                h1, r, o = h1t[0], rr[0], oo[t % 2]
                S.dma("sp", h1[:], K.d["h1"][rows, :], R=[K.h1buf[tg]], W=[h1])
                S.op("dve", lambda: nc.vector.scalar_tensor_tensor(out=r[:], in0=h1[:], scalar=ALPHA, in1=yacc[:, t, :], op0=ALU.mult, op1=ALU.add),
                     R=[h1, yacc], W=[r])
                _ln_tile(K, r, o, g, b, sm)
                if not last:
                    S.dma("pool", K.d["h"][rows, :], o[:], R=[o], W=[K.hbuf[tg]])
                else:
                    lo = 128 * tg - 64
                    r0, r1 = max(lo, 0), min(lo + 128, K.SEQ)
                    if r1 > r0:
                        S.dma("pool", K.d["out"][r0:r1, :], o[r0 - lo:r1 - lo, :], R=[o], W=[K.outbuf])


def make_consts():
    c = {}
    c["identf"] = np.eye(128, dtype=np.float32)
    c["identb"] = np.eye(128, dtype=np.float32).astype(ml_dtypes.bfloat16)
    s = np.arange(128)[:, None]
    masks = np.zeros((6, 128, 512), np.float32)
    for rel in range(4):
        for qt in range(4):
            blk = masks[rel, :, qt * 128:(qt + 1) * 128]
            if qt < rel:
                blk[:] = NEG
            elif qt == rel:
                blk[:] = np.where(s < np.arange(128)[None, :], 0.0, NEG)
    masks[4] = masks[0]
    masks[4, 0:48, :] = NEG
    masks[5, 0:48, :] = NEG
    c["masks"] = np.ascontiguousarray(masks.transpose(1, 0, 2)).astype(ml_dtypes.bfloat16)
    c["negtri"] = np.where(s >= np.arange(128)[None, :], -1.0, 0.0).astype(ml_dtypes.bfloat16)
    c["onesb"] = np.ones((128, 1), np.float32).astype(ml_dtypes.bfloat16)
    c["onesf"] = np.ones((128, 128), np.float32)
    m = np.arange(64)[:, None]
    i = np.arange(64)[None, :]
    triu = (m <= i).astype(np.float32)
    c["triu"] = np.ascontiguousarray(np.repeat(triu[:, None, :], 4, axis=1))
    c["sgt"] = (m > i).astype(np.float32)
    c["ones64"] = np.ones((64, 128), np.float32)
    c["ones4"] = np.ones((64, 4, 128), np.float32)
    c["trilsn"] = np.ascontiguousarray(np.repeat((-(m > i).astype(np.float32))[:, None, :], 4, axis=1))
    c["ident4"] = np.ascontiguousarray(np.repeat(np.eye(64, dtype=np.float32)[:, None, :], 4, axis=1))
    c["cvec"] = np.tile(np.array([[1.0, LN_EPS, RMS_EPS, 0.0, 64.0 * RMS_EPS, 128.0 * RMS_EPS]], np.float32), (128, 1))
    return c


CONST_DT = {"identb": BF16, "masks": BF16, "negtri": BF16, "onesb": BF16}

IN_SHAPES = lambda SEQ, depth: {
    "x": [SEQ, D], "meta": [16, D], "ln_in_g": [D], "ln_in_b": [D], "w_in": [depth, D, IN_W], "conv_wT": [depth, 1536, 4],
    "a_log": [depth, 4], "dt_bias": [depth, 4], "sb_norm_g": [depth, 64], "gdn_norm_g": [depth, 128], "w_out": [depth, D, D],
    "ln1_g": [depth, D], "ln1_b": [depth, D], "w_group": [depth, D, 4], "b_group": [depth, 4], "w_expert": [depth, D, 32],
    "b_expert": [depth, 32], "w1": [depth, 32, D, 256], "w3": [depth, 32, D, 256], "w2": [depth, 32, 256, D],
    "ln2_g": [depth, D], "ln2_b": [depth, D]}


def build(SEQ, depth, debug=False, same_eng=True, phases=None, max_ops=None, log=None):
    nc = bass.Bass("TRN2", target_bir_lowering=False)
    K = Ctx()
    K.nc = nc
    K.SEQ = SEQ
    PT = SEQ + 64
    K.NT = NT = (PT + 127) // 128
    K.NC = PT // 64
    P = NT * 128
    K.d = {}
    for name, shp in IN_SHAPES(SEQ, depth).items():
        K.d[name] = nc.dram_tensor(name, shp, F32, kind="ExternalInput").ap()
    consts = make_consts()
    for name, arr in consts.items():
        K.d["c_" + name] = nc.dram_tensor("c_" + name, list(arr.shape), CONST_DT.get(name, F32), kind="ExternalInput").ap()
    K.d["out"] = nc.dram_tensor("out", [SEQ, D], F32, kind="ExternalOutput").ap()
    kind = "ExternalOutput" if debug else "Internal"
    for name, shp, dt in (("h", [P, D], F32), ("qT", [4, 128, P], BF16), ("kT", [4, 128, P], BF16), ("V", [P, 512], BF16),
                          ("gT", [12, 128, P], F32), ("zs", [P, 512], F32), ("bg", [P, 8], F32), ("mixed", [P, D], BF16),
                          ("h1", [P, D], F32), ("h1T", [KC, 128, P], BF16), ("comb", [P, 32], F32)):
        K.d[name] = nc.dram_tensor("s_" + name, shp, dt, kind=kind).ap()
    K.hbuf = [Buf() for _ in range(NT)]
    K.h1buf = [Buf() for _ in range(NT)]
    for nm in ("sbuf_q", "sbuf_k", "sbuf_v", "sbuf_g", "sbuf_z", "sbuf_bg", "sbuf_mx_sb", "sbuf_mx_gd", "sbuf_h1T", "sbuf_comb", "outbuf"):
        setattr(K, nm, Buf())
    with contextlib.ExitStack() as es:
        K.S = S = Sched(nc, es, same_eng=same_eng)
        S.max_ops = max_ops
        S.log = log
        K.psum = [Tl(es.enter_context(nc.psum_tensor("ps%d" % i, [128, 512], F32)), "ps%d" % i) for i in range(8)]
        K.psi = 0
        for p_ in K.psum:
            p_.b.excl = True
        K.stgi = 0
        for name, arr in consts.items():
            if name == "cvec":
                continue
            tl = _sb(K, es, "k_" + name, list(arr.shape), CONST_DT.get(name, F32))
            setattr(K, name, tl)
            S.dma("sp", tl[:], K.d["c_" + name], W=[tl])
        cv = _sb(K, es, "k_cvec", [128, 6], F32)
        S.dma("sp", cv[:], K.d["c_cvec"], W=[cv])
        K.one_c = Tl(cv.h[:, 0:1]); K.one_c.b = cv.b
        K.eps_ln = Tl(cv.h[:, 1:2]); K.eps_ln.b = cv.b
        K.eps_rms = Tl(cv.h[:, 2:3]); K.eps_rms.b = cv.b
        K.eps_rms64 = Tl(cv.h[:, 4:5]); K.eps_rms64.b = cv.b
        K.eps_rms128 = Tl(cv.h[:, 5:6]); K.eps_rms128.b = cv.b
        ph = phases or ("in", "A1", "SB", "GDN", "A3", "B")
        if "in" in ph:
            phase_input(K)
            S.barrier()
        for l in range(depth):
            if "A1" in ph:
                phase_A1(K, l)
                S.barrier()
            if INTERLEAVE_GDN:
                if "SB" in ph or "GDN" in ph:
                    phase_SBGDN(K, l, run_sb=("SB" in ph), run_gdn=("GDN" in ph))
                    S.barrier()
            else:
                if "SB" in ph:
                    phase_SBGDN(K, l, run_sb=True, run_gdn=False)
                    S.barrier()
                if "GDN" in ph:
                    phase_GDN_old(K, l)
                    S.barrier()
            if "A3" in ph:
                phase_A3(K, l)
                S.barrier()
            if "B" in ph:
                phase_B(K, l, l == depth - 1)
                S.barrier()
        S.finish()
    K.consts = consts
    return nc, K


def make_in_maps(inputs, SEQ, depth, consts, n_cores=8):
    shared = {}
    for k in ("ln_in_g", "ln_in_b", "w_in", "a_log", "dt_bias", "sb_norm_g", "gdn_norm_g", "w_out", "ln1_g", "ln1_b",
              "w_group", "b_group", "w_expert", "b_expert", "w1", "w3", "w2", "ln2_g", "ln2_b"):
        shared[k] = np.ascontiguousarray(np.asarray(inputs[k], dtype=np.float32))
    shared["meta"] = np.ascontiguousarray(np.asarray(inputs["meta_tokens"], dtype=np.float32))
    shared["conv_wT"] = np.ascontiguousarray(np.asarray(inputs["conv_w"], dtype=np.float32).transpose(0, 2, 1))
    for k, v in consts.items():
        shared["c_" + k] = v
    x = np.asarray(inputs["x"], dtype=np.float32)
    B = x.shape[0]
    maps = []
    for c in range(n_cores):
        m = dict(shared)
        m["x"] = np.ascontiguousarray(x[c % B])
        maps.append(m)
    return maps


def kernel(**inputs):
    x = np.asarray(inputs["x"])
    B, SEQ, _ = x.shape
    depth = np.asarray(inputs["w_in"]).shape[0]
    nc, K = build(SEQ, depth)
    maps = make_in_maps(inputs, SEQ, depth, K.consts)
    res = run_bass_kernel_spmd(nc, maps, core_ids=list(range(8)))
    out = np.stack([np.asarray(res.results[b]["out"], dtype=np.float32) for b in range(B)], axis=0)
    return out
```
